# Optimizing a Trainium2 kernel written in Bass

```python
import jax, jax.numpy as jnp
from jax import lax
import numpy as np

D_MODEL = 2048
BATCH = 4
SEQ = 4096
DEPTH = 2

CHUNK = 64
D_MIX = D_MODEL
GROUP = D_MIX // 4
LN_EPS = 1e-5
DN_ALPHA = (2 * DEPTH) ** 0.25
DN_BETA = (8 * DEPTH) ** -0.25
N_MOD = 6

CONV_WIDTH = 31

MLA_HEADS = 4
MLA_NOPE = 128
MLA_ROPE = 64
MLA_VDIM = 128
MLA_Q_RANK = 384
MLA_KV_RANK = 256
ROPE_THETA = 10000.0
Q_BLOCK = 128
MAX_STREAM_OFFSET = 65536

RW_HEAD = 64
RW_HEADS = GROUP // RW_HEAD
RW_DECAY_LORA = 96
RW_AAA_LORA = 96
RW_MV_LORA = 64
RW_GATE_LORA = 256
RW_GN_EPS = 64e-5

CA_HEADS = 4
CA_HEAD_DIM = GROUP // CA_HEADS
CA_LEFT = 8
REL_CLIP = 256

N_EXPERTS = 32
TOP_K = 4
D_FF = D_MODEL
SWIGLU_LIMIT = 7.0
SWIGLU_ALPHA = 1.702
MOE_BLOCK = 256

IN_SPLITS = (2 * GROUP, MLA_Q_RANK, MLA_KV_RANK, MLA_ROPE, 3 * GROUP, 3 * GROUP)
D_IN = sum(IN_SPLITS)
IN_OFFSETS = tuple(int(o) for o in np.cumsum(IN_SPLITS)[:-1])

kernel_name = 'hybrid_streaming_encoder_block'


def layer_norm(x, g=None, b=None, eps=LN_EPS):
    xf = x.astype(jnp.float32)
    mu = xf.mean(-1, keepdims=True)
    var = jnp.square(xf - mu).mean(-1, keepdims=True)
    y = (xf - mu) * lax.rsqrt(var + eps)
    if g is not None:
        y = y * g + b
    return y.astype(x.dtype)


def rms_norm(x, g, eps=1e-6):
    xf = x.astype(jnp.float32)
    y = xf * lax.rsqrt(jnp.mean(xf * xf, -1, keepdims=True) + eps)
    return (y * g).astype(x.dtype)


def rope_tables(positions):
    inv = ROPE_THETA ** (-jnp.arange(0, MLA_ROPE, 2, dtype=jnp.float32) / MLA_ROPE)
    ang = positions.astype(jnp.float32)[..., None] * inv
    return jnp.cos(ang), jnp.sin(ang)


def apply_rope(x, cos, sin):
    x1, x2 = jnp.split(x.astype(jnp.float32), 2, axis=-1)
    return jnp.concatenate([x1 * cos - x2 * sin, x1 * sin + x2 * cos], -1).astype(x.dtype)


def token_shift(t):
    return jnp.pad(t, ((0, 0), (1, 0), (0, 0)))[:, :-1]


def conv_module(u2, dw, db, ln_g, ln_b):
    val, gate = jnp.split(u2, 2, axis=-1)
    u = val * jax.nn.sigmoid(gate)
    u = jnp.pad(u, ((0, 0), (CONV_WIDTH - 1, 0), (0, 0)))
    u = lax.conv_general_dilated(u, dw[:, None, :].astype(u.dtype), window_strides=(1,),
                                 padding='VALID', dimension_numbers=('NWC', 'WIO', 'NWC'),
                                 feature_group_count=GROUP) + db
    return jax.nn.silu(layer_norm(u, ln_g, ln_b))


def mla(cq, ckv, kpe, q_norm, kv_norm, w_uq, w_ukv, cos, sin):
    B, S, _ = cq.shape
    q = (rms_norm(cq, q_norm) @ w_uq).reshape(B, S, MLA_HEADS, MLA_NOPE + MLA_ROPE)
    q_nope, q_pe = jnp.split(q, [MLA_NOPE], axis=-1)
    q = jnp.concatenate([q_nope, apply_rope(q_pe, cos[:, :, None], sin[:, :, None])], -1)
    kv = (rms_norm(ckv, kv_norm) @ w_ukv).reshape(B, S, MLA_HEADS, MLA_NOPE + MLA_VDIM)
    k_nope, v = jnp.split(kv, [MLA_NOPE], axis=-1)
    k_pe = apply_rope(kpe, cos, sin)[:, :, None, :]
    k = jnp.concatenate([k_nope, jnp.broadcast_to(k_pe, (B, S, MLA_HEADS, MLA_ROPE))], -1)
    scale = (MLA_NOPE + MLA_ROPE) ** -0.5
    chunk_id = jnp.arange(S) // CHUNK
    nqb = S // Q_BLOCK
    qb = jnp.moveaxis(q.reshape(B, nqb, Q_BLOCK, MLA_HEADS, MLA_NOPE + MLA_ROPE), 1, 0)
    qchunk = chunk_id.reshape(nqb, Q_BLOCK)

    def query_block(args):
        qi, qc = args
        s = jnp.einsum('bqhd,bkhd->bhqk', qi, k).astype(jnp.float32) * scale
        s = jnp.where(chunk_id[None, :] <= qc[:, None], s, -jnp.inf)
        p = jax.nn.softmax(s, axis=-1).astype(v.dtype)
        return jnp.einsum('bhqk,bkhd->bqhd', p, v)

    o = lax.map(query_block, (qb, qchunk))
    return jnp.moveaxis(o, 0, 1).reshape(B, S, MLA_HEADS * MLA_VDIM)


def rwkv7_time_mix(h, r, k, v, v_first, mu_rkv, mu_x, w0, w1, w2, a0, a1, a2,
                   g1, g2, k_k, k_a, r_k, gn_g, gn_b, vres):
    B, S, _ = h.shape
    f32 = jnp.float32
    dh = token_shift(h) - h
    xw, xa, xg = h + dh * mu_x[0], h + dh * mu_x[1], h + dh * mu_x[2]
    r = r + (token_shift(r) - r) * mu_rkv[0]
    k = k + (token_shift(k) - k) * mu_rkv[1]
    v = v + (token_shift(v) - v) * mu_rkv[2]
    w = -jax.nn.softplus(-(w0 + jnp.tanh(xw @ w1) @ w2)) - 0.5
    decay = jnp.exp(-jnp.exp(w.astype(f32)))
    a = jax.nn.sigmoid(a0 + (xa @ a1) @ a2)
    g = jax.nn.sigmoid(xg @ g1) @ g2
    heads = lambda t: t.reshape(B, S, RW_HEADS, RW_HEAD)
    kk = heads(k * k_k).astype(f32)
    kk = kk / jnp.maximum(jnp.sqrt(jnp.sum(kk * kk, -1, keepdims=True)), 1e-12)
    k = k * (1 + (a - 1) * k_a)
    if vres is None:
        v_first = v
    else:
        mu_v, v0, v1, v2 = vres
        xv = h + dh * mu_v
        v = v + (v_first - v) * jax.nn.sigmoid(v0 + (xv @ v1) @ v2)
    seq = lambda t: jnp.moveaxis(heads(t).astype(f32), 1, 0)
    xs = (seq(r), seq(decay), seq(k), seq(v), jnp.moveaxis(kk, 1, 0), seq(a))

    def step(state, inp):
        rt, wt, kt, vt, kkt, at = inp
        s_kk = jnp.einsum('bhvk,bhk->bhv', state, kkt)
        state = (state * wt[:, :, None, :]
                 - s_kk[..., None] * (kkt * at)[:, :, None, :]
                 + vt[..., None] * kt[:, :, None, :])
        return state, jnp.einsum('bhvk,bhk->bhv', state, rt)

    state0 = jnp.zeros((B, RW_HEADS, RW_HEAD, RW_HEAD), f32)
    _, y = lax.scan(step, state0, xs)
    y = jnp.moveaxis(y, 0, 1)
    mu = y.mean(-1, keepdims=True)
    var = jnp.square(y - mu).mean(-1, keepdims=True)
    y = ((y - mu) * lax.rsqrt(var + RW_GN_EPS)).reshape(B, S, GROUP) * gn_g + gn_b
    bonus = jnp.sum(heads(r * k).astype(f32) * r_k, -1, keepdims=True) * heads(v).astype(f32)
    y = (y + bonus.reshape(B, S, GROUP)) * g
    return y.astype(h.dtype), v_first


def chunk_band_attention(q, k, v, rel_table):
    B, S, _ = q.shape
    NC = S // CHUNK
    W = (CA_LEFT + 1) * CHUNK
    qc = q.reshape(B, NC, CHUNK, CA_HEADS, CA_HEAD_DIM)

    def band(t):
        tc = t.reshape(B, NC, CHUNK, CA_HEADS, CA_HEAD_DIM)
        tp = jnp.pad(tc, ((0, 0), (CA_LEFT, 0), (0, 0), (0, 0), (0, 0)))
        return jnp.concatenate([tp[:, j:j + NC] for j in range(CA_LEFT + 1)], axis=2)

    kb, vb = band(k), band(v)
    q_pos = CA_LEFT * CHUNK + jnp.arange(CHUNK)[:, None]
    k_pos = jnp.arange(W)[None, :]
    rel = jnp.clip(q_pos - k_pos, -REL_CLIP, REL_CLIP) + REL_CLIP
    bias = rel_table[:, rel].astype(jnp.float32)
    key_chunk = jnp.arange(NC)[:, None] - CA_LEFT + jnp.arange(W)[None, :] // CHUNK
    valid = key_chunk >= 0
    s = jnp.einsum('bnqhd,bnkhd->bhnqk', qc, kb).astype(jnp.float32) * CA_HEAD_DIM ** -0.5
    s = s + bias[:, None]
    s = jnp.where(valid[None, None, :, None, :], s, -jnp.inf)
    p = jax.nn.softmax(s, axis=-1).astype(v.dtype)
    o = jnp.einsum('bhnqk,bnkhd->bnqhd', p, vb)
    return o.reshape(B, S, GROUP)


def moe(h, router_w, router_b, w1, b1, w2, b2):
    T, D = h.shape
    logits = (h @ router_w + router_b).astype(jnp.float32)
    top_val, top_idx = lax.top_k(logits, TOP_K)
    gate = jax.nn.softmax(top_val, axis=-1)
    flat_e = top_idx.reshape(-1)
    flat_tok = jnp.repeat(jnp.arange(T, dtype=jnp.int32), TOP_K)
    order = jnp.argsort(flat_e)
    e_sorted = flat_e[order]
    counts = jnp.bincount(flat_e, length=N_EXPERTS)
    padded = (counts + MOE_BLOCK - 1) // MOE_BLOCK * MOE_BLOCK
    start = jnp.cumsum(counts) - counts
    pad_end = jnp.cumsum(padded)
    pad_start = pad_end - padded
    dest = pad_start[e_sorted] + jnp.arange(T * TOP_K) - start[e_sorted]
    n_blocks = -(-(T * TOP_K + N_EXPERTS * (MOE_BLOCK - 1)) // MOE_BLOCK)
    n_slots = n_blocks * MOE_BLOCK
    slot_tok = jnp.full((n_slots,), T, jnp.int32).at[dest].set(flat_tok[order])
    slot_w = jnp.zeros((n_slots,), jnp.float32).at[dest].set(gate.reshape(-1)[order])
    block_e = jnp.minimum(jnp.searchsorted(pad_end, jnp.arange(n_blocks) * MOE_BLOCK, side='right'),
                          N_EXPERTS - 1).astype(jnp.int32)
    h_pad = jnp.concatenate([h, jnp.zeros((1, D), h.dtype)], 0)

    def expert_block(acc, blk):
        tok, wt, e = blk
        w1e = lax.dynamic_index_in_dim(w1, e, keepdims=False)
        b1e = lax.dynamic_index_in_dim(b1, e, keepdims=False)
        w2e = lax.dynamic_index_in_dim(w2, e, keepdims=False)
        b2e = lax.dynamic_index_in_dim(b2, e, keepdims=False)
        hid = h_pad[tok] @ w1e + b1e
        glu, lin = jnp.split(hid, 2, axis=-1)
        glu = jnp.minimum(glu, SWIGLU_LIMIT)
        lin = jnp.clip(lin, -SWIGLU_LIMIT, SWIGLU_LIMIT)
        act = glu * jax.nn.sigmoid(SWIGLU_ALPHA * glu) * (lin + 1)
        out = (act @ w2e + b2e) * wt[:, None].astype(h.dtype)
        return acc.at[tok].add(out), None

    acc, _ = lax.scan(expert_block, jnp.zeros((T + 1, D), h.dtype),
                      (slot_tok.reshape(n_blocks, MOE_BLOCK), slot_w.reshape(n_blocks, MOE_BLOCK), block_e))
    return acc[:T]


def setup_inputs(seed: int = 0) -> dict:
    key = jax.random.key(seed)
    ks = iter(jax.random.split(key, 64))
    L = DEPTH

    def nrm(shape, scale):
        return jax.random.normal(next(ks), shape, jnp.float32) * scale

    def unif(shape):
        return jax.random.uniform(next(ks), shape, jnp.float32)

    x = nrm((BATCH, SEQ, D_MODEL), 1.0)
    c = nrm((BATCH, D_MODEL), 1.0)
    offsets = jax.random.randint(next(ks), (BATCH, 1), 0, MAX_STREAM_OFFSET, dtype=jnp.int32)
    positions = offsets + jnp.arange(SEQ, dtype=jnp.int32)[None, :]
    return {
        'x': x, 'c': c, 'positions': positions,
        'mod_w': nrm((D_MODEL, N_MOD * D_MODEL), 0.2 * D_MODEL ** -0.5),
        'mod_b': nrm((N_MOD * D_MODEL,), 0.02),
        'mod_offset': nrm((L, N_MOD * D_MODEL), 0.02),
        'ln_post_g': 1.0 + nrm((L, 2, D_MODEL), 0.02),
        'ln_post_b': nrm((L, 2, D_MODEL), 0.02),
        'w_in': nrm((L, D_MODEL, D_IN), D_MODEL ** -0.5),
        'w_out': nrm((L, D_MIX, D_MODEL), DN_BETA * D_MIX ** -0.5),
        'conv_dw': nrm((L, CONV_WIDTH, GROUP), CONV_WIDTH ** -0.5),
        'conv_b': nrm((L, GROUP), 0.02),
        'conv_ln_g': 1.0 + nrm((L, GROUP), 0.02),
        'conv_ln_b': nrm((L, GROUP), 0.02),
        'mla_q_norm': 1.0 + nrm((L, MLA_Q_RANK), 0.02),
        'mla_kv_norm': 1.0 + nrm((L, MLA_KV_RANK), 0.02),
        'mla_w_uq': nrm((L, MLA_Q_RANK, MLA_HEADS * (MLA_NOPE + MLA_ROPE)), MLA_Q_RANK ** -0.5),
        'mla_w_ukv': nrm((L, MLA_KV_RANK, MLA_HEADS * (MLA_NOPE + MLA_VDIM)), MLA_KV_RANK ** -0.5),
        'rw_mu_rkv': unif((L, 3, GROUP)),
        'rw_mu_x': unif((L, 3, D_MODEL)),
        'rw_w0': nrm((L, GROUP), 0.5),
        'rw_w1': nrm((L, D_MODEL, RW_DECAY_LORA), D_MODEL ** -0.5),
        'rw_w2': nrm((L, RW_DECAY_LORA, GROUP), 0.5 * RW_DECAY_LORA ** -0.5),
        'rw_a0': nrm((L, GROUP), 0.1),
        'rw_a1': nrm((L, D_MODEL, RW_AAA_LORA), D_MODEL ** -0.5),
        'rw_a2': nrm((L, RW_AAA_LORA, GROUP), 0.5 * RW_AAA_LORA ** -0.5),
        'rw_g1': nrm((L, D_MODEL, RW_GATE_LORA), D_MODEL ** -0.5),
        'rw_g2': nrm((L, RW_GATE_LORA, GROUP), RW_GATE_LORA ** -0.5),
        'rw_k_k': 0.85 + nrm((L, GROUP), 0.02),
        'rw_k_a': 1.0 + nrm((L, GROUP), 0.02),
        'rw_r_k': nrm((L, RW_HEADS, RW_HEAD), 0.1),
        'rw_gn_g': 1.0 + nrm((L, GROUP), 0.02),
        'rw_gn_b': nrm((L, GROUP), 0.02),
        'rw_vres_mu': unif((L - 1, D_MODEL)),
        'rw_v0': nrm((L - 1, GROUP), 0.1),
        'rw_v1': nrm((L - 1, D_MODEL, RW_MV_LORA), D_MODEL ** -0.5),
        'rw_v2': nrm((L - 1, RW_MV_LORA, GROUP), 0.5 * RW_MV_LORA ** -0.5),
        'ca_rel_bias': nrm((L, CA_HEADS, 2 * REL_CLIP + 1), 0.1),
        'moe_router_w': nrm((L, D_MODEL, N_EXPERTS), D_MODEL ** -0.5),
        'moe_router_b': nrm((L, N_EXPERTS), 0.01),
        'moe_w1': nrm((L, N_EXPERTS, D_MODEL, 2 * D_FF), D_MODEL ** -0.5),
        'moe_b1': nrm((L, N_EXPERTS, 2 * D_FF), 0.02),
        'moe_w2': nrm((L, N_EXPERTS, D_FF, D_MODEL), DN_BETA * D_FF ** -0.5),
        'moe_b2': nrm((L, N_EXPERTS, D_MODEL), 0.02),
    }


def reference(x, c, positions, mod_w, mod_b, mod_offset, ln_post_g, ln_post_b, w_in, w_out,
              conv_dw, conv_b, conv_ln_g, conv_ln_b, mla_q_norm, mla_kv_norm, mla_w_uq, mla_w_ukv,
              rw_mu_rkv, rw_mu_x, rw_w0, rw_w1, rw_w2, rw_a0, rw_a1, rw_a2, rw_g1, rw_g2,
              rw_k_k, rw_k_a, rw_r_k, rw_gn_g, rw_gn_b, rw_vres_mu, rw_v0, rw_v1, rw_v2,
              ca_rel_bias, moe_router_w, moe_router_b, moe_w1, moe_b1, moe_w2, moe_b2):
    B, S, D = x.shape
    cos, sin = rope_tables(positions)
    mod_shared = jax.nn.silu(c) @ mod_w + mod_b
    v_first = None
    for l in range(DEPTH):
        mod = mod_shared + mod_offset[l]
        sh1, sc1, g1, sh2, sc2, g2 = [m[:, None, :] for m in jnp.split(mod, N_MOD, axis=-1)]

        h = layer_norm(x) * (1 + sc1) + sh1
        u_conv, cq, ckv, kpe, rkv, qkv = jnp.split(h @ w_in[l], IN_OFFSETS, axis=-1)
        y_conv = conv_module(u_conv, conv_dw[l], conv_b[l], conv_ln_g[l], conv_ln_b[l])
        y_mla = mla(cq, ckv, kpe, mla_q_norm[l], mla_kv_norm[l], mla_w_uq[l], mla_w_ukv[l], cos, sin)
        r, k, v = jnp.split(rkv, 3, axis=-1)
        vres = None if l == 0 else (rw_vres_mu[l - 1], rw_v0[l - 1], rw_v1[l - 1], rw_v2[l - 1])
        y_rw, v_first = rwkv7_time_mix(h, r, k, v, v_first, rw_mu_rkv[l], rw_mu_x[l], rw_w0[l],
                                       rw_w1[l], rw_w2[l], rw_a0[l], rw_a1[l], rw_a2[l],
                                       rw_g1[l], rw_g2[l], rw_k_k[l], rw_k_a[l], rw_r_k[l],
                                       rw_gn_g[l], rw_gn_b[l], vres)
        qa, ka, va = jnp.split(qkv, 3, axis=-1)
        y_ca = chunk_band_attention(qa, ka, va, ca_rel_bias[l])
        y = jnp.concatenate([y_conv, y_mla, y_rw, y_ca], axis=-1) @ w_out[l]
        x = layer_norm(DN_ALPHA * x + (1 + g1) * y, ln_post_g[l, 0], ln_post_b[l, 0])

        h = layer_norm(x) * (1 + sc2) + sh2
        y = moe(h.reshape(B * S, D), moe_router_w[l], moe_router_b[l], moe_w1[l], moe_b1[l],
                moe_w2[l], moe_b2[l]).reshape(B, S, D)
        x = layer_norm(DN_ALPHA * x + (1 + g2) * y, ln_post_g[l, 1], ln_post_b[l, 1])
    return x
```

```python
import numpy as np
from contextlib import ExitStack
import concourse.bass as bass
import concourse.mybir as mybir
from concourse.bass_utils import run_bass_kernel_spmd

F32 = mybir.dt.float32
BF16 = mybir.dt.bfloat16
I32 = mybir.dt.int32
AF = mybir.ActivationFunctionType
ALU = mybir.AluOpType
AX = mybir.AxisListType

ENGS = ("pe", "act", "dve", "pool", "sp")
ND = 6

D = 2048
SEQ = 4096
NB = 4
TOK = 2048
HALO = 128
TT = TOK + HALO
DIN = 4800
LN_EPS = 1e-5
DEPTH = 2
DN_ALPHA = (2 * DEPTH) ** 0.25


class _Rec:
    def __init__(self):
        self.calls = []

    def __getattr__(self, name):
        def f(*a, **kw):
            self.calls.append((name, a, kw))
        return f


class Prog:
    def __init__(self):
        self.nc = bass.Bass("TRN2", target_bir_lowering=False)
        self.es = ExitStack()
        self.ops = {e: [] for e in ENGS}
        self.cnt = {e: 0 for e in ENGS}
        self.dcnt = {e: 0 for e in ENGS}
        self.waited = {e: {} for e in ENGS}
        self.lastw = {}
        self.readers = {}
        self.sems = {}
        self.n_t = 0
        self.rr = {}
        self.same_engine_sync = True

    def sem(self, key):
        if key not in self.sems:
            nm = "s_" + "_".join(str(x) for x in (key if isinstance(key, tuple) else (key,)))
            self.sems[key] = self.es.enter_context(self.nc.semaphore(nm))
        return self.sems[key]

    def sb(self, shape, dt=F32, name=None):
        self.n_t += 1
        return self.es.enter_context(self.nc.sbuf_tensor("sb_" + (name or "t%d" % self.n_t), list(shape), dt))

    def ps(self, shape, dt=F32, name=None):
        self.n_t += 1
        return self.es.enter_context(self.nc.psum_tensor("ps_" + (name or "p%d" % self.n_t), list(shape), dt))

    def din(self, name, shape, dt=F32):
        return self.nc.dram_tensor(name, list(shape), dt, kind="ExternalInput")

    def dout(self, name, shape, dt=F32):
        return self.nc.dram_tensor(name, list(shape), dt, kind="ExternalOutput")

    def pool(self, name, n, shape, dt=F32, psum=False):
        bufs = [(self.ps if psum else self.sb)(shape, dt, name="%s%d" % (name, i)) for i in range(n)]
        self.rr[name] = 0

        def nxt():
            b = bufs[self.rr[name] % n]
            self.rr[name] += 1
            return b
        return nxt

    @staticmethod
    def _k(k):
        if isinstance(k, (str, int)):
            return k
        if isinstance(k, tuple):
            return tuple(Prog._k(x) for x in k)
        return "T:" + k.name

    def _tok_wait(self, tok):
        if tok[0] == "c":
            return (("c", tok[1]), tok[2] + 1)
        q, d = tok[1], tok[2]
        return (("d", q, d % ND), 16 * (d // ND + 1))

    def _deps(self, eng, reads, writes):
        toks = set()
        for k in list(reads) + list(writes):
            if k in self.lastw:
                toks.add(self.lastw[k])
        for k in writes:
            for t in self.readers.get(k, ()):
                toks.add(t)
        waits = []
        for t in toks:
            if t[0] == "c" and t[1] == eng and (eng == "pe" or not self.same_engine_sync):
                continue
            sk, v = self._tok_wait(t)
            if self.waited[eng].get(sk, 0) >= v:
                continue
            self.waited[eng][sk] = v
            waits.append((sk, v))
        return waits

    def _commit(self, tok, reads, writes):
        for k in writes:
            self.lastw[k] = tok
            self.readers[k] = []
        for k in reads:
            self.readers.setdefault(k, []).append(tok)

    def op(self, eng, fn, reads=(), writes=()):
        rec = _Rec()
        fn(rec)
        (mname, margs, mkw), = rec.calls
        fn = lambda e, mname=mname, margs=margs, mkw=mkw: getattr(e, mname)(*margs, **mkw)
        reads = [self._k(k) for k in reads]
        writes = [self._k(k) for k in writes]
        waits = self._deps(eng, reads, writes)
        idx = self.cnt[eng]
        self.cnt[eng] += 1
        self.ops[eng].append((waits, fn, (("c", eng), 1)))
        self._commit(("c", eng, idx), reads, writes)

    def dma(self, q, out, in_, reads=(), writes=(), **kw):
        reads = [self._k(k) for k in reads]
        writes = [self._k(k) for k in writes]
        d = self.dcnt[q]
        waits = self._deps(q, reads, writes)
        if d >= ND:
            sk, v = self._tok_wait(("d", q, d - ND))
            if self.waited[q].get(sk, 0) < v:
                self.waited[q][sk] = v
                waits.append((sk, v))
        self.dcnt[q] += 1
        self.ops[q].append((waits, lambda e: e.dma_start(out=out, in_=in_, **kw), (("d", q, d % ND), 16)))
        self._commit(("d", q, d), reads, writes)

    def barrier(self):
        allw = []
        for e2 in ENGS:
            if self.cnt[e2] > 0:
                allw.append((("c", e2), self.cnt[e2]))
            for d in range(max(0, self.dcnt[e2] - ND), self.dcnt[e2]):
                allw.append(self._tok_wait(("d", e2, d)))
        for e in ENGS:
            waits = []
            for sk, v in allw:
                if self.waited[e].get(sk, 0) < v:
                    self.waited[e][sk] = v
                    waits.append((sk, v))
            if waits:
                self.ops[e].append((waits, None, None))

    def scope(self):
        prog = self

        class _S:
            def __enter__(s2):
                s2.outer = prog.es
                prog.es = ExitStack()
                return s2

            def __exit__(s2, *a):
                prog.barrier()
                prog.es.close()
                prog.es = s2.outer
                return False
        return _S()

    def finish(self, final_keys):
        final_keys = [self._k(k) for k in final_keys]
        waits = self._deps("sp", final_keys, ())
        self.ops["sp"].append((waits, None, None))
        nc = self.nc
        for e in ENGS:
            for waits, fn, inc in self.ops[e]:
                for sk, v in waits:
                    self.sem(sk)
                if inc is not None:
                    self.sem(inc[0])
        with nc.Block() as block:
            def emit(eng_name):
                def body(e):
                    for waits, fn, inc in self.ops[eng_name]:
                        for sk, v in waits:
                            e.wait_ge(self.sem(sk), v)
                        if fn is not None:
                            fn(e).then_inc(self.sem(inc[0]), inc[1])
                return body
            block.tensor(emit("pe"))
            block.scalar(emit("act"))
            block.vector(emit("dve"))
            block.gpsimd(emit("pool"))
            block.sync(emit("sp"))
        self.es.close()
        return nc


def fm(v):
    v = np.asarray(v, np.float32).reshape(-1, 128)
    return np.ascontiguousarray(v.T)


PKA = {}
_o = 0
for _n, _w in (("modb", 96), ("modoff", 96), ("cT", 16), ("dw", 124), ("convb", 4), ("clng", 4),
               ("clnb", 4), ("qn", 3), ("kvn", 2), ("murkv", 12), ("mux", 48), ("w0", 4), ("a0", 4),
               ("kk", 4), ("ka", 4), ("rk", 4), ("v0", 4), ("vmu", 16), ("invf", 1), ("flag", 1)):
    PKA[_n] = (_o, _w)
    _o += _w
NPKA = _o


def pack_A(inp, l, b, hf):
    pk = np.zeros((128, NPKA), np.float32)

    def put(n, a):
        o, w = PKA[n]
        a = np.asarray(a, np.float32).reshape(128, w)
        pk[:, o:o + w] = a
    put("modb", inp["modsh"][b])
    put("modoff", fm(inp["mod_offset"][l]))
    put("dw", np.transpose(inp["conv_dw"][l].T.reshape(4, 128, 31), (1, 0, 2)))
    put("convb", fm(inp["conv_b"][l]))
    put("clng", fm(inp["conv_ln_g"][l]))
    put("clnb", fm(inp["conv_ln_b"][l]))
    put("qn", fm(inp["mla_q_norm"][l]))
    put("kvn", fm(inp["mla_kv_norm"][l]))
    put("murkv", np.transpose(inp["rw_mu_rkv"][l].reshape(3, 4, 128), (2, 0, 1)))
    put("mux", np.transpose(inp["rw_mu_x"][l].reshape(3, 16, 128), (2, 0, 1)))
    put("w0", fm(inp["rw_w0"][l]))
    put("a0", fm(inp["rw_a0"][l]))
    put("kk", fm(inp["rw_k_k"][l]))
    put("ka", fm(inp["rw_k_a"][l]))
    put("rk", fm(inp["rw_r_k"][l].reshape(-1)))
    if l > 0:
        put("v0", fm(inp["rw_v0"][l - 1]))
        put("vmu", fm(inp["rw_vres_mu"][l - 1]))
    inv = (10000.0 ** (-np.arange(0, 64, 2, dtype=np.float32) / 64)).astype(np.float32)
    iv = np.zeros((128, 1), np.float32)
    iv[0:32, 0] = inv
    iv[32:64, 0] = inv
    put("invf", iv)
    put("flag", np.full((128, 1), 0.0 if hf == 0 else 1.0, np.float32))
    return pk


TWO_PI = float(2 * np.pi)
CW1 = 6.28125
CW2 = float(np.float32(2 * np.pi - 6.28125).view(np.int32) & ~0xFFF) if False else 0.0019350052
_c2 = np.float32(2 * np.pi - CW1)
_c2i = np.frombuffer(np.float32(_c2).tobytes(), np.uint32)[0] & np.uint32(0xFFFFF000)
CW2 = float(np.frombuffer(np.uint32(_c2i).tobytes(), np.float32)[0])
CW3 = float(2 * np.pi - CW1 - CW2)
MAGIC = 12582912.0


def build_A(l, dbg=False):
    P = Prog()
    xh = P.din("xh", [TT, D])
    pk_d = P.din("pk", [128, NPKA])
    pos_d = P.din("pos", [1, TOK], I32)
    ident_d = P.din("ident", [128, 128])
    win_d = P.din("w_in", [D, DIN])
    wuq_d = P.din("w_uq", [384, 768])
    wukv_d = P.din("w_ukv", [256, 1024])
    w1_d = P.din("rw_w1", [D, 96]); w2_d = P.din("rw_w2", [96, 512])
    a1_d = P.din("rw_a1", [D, 96]); a2_d = P.din("rw_a2", [96, 512])
    g1_d = P.din("rw_g1", [D, 256]); g2_d = P.din("rw_g2", [256, 512])
    if l > 0:
        v1_d = P.din("rw_v1", [D, 64]); v2_d = P.din("rw_v2", [64, 512])
        vf_d = P.din("vfirstT", [512, TOK])
    o_modp = P.dout("modp", [128, 96])
    o_yconv = P.dout("yconvT", [512, TOK], BF16)
    o_mq = P.dout("mla_qT", [4, 192, TOK], BF16)
    o_mk = P.dout("mla_kT", [4, 128, TOK], BF16)
    o_mkpe = P.dout("mla_kpeT", [64, TOK], BF16)
    o_mv = P.dout("mla_v", [TOK, 512], BF16)
    o_cq = P.dout("ca_qT", [512, TOK], BF16)
    o_ck = P.dout("ca_kT", [512, TOK], BF16)
    o_cv = P.dout("ca_v", [TOK, 512], BF16)
    o_rtm = {n: P.dout("rw_%s_tm" % n, [TOK, 512]) for n in ("r", "w", "k", "kk", "kka")}
    rfm_names = ("v", "bonus", "g") + (("vfirst",) if l == 0 else ())
    o_rfm = {n: P.dout("rw_%sT" % n, [512, TOK]) for n in rfm_names}
    finals = ["o_modp", "o_yconv", "o_mq", "o_mk", "o_mkpe", "o_mv", "o_cq", "o_ck", "o_cv"] + \
        ["o_rtm_" + n for n in o_rtm] + ["o_rfm_" + n for n in o_rfm]

    pk = P.sb([128, NPKA], name="pk")
    ident = P.sb([128, 128], name="ident")
    ones = P.sb([128, 128], name="ones")
    bones = P.sb([128, 128], name="bones")
    epsc = P.sb([128, 4], name="epsc")
    hT = P.sb([128, 16, TT], BF16, name="hT")
    modp = P.sb([128, 96], name="modp")
    sc1p = P.sb([128, 16], name="sc1p")
    cosT = P.sb([64, TOK], name="cosT")
    sinT = P.sb([64, TOK], name="sinT")
    wkb = P.pool("wkb", 6, [128, 512], BF16)
    wb = P.pool("wb", 4, [128, 16, 128], BF16)
    pp = P.pool("pp", 7, [128, 512], psum=True)

    def col(n, j=0, w=1):
        o, _ = PKA[n]
        return pk[:, o + j:o + j + w]

    P.dma("sp", pk[:], pk_d.ap(), writes=[pk])
    P.dma("sp", ident[:], ident_d.ap(), writes=[ident])
    P.op("pool", lambda e: e.memset(epsc[:, 0:1], LN_EPS), writes=[epsc])
    P.op("pool", lambda e: e.memset(epsc[:, 1:2], 1e-6), reads=[epsc], writes=[epsc])
    P.op("pool", lambda e: e.memset(epsc[:, 2:3], 64e-5), reads=[epsc], writes=[epsc])
    P.op("pool", lambda e: e.memset(ones[:], 1.0), writes=[ones])
    P.op("pool", lambda e: e.memset(bones[:], 0.0), writes=[bones])
    P.op("pool", lambda e: e.memset(bones[0:64, 0:64], 1.0), reads=[bones], writes=[bones])
    P.op("pool", lambda e: e.memset(bones[64:128, 64:128], 1.0), reads=[bones], writes=[bones])

    NTI = TT // 128
    hkeys = [("hT", i) for i in range(NTI)]
    with P.scope():
        big = P.sb([128, 8192], name="big")
        junk = P.sb([128, D], BF16, name="junk")
        st = P.pool("st", 8, [128, 4])
        P.op("dve", lambda e: e.tensor_tensor(modp[:], col("modb", 0, 96), col("modoff", 0, 96), ALU.add),
             reads=[pk], writes=[modp])
        P.op("dve", lambda e: e.tensor_scalar_add(sc1p[:], modp[:, 16:32], 1.0), reads=[modp], writes=[sc1p])
        P.dma("sp", o_modp.ap(), modp[:], reads=[modp], writes=["o_modp"])
        P.barrier()
        posi = big[0:64, 4096:6144].bitcast(I32)
        ang = big[0:64, 0:2048]
        rn = big[0:64, 2048:4096]
        P.dma("sp", posi, pos_d.ap().partition_broadcast(64), writes=["posi"])
        P.op("dve", lambda e: e.tensor_copy(ang, posi), reads=["posi"], writes=["ang"])
        P.op("dve", lambda e: e.tensor_scalar_mul(ang, ang, col("invf")[0:64, :]), reads=["ang", pk], writes=["ang"])
        P.op("dve", lambda e: e.tensor_scalar(rn, ang, 1.0 / TWO_PI, MAGIC, ALU.mult, ALU.add), reads=["ang"], writes=["rn"])
        P.op("dve", lambda e: e.tensor_scalar_add(rn, rn, -MAGIC), reads=["rn"], writes=["rn"])
        for cw in (CW1, CW2, CW3):
            P.op("dve", lambda e, cw=cw: e.scalar_tensor_tensor(ang, rn, -cw, ang, ALU.mult, ALU.add),
                 reads=["rn", "ang"], writes=["ang"])
        P.op("dve", lambda e: e.tensor_scalar(sinT[:], ang, float(np.pi), -float(np.pi), ALU.min, ALU.max),
             reads=["ang"], writes=[sinT])
        P.op("dve", lambda e: e.tensor_scalar_add(cosT[:], sinT[:], float(np.pi / 2)), reads=[sinT], writes=[cosT])
        P.op("dve", lambda e: e.tensor_scalar(rn, cosT[:], float(np.pi), -TWO_PI, ALU.is_gt, ALU.mult), reads=[cosT], writes=["rn"])
        P.op("dve", lambda e: e.tensor_tensor(cosT[:], cosT[:], rn, ALU.add), reads=[cosT, "rn"], writes=[cosT])
        P.op("dve", lambda e: e.tensor_scalar(cosT[:], cosT[:], float(np.pi), -float(np.pi), ALU.min, ALU.max), reads=[cosT], writes=[cosT])
        P.op("act", lambda e: e.activation(sinT[:], sinT[:], AF.Sin), reads=[sinT], writes=[sinT])
        P.op("act", lambda e: e.activation(cosT[:], cosT[:], AF.Sin), reads=[cosT], writes=[cosT])
        P.barrier()
        for i in range(NTI):
            xv = big[:, (i % 2) * 2048:(i % 2 + 1) * 2048]
            xk = ("xt", i % 2)
            P.dma("sp" if i % 2 == 0 else "act", xv, xh.ap()[i * 128:(i + 1) * 128, :], writes=[xk])
            s = st()
            P.op("act", lambda e, xv=xv, s=s: e.activation(junk[:], xv, AF.Identity, accum_out=s[:, 0:1]),
                 reads=[xk], writes=[junk, (s, 0)])
            P.op("act", lambda e, xv=xv, s=s: e.activation(junk[:], xv, AF.Square, accum_out=s[:, 1:2]),
                 reads=[xk], writes=[junk, (s, 1)])
            P.op("dve", lambda e, s=s: e.tensor_scalar_mul(s[:, 0:2], s[:, 0:2], 1.0 / D), reads=[(s, 0), (s, 1)], writes=[(s, 0), (s, 1)])
            P.op("dve", lambda e, s=s: e.tensor_tensor(s[:, 2:3], s[:, 0:1], s[:, 0:1], ALU.mult), reads=[(s, 0)], writes=[(s, 2)])
            P.op("dve", lambda e, s=s: e.tensor_tensor(s[:, 1:2], s[:, 1:2], s[:, 2:3], ALU.subtract), reads=[(s, 1), (s, 2)], writes=[(s, 1)])
            P.op("act", lambda e, s=s: e.activation(s[:, 1:2], s[:, 1:2], AF.Sqrt, bias=epsc[:, 0:1]), reads=[(s, 1), epsc], writes=[(s, 1)])
            P.op("dve", lambda e, s=s: e.reciprocal(s[:, 1:2], s[:, 1:2]), reads=[(s, 1)], writes=[(s, 1)])
            P.op("dve", lambda e, xv=xv, s=s: e.tensor_scalar(xv, xv, s[:, 0:1], s[:, 1:2], ALU.subtract, ALU.mult),
                 reads=[xk, (s, 0), (s, 1)], writes=[xk])
            for g in range(4):
                pt = pp()
                for q in range(4):
                    k = g * 4 + q
                    P.op("pe", lambda e, pt=pt, q=q, k=k, xv=xv: e.transpose(pt[:, q * 128:(q + 1) * 128], xv[:, k * 128:(k + 1) * 128], ident[:]),
                         reads=[xk, ident], writes=[pt])
                for q in range(4):
                    k = g * 4 + q
                    P.op("act", lambda e, pt=pt, q=q, k=k, i=i: e.activation(
                        hT[:, k, i * 128:(i + 1) * 128], pt[:, q * 128:(q + 1) * 128], AF.Identity,
                        bias=modp[:, k:k + 1], scale=sc1p[:, k:k + 1]),
                        reads=[pt, modp, sc1p], writes=[("hT", i)])
        P.op("dve", lambda e: e.tensor_scalar_mul(hT[:, :, 0:128], hT[:, :, 0:128], col("flag")), reads=[("hT", 0), pk], writes=[("hT", 0)])

    cnt = {"q": 0}

    def dq():
        cnt["q"] += 1
        return ("sp", "act")[cnt["q"] % 2]

    def load_wblock(src_ap, ncols=128, rows=16):
        w = wb()
        P.dma("pool", w[:, 0:rows, 0:ncols], src_ap.rearrange("(k p) n -> p k n", p=128), writes=[w])
        return w

    def proj(w, ncols, t0, n, shift=0):
        ps = pp()
        for k in range(16):
            P.op("pe", lambda e, ps=ps, w=w, k=k: e.matmul(ps[0:ncols, 0:n], w[:, k, 0:ncols], hT[:, k, t0 - shift:t0 - shift + n],
                                                       start=(k == 0), stop=(k == 15)),
                 reads=[w] + hkeys, writes=[ps])
        return ps

    def evac(dst, src, i, reads, writes):
        if i % 2 == 0:
            P.op("act", lambda e: e.activation(dst, src, AF.Copy), reads=reads, writes=writes)
        else:
            P.op("dve", lambda e: e.tensor_copy(dst, src), reads=reads, writes=writes)

    TILES = [(HALO + n * 512, 512) for n in range(4)]

    with P.scope():
        for name, c0, o_d in (("cq", 3264, o_cq), ("ck", 3776, o_ck)):
            for j in range(4):
                w = load_wblock(win_d.ap()[:, c0 + j * 128:c0 + (j + 1) * 128])
                for n, (t0, nn) in enumerate(TILES):
                    ps = proj(w, 128, t0, nn)
                    ob = wkb()
                    evac(ob[:], ps[:], n, [ps], [ob])
                    P.dma(dq(), o_d.ap()[j * 128:(j + 1) * 128, t0 - HALO:t0 - HALO + nn], ob[:], reads=[ob], writes=["o_" + name])
        wv = P.sb([128, 16, 512], BF16, name="wv")
        P.dma("pool", wv[:], win_d.ap()[:, 4288:4800].rearrange("(k p) n -> p k n", p=128), writes=[wv])
        for i in range(16):
            ps = pp()
            for k in range(16):
                P.op("pe", lambda e, ps=ps, k=k, i=i: e.matmul(ps[:], hT[:, k, HALO + i * 128:HALO + (i + 1) * 128], wv[:, k, :],
                                                            start=(k == 0), stop=(k == 15)),
                     reads=[wv] + hkeys, writes=[ps])
            ob = wkb()
            evac(ob[:], ps[:], i, [ps], [ob])
            P.dma(dq(), o_cv.ap()[i * 128:(i + 1) * 128, :], ob[:], reads=[ob], writes=["o_cv"])

    with P.scope():
        cqs = P.sb([128, 5, TOK], name="cqs")
        wuq = P.sb([128, 3, 768], BF16, name="wuq")
        wuqr = P.sb([128, 3, 4, 64], BF16, name="wuqr")
        wukv = P.sb([128, 2, 1024], BF16, name="wukv")
        wukvv = P.sb([128, 2, 512], BF16, name="wukvv")
        wk = P.pool("wkm", 10, [128, 512])
        cqn_p = P.pool("cqn", 2, [128, 3, 512], BF16)
        ckvn_p = P.pool("ckvn", 2, [128, 2, 512], BF16)
        P.dma("pool", wuq[:], wuq_d.ap().rearrange("(k p) n -> p k n", p=128), writes=[wuq])
        P.dma("pool", wukv[:], wukv_d.ap().rearrange("(k p) n -> p k n", p=128), writes=[wukv])
        for k in range(3):
            for h in range(4):
                P.op("dve", lambda e, k=k, h=h: e.tensor_scalar_mul(wuqr[:, k, h, 0:32], wuq[:, k, h * 192 + 160:h * 192 + 192], -1.0),
                     reads=[wuq], writes=[wuqr])
                P.op("dve", lambda e, k=k, h=h: e.tensor_copy(wuqr[:, k, h, 32:64], wuq[:, k, h * 192 + 128:h * 192 + 160]),
                     reads=[wuq], writes=[wuqr])
        for k in range(2):
            for h in range(4):
                P.op("dve", lambda e, k=k, h=h: e.tensor_copy(wukvv[:, k, h * 128:(h + 1) * 128], wukv[:, k, h * 256 + 128:h * 256 + 256]),
                     reads=[wukv], writes=[wukvv])
        for j in range(5):
            w = load_wblock(win_d.ap()[:, 1024 + j * 128:1024 + (j + 1) * 128])
            for n, (t0, nn) in enumerate(TILES):
                ps = proj(w, 128, t0, nn)
                evac(cqs[:, j, t0 - HALO:t0 - HALO + nn], ps[:], n, [ps], [("cqs", j, n)])
        wkp = load_wblock(win_d.ap()[:, 1664:1728], ncols=64)
        wkr = wb()
        P.op("dve", lambda e: e.tensor_scalar_mul(wkr[:, :, 0:32], wkp[:, :, 32:64], -1.0), reads=[wkp], writes=[wkr])
        P.op("dve", lambda e: e.tensor_copy(wkr[:, :, 32:64], wkp[:, :, 0:32]), reads=[wkp, wkr], writes=[wkr])

        def rope_out(ps1, ps2, m0, dst_ap, okey):
            t1 = wk(); t2 = wk(); ob = wkb()
            P.op("dve", lambda e: e.tensor_tensor(t1[0:64, :], ps1[0:64, :], cosT[:, m0:m0 + 512], ALU.mult), reads=[ps1, cosT], writes=[t1])
            P.op("dve", lambda e: e.tensor_tensor(t2[0:64, :], ps2[0:64, :], sinT[:, m0:m0 + 512], ALU.mult), reads=[ps2, sinT], writes=[t2])
            P.op("dve", lambda e: e.tensor_tensor(ob[0:64, :], t1[0:64, :], t2[0:64, :], ALU.add), reads=[t1, t2], writes=[ob])
            P.dma(dq(), dst_ap, ob[0:64, :], reads=[ob], writes=[okey])

        for n, (t0, nn) in enumerate(TILES):
            m0 = t0 - HALO
            ps1 = proj(wkp, 64, t0, nn)
            ps2 = proj(wkr, 64, t0, nn)
            rope_out(ps1, ps2, m0, o_mkpe.ap()[:, m0:m0 + 512], "o_mkpe")
        for n, (t0, nn) in enumerate(TILES):
            m0 = t0 - HALO
            normed = []
            for (j0, nj, pkn, inv_d, pool_) in ((0, 3, "qn", 1.0 / 384, cqn_p), (3, 2, "kvn", 1.0 / 256, ckvn_p)):
                pss = pp()
                for j in range(nj):
                    sq = wk()
                    P.op("act", lambda e, sq=sq, j=j: e.activation(sq[:], cqs[:, j0 + j, m0:m0 + 512], AF.Square),
                         reads=[("cqs", j0 + j, n)], writes=[sq])
                    P.op("pe", lambda e, sq=sq, j=j, pss=pss: e.matmul(pss[:], ones[:], sq[:], start=(j == 0), stop=(j == nj - 1)),
                         reads=[sq, ones], writes=[pss])
                rq = wk()
                P.op("act", lambda e, rq=rq, pss=pss: e.activation(rq[:], pss[:], AF.Sqrt, bias=epsc[:, 1:2], scale=inv_d),
                     reads=[pss, epsc], writes=[rq])
                P.op("dve", lambda e, rq=rq: e.reciprocal(rq[:], rq[:]), reads=[rq], writes=[rq])
                nb_ = pool_()
                for j in range(nj):
                    P.op("dve", lambda e, j=j, nb_=nb_, rq=rq: e.scalar_tensor_tensor(
                        nb_[:, j, :], cqs[:, j0 + j, m0:m0 + 512], col(pkn, j), rq[:], ALU.mult, ALU.mult),
                        reads=[("cqs", j0 + j, n), pk, rq], writes=[nb_])
                normed.append(nb_)
            cqn, ckvn = normed
            for h in range(4):
                ps = pp()
                for k in range(3):
                    P.op("pe", lambda e, ps=ps, k=k, h=h: e.matmul(ps[:], wuq[:, k, h * 192:h * 192 + 128], cqn[:, k, :], start=(k == 0), stop=(k == 2)),
                         reads=[wuq, cqn], writes=[ps])
                ob = wkb()
                evac(ob[:], ps[:], h, [ps], [ob])
                P.dma(dq(), o_mq.ap()[h, 0:128, m0:m0 + 512], ob[:], reads=[ob], writes=["o_mq"])
                ps1 = pp(); ps2 = pp()
                for k in range(3):
                    P.op("pe", lambda e, ps1=ps1, k=k, h=h: e.matmul(ps1[0:64, :], wuq[:, k, h * 192 + 128:h * 192 + 192], cqn[:, k, :], start=(k == 0), stop=(k == 2)),
                         reads=[wuq, cqn], writes=[ps1])
                for k in range(3):
                    P.op("pe", lambda e, ps2=ps2, k=k, h=h: e.matmul(ps2[0:64, :], wuqr[:, k, h, :], cqn[:, k, :], start=(k == 0), stop=(k == 2)),
                         reads=[wuqr, cqn], writes=[ps2])
                rope_out(ps1, ps2, m0, o_mq.ap()[h, 128:192, m0:m0 + 512], "o_mq")
                ps = pp()
                for k in range(2):
                    P.op("pe", lambda e, ps=ps, k=k, h=h: e.matmul(ps[:], wukv[:, k, h * 256:h * 256 + 128], ckvn[:, k, :], start=(k == 0), stop=(k == 1)),
                         reads=[wukv, ckvn], writes=[ps])
                ob = wkb()
                evac(ob[:], ps[:], h + 1, [ps], [ob])
                P.dma(dq(), o_mk.ap()[h, :, m0:m0 + 512], ob[:], reads=[ob], writes=["o_mk"])
            for s in range(4):
                ps = pp()
                for k in range(2):
                    P.op("pe", lambda e, ps=ps, k=k, s=s: e.matmul(ps[:], ckvn[:, k, s * 128:(s + 1) * 128], wukvv[:, k, :], start=(k == 0), stop=(k == 1)),
                         reads=[wukvv, ckvn], writes=[ps])
                ob = wkb()
                evac(ob[:], ps[:], s, [ps], [ob])
                P.dma(dq(), o_mv.ap()[m0 + s * 128:m0 + (s + 1) * 128, :], ob[:], reads=[ob], writes=["o_mv"])

    with P.scope():
        u = P.sb([128, TT], name="u")
        uc = P.sb([128, 4, TOK], name="uc")
        wk = P.pool("wkc", 10, [128, 512])
        CT = [(0, 512), (512, 512), (1024, 512), (1536, 512), (2048, 128)]
        for j in range(4):
            wva = load_wblock(win_d.ap()[:, j * 128:(j + 1) * 128])
            wga = load_wblock(win_d.ap()[:, 512 + j * 128:512 + (j + 1) * 128])
            for n, (t0, nn) in enumerate(CT):
                psv = proj(wva, 128, t0, nn)
                psg = proj(wga, 128, t0, nn)
                sg = wk()
                P.op("act", lambda e, sg=sg, psg=psg, nn=nn: e.activation(sg[:, 0:nn], psg[:, 0:nn], AF.Sigmoid), reads=[psg], writes=[sg])
                P.op("dve", lambda e, sg=sg, psv=psv, t0=t0, nn=nn: e.tensor_tensor(u[:, t0:t0 + nn], psv[:, 0:nn], sg[:, 0:nn], ALU.mult),
                     reads=[psv, sg], writes=[("u", n)])
            ukeys = [("u", n) for n in range(5)]
            o_dw = PKA["dw"][0]
            P.op("dve", lambda e, j=j: e.tensor_scalar(uc[:, j, :], u[:, 98:98 + TOK], pk[:, o_dw + j * 31:o_dw + j * 31 + 1], col("convb", j), ALU.mult, ALU.add),
                 reads=ukeys + [pk], writes=[("uc", j)])
            for i in range(1, 31):
                P.op("dve", lambda e, j=j, i=i: e.scalar_tensor_tensor(uc[:, j, :], u[:, 98 + i:98 + i + TOK], pk[:, o_dw + j * 31 + i:o_dw + j * 31 + i + 1], uc[:, j, :], ALU.mult, ALU.add),
                     reads=ukeys + [pk, ("uc", j)], writes=[("uc", j)])
        uckeys = [("uc", j) for j in range(4)]
        for n in range(4):
            m0 = n * 512
            pssum = pp(); pssq = pp()
            for j in range(4):
                sq = wk()
                P.op("act", lambda e, sq=sq, j=j: e.activation(sq[:], uc[:, j, m0:m0 + 512], AF.Square), reads=[("uc", j)], writes=[sq])
                P.op("pe", lambda e, j=j, pssum=pssum: e.matmul(pssum[:], ones[:], uc[:, j, m0:m0 + 512], start=(j == 0), stop=(j == 3)),
                     reads=[("uc", j), ones], writes=[pssum])
                P.op("pe", lambda e, j=j, pssq=pssq, sq=sq: e.matmul(pssq[:], ones[:], sq[:], start=(j == 0), stop=(j == 3)),
                     reads=[sq, ones], writes=[pssq])
            mean = wk(); msq = wk(); rstd = wk()
            P.op("act", lambda e, mean=mean, pssum=pssum: e.activation(mean[:], pssum[:], AF.Copy, scale=1.0 / 512), reads=[pssum], writes=[mean])
            P.op("dve", lambda e, mean=mean, msq=msq: e.tensor_tensor(msq[:], mean[:], mean[:], ALU.mult), reads=[mean], writes=[msq])
            P.op("dve", lambda e, msq=msq, pssq=pssq, rstd=rstd: e.scalar_tensor_tensor(rstd[:], pssq[:], 1.0 / 512, msq[:], ALU.mult, ALU.subtract),
                 reads=[pssq, msq], writes=[rstd])
            P.op("act", lambda e, rstd=rstd: e.activation(rstd[:], rstd[:], AF.Sqrt, bias=epsc[:, 0:1]), reads=[rstd, epsc], writes=[rstd])
            P.op("dve", lambda e, rstd=rstd: e.reciprocal(rstd[:], rstd[:]), reads=[rstd], writes=[rstd])
            for j in range(4):
                t1 = wk(); ob = wkb()
                P.op("dve", lambda e, t1=t1, j=j, mean=mean: e.tensor_tensor(t1[:], uc[:, j, m0:m0 + 512], mean[:], ALU.subtract), reads=[("uc", j), mean], writes=[t1])
                P.op("dve", lambda e, t1=t1, rstd=rstd: e.tensor_tensor(t1[:], t1[:], rstd[:], ALU.mult), reads=[t1, rstd], writes=[t1])
                P.op("act", lambda e, t1=t1, ob=ob, j=j: e.activation(ob[:], t1[:], AF.Silu, bias=col("clnb", j), scale=col("clng", j)), reads=[t1, pk], writes=[ob])
                P.dma(dq(), o_yconv.ap()[j * 128:(j + 1) * 128, m0:m0 + 512], ob[:], reads=[ob], writes=["o_yconv"])

    with P.scope():
        loras = [("w", w1_d, 96, 0, AF.Tanh), ("a", a1_d, 96, 1, AF.Identity), ("g", g1_d, 256, 2, AF.Sigmoid)]
        if l > 0:
            loras.append(("v", v1_d, 64, None, AF.Identity))
        lo = {}
        for nm, _, r, _, _ in loras:
            lo[nm] = P.sb([128, (r + 127) // 128, TOK], BF16, name="lo_" + nm)
        with P.scope():
            stg = P.sb([128, 16, 256], name="stg")
            stb = P.sb([128, 16, 256], name="stb")
            for nm, wd, r, murow, fn in loras:
                wa = P.sb([128, 16, r], BF16, name="wa_" + nm)
                wbb = P.sb([128, 16, r], BF16, name="wb_" + nm)
                P.dma("sp", stg[:, :, 0:r], wd.ap().rearrange("(k p) n -> p k n", p=128), writes=[stg])
                for k in range(16):
                    mcol = col("mux", murow * 16 + k) if murow is not None else col("vmu", k)
                    P.op("dve", lambda e, k=k, mcol=mcol, r=r: e.tensor_scalar_mul(stb[:, k, 0:r], stg[:, k, 0:r], mcol), reads=[stg, pk], writes=[stb])
                P.op("dve", lambda e, r=r, wbb=wbb: e.tensor_copy(wbb[:], stb[:, :, 0:r]), reads=[stb], writes=[wbb])
                P.op("dve", lambda e, r=r, wa=wa: e.tensor_tensor(wa[:], stg[:, :, 0:r], stb[:, :, 0:r], ALU.subtract), reads=[stg, stb], writes=[wa])
                for n, (t0, nn) in enumerate(TILES):
                    m0 = t0 - HALO
                    for c0 in range(0, r, 128):
                        cw = min(128, r - c0)
                        ps = pp()
                        for k in range(16):
                            P.op("pe", lambda e, ps=ps, k=k, c0=c0, cw=cw, wa=wa: e.matmul(ps[0:cw, :], wa[:, k, c0:c0 + cw], hT[:, k, t0:t0 + 512], start=(k == 0), stop=False),
                                 reads=[wa] + hkeys, writes=[ps])
                        for k in range(16):
                            P.op("pe", lambda e, ps=ps, k=k, c0=c0, cw=cw, wbb=wbb: e.matmul(ps[0:cw, :], wbb[:, k, c0:c0 + cw], hT[:, k, t0 - 1:t0 - 1 + 512], start=False, stop=(k == 15)),
                                 reads=[wbb] + hkeys, writes=[ps])
                        P.op("act", lambda e, ps=ps, c0=c0, cw=cw, nm=nm, fn=fn: e.activation(lo[nm][0:cw, c0 // 128, m0:m0 + 512], ps[0:cw, :], fn),
                             reads=[ps], writes=[("lo", nm, n)])
        w2s = P.sb([96, 512], BF16, name="w2s"); a2s = P.sb([96, 512], BF16, name="a2s")
        g2s = P.sb([128, 2, 512], BF16, name="g2s")
        P.dma("pool", w2s[:], w2_d.ap(), writes=[w2s])
        P.dma("pool", a2s[:], a2_d.ap(), writes=[a2s])
        P.dma("pool", g2s[:], g2_d.ap().rearrange("(k p) n -> p k n", p=128), writes=[g2s])
        if l > 0:
            v2s = P.sb([64, 512], BF16, name="v2s")
            P.dma("pool", v2s[:], v2_d.ap(), writes=[v2s])
        wk = P.pool("wkr", 30, [128, 512])

        def fm_out(name, src, j, m0):
            P.dma(dq(), o_rfm[name].ap()[j * 128:(j + 1) * 128, m0:m0 + 512], src[:], reads=[src], writes=["o_rfm_" + name])

        tcount = [0]

        def tm_out(name, src, j, m0):
            pt = pp()
            for s in range(4):
                P.op("pe", lambda e, pt=pt, s=s: e.transpose(pt[:, s * 128:(s + 1) * 128], src[:, s * 128:(s + 1) * 128], ident[:]),
                     reads=[src, ident], writes=[pt])
            ob = wk()
            tcount[0] += 1
            evac(ob[:], pt[:], tcount[0], [pt], [ob])
            P.dma(dq(), o_rtm[name].ap()[m0:m0 + 512, j * 128:(j + 1) * 128].rearrange("(s p) c -> p s c", p=128),
                  ob[:].rearrange("p (s c) -> p s c", s=4), reads=[ob], writes=["o_rtm_" + name])

        for j in range(4):
            wr_ = load_wblock(win_d.ap()[:, 1728 + j * 128:1728 + (j + 1) * 128])
            wk_ = load_wblock(win_d.ap()[:, 2240 + j * 128:2240 + (j + 1) * 128])
            wv_ = load_wblock(win_d.ap()[:, 2752 + j * 128:2752 + (j + 1) * 128])
            for n, (t0, nn) in enumerate(TILES):
                m0 = t0 - HALO
                mixed = []
                for idx, w_ in enumerate((wr_, wk_, wv_)):
                    ps_a = proj(w_, 128, t0, 512)
                    ps_s = proj(w_, 128, t0, 512, shift=1)
                    base = wk(); dd = wk(); mx = wk()
                    P.op("act", lambda e, base=base, ps_a=ps_a: e.activation(base[:], ps_a[:], AF.Copy), reads=[ps_a], writes=[base])
                    P.op("dve", lambda e, dd=dd, ps_s=ps_s, base=base: e.tensor_tensor(dd[:], ps_s[:], base[:], ALU.subtract), reads=[ps_s, base], writes=[dd])
                    P.op("dve", lambda e, dd=dd, mx=mx, base=base, idx=idx: e.scalar_tensor_tensor(mx[:], dd[:], col("murkv", idx * 4 + j), base[:], ALU.mult, ALU.add),
                         reads=[dd, base, pk], writes=[mx])
                    mixed.append(mx)
                rp, kp, vp = mixed
                ps = pp()
                P.op("pe", lambda e, ps=ps: e.matmul(ps[:], w2s[:, j * 128:(j + 1) * 128], lo["w"][0:96, 0, m0:m0 + 512], start=True, stop=True),
                     reads=[w2s, ("lo", "w", n)], writes=[ps])
                dec = wk()
                P.op("act", lambda e, ps=ps, dec=dec: e.activation(dec[:], ps[:], AF.Sigmoid, bias=col("w0", j)), reads=[ps, pk], writes=[dec])
                P.op("act", lambda e, dec=dec: e.activation(dec[:], dec[:], AF.Exp, scale=-float(np.exp(-0.5))), reads=[dec], writes=[dec])
                ps = pp()
                P.op("pe", lambda e, ps=ps: e.matmul(ps[:], a2s[:, j * 128:(j + 1) * 128], lo["a"][0:96, 0, m0:m0 + 512], start=True, stop=True),
                     reads=[a2s, ("lo", "a", n)], writes=[ps])
                at = wk()
                P.op("act", lambda e, ps=ps, at=at: e.activation(at[:], ps[:], AF.Sigmoid, bias=col("a0", j)), reads=[ps, pk], writes=[at])
                ps = pp()
                for k in range(2):
                    P.op("pe", lambda e, ps=ps, k=k: e.matmul(ps[:], g2s[:, k, j * 128:(j + 1) * 128], lo["g"][:, k, m0:m0 + 512], start=(k == 0), stop=(k == 1)),
                         reads=[g2s, ("lo", "g", n)], writes=[ps])
                gt = wk()
                P.op("dve", lambda e, ps=ps, gt=gt: e.tensor_copy(gt[:], ps[:]), reads=[ps], writes=[gt])
                fm_out("g", gt, j, m0)
                if l > 0:
                    ps = pp()
                    P.op("pe", lambda e, ps=ps: e.matmul(ps[:], v2s[:, j * 128:(j + 1) * 128], lo["v"][0:64, 0, m0:m0 + 512], start=True, stop=True),
                         reads=[v2s, ("lo", "v", n)], writes=[ps])
                    vg = wk(); vf = wk(); vpp = wk()
                    P.op("act", lambda e, ps=ps, vg=vg: e.activation(vg[:], ps[:], AF.Sigmoid, bias=col("v0", j)), reads=[ps, pk], writes=[vg])
                    P.dma(dq(), vf[:], vf_d.ap()[j * 128:(j + 1) * 128, m0:m0 + 512], writes=[vf])
                    P.op("dve", lambda e, vf=vf, vp=vp: e.tensor_tensor(vf[:], vf[:], vp[:], ALU.subtract), reads=[vf, vp], writes=[vf])
                    P.op("dve", lambda e, vf=vf, vg=vg: e.tensor_tensor(vf[:], vf[:], vg[:], ALU.mult), reads=[vf, vg], writes=[vf])
                    P.op("dve", lambda e, vf=vf, vp=vp, vpp=vpp: e.tensor_tensor(vpp[:], vf[:], vp[:], ALU.add), reads=[vf, vp], writes=[vpp])
                else:
                    vpp = vp
                    fm_out("vfirst", vp, j, m0)
                fm_out("v", vpp, j, m0)
                kkr = wk(); sq = wk(); nrm = wk(); kkn = wk()
                P.op("dve", lambda e, kkr=kkr, kp=kp: e.tensor_scalar_mul(kkr[:], kp[:], col("kk", j)), reads=[kp, pk], writes=[kkr])
                P.op("act", lambda e, kkr=kkr, sq=sq: e.activation(sq[:], kkr[:], AF.Square), reads=[kkr], writes=[sq])
                ps = pp()
                P.op("pe", lambda e, ps=ps, sq=sq: e.matmul(ps[:], bones[:], sq[:], start=True, stop=True), reads=[bones, sq], writes=[ps])
                P.op("act", lambda e, ps=ps, nrm=nrm: e.activation(nrm[:], ps[:], AF.Sqrt), reads=[ps], writes=[nrm])
                P.op("dve", lambda e, nrm=nrm: e.tensor_scalar_max(nrm[:], nrm[:], 1e-12), reads=[nrm], writes=[nrm])
                P.op("dve", lambda e, nrm=nrm: e.reciprocal(nrm[:], nrm[:]), reads=[nrm], writes=[nrm])
                P.op("dve", lambda e, nrm=nrm, kkr=kkr, kkn=kkn: e.tensor_tensor(kkn[:], kkr[:], nrm[:], ALU.mult), reads=[kkr, nrm], writes=[kkn])
                tt = wk(); k2 = wk(); kka = wk()
                P.op("dve", lambda e, tt=tt, at=at: e.tensor_scalar(tt[:], at[:], -1.0, col("ka", j), ALU.add, ALU.mult), reads=[at, pk], writes=[tt])
                P.op("dve", lambda e, tt=tt, k2=k2, kp=kp: e.scalar_tensor_tensor(k2[:], tt[:], 1.0, kp[:], ALU.add, ALU.mult), reads=[tt, kp], writes=[k2])
                P.op("dve", lambda e, kka=kka, kkn=kkn, at=at: e.tensor_tensor(kka[:], kkn[:], at[:], ALU.mult), reads=[kkn, at], writes=[kka])
                rk = wk(); bon = wk()
                P.op("dve", lambda e, rk=rk, rp=rp, k2=k2: e.scalar_tensor_tensor(rk[:], rp[:], col("rk", j), k2[:], ALU.mult, ALU.mult), reads=[rp, k2, pk], writes=[rk])
                ps = pp()
                P.op("pe", lambda e, ps=ps, rk=rk: e.matmul(ps[:], bones[:], rk[:], start=True, stop=True), reads=[bones, rk], writes=[ps])
                P.op("dve", lambda e, ps=ps, bon=bon, vpp=vpp: e.tensor_tensor(bon[:], ps[:], vpp[:], ALU.mult), reads=[ps, vpp], writes=[bon])
                fm_out("bonus", bon, j, m0)
                for name, src in (("r", rp), ("w", dec), ("k", k2), ("kk", kkn), ("kka", kka)):
                    tm_out(name, src, j, m0)

    nc = P.finish(finals)
    return nc, P


TC = 8
NEG = -30000.0


def band_bias(rel_table_h):
    q = np.arange(128)
    a, c = q // 64, q % 64
    kk = np.arange(640)
    u, kq = kk // 64, kk % 64
    w = 64 * (u[None, :] - a[:, None]) + kq[None, :]
    valid = np.where(a[:, None] == 0, u[None, :] <= 8, u[None, :] >= 1)
    rel = np.clip((512 + c[:, None]) - w, -256, 256) + 256
    out = rel_table_h[np.clip(rel, 0, 512)].astype(np.float32)
    return np.where(valid, out, np.float32(NEG)).astype(np.float32)


def build_B(l):
    P = Prog()
    mq_d = P.din("mla_qT", [2, 192, SEQ], BF16)
    mk_d = P.din("mla_kT", [2, 128, SEQ], BF16)
    mkpe_d = P.din("mla_kpeT", [64, SEQ], BF16)
    mv_d = P.din("mla_v", [SEQ, 256], BF16)
    cq_d = P.din("ca_qT", [2, 128, SEQ], BF16)
    ck_d = P.din("ca_kT", [2, 128, SEQ], BF16)
    cv_d = P.din("ca_v", [SEQ, 256], BF16)
    bias_d = P.din("ca_bias", [2, 128, 640])
    ident_d = P.din("ident", [128, 128])
    rtm = {n: P.din("rw_%s_tm" % n, [SEQ, 256]) for n in ("r", "w", "k", "kk", "kka")}
    rv_d = P.din("rw_vT", [256, SEQ])
    rb_d = P.din("rw_bonusT", [256, SEQ])
    rg_d = P.din("rw_gT", [256, SEQ])
    gn_d = P.din("rw_gn", [128, 4])
    o_mla = P.dout("y_mlaT", [256, SEQ], BF16)
    o_ca = P.dout("y_caT", [256, SEQ], BF16)
    o_rw = P.dout("y_rwT", [256, SEQ], BF16)

    ident = P.sb([128, 128], name="ident")
    identb = P.sb([128, 128], BF16, name="identb")
    bones = P.sb([128, 128], name="bones")
    epsc = P.sb([128, 2], name="epsc")
    gn = P.sb([128, 4], name="gn")
    bias = P.sb([128, 2, 640], name="bias")
    qn = P.sb([128, SEQ], BF16, name="qn")
    qp = P.sb([64, SEQ], BF16, name="qp")
    kn = P.sb([128, SEQ], BF16, name="kn")
    kp = P.sb([64, SEQ], BF16, name="kp")
    vts = [P.sb([128, 32, 128], BF16, name="vt%d" % i) for i in range(2)]
    scb = [P.sb([128, SEQ], name="sc%d" % i) for i in range(2)]
    pb = [P.sb([128, SEQ], BF16, name="pb%d" % i) for i in range(2)]
    PT = P.pool("PT", 3, [128, 5, 128], BF16)
    outb = P.sb([128, SEQ], BF16, name="outb")
    stt_ = P.pool("stt", 6, [128, 4])
    dg = P.pool("dg", 3, [128, 128], BF16)
    psc = P.pool("psc", 3, [128, 512], psum=True)
    ppt = P.pool("ppt", 2, [128, 512], psum=True)
    pot = P.ps([128, 512], name="pot")
    S = P.sb([128, 128], name="S")
    tmp = P.sb([128, 128], name="tmp")
    nsk = P.pool("nsk", 4, [128, 2])
    bc = [P.sb([128, TC, 2, 5, 64], name="bc%d" % i) for i in range(2)]
    vsb = [P.sb([128, 2, 512], name="vsb%d" % i) for i in range(2)]
    ysb = P.sb([128, 2, SEQ], name="ysb")

    P.dma("act", ident[:], ident_d.ap(), writes=[ident])
    P.dma("act", gn[:], gn_d.ap(), writes=[gn])
    P.dma("act", bias[:], bias_d.ap().rearrange("h q k -> q h k"), writes=[bias])
    P.dma("act", kp[:], mkpe_d.ap(), writes=[kp])
    P.op("act", lambda e: e.activation(identb[:], ident[:], AF.Copy), reads=[ident], writes=[identb])
    P.op("pool", lambda e: e.memset(epsc[:, 0:1], 64e-5), writes=[epsc])
    P.op("pool", lambda e: e.memset(bones[:], 0.0), writes=[bones])
    P.op("pool", lambda e: e.memset(bones[0:64, 0:64], 1.0), reads=[bones], writes=[bones])
    P.op("pool", lambda e: e.memset(bones[64:128, 64:128], 1.0), reads=[bones], writes=[bones])
    P.op("dve", lambda e: e.memset(S[:], 0.0), writes=[S])

    dqc = [0]

    def dq():
        dqc[0] += 1
        return ("act", "pool")[dqc[0] % 2]

    units = []
    otc = [0]

    def make_unit(kind, hh, i):
        ub = len(units) % 2
        sc = scb[ub]; pbuf = pb[ub]
        vt = vts[(len(units) // 32) % 2]
        st = {}
        if kind == "mla":
            kt0 = 0
            scale = 192.0 ** -0.5
        else:
            kt0 = max(0, i - 4)
            scale = 128.0 ** -0.5
        nkt = i - kt0 + 1
        nk = nkt * 128

        def stageA():
            if i == 0:
                if kind == "mla":
                    P.dma(dq(), qn[:], mq_d.ap()[hh, 0:128, :], writes=[qn])
                    P.dma(dq(), qp[:], mq_d.ap()[hh, 128:192, :], writes=[qp])
                    P.dma(dq(), kn[:], mk_d.ap()[hh], writes=[kn])
                    P.dma(dq(), vt[:], mv_d.ap()[:, hh * 128:(hh + 1) * 128].rearrange("(n p) d -> p n d", p=128), writes=[vt])
                else:
                    P.dma(dq(), qn[:], cq_d.ap()[hh], writes=[qn])
                    P.dma(dq(), kn[:], ck_d.ap()[hh], writes=[kn])
                    P.dma(dq(), vt[:], cv_d.ap()[:, hh * 128:(hh + 1) * 128].rearrange("(n p) d -> p n d", p=128), writes=[vt])
            qs = slice(i * 128, (i + 1) * 128)
            c0 = 0
            while c0 < nk:
                n_c = min(512, nk - c0)
                ps = psc()
                k0 = kt0 * 128 + c0
                P.op("pe", lambda e: e.matmul(ps[:, 0:n_c], qn[:, qs], kn[:, k0:k0 + n_c], start=True, stop=(kind != "mla")),
                     reads=[qn, kn], writes=[ps])
                if kind == "mla":
                    P.op("pe", lambda e: e.matmul(ps[:, 0:n_c], qp[:, qs], kp[:, k0:k0 + n_c], start=False, stop=True),
                         reads=[qp, kp], writes=[ps])
                    P.op("act", lambda e: e.activation(sc[:, c0:c0 + n_c], ps[:, 0:n_c], AF.Copy), reads=[ps], writes=[sc])
                else:
                    P.op("act", lambda e: e.activation(sc[:, c0:c0 + n_c], ps[:, 0:n_c], AF.Copy, scale=scale), reads=[ps], writes=[sc])
                c0 += n_c
            if kind == "mla":
                P.op("pool", lambda e: e.memset(sc[0:64, nk - 64:nk], NEG / scale), reads=[sc], writes=[sc])
            else:
                p0 = (kt0 - (i - 4)) * 128
                P.op("pool", lambda e: e.tensor_tensor(sc[:, 0:nk], sc[:, 0:nk], bias[:, hh, p0:p0 + nk], ALU.add), reads=[sc, bias], writes=[sc])

        def stageB():
            s4 = stt_()
            st["s4"] = s4
            P.op("dve", lambda e: e.tensor_reduce(s4[:, 0:1], sc[:, 0:nk], AX.X, ALU.max), reads=[sc], writes=[s4])
            esc = scale if kind == "mla" else 1.0
            P.op("dve", lambda e: e.tensor_scalar_mul(s4[:, 1:2], s4[:, 0:1], -esc), reads=[s4], writes=[s4])
            P.op("act", lambda e: e.activation(pbuf[:, 0:nk], sc[:, 0:nk], AF.Exp, bias=s4[:, 1:2], scale=esc, accum_out=s4[:, 2:3]),
                 reads=[sc, s4], writes=[pbuf, s4])

        def stageC():
            s4 = st["s4"]
            P.op("dve", lambda e: e.reciprocal(s4[:, 3:4], s4[:, 2:3]), reads=[s4], writes=[s4])
            d = dg()
            P.op("act", lambda e: e.activation(d[:], identb[:], AF.Copy, scale=s4[:, 3:4]), reads=[identb, s4], writes=[d])
            oc = (otc[0] % 4) * 128
            otc[0] += 1
            okey = ("pot", oc)
            for g0 in range(0, nkt, 4):
                gn_ = min(4, nkt - g0)
                pt = ppt()
                for t in range(gn_):
                    kt = g0 + t
                    P.op("pe", lambda e: e.matmul(pt[:, t * 128:(t + 1) * 128], pbuf[:, kt * 128:(kt + 1) * 128], d[:], start=True, stop=True),
                         reads=[pbuf, d], writes=[pt])
                ptb = PT()
                P.op("act", lambda e: e.activation(ptb[:, 0:gn_, :], pt[:, 0:gn_ * 128].rearrange("p (t k) -> p t k", t=gn_), AF.Copy),
                     reads=[pt], writes=[ptb])
                for t in range(gn_):
                    kt = g0 + t
                    P.op("pe", lambda e: e.matmul(pot[:, oc:oc + 128], vt[:, kt0 + kt, :], ptb[:, t, :], start=(kt == 0), stop=(kt == nkt - 1)),
                         reads=[vt, ptb], writes=[okey])
            P.op("act", lambda e: e.activation(outb[:, i * 128:(i + 1) * 128], pot[:, oc:oc + 128], AF.Copy), reads=[okey], writes=[outb])
            if i == 31:
                od = o_mla if kind == "mla" else o_ca
                P.dma(dq(), od.ap()[hh * 128:(hh + 1) * 128, :], outb[:], reads=[outb], writes=["o_" + kind])
        units.append((stageA, stageB, stageC))

    for kind in ("mla", "ca"):
        for hh in range(2):
            for i in range(32):
                make_unit(kind, hh, i)

    Sg = S[:].rearrange("p (g k) -> p g k", g=2)
    tg = tmp[:].rearrange("p (g k) -> p g k", g=2)

    def scan_chunk(c):
        t0 = c * TC
        b = bc[c % 2]
        bkey = ("bc", c % 2)
        for ai, nm in enumerate(("kk", "w", "kka", "k", "r")):
            for half in range(2):
                src = rtm[nm].ap()[t0:t0 + TC, :].rearrange("t (g h k) -> t g h k", g=2, h=2)[:, :, half, :]
                P.dma("sp", b[half * 64:(half + 1) * 64, :, :, ai, :],
                      src.partition_broadcast(64), writes=[bkey])
        if t0 % 512 == 0:
            vb = vsb[(t0 // 512) % 2]
            P.dma("sp", vb[:], rv_d.ap()[:, t0:t0 + 512].rearrange("(g p) t -> p g t", p=128), writes=[vb])
        vb = vsb[(t0 // 512) % 2]
        for tt in range(TC):
            t = t0 + tt
            ns = nsk()
            P.op("dve", lambda e: e.tensor_tensor(tg, Sg, b[:, tt, :, 0, :], ALU.mult), reads=[S, bkey], writes=[tmp])
            P.op("dve", lambda e: e.tensor_reduce(ns[:], tg, AX.X, ALU.add, negate=True), reads=[tmp], writes=[ns])
            P.op("dve", lambda e: e.tensor_tensor(Sg, Sg, b[:, tt, :, 1, :], ALU.mult), reads=[S, bkey], writes=[S])
            for g in range(2):
                P.op("dve", lambda e: e.scalar_tensor_tensor(S[:, g * 64:(g + 1) * 64], b[:, tt, g, 2, :], ns[:, g:g + 1],
                                                            S[:, g * 64:(g + 1) * 64], ALU.mult, ALU.add), reads=[S, bkey, ns], writes=[S])
            for g in range(2):
                P.op("dve", lambda e: e.scalar_tensor_tensor(S[:, g * 64:(g + 1) * 64], b[:, tt, g, 3, :], vb[:, g, (t % 512):(t % 512) + 1],
                                                            S[:, g * 64:(g + 1) * 64], ALU.mult, ALU.add), reads=[S, bkey, vb], writes=[S])
            P.op("dve", lambda e: e.tensor_tensor(tg, Sg, b[:, tt, :, 4, :], ALU.mult), reads=[S, bkey], writes=[tmp])
            P.op("dve", lambda e: e.tensor_reduce(ysb[:, :, t], tg, AX.X, ALU.add), reads=[tmp], writes=[ysb])

    NCH = SEQ // TC
    per = NCH // len(units)
    for c in range(NCH):
        u = c // per if c % per == 0 else None
        if u is not None and u < len(units):
            units[u][0]()
        scan_chunk(c)
        if u is not None and u < len(units):
            units[u][1]()
            if u >= 1:
                units[u - 1][2]()
    units[-1][2]()

    wkv = [scb[0][:, i * 512:(i + 1) * 512] for i in range(8)] + [scb[1][:, i * 512:(i + 1) * 512] for i in range(8)]
    wi = [0]

    def wk():
        wi[0] += 1
        j = wi[0] % 16
        return wkv[j], ("wkv", j)

    first = [True]
    for g in range(2):
        for n in range(8):
            m0 = n * 512
            extra = ([scb[0], scb[1]] + [("wkv", j) for j in range(16)]) if first[0] else []
            first[0] = False
            sq, sqk = wk()
            P.op("act", lambda e: e.activation(sq, ysb[:, g, m0:m0 + 512], AF.Square), reads=[ysb], writes=[sqk] + extra)
            ps1 = psc(); ps2 = psc()
            P.op("pe", lambda e: e.matmul(ps1[:], bones[:], ysb[:, g, m0:m0 + 512], start=True, stop=True), reads=[bones, ysb], writes=[ps1])
            P.op("pe", lambda e: e.matmul(ps2[:], bones[:], sq, start=True, stop=True), reads=[bones, sqk], writes=[ps2])
            mean, mk_ = wk(); msq, msk = wk(); rstd, rsk = wk(); yn, ynk = wk(); bt, btk = wk(); gt, gtk = wk()
            P.op("act", lambda e: e.activation(mean, ps1[:], AF.Copy, scale=1.0 / 64), reads=[ps1], writes=[mk_])
            P.op("dve", lambda e: e.tensor_tensor(msq, mean, mean, ALU.mult), reads=[mk_], writes=[msk])
            P.op("dve", lambda e: e.scalar_tensor_tensor(rstd, ps2[:], 1.0 / 64, msq, ALU.mult, ALU.subtract), reads=[ps2, msk], writes=[rsk])
            P.op("act", lambda e: e.activation(rstd, rstd, AF.Sqrt, bias=epsc[:, 0:1]), reads=[rsk, epsc], writes=[rsk])
            P.op("dve", lambda e: e.reciprocal(rstd, rstd), reads=[rsk], writes=[rsk])
            P.op("dve", lambda e: e.tensor_tensor(yn, ysb[:, g, m0:m0 + 512], mean, ALU.subtract), reads=[ysb, mk_], writes=[ynk])
            P.op("dve", lambda e: e.tensor_tensor(yn, yn, rstd, ALU.mult), reads=[ynk, rsk], writes=[ynk])
            P.op("dve", lambda e: e.tensor_scalar(yn, yn, gn[:, g:g + 1], gn[:, 2 + g:3 + g], ALU.mult, ALU.add), reads=[ynk, gn], writes=[ynk])
            P.dma(dq(), bt, rb_d.ap()[g * 128:(g + 1) * 128, m0:m0 + 512], writes=[btk])
            P.dma(dq(), gt, rg_d.ap()[g * 128:(g + 1) * 128, m0:m0 + 512], writes=[gtk])
            P.op("dve", lambda e: e.tensor_tensor(yn, yn, bt, ALU.add), reads=[ynk, btk], writes=[ynk])
            ob = PT()
            obv = ob[:].rearrange("p a b -> p (a b)")[:, 0:512]
            P.op("dve", lambda e: e.tensor_tensor(obv, yn, gt, ALU.mult), reads=[ynk, gtk], writes=[ob])
            P.dma(dq(), o_rw.ap()[g * 128:(g + 1) * 128, m0:m0 + 512], obv, reads=[ob], writes=["o_rw"])
    nc = P.finish(["o_mla", "o_ca", "o_rw"])
    return nc, P


def inputs_B(inp, l, c, A):
    b, g = c // 2, c % 2
    cat = lambda name, ax: np.concatenate([A[2 * b][name], A[2 * b + 1][name]], axis=ax)
    m = {"mla_qT": np.ascontiguousarray(cat("mla_qT", 2)[2 * g:2 * g + 2]),
         "mla_kT": np.ascontiguousarray(cat("mla_kT", 2)[2 * g:2 * g + 2]),
         "mla_kpeT": cat("mla_kpeT", 1),
         "mla_v": np.ascontiguousarray(cat("mla_v", 0)[:, 256 * g:256 * (g + 1)]),
         "ca_qT": np.ascontiguousarray(cat("ca_qT", 1)[256 * g:256 * (g + 1)].reshape(2, 128, SEQ)),
         "ca_kT": np.ascontiguousarray(cat("ca_kT", 1)[256 * g:256 * (g + 1)].reshape(2, 128, SEQ)),
         "ca_v": np.ascontiguousarray(cat("ca_v", 0)[:, 256 * g:256 * (g + 1)]),
         "ca_bias": np.stack([band_bias(inp["ca_rel_bias"][l][2 * g + hh]) for hh in range(2)]),
         "ident": np.eye(128, dtype=np.float32),
         "rw_vT": np.ascontiguousarray(cat("rw_vT", 1)[256 * g:256 * (g + 1)]),
         "rw_bonusT": np.ascontiguousarray(cat("rw_bonusT", 1)[256 * g:256 * (g + 1)]),
         "rw_gT": np.ascontiguousarray(cat("rw_gT", 1)[256 * g:256 * (g + 1)]),
         "rw_gn": np.concatenate([fm(inp["rw_gn_g"][l][256 * g:256 * (g + 1)]), fm(inp["rw_gn_b"][l][256 * g:256 * (g + 1)])], 1)}
    for n in ("r", "w", "k", "kk", "kka"):
        m["rw_%s_tm" % n] = np.ascontiguousarray(cat("rw_%s_tm" % n, 0)[:, 256 * g:256 * (g + 1)])
    return m


def build_M():
    P = Prog()
    ct_d = P.din("cT4", [128, 64])
    mw_d = P.din("mod_w_s", [D, 1536])
    mb_d = P.din("mod_b_s", [128, 12])
    o = P.dout("modT", [128, 48])
    sct = P.sb([128, 64], name="sct")
    mb = P.sb([128, 12], name="mb")
    res = P.sb([128, 12, 4], name="res")
    blk = [P.sb([128, 16, 128], name="blk%d" % i) for i in range(3)]
    ps = P.ps([128, 48], name="pm")
    P.dma("sp", sct[:], ct_d.ap(), writes=[sct])
    P.dma("sp", mb[:], mb_d.ap(), writes=[mb])
    sct0 = sct
    sct = P.sb([128, 64], name="sct2")
    P.op("act", lambda e: e.activation(sct[:], sct0[:], AF.Silu), reads=[sct0], writes=[sct])
    for j in range(12):
        b = blk[j % 3]
        P.dma(("sp", "act")[j % 2], b[:], mw_d.ap()[:, j * 128:(j + 1) * 128].rearrange("(k p) n -> p k n", p=128), writes=[b])
        for k in range(16):
            P.op("pe", lambda e: e.matmul(ps[:, j * 4:(j + 1) * 4], b[:, k, :], sct[:, k * 4:(k + 1) * 4], start=(k == 0), stop=(k == 15)),
                 reads=[b, sct], writes=[("pm", j)])
    for j in range(12):
        P.op("dve", lambda e: e.tensor_scalar_add(res[:, j, :], ps[:, j * 4:(j + 1) * 4], mb[:, j:j + 1]), reads=[("pm", jj) for jj in range(12)] + [mb], writes=[res])
    P.dma("sp", o.ap(), res[:].rearrange("p j b -> p (j b)"), reads=[res], writes=["o"])
    return P.finish(["o"]), P


def ln_fm(P, z, zkey, n, ones, epsc_col, wk, pp, inv_d, stp):
    ps1 = pp(); ps2 = pp()
    for k in range(16):
        sq = wk()
        P.op("act", lambda e: e.activation(sq[:, 0:n], z[:, k, 0:n], AF.Square), reads=[zkey], writes=[sq])
        P.op("pe", lambda e: e.matmul(ps1[:, 0:n], ones[:], z[:, k, 0:n], start=(k == 0), stop=(k == 15)), reads=[ones, zkey], writes=[ps1])
        P.op("pe", lambda e: e.matmul(ps2[:, 0:n], ones[:], sq[:, 0:n], start=(k == 0), stop=(k == 15)), reads=[ones, sq], writes=[ps2])
    mean = stp(); msq = stp(); rstd = stp()
    P.op("act", lambda e: e.activation(mean[:, 0:n], ps1[:, 0:n], AF.Copy, scale=inv_d), reads=[ps1], writes=[mean])
    P.op("dve", lambda e: e.tensor_tensor(msq[:, 0:n], mean[:, 0:n], mean[:, 0:n], ALU.mult), reads=[mean], writes=[msq])
    P.op("dve", lambda e: e.scalar_tensor_tensor(rstd[:, 0:n], ps2[:, 0:n], inv_d, msq[:, 0:n], ALU.mult, ALU.subtract), reads=[ps2, msq], writes=[rstd])
    P.op("act", lambda e: e.activation(rstd[:, 0:n], rstd[:, 0:n], AF.Sqrt, bias=epsc_col), reads=[rstd], writes=[rstd])
    P.op("dve", lambda e: e.reciprocal(rstd[:, 0:n], rstd[:, 0:n]), reads=[rstd], writes=[rstd])
    return mean, rstd


PKC = {}
_o = 0
for _n, _w in (("modsh", 96), ("modoff", 96), ("lng", 16), ("lnb", 16), ("rb", 1)):
    PKC[_n] = (_o, _w)
    _o += _w
NPKC = _o


def pack_C(inp, l, which, modsh):
    pk = np.zeros((128, NPKC), np.float32)
    pk[:, 0:96] = modsh
    pk[:, 96:192] = fm(inp["mod_offset"][l])
    pk[:, 192:208] = fm(inp["ln_post_g"][l, which])
    pk[:, 208:224] = fm(inp["ln_post_b"][l, which])
    pk[0:32, 224] = inp["moe_router_b"][l]
    return pk


def build_C():
    P = Prog()
    yc_d = P.din("ycatT", [D, TOK], BF16)
    x_d = P.din("x", [TOK, D])
    wo_d = P.din("w_out", [D, D])
    pk_d = P.din("pk", [128, NPKC])
    rw_d = P.din("router_w", [D, 32])
    ident_d = P.din("ident", [128, 128])
    o_x1 = P.dout("x1T", [D, TOK])
    o_h2 = P.dout("h2T", [D, TOK], BF16)
    o_g = P.dout("gateT", [32, TOK])

    pk = P.sb([128, NPKC], name="pk")
    ident = P.sb([128, 128], name="ident")
    ones = P.sb([128, 128], name="ones")
    epsc = P.sb([128, 1], name="epsc")
    modp = P.sb([128, 96], name="modp")
    g1p = P.sb([128, 16], name="g1p")
    sc2p = P.sb([128, 16], name="sc2p")
    rw = P.sb([128, 16, 32], name="rw")
    z = P.sb([128, 16, 512], name="z")
    ycT = P.sb([128, 16, 512], BF16, name="ycT")
    h2b = P.sb([128, 16, 512], BF16, name="h2b")
    xt = P.pool("xt", 2, [128, D])
    wb = P.pool("wb", 4, [128, 16, 128], BF16)
    wk = P.pool("wk", 8, [128, 512])
    stp = P.pool("stp", 6, [128, 512])
    pp = P.pool("pp", 6, [128, 512], psum=True)
    plg = P.ps([32, 512], name="plg")
    ptk = P.ps([128, 512], name="ptk")
    lgs = P.sb([32, 512], name="lgs")
    lt = P.sb([128, 4, 32], name="lt")
    gt = P.sb([128, 4, 32], name="gt")
    ex = P.sb([128, 4, 32], name="ex")
    m8 = P.sb([128, 4, 8], name="m8")
    sm = P.sb([128, 4, 4], name="sm")
    gT = P.sb([32, 512], name="gT")

    def col(n, j=0, w=1):
        o, _ = PKC[n]
        return pk[:, o + j:o + j + w]

    P.dma("sp", pk[:], pk_d.ap(), writes=[pk])
    P.dma("sp", ident[:], ident_d.ap(), writes=[ident])
    P.dma("sp", rw[:], rw_d.ap().rearrange("(k p) n -> p k n", p=128), writes=[rw])
    P.op("pool", lambda e: e.memset(ones[:], 1.0), writes=[ones])
    P.op("pool", lambda e: e.memset(epsc[:], LN_EPS), writes=[epsc])
    P.op("dve", lambda e: e.tensor_tensor(modp[:], col("modsh", 0, 96), col("modoff", 0, 96), ALU.add), reads=[pk], writes=[modp])
    P.op("dve", lambda e: e.tensor_scalar_add(g1p[:], modp[:, 32:48], 1.0), reads=[modp], writes=[g1p])
    P.op("dve", lambda e: e.tensor_scalar_add(sc2p[:], modp[:, 64:80], 1.0), reads=[modp], writes=[sc2p])
    dqc = [0]

    def dq():
        dqc[0] += 1
        return ("sp", "act")[dqc[0] % 2]

    for n in range(4):
        m0 = n * 512
        P.dma(dq(), ycT[:], yc_d.ap()[:, m0:m0 + 512].rearrange("(k p) t -> p k t", p=128), writes=[ycT])
        for s in range(4):
            xx = xt()
            P.dma(dq(), xx[:], x_d.ap()[m0 + s * 128:m0 + (s + 1) * 128, :], writes=[xx])
            for g in range(4):
                pt = pp()
                for q in range(4):
                    k = g * 4 + q
                    P.op("pe", lambda e: e.transpose(pt[:, q * 128:(q + 1) * 128], xx[:, k * 128:(k + 1) * 128], ident[:]), reads=[xx, ident], writes=[pt])
                P.op("act", lambda e: e.activation(z[:, g * 4:(g + 1) * 4, s * 128:(s + 1) * 128], pt[:].rearrange("p (q t) -> p q t", q=4), AF.Copy, scale=DN_ALPHA),
                     reads=[pt], writes=["z"])
        for dc in range(16):
            w = wb()
            P.dma("pool", w[:], wo_d.ap()[:, dc * 128:(dc + 1) * 128].rearrange("(k p) n -> p k n", p=128), writes=[w])
            ps = pp()
            for k in range(16):
                P.op("pe", lambda e: e.matmul(ps[:], w[:, k, :], ycT[:, k, :], start=(k == 0), stop=(k == 15)), reads=[w, ycT], writes=[ps])
            P.op("dve", lambda e: e.scalar_tensor_tensor(z[:, dc, :], ps[:], g1p[:, dc:dc + 1], z[:, dc, :], ALU.mult, ALU.add), reads=[ps, g1p, "z"], writes=["z"])
        mean, rstd = ln_fm(P, z, "z", 512, ones, epsc[:, 0:1], wk, pp, 1.0 / D, stp)
        for k in range(16):
            t = wk()
            P.op("dve", lambda e: e.tensor_tensor(t[:], z[:, k, :], mean[:], ALU.subtract), reads=["z", mean], writes=[t])
            P.op("dve", lambda e: e.tensor_tensor(t[:], t[:], rstd[:], ALU.mult), reads=[t, rstd], writes=[t])
            P.op("dve", lambda e: e.tensor_scalar(z[:, k, :], t[:], col("lng", k), col("lnb", k), ALU.mult, ALU.add), reads=[t, pk, "z"], writes=["z"])
        P.dma(dq(), o_x1.ap()[:, m0:m0 + 512].rearrange("(k p) t -> p k t", p=128), z[:], reads=["z"], writes=["o_x1"])
        mean, rstd = ln_fm(P, z, "z", 512, ones, epsc[:, 0:1], wk, pp, 1.0 / D, stp)
        for k in range(16):
            t = wk()
            P.op("dve", lambda e: e.tensor_tensor(t[:], z[:, k, :], mean[:], ALU.subtract), reads=["z", mean], writes=[t])
            P.op("dve", lambda e: e.tensor_tensor(t[:], t[:], rstd[:], ALU.mult), reads=[t, rstd], writes=[t])
            P.op("dve", lambda e: e.tensor_scalar(t[:], t[:], sc2p[:, k:k + 1], modp[:, 48 + k:49 + k], ALU.mult, ALU.add), reads=[t, sc2p, modp], writes=[t])
            P.op("pe", lambda e: e.matmul(plg[:], rw[:, k, :], t[:], start=(k == 0), stop=(k == 15)), reads=[rw, t], writes=[plg])
            P.op("act", lambda e: e.activation(h2b[:, k, :], t[:], AF.Copy), reads=[t], writes=[h2b])
        P.dma(dq(), o_h2.ap()[:, m0:m0 + 512].rearrange("(k p) t -> p k t", p=128), h2b[:], reads=[h2b], writes=["o_h2"])
        P.op("act", lambda e: e.activation(lgs[:], plg[:], AF.Identity, bias=pk[0:32, PKC["rb"][0]:PKC["rb"][0] + 1]), reads=[plg, pk], writes=[lgs])
        for s in range(4):
            P.op("pe", lambda e: e.transpose(ptk[:, s * 32:(s + 1) * 32], lgs[:, s * 128:(s + 1) * 128], ident[0:32, 0:32]), reads=[lgs, ident], writes=[ptk])
        P.op("dve", lambda e: e.tensor_copy(lt[:], ptk[:, 0:128].rearrange("p (s e) -> p s e", s=4)), reads=[ptk], writes=[lt])
        for s in range(4):
            P.op("dve", lambda e: e.max(m8[:, s, :], lt[:, s, :]), reads=[lt], writes=[m8])
        P.op("dve", lambda e: e.tensor_scalar_mul(sm[:, :, 0:1], m8[:, :, 0:1], -1.0), reads=[m8], writes=[sm])
        for s in range(4):
            P.op("act", lambda e: e.activation(ex[:, s, :], lt[:, s, :], AF.Exp, bias=sm[:, s, 0:1]), reads=[lt, sm], writes=[ex])
            P.op("dve", lambda e: e.tensor_scalar(gt[:, s, :], lt[:, s, :], m8[:, s, 3:4], None, ALU.is_ge), reads=[lt, m8], writes=[gt])
            P.op("dve", lambda e: e.tensor_tensor(gt[:, s, :], gt[:, s, :], ex[:, s, :], ALU.mult), reads=[gt, ex], writes=[gt])
            P.op("dve", lambda e: e.tensor_reduce(sm[:, s, 1:2], gt[:, s, :], AX.X, ALU.add), reads=[gt], writes=[sm])
            P.op("dve", lambda e: e.reciprocal(sm[:, s, 2:3], sm[:, s, 1:2]), reads=[sm], writes=[sm])
            P.op("dve", lambda e: e.tensor_scalar_mul(gt[:, s, :], gt[:, s, :], sm[:, s, 2:3]), reads=[gt, sm], writes=[gt])
        for s in range(4):
            P.op("pe", lambda e: e.transpose(ptk[0:32, 128 + s * 128:128 + (s + 1) * 128] if False else plg[:, s * 128:(s + 1) * 128], gt[:, s, :], ident[:]),
                 reads=[gt, ident, lgs], writes=[plg])
        P.op("act", lambda e: e.activation(gT[:], plg[:], AF.Copy), reads=[plg], writes=[gT])
        P.dma(dq(), o_g.ap()[:, m0:m0 + 512], gT[:], reads=[gT], writes=["o_g"])
    return P.finish(["o_x1", "o_h2", "o_g"]), P


NTOK = NB * SEQ
TP = 1024


def build_D():
    P = Prog()
    h2_d = P.din("h2T_all", [D, NTOK], BF16)
    g_d = P.din("gate4", [4, NTOK])
    w1_d = P.din("w1", [4, D, 2 * D])
    w2_d = P.din("w2", [4, D, D])
    b1_d = P.din("b1T", [128, 4 * 32])
    b2_d = P.din("b2", [4, D])
    o_p = P.dout("partT", [D, NTOK])

    b1T = P.sb([128, 4, 32], name="b1T")
    b2s = P.sb([4, D], name="b2s")
    h2 = P.sb([128, 16, TP], BF16, name="h2")
    acc = P.sb([128, 16, TP], name="acc")
    act = P.sb([128, 16, TP], BF16, name="act")
    g4 = P.sb([4, TP], name="g4")
    wbc = P.pool("wbc", 2, [128, TP])
    w1b = P.pool("w1b", 2, [128, 16, 256], BF16)
    w2b = P.pool("w2b", 3, [128, 16, 128], BF16)
    wk = P.pool("wk", 8, [128, 512])
    pp = P.pool("pp", 8, [128, 512], psum=True)
    P.dma("sp", b1T[:], b1_d.ap().rearrange("p (e j) -> p e j", e=4), writes=[b1T])
    P.dma("sp", b2s[:], b2_d.ap(), writes=[b2s])
    for p_ in range(NTOK // TP):
        t0 = p_ * TP
        P.dma("sp", h2[:], h2_d.ap()[:, t0:t0 + TP].rearrange("(k p) t -> p k t", p=128), writes=[h2])
        P.dma("act", g4[:], g_d.ap()[:, t0:t0 + TP], writes=[g4])
        for dc in range(16):
            for tt in range(2):
                ps = pp()
                P.op("pe", lambda e: e.matmul(ps[:], b2s[:, dc * 128:(dc + 1) * 128], g4[:, tt * 512:(tt + 1) * 512], start=True, stop=True), reads=[b2s, g4], writes=[ps])
                P.op("act", lambda e: e.activation(acc[:, dc, tt * 512:(tt + 1) * 512], ps[:], AF.Copy), reads=[ps], writes=[("acc", dc, tt)])
        for ex_ in range(4):
            wb_ = wbc()
            P.dma("act", wb_[:], g_d.ap()[ex_:ex_ + 1, t0:t0 + TP].partition_broadcast(128), writes=[wb_])
            for j in range(16):
                w = w1b()
                P.dma("pool", w[:, :, 0:128], w1_d.ap()[ex_, :, j * 128:(j + 1) * 128].rearrange("(k p) n -> p k n", p=128), writes=[w])
                P.dma("pool", w[:, :, 128:256], w1_d.ap()[ex_, :, D + j * 128:D + (j + 1) * 128].rearrange("(k p) n -> p k n", p=128), reads=[w], writes=[w])
                for tt in range(2):
                    ts = slice(tt * 512, (tt + 1) * 512)
                    psg = pp(); psl = pp()
                    for k in range(16):
                        P.op("pe", lambda e: e.matmul(psg[:], w[:, k, 0:128], h2[:, k, ts], start=(k == 0), stop=(k == 15)), reads=[w, h2], writes=[psg])
                    for k in range(16):
                        P.op("pe", lambda e: e.matmul(psl[:], w[:, k, 128:256], h2[:, k, ts], start=(k == 0), stop=(k == 15)), reads=[w, h2], writes=[psl])
                    gl = wk(); sg = wk(); ln = wk()
                    P.op("dve", lambda e: e.tensor_scalar(gl[:], psg[:], b1T[:, ex_, j:j + 1], 7.0, ALU.add, ALU.min), reads=[psg, b1T], writes=[gl])
                    P.op("act", lambda e: e.activation(sg[:], gl[:], AF.Sigmoid, scale=1.702), reads=[gl], writes=[sg])
                    P.op("dve", lambda e: e.tensor_scalar(ln[:], psl[:], b1T[:, ex_, 16 + j:17 + j], 7.0, ALU.add, ALU.min), reads=[psl, b1T], writes=[ln])
                    P.op("dve", lambda e: e.tensor_scalar(ln[:], ln[:], -7.0, 1.0, ALU.max, ALU.add), reads=[ln], writes=[ln])
                    P.op("dve", lambda e: e.tensor_tensor(gl[:], gl[:], sg[:], ALU.mult), reads=[gl, sg], writes=[gl])
                    P.op("dve", lambda e: e.tensor_tensor(gl[:], gl[:], ln[:], ALU.mult), reads=[gl, ln], writes=[gl])
                    P.op("dve", lambda e: e.tensor_tensor(act[:, j, ts], gl[:], wb_[:, ts], ALU.mult), reads=[gl, wb_], writes=[("act", j, tt)])
            for dc in range(16):
                w = w2b()
                P.dma("pool", w[:], w2_d.ap()[ex_, :, dc * 128:(dc + 1) * 128].rearrange("(k p) n -> p k n", p=128), writes=[w])
                for tt in range(2):
                    ts = slice(tt * 512, (tt + 1) * 512)
                    ps = pp()
                    for k in range(16):
                        P.op("pe", lambda e: e.matmul(ps[:], w[:, k, :], act[:, k, ts], start=(k == 0), stop=(k == 15)), reads=[w, ("act", k, tt)], writes=[ps])
                    P.op("dve", lambda e: e.tensor_tensor(acc[:, dc, ts], ps[:], acc[:, dc, ts], ALU.add), reads=[ps, ("acc", dc, tt)], writes=[("acc", dc, tt)])
        P.dma("sp", o_p.ap()[:, t0:t0 + TP].rearrange("(k p) t -> p k t", p=128), acc[:],
              reads=[("acc", dc, tt) for dc in range(16) for tt in range(2)], writes=["o_p"])
    return P.finish(["o_p"]), P


def build_E():
    P = Prog()
    part_d = P.din("parts", [8, D, TOK])
    x1_d = P.din("x1T", [D, TOK])
    pk_d = P.din("pk", [128, NPKC])
    ident_d = P.din("ident", [128, 128])
    o_x = P.dout("xout", [TOK, D])
    pk = P.sb([128, NPKC], name="pk")
    ident = P.sb([128, 128], name="ident")
    ones = P.sb([128, 128], name="ones")
    epsc = P.sb([128, 1], name="epsc")
    modp = P.sb([128, 96], name="modp")
    g2p = P.sb([128, 16], name="g2p")
    u = P.sb([128, 16, 512], name="u")
    x1 = P.sb([128, 16, 512], name="x1")
    pb_ = P.pool("pb", 2, [128, 16, 512])
    ot = P.pool("ot", 2, [128, D])
    wk = P.pool("wk", 8, [128, 512])
    stp = P.pool("stp", 6, [128, 512])
    pp = P.pool("pp", 7, [128, 512], psum=True)

    def col(n, j=0, w=1):
        o, _ = PKC[n]
        return pk[:, o + j:o + j + w]
    P.dma("sp", pk[:], pk_d.ap(), writes=[pk])
    P.dma("sp", ident[:], ident_d.ap(), writes=[ident])
    P.op("pool", lambda e: e.memset(ones[:], 1.0), writes=[ones])
    P.op("pool", lambda e: e.memset(epsc[:], LN_EPS), writes=[epsc])
    P.op("dve", lambda e: e.tensor_tensor(modp[:], col("modsh", 0, 96), col("modoff", 0, 96), ALU.add), reads=[pk], writes=[modp])
    P.op("dve", lambda e: e.tensor_scalar_add(g2p[:], modp[:, 80:96], 1.0), reads=[modp], writes=[g2p])
    dqc = [0]

    def dq():
        dqc[0] += 1
        return ("sp", "act")[dqc[0] % 2]
    for n in range(4):
        m0 = n * 512
        P.dma(dq(), x1[:], x1_d.ap()[:, m0:m0 + 512].rearrange("(k p) t -> p k t", p=128), writes=[x1])
        P.dma(dq(), u[:], part_d.ap()[0, :, m0:m0 + 512].rearrange("(k p) t -> p k t", p=128), writes=[u])
        for c in range(1, 8):
            pb = pb_()
            P.dma(dq(), pb[:], part_d.ap()[c, :, m0:m0 + 512].rearrange("(k p) t -> p k t", p=128), writes=[pb])
            P.op("dve" if c % 2 else "pool", lambda e: e.tensor_tensor(u[:], u[:], pb[:], ALU.add), reads=[u, pb], writes=[u])
        for k in range(16):
            P.op("dve", lambda e: e.tensor_scalar_mul(u[:, k, :], u[:, k, :], g2p[:, k:k + 1]), reads=[u, g2p], writes=[u])
        P.op("dve", lambda e: e.scalar_tensor_tensor(u[:], x1[:], DN_ALPHA, u[:], ALU.mult, ALU.add), reads=[u, x1], writes=[u])
        mean, rstd = ln_fm(P, u, u, 512, ones, epsc[:, 0:1], wk, pp, 1.0 / D, stp)
        for k in range(16):
            t = wk()
            P.op("dve", lambda e: e.tensor_tensor(t[:], u[:, k, :], mean[:], ALU.subtract), reads=[u, mean], writes=[t])
            P.op("dve", lambda e: e.tensor_tensor(t[:], t[:], rstd[:], ALU.mult), reads=[t, rstd], writes=[t])
            P.op("dve", lambda e: e.tensor_scalar(x1[:, k, :], t[:], col("lng", k), col("lnb", k), ALU.mult, ALU.add), reads=[t, pk, x1], writes=[x1])
        for s in range(4):
            o = ot()
            for g in range(4):
                pt = pp()
                for q in range(4):
                    k = g * 4 + q
                    P.op("pe", lambda e: e.transpose(pt[:, q * 128:(q + 1) * 128], x1[:, k, s * 128:(s + 1) * 128], ident[:]), reads=[x1, ident], writes=[pt])
                if g % 2 == 0:
                    P.op("act", lambda e: e.activation(o[:, g * 512:(g + 1) * 512], pt[:], AF.Copy), reads=[pt], writes=[o])
                else:
                    P.op("dve", lambda e: e.tensor_copy(o[:, g * 512:(g + 1) * 512], pt[:]), reads=[pt], writes=[o])
            P.dma(dq(), o_x.ap()[m0 + s * 128:m0 + (s + 1) * 128, :], o[:], reads=[o], writes=["o_x"])
    return P.finish(["o_x"]), P


def inputs_A(inp, l, c, x, vfirst):
    b, hf = c // 2, c % 2
    xh = np.zeros((TT, D), np.float32)
    if hf == 0:
        xh[HALO:] = x[b, 0:TOK]
    else:
        xh[:] = x[b, TOK - HALO:2 * TOK]
    m = {"xh": xh, "pk": pack_A(inp, l, b, hf),
         "pos": np.ascontiguousarray(inp["positions"][b:b + 1, hf * TOK:(hf + 1) * TOK]).astype(np.int32),
         "ident": np.eye(128, dtype=np.float32), "w_in": inp["w_in"][l],
         "w_uq": inp["mla_w_uq"][l], "w_ukv": inp["mla_w_ukv"][l], "rw_w1": inp["rw_w1"][l], "rw_w2": inp["rw_w2"][l],
         "rw_a1": inp["rw_a1"][l], "rw_a2": inp["rw_a2"][l], "rw_g1": inp["rw_g1"][l], "rw_g2": inp["rw_g2"][l]}
    if l > 0:
        m["rw_v1"] = inp["rw_v1"][l - 1]
        m["rw_v2"] = inp["rw_v2"][l - 1]
        m["vfirstT"] = np.ascontiguousarray(vfirst[b, hf * TOK:(hf + 1) * TOK].T)
    return m


_PROGS = {}


def _prog(name, fn):
    if name not in _PROGS:
        _PROGS[name] = fn()[0]
    return _PROGS[name]


def _run(nc, ims):
    return run_bass_kernel_spmd(nc, ims, core_ids=list(range(8))).results


def run_M(inp):
    ims = []
    cT4 = np.zeros((128, 64), np.float32)
    for b in range(NB):
        cT4[:, b::4] = fm(inp["c"][b])
    for c in range(8):
        ims.append({"cT4": cT4, "mod_w_s": np.ascontiguousarray(inp["mod_w"][:, c * 1536:(c + 1) * 1536]),
                    "mod_b_s": fm(inp["mod_b"][c * 1536:(c + 1) * 1536])})
    r = _run(_prog("M", build_M), ims)
    modsh = np.zeros((NB, 128, 96), np.float32)
    for c in range(8):
        mt = r[c]["modT"].reshape(128, 12, 4)
        for b in range(NB):
            modsh[b][:, c * 12:(c + 1) * 12] = mt[:, :, b]
    return modsh


def inputs_C(inp, l, c, x, A, Bres):
    b, hf = c // 2, c % 2
    ts = slice(hf * TOK, (hf + 1) * TOK)
    pair = lambda n: np.concatenate([Bres[2 * b][n], Bres[2 * b + 1][n]], 0)[:, ts]
    ycat = np.concatenate([A[c]["yconvT"], pair("y_mlaT"), pair("y_rwT"), pair("y_caT")], 0)
    return {"ycatT": np.ascontiguousarray(ycat), "x": np.ascontiguousarray(x[b, ts]), "w_out": inp["w_out"][l],
            "pk": pack_C(inp, l, 0, inp["modsh"][b]), "router_w": inp["moe_router_w"][l], "ident": np.eye(128, dtype=np.float32)}


def inputs_D(inp, l, c, Cres):
    h2 = np.concatenate([Cres[i]["h2T"] for i in range(8)], 1)
    g = np.concatenate([Cres[i]["gateT"] for i in range(8)], 1)
    b1 = inp["moe_b1"][l][4 * c:4 * c + 4]
    b1T = np.concatenate([fm(b1[e]) for e in range(4)], 1)
    return {"h2T_all": h2, "gate4": np.ascontiguousarray(g[4 * c:4 * c + 4]),
            "w1": inp["moe_w1"][l][4 * c:4 * c + 4], "w2": inp["moe_w2"][l][4 * c:4 * c + 4],
            "b1T": b1T, "b2": np.ascontiguousarray(inp["moe_b2"][l][4 * c:4 * c + 4])}


def inputs_E(inp, l, c, Cres, Dres):
    b = c // 2
    parts = np.stack([Dres[i]["partT"][:, c * TOK:(c + 1) * TOK] for i in range(8)], 0)
    return {"parts": parts, "x1T": Cres[c]["x1T"], "pk": pack_C(inp, l, 1, inp["modsh"][b]), "ident": np.eye(128, dtype=np.float32)}


def kernel(**inputs):
    inp = {k: np.asarray(v) for k, v in inputs.items()}
    inp["modsh"] = run_M(inp)
    x = inp["x"].astype(np.float32)
    vfirst = None
    for l in range(DEPTH):
        A = _run(_prog("A%d" % l, lambda: build_A(l)), [inputs_A(inp, l, c, x, vfirst) for c in range(8)])
        if l == 0:
            vfirst = np.zeros((NB, SEQ, 512), np.float32)
            for c in range(8):
                vfirst[c // 2, (c % 2) * TOK:(c % 2 + 1) * TOK] = A[c]["rw_vfirstT"].T
        Bres = _run(_prog("B", lambda: build_B(l)), [inputs_B(inp, l, c, A) for c in range(8)])
        Cres = _run(_prog("C", build_C), [inputs_C(inp, l, c, x, A, Bres) for c in range(8)])
        del A, Bres
        Dres = _run(_prog("D", build_D), [inputs_D(inp, l, c, Cres) for c in range(8)])
        Eres = _run(_prog("E", build_E), [inputs_E(inp, l, c, Cres, Dres) for c in range(8)])
        del Cres, Dres
        x = np.zeros((NB, SEQ, D), np.float32)
        for c in range(8):
            x[c // 2, (c % 2) * TOK:(c % 2 + 1) * TOK] = Eres[c]["xout"]
    return x
```

```python
import numpy as np
from contextlib import ExitStack
import concourse.bass as bass
import concourse.mybir as mybir
from concourse.bass_utils import run_bass_kernel_spmd

F32 = mybir.dt.float32
BF16 = mybir.dt.bfloat16
I32 = mybir.dt.int32
AF = mybir.ActivationFunctionType
ALU = mybir.AluOpType
AX = mybir.AxisListType

ENGS = ("pe", "act", "dve", "pool", "sp")
ND = 6

D = 2048
SEQ = 4096
NB = 4
TOK = 2048
HALO = 128
TT = TOK + HALO
DIN = 4800
LN_EPS = 1e-5
DEPTH = 2
DN_ALPHA = (2 * DEPTH) ** 0.25


class _Rec:
    def __init__(self):
        self.calls = []

    def __getattr__(self, name):
        def f(*a, **kw):
            self.calls.append((name, a, kw))
        return f


class Prog:
    def __init__(self):
        self.nc = bass.Bass("TRN2", target_bir_lowering=False)
        self.es = ExitStack()
        self.ops = {e: [] for e in ENGS}
        self.cnt = {e: 0 for e in ENGS}
        self.dcnt = {e: 0 for e in ENGS}
        self.waited = {e: {} for e in ENGS}
        self.lastw = {}
        self.readers = {}
        self.sems = {}
        self.n_t = 0
        self.rr = {}
        self.same_engine_sync = True

    def sem(self, key):
        if key not in self.sems:
            nm = "s_" + "_".join(str(x) for x in (key if isinstance(key, tuple) else (key,)))
            self.sems[key] = self.es.enter_context(self.nc.semaphore(nm))
        return self.sems[key]

    def sb(self, shape, dt=F32, name=None):
        self.n_t += 1
        return self.es.enter_context(self.nc.sbuf_tensor("sb_" + (name or "t%d" % self.n_t), list(shape), dt))

    def ps(self, shape, dt=F32, name=None):
        self.n_t += 1
        return self.es.enter_context(self.nc.psum_tensor("ps_" + (name or "p%d" % self.n_t), list(shape), dt))

    def din(self, name, shape, dt=F32):
        return self.nc.dram_tensor(name, list(shape), dt, kind="ExternalInput")

    def dout(self, name, shape, dt=F32):
        return self.nc.dram_tensor(name, list(shape), dt, kind="ExternalOutput")

    def pool(self, name, n, shape, dt=F32, psum=False):
        bufs = [(self.ps if psum else self.sb)(shape, dt, name="%s%d" % (name, i)) for i in range(n)]
        self.rr[name] = 0

        def nxt():
            b = bufs[self.rr[name] % n]
            self.rr[name] += 1
            return b
        return nxt

    @staticmethod
    def _k(k):
        if isinstance(k, (str, int)):
            return k
        if isinstance(k, tuple):
            return tuple(Prog._k(x) for x in k)
        return "T:" + k.name

    def _tok_wait(self, tok):
        if tok[0] == "c":
            return (("c", tok[1]), tok[2] + 1)
        q, d = tok[1], tok[2]
        return (("d", q, d % ND), 16 * (d // ND + 1))

    def _deps(self, eng, reads, writes):
        toks = set()
        for k in list(reads) + list(writes):
            if k in self.lastw:
                toks.add(self.lastw[k])
        for k in writes:
            for t in self.readers.get(k, ()):
                toks.add(t)
        waits = []
        for t in toks:
            if t[0] == "c" and t[1] == eng and (eng == "pe" or not self.same_engine_sync):
                continue
            sk, v = self._tok_wait(t)
            if self.waited[eng].get(sk, 0) >= v:
                continue
            self.waited[eng][sk] = v
            waits.append((sk, v))
        return waits

    def _commit(self, tok, reads, writes):
        for k in writes:
            self.lastw[k] = tok
            self.readers[k] = []
        for k in reads:
            self.readers.setdefault(k, []).append(tok)

    def op(self, eng, fn, reads=(), writes=()):
        rec = _Rec()
        fn(rec)
        (mname, margs, mkw), = rec.calls
        fn = lambda e, mname=mname, margs=margs, mkw=mkw: getattr(e, mname)(*margs, **mkw)
        reads = [self._k(k) for k in reads]
        writes = [self._k(k) for k in writes]
        waits = self._deps(eng, reads, writes)
        idx = self.cnt[eng]
        self.cnt[eng] += 1
        self.ops[eng].append((waits, fn, (("c", eng), 1)))
        self._commit(("c", eng, idx), reads, writes)

    def dma(self, q, out, in_, reads=(), writes=(), **kw):
        reads = [self._k(k) for k in reads]
        writes = [self._k(k) for k in writes]
        d = self.dcnt[q]
        waits = self._deps(q, reads, writes)
        if d >= ND:
            sk, v = self._tok_wait(("d", q, d - ND))
            if self.waited[q].get(sk, 0) < v:
                self.waited[q][sk] = v
                waits.append((sk, v))
        self.dcnt[q] += 1
        self.ops[q].append((waits, lambda e: e.dma_start(out=out, in_=in_, **kw), (("d", q, d % ND), 16)))
        self._commit(("d", q, d), reads, writes)

    def barrier(self):
        allw = []
        for e2 in ENGS:
            if self.cnt[e2] > 0:
                allw.append((("c", e2), self.cnt[e2]))
            for d in range(max(0, self.dcnt[e2] - ND), self.dcnt[e2]):
                allw.append(self._tok_wait(("d", e2, d)))
        for e in ENGS:
            waits = []
            for sk, v in allw:
                if self.waited[e].get(sk, 0) < v:
                    self.waited[e][sk] = v
                    waits.append((sk, v))
            if waits:
                self.ops[e].append((waits, None, None))

    def scope(self):
        prog = self

        class _S:
            def __enter__(s2):
                s2.outer = prog.es
                prog.es = ExitStack()
                return s2

            def __exit__(s2, *a):
                prog.barrier()
                prog.es.close()
                prog.es = s2.outer
                return False
        return _S()

    def finish(self, final_keys):
        final_keys = [self._k(k) for k in final_keys]
        waits = self._deps("sp", final_keys, ())
        self.ops["sp"].append((waits, None, None))
        nc = self.nc
        for e in ENGS:
            for waits, fn, inc in self.ops[e]:
                for sk, v in waits:
                    self.sem(sk)
                if inc is not None:
                    self.sem(inc[0])
        with nc.Block() as block:
            def emit(eng_name):
                def body(e):
                    for waits, fn, inc in self.ops[eng_name]:
                        for sk, v in waits:
                            e.wait_ge(self.sem(sk), v)
                        if fn is not None:
                            fn(e).then_inc(self.sem(inc[0]), inc[1])
                return body
            block.tensor(emit("pe"))
            block.scalar(emit("act"))
            block.vector(emit("dve"))
            block.gpsimd(emit("pool"))
            block.sync(emit("sp"))
        self.es.close()
        return nc


def fm(v):
    v = np.asarray(v, np.float32).reshape(-1, 128)
    return np.ascontiguousarray(v.T)


PKA = {}
_o = 0
for _n, _w in (("modb", 96), ("modoff", 96), ("cT", 16), ("dw", 124), ("convb", 4), ("clng", 4),
               ("clnb", 4), ("qn", 3), ("kvn", 2), ("murkv", 12), ("mux", 48), ("w0", 4), ("a0", 4),
               ("kk", 4), ("ka", 4), ("rk", 4), ("v0", 4), ("vmu", 16), ("invf", 1), ("flag", 1)):
    PKA[_n] = (_o, _w)
    _o += _w
NPKA = _o


def pack_A(inp, l, b, hf):
    pk = np.zeros((128, NPKA), np.float32)

    def put(n, a):
        o, w = PKA[n]
        a = np.asarray(a, np.float32).reshape(128, w)
        pk[:, o:o + w] = a
    put("modb", inp["modsh"][b])
    put("modoff", fm(inp["mod_offset"][l]))
    put("dw", np.transpose(inp["conv_dw"][l].T.reshape(4, 128, 31), (1, 0, 2)))
    put("convb", fm(inp["conv_b"][l]))
    put("clng", fm(inp["conv_ln_g"][l]))
    put("clnb", fm(inp["conv_ln_b"][l]))
    put("qn", fm(inp["mla_q_norm"][l]))
    put("kvn", fm(inp["mla_kv_norm"][l]))
    put("murkv", np.transpose(inp["rw_mu_rkv"][l].reshape(3, 4, 128), (2, 0, 1)))
    put("mux", np.transpose(inp["rw_mu_x"][l].reshape(3, 16, 128), (2, 0, 1)))
    put("w0", fm(inp["rw_w0"][l]))
    put("a0", fm(inp["rw_a0"][l]))
    put("kk", fm(inp["rw_k_k"][l]))
    put("ka", fm(inp["rw_k_a"][l]))
    put("rk", fm(inp["rw_r_k"][l].reshape(-1)))
    if l > 0:
        put("v0", fm(inp["rw_v0"][l - 1]))
        put("vmu", fm(inp["rw_vres_mu"][l - 1]))
    inv = (10000.0 ** (-np.arange(0, 64, 2, dtype=np.float32) / 64)).astype(np.float32)
    iv = np.zeros((128, 1), np.float32)
    iv[0:32, 0] = inv
    iv[32:64, 0] = inv
    put("invf", iv)
    put("flag", np.full((128, 1), 0.0 if hf == 0 else 1.0, np.float32))
    return pk


TWO_PI = float(2 * np.pi)
CW1 = 6.28125
CW2 = float(np.float32(2 * np.pi - 6.28125).view(np.int32) & ~0xFFF) if False else 0.0019350052
_c2 = np.float32(2 * np.pi - CW1)
_c2i = np.frombuffer(np.float32(_c2).tobytes(), np.uint32)[0] & np.uint32(0xFFFFF000)
CW2 = float(np.frombuffer(np.uint32(_c2i).tobytes(), np.float32)[0])
CW3 = float(2 * np.pi - CW1 - CW2)
MAGIC = 12582912.0


def build_A(l, dbg=False):
    P = Prog()
    xh = P.din("xh", [TT, D])
    pk_d = P.din("pk", [128, NPKA])
    pos_d = P.din("pos", [1, TOK], I32)
    ident_d = P.din("ident", [128, 128])
    win_d = P.din("w_in", [D, DIN])
    wuq_d = P.din("w_uq", [384, 768])
    wukv_d = P.din("w_ukv", [256, 1024])
    w1_d = P.din("rw_w1", [D, 96]); w2_d = P.din("rw_w2", [96, 512])
    a1_d = P.din("rw_a1", [D, 96]); a2_d = P.din("rw_a2", [96, 512])
    g1_d = P.din("rw_g1", [D, 256]); g2_d = P.din("rw_g2", [256, 512])
    if l > 0:
        v1_d = P.din("rw_v1", [D, 64]); v2_d = P.din("rw_v2", [64, 512])
        vf_d = P.din("vfirstT", [512, TOK])
    o_modp = P.dout("modp", [128, 96])
    o_yconv = P.dout("yconvT", [512, TOK], BF16)
    o_mq = P.dout("mla_qT", [4, 192, TOK], BF16)
    o_mk = P.dout("mla_kT", [4, 128, TOK], BF16)
    o_mkpe = P.dout("mla_kpeT", [64, TOK], BF16)
    o_mv = P.dout("mla_v", [TOK, 512], BF16)
    o_cq = P.dout("ca_qT", [512, TOK], BF16)
    o_ck = P.dout("ca_kT", [512, TOK], BF16)
    o_cv = P.dout("ca_v", [TOK, 512], BF16)
    o_rtm = {n: P.dout("rw_%s_tm" % n, [TOK, 512]) for n in ("r", "w", "k", "kk", "kka")}
    rfm_names = ("v", "bonus", "g") + (("vfirst",) if l == 0 else ())
    o_rfm = {n: P.dout("rw_%sT" % n, [512, TOK]) for n in rfm_names}
    finals = ["o_modp", "o_yconv", "o_mq", "o_mk", "o_mkpe", "o_mv", "o_cq", "o_ck", "o_cv"] + \
        ["o_rtm_" + n for n in o_rtm] + ["o_rfm_" + n for n in o_rfm]

    pk = P.sb([128, NPKA], name="pk")
    ident = P.sb([128, 128], name="ident")
    ones = P.sb([128, 128], name="ones")
    bones = P.sb([128, 128], name="bones")
    epsc = P.sb([128, 4], name="epsc")
    hT = P.sb([128, 16, TT], BF16, name="hT")
    modp = P.sb([128, 96], name="modp")
    sc1p = P.sb([128, 16], name="sc1p")
    cosT = P.sb([64, TOK], name="cosT")
    sinT = P.sb([64, TOK], name="sinT")
    wkb = P.pool("wkb", 6, [128, 512], BF16)
    wb = P.pool("wb", 4, [128, 16, 128], BF16)
    pp = P.pool("pp", 7, [128, 512], psum=True)

    def col(n, j=0, w=1):
        o, _ = PKA[n]
        return pk[:, o + j:o + j + w]

    P.dma("sp", pk[:], pk_d.ap(), writes=[pk])
    P.dma("sp", ident[:], ident_d.ap(), writes=[ident])
    P.op("pool", lambda e: e.memset(epsc[:, 0:1], LN_EPS), writes=[epsc])
    P.op("pool", lambda e: e.memset(epsc[:, 1:2], 1e-6), reads=[epsc], writes=[epsc])
    P.op("pool", lambda e: e.memset(epsc[:, 2:3], 64e-5), reads=[epsc], writes=[epsc])
    P.op("pool", lambda e: e.memset(ones[:], 1.0), writes=[ones])
    P.op("pool", lambda e: e.memset(bones[:], 0.0), writes=[bones])
    P.op("pool", lambda e: e.memset(bones[0:64, 0:64], 1.0), reads=[bones], writes=[bones])
    P.op("pool", lambda e: e.memset(bones[64:128, 64:128], 1.0), reads=[bones], writes=[bones])

    NTI = TT // 128
    hkeys = [("hT", i) for i in range(NTI)]
    with P.scope():
        big = P.sb([128, 8192], name="big")
        junk = P.sb([128, D], BF16, name="junk")
        st = P.pool("st", 8, [128, 4])
        P.op("dve", lambda e: e.tensor_tensor(modp[:], col("modb", 0, 96), col("modoff", 0, 96), ALU.add),
             reads=[pk], writes=[modp])
        P.op("dve", lambda e: e.tensor_scalar_add(sc1p[:], modp[:, 16:32], 1.0), reads=[modp], writes=[sc1p])
        P.dma("sp", o_modp.ap(), modp[:], reads=[modp], writes=["o_modp"])
        P.barrier()
        posi = big[0:64, 4096:6144].bitcast(I32)
        ang = big[0:64, 0:2048]
        rn = big[0:64, 2048:4096]
        P.dma("sp", posi, pos_d.ap().partition_broadcast(64), writes=["posi"])
        P.op("dve", lambda e: e.tensor_copy(ang, posi), reads=["posi"], writes=["ang"])
        P.op("dve", lambda e: e.tensor_scalar_mul(ang, ang, col("invf")[0:64, :]), reads=["ang", pk], writes=["ang"])
        P.op("dve", lambda e: e.tensor_scalar(rn, ang, 1.0 / TWO_PI, MAGIC, ALU.mult, ALU.add), reads=["ang"], writes=["rn"])
        P.op("dve", lambda e: e.tensor_scalar_add(rn, rn, -MAGIC), reads=["rn"], writes=["rn"])
        for cw in (CW1, CW2, CW3):
            P.op("dve", lambda e, cw=cw: e.scalar_tensor_tensor(ang, rn, -cw, ang, ALU.mult, ALU.add),
                 reads=["rn", "ang"], writes=["ang"])
        P.op("dve", lambda e: e.tensor_scalar(sinT[:], ang, float(np.pi), -float(np.pi), ALU.min, ALU.max),
             reads=["ang"], writes=[sinT])
        P.op("dve", lambda e: e.tensor_scalar_add(cosT[:], sinT[:], float(np.pi / 2)), reads=[sinT], writes=[cosT])
        P.op("dve", lambda e: e.tensor_scalar(rn, cosT[:], float(np.pi), -TWO_PI, ALU.is_gt, ALU.mult), reads=[cosT], writes=["rn"])
        P.op("dve", lambda e: e.tensor_tensor(cosT[:], cosT[:], rn, ALU.add), reads=[cosT, "rn"], writes=[cosT])
        P.op("dve", lambda e: e.tensor_scalar(cosT[:], cosT[:], float(np.pi), -float(np.pi), ALU.min, ALU.max), reads=[cosT], writes=[cosT])
        P.op("act", lambda e: e.activation(sinT[:], sinT[:], AF.Sin), reads=[sinT], writes=[sinT])
        P.op("act", lambda e: e.activation(cosT[:], cosT[:], AF.Sin), reads=[cosT], writes=[cosT])
        P.barrier()
        for i in range(NTI):
            xv = big[:, (i % 2) * 2048:(i % 2 + 1) * 2048]
            xk = ("xt", i % 2)
            P.dma("sp" if i % 2 == 0 else "act", xv, xh.ap()[i * 128:(i + 1) * 128, :], writes=[xk])
            s = st()
            P.op("act", lambda e, xv=xv, s=s: e.activation(junk[:], xv, AF.Identity, accum_out=s[:, 0:1]),
                 reads=[xk], writes=[junk, (s, 0)])
            P.op("act", lambda e, xv=xv, s=s: e.activation(junk[:], xv, AF.Square, accum_out=s[:, 1:2]),
                 reads=[xk], writes=[junk, (s, 1)])
            P.op("dve", lambda e, s=s: e.tensor_scalar_mul(s[:, 0:2], s[:, 0:2], 1.0 / D), reads=[(s, 0), (s, 1)], writes=[(s, 0), (s, 1)])
            P.op("dve", lambda e, s=s: e.tensor_tensor(s[:, 2:3], s[:, 0:1], s[:, 0:1], ALU.mult), reads=[(s, 0)], writes=[(s, 2)])
            P.op("dve", lambda e, s=s: e.tensor_tensor(s[:, 1:2], s[:, 1:2], s[:, 2:3], ALU.subtract), reads=[(s, 1), (s, 2)], writes=[(s, 1)])
            P.op("act", lambda e, s=s: e.activation(s[:, 1:2], s[:, 1:2], AF.Sqrt, bias=epsc[:, 0:1]), reads=[(s, 1), epsc], writes=[(s, 1)])
            P.op("dve", lambda e, s=s: e.reciprocal(s[:, 1:2], s[:, 1:2]), reads=[(s, 1)], writes=[(s, 1)])
            P.op("dve", lambda e, xv=xv, s=s: e.tensor_scalar(xv, xv, s[:, 0:1], s[:, 1:2], ALU.subtract, ALU.mult),
                 reads=[xk, (s, 0), (s, 1)], writes=[xk])
            for g in range(4):
                pt = pp()
                for q in range(4):
                    k = g * 4 + q
                    P.op("pe", lambda e, pt=pt, q=q, k=k, xv=xv: e.transpose(pt[:, q * 128:(q + 1) * 128], xv[:, k * 128:(k + 1) * 128], ident[:]),
                         reads=[xk, ident], writes=[pt])
                for q in range(4):
                    k = g * 4 + q
                    P.op("act", lambda e, pt=pt, q=q, k=k, i=i: e.activation(
                        hT[:, k, i * 128:(i + 1) * 128], pt[:, q * 128:(q + 1) * 128], AF.Identity,
                        bias=modp[:, k:k + 1], scale=sc1p[:, k:k + 1]),
                        reads=[pt, modp, sc1p], writes=[("hT", i)])
        P.op("dve", lambda e: e.tensor_scalar_mul(hT[:, :, 0:128], hT[:, :, 0:128], col("flag")), reads=[("hT", 0), pk], writes=[("hT", 0)])

    cnt = {"q": 0}

    def dq():
        cnt["q"] += 1
        return ("sp", "act")[cnt["q"] % 2]

    def load_wblock(src_ap, ncols=128, rows=16):
        w = wb()
        P.dma("pool", w[:, 0:rows, 0:ncols], src_ap.rearrange("(k p) n -> p k n", p=128), writes=[w])
        return w

    def proj(w, ncols, t0, n, shift=0):
        ps = pp()
        for k in range(16):
            P.op("pe", lambda e, ps=ps, w=w, k=k: e.matmul(ps[0:ncols, 0:n], w[:, k, 0:ncols], hT[:, k, t0 - shift:t0 - shift + n],
                                                       start=(k == 0), stop=(k == 15)),
                 reads=[w] + hkeys, writes=[ps])
        return ps

    def evac(dst, src, i, reads, writes):
        if i % 2 == 0:
            P.op("act", lambda e: e.activation(dst, src, AF.Copy), reads=reads, writes=writes)
        else:
            P.op("dve", lambda e: e.tensor_copy(dst, src), reads=reads, writes=writes)

    TILES = [(HALO + n * 512, 512) for n in range(4)]

    with P.scope():
        for name, c0, o_d in (("cq", 3264, o_cq), ("ck", 3776, o_ck)):
            for j in range(4):
                w = load_wblock(win_d.ap()[:, c0 + j * 128:c0 + (j + 1) * 128])
                for n, (t0, nn) in enumerate(TILES):
                    ps = proj(w, 128, t0, nn)
                    ob = wkb()
                    evac(ob[:], ps[:], n, [ps], [ob])
                    P.dma(dq(), o_d.ap()[j * 128:(j + 1) * 128, t0 - HALO:t0 - HALO + nn], ob[:], reads=[ob], writes=["o_" + name])
        wv = P.sb([128, 16, 512], BF16, name="wv")
        P.dma("pool", wv[:], win_d.ap()[:, 4288:4800].rearrange("(k p) n -> p k n", p=128), writes=[wv])
        for i in range(16):
            ps = pp()
            for k in range(16):
                P.op("pe", lambda e, ps=ps, k=k, i=i: e.matmul(ps[:], hT[:, k, HALO + i * 128:HALO + (i + 1) * 128], wv[:, k, :],
                                                            start=(k == 0), stop=(k == 15)),
                     reads=[wv] + hkeys, writes=[ps])
            ob = wkb()
            evac(ob[:], ps[:], i, [ps], [ob])
            P.dma(dq(), o_cv.ap()[i * 128:(i + 1) * 128, :], ob[:], reads=[ob], writes=["o_cv"])

    with P.scope():
        cqs = P.sb([128, 5, TOK], name="cqs")
        wuq = P.sb([128, 3, 768], BF16, name="wuq")
        wuqr = P.sb([128, 3, 4, 64], BF16, name="wuqr")
        wukv = P.sb([128, 2, 1024], BF16, name="wukv")
        wukvv = P.sb([128, 2, 512], BF16, name="wukvv")
        wk = P.pool("wkm", 10, [128, 512])
        cqn_p = P.pool("cqn", 2, [128, 3, 512], BF16)
        ckvn_p = P.pool("ckvn", 2, [128, 2, 512], BF16)
        P.dma("pool", wuq[:], wuq_d.ap().rearrange("(k p) n -> p k n", p=128), writes=[wuq])
        P.dma("pool", wukv[:], wukv_d.ap().rearrange("(k p) n -> p k n", p=128), writes=[wukv])
        for k in range(3):
            for h in range(4):
                P.op("dve", lambda e, k=k, h=h: e.tensor_scalar_mul(wuqr[:, k, h, 0:32], wuq[:, k, h * 192 + 160:h * 192 + 192], -1.0),
                     reads=[wuq], writes=[wuqr])
                P.op("dve", lambda e, k=k, h=h: e.tensor_copy(wuqr[:, k, h, 32:64], wuq[:, k, h * 192 + 128:h * 192 + 160]),
                     reads=[wuq], writes=[wuqr])
        for k in range(2):
            for h in range(4):
                P.op("dve", lambda e, k=k, h=h: e.tensor_copy(wukvv[:, k, h * 128:(h + 1) * 128], wukv[:, k, h * 256 + 128:h * 256 + 256]),
                     reads=[wukv], writes=[wukvv])
        for j in range(5):
            w = load_wblock(win_d.ap()[:, 1024 + j * 128:1024 + (j + 1) * 128])
            for n, (t0, nn) in enumerate(TILES):
                ps = proj(w, 128, t0, nn)
                evac(cqs[:, j, t0 - HALO:t0 - HALO + nn], ps[:], n, [ps], [("cqs", j, n)])
        wkp = load_wblock(win_d.ap()[:, 1664:1728], ncols=64)
        wkr = wb()
        P.op("dve", lambda e: e.tensor_scalar_mul(wkr[:, :, 0:32], wkp[:, :, 32:64], -1.0), reads=[wkp], writes=[wkr])
        P.op("dve", lambda e: e.tensor_copy(wkr[:, :, 32:64], wkp[:, :, 0:32]), reads=[wkp, wkr], writes=[wkr])

        def rope_out(ps1, ps2, m0, dst_ap, okey):
            t1 = wk(); t2 = wk(); ob = wkb()
            P.op("dve", lambda e: e.tensor_tensor(t1[0:64, :], ps1[0:64, :], cosT[:, m0:m0 + 512], ALU.mult), reads=[ps1, cosT], writes=[t1])
            P.op("dve", lambda e: e.tensor_tensor(t2[0:64, :], ps2[0:64, :], sinT[:, m0:m0 + 512], ALU.mult), reads=[ps2, sinT], writes=[t2])
            P.op("dve", lambda e: e.tensor_tensor(ob[0:64, :], t1[0:64, :], t2[0:64, :], ALU.add), reads=[t1, t2], writes=[ob])
            P.dma(dq(), dst_ap, ob[0:64, :], reads=[ob], writes=[okey])

        for n, (t0, nn) in enumerate(TILES):
            m0 = t0 - HALO
            ps1 = proj(wkp, 64, t0, nn)
            ps2 = proj(wkr, 64, t0, nn)
            rope_out(ps1, ps2, m0, o_mkpe.ap()[:, m0:m0 + 512], "o_mkpe")
        for n, (t0, nn) in enumerate(TILES):
            m0 = t0 - HALO
            normed = []
            for (j0, nj, pkn, inv_d, pool_) in ((0, 3, "qn", 1.0 / 384, cqn_p), (3, 2, "kvn", 1.0 / 256, ckvn_p)):
                pss = pp()
                for j in range(nj):
                    sq = wk()
                    P.op("act", lambda e, sq=sq, j=j: e.activation(sq[:], cqs[:, j0 + j, m0:m0 + 512], AF.Square),
                         reads=[("cqs", j0 + j, n)], writes=[sq])
                    P.op("pe", lambda e, sq=sq, j=j, pss=pss: e.matmul(pss[:], ones[:], sq[:], start=(j == 0), stop=(j == nj - 1)),
                         reads=[sq, ones], writes=[pss])
                rq = wk()
                P.op("act", lambda e, rq=rq, pss=pss: e.activation(rq[:], pss[:], AF.Sqrt, bias=epsc[:, 1:2], scale=inv_d),
                     reads=[pss, epsc], writes=[rq])
                P.op("dve", lambda e, rq=rq: e.reciprocal(rq[:], rq[:]), reads=[rq], writes=[rq])
                nb_ = pool_()
                for j in range(nj):
                    P.op("dve", lambda e, j=j, nb_=nb_, rq=rq: e.scalar_tensor_tensor(
                        nb_[:, j, :], cqs[:, j0 + j, m0:m0 + 512], col(pkn, j), rq[:], ALU.mult, ALU.mult),
                        reads=[("cqs", j0 + j, n), pk, rq], writes=[nb_])
                normed.append(nb_)
            cqn, ckvn = normed
            for h in range(4):
                ps = pp()
                for k in range(3):
                    P.op("pe", lambda e, ps=ps, k=k, h=h: e.matmul(ps[:], wuq[:, k, h * 192:h * 192 + 128], cqn[:, k, :], start=(k == 0), stop=(k == 2)),
                         reads=[wuq, cqn], writes=[ps])
                ob = wkb()
                evac(ob[:], ps[:], h, [ps], [ob])
                P.dma(dq(), o_mq.ap()[h, 0:128, m0:m0 + 512], ob[:], reads=[ob], writes=["o_mq"])
                ps1 = pp(); ps2 = pp()
                for k in range(3):
                    P.op("pe", lambda e, ps1=ps1, k=k, h=h: e.matmul(ps1[0:64, :], wuq[:, k, h * 192 + 128:h * 192 + 192], cqn[:, k, :], start=(k == 0), stop=(k == 2)),
                         reads=[wuq, cqn], writes=[ps1])
                for k in range(3):
                    P.op("pe", lambda e, ps2=ps2, k=k, h=h: e.matmul(ps2[0:64, :], wuqr[:, k, h, :], cqn[:, k, :], start=(k == 0), stop=(k == 2)),
                         reads=[wuqr, cqn], writes=[ps2])
                rope_out(ps1, ps2, m0, o_mq.ap()[h, 128:192, m0:m0 + 512], "o_mq")
                ps = pp()
                for k in range(2):
                    P.op("pe", lambda e, ps=ps, k=k, h=h: e.matmul(ps[:], wukv[:, k, h * 256:h * 256 + 128], ckvn[:, k, :], start=(k == 0), stop=(k == 1)),
                         reads=[wukv, ckvn], writes=[ps])
                ob = wkb()
                evac(ob[:], ps[:], h + 1, [ps], [ob])
                P.dma(dq(), o_mk.ap()[h, :, m0:m0 + 512], ob[:], reads=[ob], writes=["o_mk"])
            for s in range(4):
                ps = pp()
                for k in range(2):
                    P.op("pe", lambda e, ps=ps, k=k, s=s: e.matmul(ps[:], ckvn[:, k, s * 128:(s + 1) * 128], wukvv[:, k, :], start=(k == 0), stop=(k == 1)),
                         reads=[wukvv, ckvn], writes=[ps])
                ob = wkb()
                evac(ob[:], ps[:], s, [ps], [ob])
                P.dma(dq(), o_mv.ap()[m0 + s * 128:m0 + (s + 1) * 128, :], ob[:], reads=[ob], writes=["o_mv"])

    with P.scope():
        u = P.sb([128, TT], name="u")
        uc = P.sb([128, 4, TOK], name="uc")
        wk = P.pool("wkc", 10, [128, 512])
        CT = [(0, 512), (512, 512), (1024, 512), (1536, 512), (2048, 128)]
        for j in range(4):
            wva = load_wblock(win_d.ap()[:, j * 128:(j + 1) * 128])
            wga = load_wblock(win_d.ap()[:, 512 + j * 128:512 + (j + 1) * 128])
            for n, (t0, nn) in enumerate(CT):
                psv = proj(wva, 128, t0, nn)
                psg = proj(wga, 128, t0, nn)
                sg = wk()
                P.op("act", lambda e, sg=sg, psg=psg, nn=nn: e.activation(sg[:, 0:nn], psg[:, 0:nn], AF.Sigmoid), reads=[psg], writes=[sg])
                P.op("dve", lambda e, sg=sg, psv=psv, t0=t0, nn=nn: e.tensor_tensor(u[:, t0:t0 + nn], psv[:, 0:nn], sg[:, 0:nn], ALU.mult),
                     reads=[psv, sg], writes=[("u", n)])
            ukeys = [("u", n) for n in range(5)]
            o_dw = PKA["dw"][0]
            P.op("dve", lambda e, j=j: e.tensor_scalar(uc[:, j, :], u[:, 98:98 + TOK], pk[:, o_dw + j * 31:o_dw + j * 31 + 1], col("convb", j), ALU.mult, ALU.add),
                 reads=ukeys + [pk], writes=[("uc", j)])
            for i in range(1, 31):
                P.op("dve", lambda e, j=j, i=i: e.scalar_tensor_tensor(uc[:, j, :], u[:, 98 + i:98 + i + TOK], pk[:, o_dw + j * 31 + i:o_dw + j * 31 + i + 1], uc[:, j, :], ALU.mult, ALU.add),
                     reads=ukeys + [pk, ("uc", j)], writes=[("uc", j)])
        uckeys = [("uc", j) for j in range(4)]
        for n in range(4):
            m0 = n * 512
            pssum = pp(); pssq = pp()
            for j in range(4):
                sq = wk()
                P.op("act", lambda e, sq=sq, j=j: e.activation(sq[:], uc[:, j, m0:m0 + 512], AF.Square), reads=[("uc", j)], writes=[sq])
                P.op("pe", lambda e, j=j, pssum=pssum: e.matmul(pssum[:], ones[:], uc[:, j, m0:m0 + 512], start=(j == 0), stop=(j == 3)),
                     reads=[("uc", j), ones], writes=[pssum])
                P.op("pe", lambda e, j=j, pssq=pssq, sq=sq: e.matmul(pssq[:], ones[:], sq[:], start=(j == 0), stop=(j == 3)),
                     reads=[sq, ones], writes=[pssq])
            mean = wk(); msq = wk(); rstd = wk()
            P.op("act", lambda e, mean=mean, pssum=pssum: e.activation(mean[:], pssum[:], AF.Copy, scale=1.0 / 512), reads=[pssum], writes=[mean])
            P.op("dve", lambda e, mean=mean, msq=msq: e.tensor_tensor(msq[:], mean[:], mean[:], ALU.mult), reads=[mean], writes=[msq])
            P.op("dve", lambda e, msq=msq, pssq=pssq, rstd=rstd: e.scalar_tensor_tensor(rstd[:], pssq[:], 1.0 / 512, msq[:], ALU.mult, ALU.subtract),
                 reads=[pssq, msq], writes=[rstd])
            P.op("act", lambda e, rstd=rstd: e.activation(rstd[:], rstd[:], AF.Sqrt, bias=epsc[:, 0:1]), reads=[rstd, epsc], writes=[rstd])
            P.op("dve", lambda e, rstd=rstd: e.reciprocal(rstd[:], rstd[:]), reads=[rstd], writes=[rstd])
            for j in range(4):
                t1 = wk(); ob = wkb()
                P.op("dve", lambda e, t1=t1, j=j, mean=mean: e.tensor_tensor(t1[:], uc[:, j, m0:m0 + 512], mean[:], ALU.subtract), reads=[("uc", j), mean], writes=[t1])
                P.op("dve", lambda e, t1=t1, rstd=rstd: e.tensor_tensor(t1[:], t1[:], rstd[:], ALU.mult), reads=[t1, rstd], writes=[t1])
                P.op("act", lambda e, t1=t1, ob=ob, j=j: e.activation(ob[:], t1[:], AF.Silu, bias=col("clnb", j), scale=col("clng", j)), reads=[t1, pk], writes=[ob])
                P.dma(dq(), o_yconv.ap()[j * 128:(j + 1) * 128, m0:m0 + 512], ob[:], reads=[ob], writes=["o_yconv"])

    with P.scope():
        loras = [("w", w1_d, 96, 0, AF.Tanh), ("a", a1_d, 96, 1, AF.Identity), ("g", g1_d, 256, 2, AF.Sigmoid)]
        if l > 0:
            loras.append(("v", v1_d, 64, None, AF.Identity))
        lo = {}
        for nm, _, r, _, _ in loras:
            lo[nm] = P.sb([128, (r + 127) // 128, TOK], BF16, name="lo_" + nm)
        with P.scope():
            stg = P.sb([128, 16, 256], name="stg")
            stb = P.sb([128, 16, 256], name="stb")
            for nm, wd, r, murow, fn in loras:
                wa = P.sb([128, 16, r], BF16, name="wa_" + nm)
                wbb = P.sb([128, 16, r], BF16, name="wb_" + nm)
                P.dma("sp", stg[:, :, 0:r], wd.ap().rearrange("(k p) n -> p k n", p=128), writes=[stg])
                for k in range(16):
                    mcol = col("mux", murow * 16 + k) if murow is not None else col("vmu", k)
                    P.op("dve", lambda e, k=k, mcol=mcol, r=r: e.tensor_scalar_mul(stb[:, k, 0:r], stg[:, k, 0:r], mcol), reads=[stg, pk], writes=[stb])
                P.op("dve", lambda e, r=r, wbb=wbb: e.tensor_copy(wbb[:], stb[:, :, 0:r]), reads=[stb], writes=[wbb])
                P.op("dve", lambda e, r=r, wa=wa: e.tensor_tensor(wa[:], stg[:, :, 0:r], stb[:, :, 0:r], ALU.subtract), reads=[stg, stb], writes=[wa])
                for n, (t0, nn) in enumerate(TILES):
                    m0 = t0 - HALO
                    for c0 in range(0, r, 128):
                        cw = min(128, r - c0)
                        ps = pp()
                        for k in range(16):
                            P.op("pe", lambda e, ps=ps, k=k, c0=c0, cw=cw, wa=wa: e.matmul(ps[0:cw, :], wa[:, k, c0:c0 + cw], hT[:, k, t0:t0 + 512], start=(k == 0), stop=False),
                                 reads=[wa] + hkeys, writes=[ps])
                        for k in range(16):
                            P.op("pe", lambda e, ps=ps, k=k, c0=c0, cw=cw, wbb=wbb: e.matmul(ps[0:cw, :], wbb[:, k, c0:c0 + cw], hT[:, k, t0 - 1:t0 - 1 + 512], start=False, stop=(k == 15)),
                                 reads=[wbb] + hkeys, writes=[ps])
                        P.op("act", lambda e, ps=ps, c0=c0, cw=cw, nm=nm, fn=fn: e.activation(lo[nm][0:cw, c0 // 128, m0:m0 + 512], ps[0:cw, :], fn),
                             reads=[ps], writes=[("lo", nm, n)])
        w2s = P.sb([96, 512], BF16, name="w2s"); a2s = P.sb([96, 512], BF16, name="a2s")
        g2s = P.sb([128, 2, 512], BF16, name="g2s")
        P.dma("pool", w2s[:], w2_d.ap(), writes=[w2s])
        P.dma("pool", a2s[:], a2_d.ap(), writes=[a2s])
        P.dma("pool", g2s[:], g2_d.ap().rearrange("(k p) n -> p k n", p=128), writes=[g2s])
        if l > 0:
            v2s = P.sb([64, 512], BF16, name="v2s")
            P.dma("pool", v2s[:], v2_d.ap(), writes=[v2s])
        wk = P.pool("wkr", 30, [128, 512])

        def fm_out(name, src, j, m0):
            P.dma(dq(), o_rfm[name].ap()[j * 128:(j + 1) * 128, m0:m0 + 512], src[:], reads=[src], writes=["o_rfm_" + name])

        tcount = [0]

        def tm_out(name, src, j, m0):
            pt = pp()
            for s in range(4):
                P.op("pe", lambda e, pt=pt, s=s: e.transpose(pt[:, s * 128:(s + 1) * 128], src[:, s * 128:(s + 1) * 128], ident[:]),
                     reads=[src, ident], writes=[pt])
            ob = wk()
            tcount[0] += 1
            evac(ob[:], pt[:], tcount[0], [pt], [ob])
            P.dma(dq(), o_rtm[name].ap()[m0:m0 + 512, j * 128:(j + 1) * 128].rearrange("(s p) c -> p s c", p=128),
                  ob[:].rearrange("p (s c) -> p s c", s=4), reads=[ob], writes=["o_rtm_" + name])

        for j in range(4):
            wr_ = load_wblock(win_d.ap()[:, 1728 + j * 128:1728 + (j + 1) * 128])
            wk_ = load_wblock(win_d.ap()[:, 2240 + j * 128:2240 + (j + 1) * 128])
            wv_ = load_wblock(win_d.ap()[:, 2752 + j * 128:2752 + (j + 1) * 128])
            for n, (t0, nn) in enumerate(TILES):
                m0 = t0 - HALO
                mixed = []
                for idx, w_ in enumerate((wr_, wk_, wv_)):
                    ps_a = proj(w_, 128, t0, 512)
                    ps_s = proj(w_, 128, t0, 512, shift=1)
                    base = wk(); dd = wk(); mx = wk()
                    P.op("act", lambda e, base=base, ps_a=ps_a: e.activation(base[:], ps_a[:], AF.Copy), reads=[ps_a], writes=[base])
                    P.op("dve", lambda e, dd=dd, ps_s=ps_s, base=base: e.tensor_tensor(dd[:], ps_s[:], base[:], ALU.subtract), reads=[ps_s, base], writes=[dd])
                    P.op("dve", lambda e, dd=dd, mx=mx, base=base, idx=idx: e.scalar_tensor_tensor(mx[:], dd[:], col("murkv", idx * 4 + j), base[:], ALU.mult, ALU.add),
                         reads=[dd, base, pk], writes=[mx])
                    mixed.append(mx)
                rp, kp, vp = mixed
                ps = pp()
                P.op("pe", lambda e, ps=ps: e.matmul(ps[:], w2s[:, j * 128:(j + 1) * 128], lo["w"][0:96, 0, m0:m0 + 512], start=True, stop=True),
                     reads=[w2s, ("lo", "w", n)], writes=[ps])
                dec = wk()
                P.op("act", lambda e, ps=ps, dec=dec: e.activation(dec[:], ps[:], AF.Sigmoid, bias=col("w0", j)), reads=[ps, pk], writes=[dec])
                P.op("act", lambda e, dec=dec: e.activation(dec[:], dec[:], AF.Exp, scale=-float(np.exp(-0.5))), reads=[dec], writes=[dec])
                ps = pp()
                P.op("pe", lambda e, ps=ps: e.matmul(ps[:], a2s[:, j * 128:(j + 1) * 128], lo["a"][0:96, 0, m0:m0 + 512], start=True, stop=True),
                     reads=[a2s, ("lo", "a", n)], writes=[ps])
                at = wk()
                P.op("act", lambda e, ps=ps, at=at: e.activation(at[:], ps[:], AF.Sigmoid, bias=col("a0", j)), reads=[ps, pk], writes=[at])
                ps = pp()
                for k in range(2):
                    P.op("pe", lambda e, ps=ps, k=k: e.matmul(ps[:], g2s[:, k, j * 128:(j + 1) * 128], lo["g"][:, k, m0:m0 + 512], start=(k == 0), stop=(k == 1)),
                         reads=[g2s, ("lo", "g", n)], writes=[ps])
                gt = wk()
                P.op("dve", lambda e, ps=ps, gt=gt: e.tensor_copy(gt[:], ps[:]), reads=[ps], writes=[gt])
                fm_out("g", gt, j, m0)
                if l > 0:
                    ps = pp()
                    P.op("pe", lambda e, ps=ps: e.matmul(ps[:], v2s[:, j * 128:(j + 1) * 128], lo["v"][0:64, 0, m0:m0 + 512], start=True, stop=True),
                         reads=[v2s, ("lo", "v", n)], writes=[ps])
                    vg = wk(); vf = wk(); vpp = wk()
                    P.op("act", lambda e, ps=ps, vg=vg: e.activation(vg[:], ps[:], AF.Sigmoid, bias=col("v0", j)), reads=[ps, pk], writes=[vg])
                    P.dma(dq(), vf[:], vf_d.ap()[j * 128:(j + 1) * 128, m0:m0 + 512], writes=[vf])
                    P.op("dve", lambda e, vf=vf, vp=vp: e.tensor_tensor(vf[:], vf[:], vp[:], ALU.subtract), reads=[vf, vp], writes=[vf])
                    P.op("dve", lambda e, vf=vf, vg=vg: e.tensor_tensor(vf[:], vf[:], vg[:], ALU.mult), reads=[vf, vg], writes=[vf])
                    P.op("dve", lambda e, vf=vf, vp=vp, vpp=vpp: e.tensor_tensor(vpp[:], vf[:], vp[:], ALU.add), reads=[vf, vp], writes=[vpp])
                else:
                    vpp = vp
                    fm_out("vfirst", vp, j, m0)
                fm_out("v", vpp, j, m0)
                kkr = wk(); sq = wk(); nrm = wk(); kkn = wk()
                P.op("dve", lambda e, kkr=kkr, kp=kp: e.tensor_scalar_mul(kkr[:], kp[:], col("kk", j)), reads=[kp, pk], writes=[kkr])
                P.op("act", lambda e, kkr=kkr, sq=sq: e.activation(sq[:], kkr[:], AF.Square), reads=[kkr], writes=[sq])
                ps = pp()
                P.op("pe", lambda e, ps=ps, sq=sq: e.matmul(ps[:], bones[:], sq[:], start=True, stop=True), reads=[bones, sq], writes=[ps])
                P.op("act", lambda e, ps=ps, nrm=nrm: e.activation(nrm[:], ps[:], AF.Sqrt), reads=[ps], writes=[nrm])
                P.op("dve", lambda e, nrm=nrm: e.tensor_scalar_max(nrm[:], nrm[:], 1e-12), reads=[nrm], writes=[nrm])
                P.op("dve", lambda e, nrm=nrm: e.reciprocal(nrm[:], nrm[:]), reads=[nrm], writes=[nrm])
                P.op("dve", lambda e, nrm=nrm, kkr=kkr, kkn=kkn: e.tensor_tensor(kkn[:], kkr[:], nrm[:], ALU.mult), reads=[kkr, nrm], writes=[kkn])
                tt = wk(); k2 = wk(); kka = wk()
                P.op("dve", lambda e, tt=tt, at=at: e.tensor_scalar(tt[:], at[:], -1.0, col("ka", j), ALU.add, ALU.mult), reads=[at, pk], writes=[tt])
                P.op("dve", lambda e, tt=tt, k2=k2, kp=kp: e.scalar_tensor_tensor(k2[:], tt[:], 1.0, kp[:], ALU.add, ALU.mult), reads=[tt, kp], writes=[k2])
                P.op("dve", lambda e, kka=kka, kkn=kkn, at=at: e.tensor_tensor(kka[:], kkn[:], at[:], ALU.mult), reads=[kkn, at], writes=[kka])
                rk = wk(); bon = wk()
                P.op("dve", lambda e, rk=rk, rp=rp, k2=k2: e.scalar_tensor_tensor(rk[:], rp[:], col("rk", j), k2[:], ALU.mult, ALU.mult), reads=[rp, k2, pk], writes=[rk])
                ps = pp()
                P.op("pe", lambda e, ps=ps, rk=rk: e.matmul(ps[:], bones[:], rk[:], start=True, stop=True), reads=[bones, rk], writes=[ps])
                P.op("dve", lambda e, ps=ps, bon=bon, vpp=vpp: e.tensor_tensor(bon[:], ps[:], vpp[:], ALU.mult), reads=[ps, vpp], writes=[bon])
                fm_out("bonus", bon, j, m0)
                for name, src in (("r", rp), ("w", dec), ("k", k2), ("kk", kkn), ("kka", kka)):
                    tm_out(name, src, j, m0)

    nc = P.finish(finals)
    return nc, P


TC = 8
NEG = -30000.0


def band_bias(rel_table_h):
    q = np.arange(128)
    a, c = q // 64, q % 64
    kk = np.arange(640)
    u, kq = kk // 64, kk % 64
    w = 64 * (u[None, :] - a[:, None]) + kq[None, :]
    valid = np.where(a[:, None] == 0, u[None, :] <= 8, u[None, :] >= 1)
    rel = np.clip((512 + c[:, None]) - w, -256, 256) + 256
    out = rel_table_h[np.clip(rel, 0, 512)].astype(np.float32)
    return np.where(valid, out, np.float32(NEG)).astype(np.float32)


def build_B(l):
    P = Prog()
    mq_d = P.din("mla_qT", [2, 192, SEQ], BF16)
    mk_d = P.din("mla_kT", [2, 128, SEQ], BF16)
    mkpe_d = P.din("mla_kpeT", [64, SEQ], BF16)
    mv_d = P.din("mla_v", [SEQ, 256], BF16)
    cq_d = P.din("ca_qT", [2, 128, SEQ], BF16)
    ck_d = P.din("ca_kT", [2, 128, SEQ], BF16)
    cv_d = P.din("ca_v", [SEQ, 256], BF16)
    bias_d = P.din("ca_bias", [2, 128, 640])
    ident_d = P.din("ident", [128, 128])
    rwbc_d = P.din("rw_bc", [2, SEQ, 640])
    rv_d = P.din("rw_vT", [256, SEQ])
    rb_d = P.din("rw_bonusT", [256, SEQ])
    rg_d = P.din("rw_gT", [256, SEQ])
    gn_d = P.din("rw_gn", [128, 4])
    o_mla = P.dout("y_mlaT", [256, SEQ], BF16)
    o_ca = P.dout("y_caT", [256, SEQ], BF16)
    o_rw = P.dout("y_rwT", [256, SEQ], BF16)

    ident = P.sb([128, 128], name="ident")
    identb = P.sb([128, 128], BF16, name="identb")
    bones = P.sb([128, 128], name="bones")
    epsc = P.sb([128, 2], name="epsc")
    gn = P.sb([128, 4], name="gn")
    bias = P.sb([128, 2, 640], name="bias")
    qn = P.sb([128, SEQ], BF16, name="qn")
    qp = P.sb([64, SEQ], BF16, name="qp")
    kn = P.sb([128, SEQ], BF16, name="kn")
    kp = P.sb([64, SEQ], BF16, name="kp")
    vts = [P.sb([128, 32, 128], BF16, name="vt%d" % i) for i in range(2)]
    scb = [P.sb([128, SEQ], name="sc%d" % i) for i in range(2)]
    pb = [P.sb([128, SEQ], BF16, name="pb%d" % i) for i in range(2)]
    PT = P.pool("PT", 3, [128, 5, 128], BF16)
    outb = P.sb([128, SEQ], BF16, name="outb")
    stt_ = P.pool("stt", 6, [128, 4])
    dg = P.pool("dg", 3, [128, 128], BF16)
    psc = P.pool("psc", 3, [128, 512], psum=True)
    ppt = P.pool("ppt", 2, [128, 512], psum=True)
    pot = P.ps([128, 512], name="pot")
    Ss = [P.sb([128, 128], name="S%d" % i) for i in range(2)]
    tmps = [P.sb([128, 128], name="tmp%d" % i) for i in range(4)]
    nsk = P.pool("nsk", 8, [128, 2])
    bc = [P.sb([128, TC, 2, 5, 64], name="bc%d" % i) for i in range(2)]
    vsb = [P.sb([128, 2, 512], name="vsb%d" % i) for i in range(2)]
    ysb = P.sb([128, 2, SEQ], name="ysb")

    P.dma("act", ident[:], ident_d.ap(), writes=[ident])
    P.dma("act", gn[:], gn_d.ap(), writes=[gn])
    P.dma("act", bias[:], bias_d.ap().rearrange("h q k -> q h k"), writes=[bias])
    P.dma("act", kp[:], mkpe_d.ap(), writes=[kp])
    P.op("act", lambda e: e.activation(identb[:], ident[:], AF.Copy), reads=[ident], writes=[identb])
    P.op("pool", lambda e: e.memset(epsc[:, 0:1], 64e-5), writes=[epsc])
    P.op("pool", lambda e: e.memset(bones[:], 0.0), writes=[bones])
    P.op("pool", lambda e: e.memset(bones[0:64, 0:64], 1.0), reads=[bones], writes=[bones])
    P.op("pool", lambda e: e.memset(bones[64:128, 64:128], 1.0), reads=[bones], writes=[bones])
    P.op("dve", lambda e: e.memset(Ss[0][:], 0.0), writes=[(Ss[0], 0), (Ss[0], 1)])

    dqc = [0]

    def dq():
        dqc[0] += 1
        return ("act", "pool")[dqc[0] % 2]

    units = []
    otc = [0]

    def make_unit(kind, hh, i):
        ub = len(units) % 2
        sc = scb[ub]; pbuf = pb[ub]
        vt = vts[(len(units) // 32) % 2]
        st = {}
        if kind == "mla":
            kt0 = 0
            scale = 192.0 ** -0.5
        else:
            kt0 = max(0, i - 4)
            scale = 128.0 ** -0.5
        nkt = i - kt0 + 1
        nk = nkt * 128

        def stageA():
            if i == 0:
                if kind == "mla":
                    P.dma(dq(), qn[:], mq_d.ap()[hh, 0:128, :], writes=[qn])
                    P.dma(dq(), qp[:], mq_d.ap()[hh, 128:192, :], writes=[qp])
                    P.dma(dq(), kn[:], mk_d.ap()[hh], writes=[kn])
                    P.dma(dq(), vt[:], mv_d.ap()[:, hh * 128:(hh + 1) * 128].rearrange("(n p) d -> p n d", p=128), writes=[vt])
                else:
                    P.dma(dq(), qn[:], cq_d.ap()[hh], writes=[qn])
                    P.dma(dq(), kn[:], ck_d.ap()[hh], writes=[kn])
                    P.dma(dq(), vt[:], cv_d.ap()[:, hh * 128:(hh + 1) * 128].rearrange("(n p) d -> p n d", p=128), writes=[vt])
            qs = slice(i * 128, (i + 1) * 128)
            c0 = 0
            while c0 < nk:
                n_c = min(512, nk - c0)
                ps = psc()
                k0 = kt0 * 128 + c0
                P.op("pe", lambda e: e.matmul(ps[:, 0:n_c], qn[:, qs], kn[:, k0:k0 + n_c], start=True, stop=(kind != "mla")),
                     reads=[qn, kn], writes=[ps])
                if kind == "mla":
                    P.op("pe", lambda e: e.matmul(ps[:, 0:n_c], qp[:, qs], kp[:, k0:k0 + n_c], start=False, stop=True),
                         reads=[qp, kp], writes=[ps])
                    P.op("act", lambda e: e.activation(sc[:, c0:c0 + n_c], ps[:, 0:n_c], AF.Copy), reads=[ps], writes=[sc])
                else:
                    P.op("act", lambda e: e.activation(sc[:, c0:c0 + n_c], ps[:, 0:n_c], AF.Copy, scale=scale), reads=[ps], writes=[sc])
                c0 += n_c
            if kind == "mla":
                P.op("pool", lambda e: e.memset(sc[0:64, nk - 64:nk], NEG / scale), reads=[sc], writes=[sc])
            else:
                p0 = (kt0 - (i - 4)) * 128
                P.op("pool", lambda e: e.tensor_tensor(sc[:, 0:nk], sc[:, 0:nk], bias[:, hh, p0:p0 + nk], ALU.add), reads=[sc, bias], writes=[sc])

        def stageB():
            s4 = stt_()
            st["s4"] = s4
            P.op("dve", lambda e: e.tensor_reduce(s4[:, 0:1], sc[:, 0:nk], AX.X, ALU.max), reads=[sc], writes=[s4])
            esc = scale if kind == "mla" else 1.0
            P.op("dve", lambda e: e.tensor_scalar_mul(s4[:, 1:2], s4[:, 0:1], -esc), reads=[s4], writes=[s4])
            P.op("act", lambda e: e.activation(pbuf[:, 0:nk], sc[:, 0:nk], AF.Exp, bias=s4[:, 1:2], scale=esc, accum_out=s4[:, 2:3]),
                 reads=[sc, s4], writes=[pbuf, s4])

        def stageC():
            s4 = st["s4"]
            P.op("dve", lambda e: e.reciprocal(s4[:, 3:4], s4[:, 2:3]), reads=[s4], writes=[s4])
            d = dg()
            P.op("act", lambda e: e.activation(d[:], identb[:], AF.Copy, scale=s4[:, 3:4]), reads=[identb, s4], writes=[d])
            oc = (otc[0] % 4) * 128
            otc[0] += 1
            okey = ("pot", oc)
            for g0 in range(0, nkt, 4):
                gn_ = min(4, nkt - g0)
                pt = ppt()
                for t in range(gn_):
                    kt = g0 + t
                    P.op("pe", lambda e: e.matmul(pt[:, t * 128:(t + 1) * 128], pbuf[:, kt * 128:(kt + 1) * 128], d[:], start=True, stop=True),
                         reads=[pbuf, d], writes=[pt])
                ptb = PT()
                P.op("act", lambda e: e.activation(ptb[:, 0:gn_, :], pt[:, 0:gn_ * 128].rearrange("p (t k) -> p t k", t=gn_), AF.Copy),
                     reads=[pt], writes=[ptb])
                for t in range(gn_):
                    kt = g0 + t
                    P.op("pe", lambda e: e.matmul(pot[:, oc:oc + 128], vt[:, kt0 + kt, :], ptb[:, t, :], start=(kt == 0), stop=(kt == nkt - 1)),
                         reads=[vt, ptb], writes=[okey])
            P.op("act", lambda e: e.activation(outb[:, i * 128:(i + 1) * 128], pot[:, oc:oc + 128], AF.Copy), reads=[okey], writes=[outb])
            if i == 31:
                od = o_mla if kind == "mla" else o_ca
                P.dma(dq(), od.ap()[hh * 128:(hh + 1) * 128, :], outb[:], reads=[outb], writes=["o_" + kind])
        units.append((stageA, stageB, stageC))

    for kind in ("mla", "ca"):
        for hh in range(2):
            for i in range(32):
                make_unit(kind, hh, i)


    def scan_chunk(c):
        t0 = c * TC
        b = bc[c % 2]
        bkey = ("bc", c % 2)
        for half in range(2):
            P.dma("sp", b[half * 64:(half + 1) * 64].rearrange("p t g a k -> p t (g a k)"),
                  rwbc_d.ap()[half, t0:t0 + TC, :].partition_broadcast(64), writes=[bkey])
        if t0 % 512 == 0:
            vb = vsb[(t0 // 512) % 2]
            P.dma("sp", vb[:], rv_d.ap()[:, t0:t0 + 512].rearrange("(g p) t -> p g t", p=128), writes=[vb])
        vb = vsb[(t0 // 512) % 2]
        for tt in range(TC):
            t = t0 + tt
            ns = nsk()
            So = Ss[t % 2]; Sn = Ss[(t + 1) % 2]
            Sog = So[:].rearrange("p (g k) -> p g k", g=2)
            Sng = Sn[:].rearrange("p (g k) -> p g k", g=2)
            tm1 = tmps[(2 * t) % 4]; tm2 = tmps[(2 * t + 1) % 4]
            tm1g = tm1[:].rearrange("p (g k) -> p g k", g=2)
            tm2g = tm2[:].rearrange("p (g k) -> p g k", g=2)
            P.op("dve", lambda e: e.tensor_tensor(tm1g, Sog, b[:, tt, :, 0, :], ALU.mult), reads=[(So, 0), (So, 1), bkey], writes=[tm1])
            P.op("dve", lambda e: e.tensor_tensor(Sng, Sog, b[:, tt, :, 1, :], ALU.mult), reads=[(So, 0), (So, 1), bkey], writes=[(Sn, 0), (Sn, 1)])
            P.op("dve", lambda e: e.tensor_reduce(ns[:], tm1g, AX.X, ALU.add, negate=True), reads=[tm1], writes=[ns])
            for g in range(2):
                P.op("dve", lambda e: e.scalar_tensor_tensor(Sn[:, g * 64:(g + 1) * 64], b[:, tt, g, 3, :], vb[:, g, (t % 512):(t % 512) + 1],
                                                            Sn[:, g * 64:(g + 1) * 64], ALU.mult, ALU.add), reads=[(Sn, g), bkey, vb], writes=[(Sn, g)])
            for g in range(2):
                P.op("dve", lambda e: e.scalar_tensor_tensor(Sn[:, g * 64:(g + 1) * 64], b[:, tt, g, 2, :], ns[:, g:g + 1],
                                                            Sn[:, g * 64:(g + 1) * 64], ALU.mult, ALU.add), reads=[(Sn, g), bkey, ns], writes=[(Sn, g)])
            P.op("dve", lambda e: e.tensor_tensor(tm2g, Sng, b[:, tt, :, 4, :], ALU.mult), reads=[(Sn, 0), (Sn, 1), bkey], writes=[tm2])
            P.op("dve", lambda e: e.tensor_reduce(ysb[:, :, t], tm2g, AX.X, ALU.add), reads=[tm2], writes=[("ysb", t // 512)])

    NCH = SEQ // TC
    per = NCH // len(units)
    for c in range(NCH):
        u = c // per if c % per == 0 else None
        if u is not None and u < len(units):
            units[u][0]()
        scan_chunk(c)
        if u is not None and u < len(units):
            units[u][1]()
            if u >= 1:
                units[u - 1][2]()
    units[-1][2]()

    wkv = [scb[0][:, i * 512:(i + 1) * 512] for i in range(8)] + [scb[1][:, i * 512:(i + 1) * 512] for i in range(8)]
    wi = [0]

    def wk():
        wi[0] += 1
        j = wi[0] % 16
        return wkv[j], ("wkv", j)

    first = [True]
    for g in range(2):
        for n in range(8):
            m0 = n * 512
            extra = ([scb[0], scb[1]] + [("wkv", j) for j in range(16)]) if first[0] else []
            first[0] = False
            sq, sqk = wk()
            P.op("act", lambda e: e.activation(sq, ysb[:, g, m0:m0 + 512], AF.Square), reads=[("ysb", n)], writes=[sqk] + extra)
            ps1 = psc(); ps2 = psc()
            P.op("pe", lambda e: e.matmul(ps1[:], bones[:], ysb[:, g, m0:m0 + 512], start=True, stop=True), reads=[bones, ("ysb", n)], writes=[ps1])
            P.op("pe", lambda e: e.matmul(ps2[:], bones[:], sq, start=True, stop=True), reads=[bones, sqk], writes=[ps2])
            mean, mk_ = wk(); msq, msk = wk(); rstd, rsk = wk(); yn, ynk = wk(); bt, btk = wk(); gt, gtk = wk()
            P.op("act", lambda e: e.activation(mean, ps1[:], AF.Copy, scale=1.0 / 64), reads=[ps1], writes=[mk_])
            P.op("dve", lambda e: e.tensor_tensor(msq, mean, mean, ALU.mult), reads=[mk_], writes=[msk])
            P.op("dve", lambda e: e.scalar_tensor_tensor(rstd, ps2[:], 1.0 / 64, msq, ALU.mult, ALU.subtract), reads=[ps2, msk], writes=[rsk])
            P.op("act", lambda e: e.activation(rstd, rstd, AF.Sqrt, bias=epsc[:, 0:1]), reads=[rsk, epsc], writes=[rsk])
            P.op("dve", lambda e: e.reciprocal(rstd, rstd), reads=[rsk], writes=[rsk])
            P.op("dve", lambda e: e.tensor_tensor(yn, ysb[:, g, m0:m0 + 512], mean, ALU.subtract), reads=[("ysb", n), mk_], writes=[ynk])
            P.op("dve", lambda e: e.tensor_tensor(yn, yn, rstd, ALU.mult), reads=[ynk, rsk], writes=[ynk])
            P.op("dve", lambda e: e.tensor_scalar(yn, yn, gn[:, g:g + 1], gn[:, 2 + g:3 + g], ALU.mult, ALU.add), reads=[ynk, gn], writes=[ynk])
            P.dma(dq(), bt, rb_d.ap()[g * 128:(g + 1) * 128, m0:m0 + 512], writes=[btk])
            P.dma(dq(), gt, rg_d.ap()[g * 128:(g + 1) * 128, m0:m0 + 512], writes=[gtk])
            P.op("dve", lambda e: e.tensor_tensor(yn, yn, bt, ALU.add), reads=[ynk, btk], writes=[ynk])
            ob = PT()
            obv = ob[:].rearrange("p a b -> p (a b)")[:, 0:512]
            P.op("dve", lambda e: e.tensor_tensor(obv, yn, gt, ALU.mult), reads=[ynk, gtk], writes=[ob])
            P.dma(dq(), o_rw.ap()[g * 128:(g + 1) * 128, m0:m0 + 512], obv, reads=[ob], writes=["o_rw"])
    nc = P.finish(["o_mla", "o_ca", "o_rw"])
    return nc, P


def inputs_B(inp, l, c, A):
    b, g = c // 2, c % 2
    cat = lambda name, ax: np.concatenate([A[2 * b][name], A[2 * b + 1][name]], axis=ax)
    m = {"mla_qT": np.ascontiguousarray(cat("mla_qT", 2)[2 * g:2 * g + 2]),
         "mla_kT": np.ascontiguousarray(cat("mla_kT", 2)[2 * g:2 * g + 2]),
         "mla_kpeT": cat("mla_kpeT", 1),
         "mla_v": np.ascontiguousarray(cat("mla_v", 0)[:, 256 * g:256 * (g + 1)]),
         "ca_qT": np.ascontiguousarray(cat("ca_qT", 1)[256 * g:256 * (g + 1)].reshape(2, 128, SEQ)),
         "ca_kT": np.ascontiguousarray(cat("ca_kT", 1)[256 * g:256 * (g + 1)].reshape(2, 128, SEQ)),
         "ca_v": np.ascontiguousarray(cat("ca_v", 0)[:, 256 * g:256 * (g + 1)]),
         "ca_bias": np.stack([band_bias(inp["ca_rel_bias"][l][2 * g + hh]) for hh in range(2)]),
         "ident": np.eye(128, dtype=np.float32),
         "rw_vT": np.ascontiguousarray(cat("rw_vT", 1)[256 * g:256 * (g + 1)]),
         "rw_bonusT": np.ascontiguousarray(cat("rw_bonusT", 1)[256 * g:256 * (g + 1)]),
         "rw_gT": np.ascontiguousarray(cat("rw_gT", 1)[256 * g:256 * (g + 1)]),
         "rw_gn": np.concatenate([fm(inp["rw_gn_g"][l][256 * g:256 * (g + 1)]), fm(inp["rw_gn_b"][l][256 * g:256 * (g + 1)])], 1)}
    arrs = [cat("rw_%s_tm" % n, 0)[:, 256 * g:256 * (g + 1)].reshape(SEQ, 2, 2, 64) for n in ("kk", "w", "kka", "k", "r")]
    st = np.stack(arrs, 0)
    m["rw_bc"] = np.ascontiguousarray(np.transpose(st, (3, 1, 2, 0, 4))).reshape(2, SEQ, 640)
    return m


def build_M():
    P = Prog()
    ct_d = P.din("cT4", [128, 64])
    mw_d = P.din("mod_w_s", [D, 1536])
    mb_d = P.din("mod_b_s", [128, 12])
    o = P.dout("modT", [128, 48])
    sct = P.sb([128, 64], name="sct")
    mb = P.sb([128, 12], name="mb")
    res = P.sb([128, 12, 4], name="res")
    blk = [P.sb([128, 16, 128], name="blk%d" % i) for i in range(3)]
    ps = P.ps([128, 48], name="pm")
    P.dma("sp", sct[:], ct_d.ap(), writes=[sct])
    P.dma("sp", mb[:], mb_d.ap(), writes=[mb])
    sct0 = sct
    sct = P.sb([128, 64], name="sct2")
    P.op("act", lambda e: e.activation(sct[:], sct0[:], AF.Silu), reads=[sct0], writes=[sct])
    for j in range(12):
        b = blk[j % 3]
        P.dma(("sp", "act")[j % 2], b[:], mw_d.ap()[:, j * 128:(j + 1) * 128].rearrange("(k p) n -> p k n", p=128), writes=[b])
        for k in range(16):
            P.op("pe", lambda e: e.matmul(ps[:, j * 4:(j + 1) * 4], b[:, k, :], sct[:, k * 4:(k + 1) * 4], start=(k == 0), stop=(k == 15)),
                 reads=[b, sct], writes=[("pm", j)])
    for j in range(12):
        P.op("dve", lambda e: e.tensor_scalar_add(res[:, j, :], ps[:, j * 4:(j + 1) * 4], mb[:, j:j + 1]), reads=[("pm", jj) for jj in range(12)] + [mb], writes=[res])
    P.dma("sp", o.ap(), res[:].rearrange("p j b -> p (j b)"), reads=[res], writes=["o"])
    return P.finish(["o"]), P


def ln_fm(P, z, zkey, n, ones, epsc_col, wk, pp, inv_d, stp):
    ps1 = pp(); ps2 = pp()
    for k in range(16):
        sq = wk()
        P.op("act", lambda e: e.activation(sq[:, 0:n], z[:, k, 0:n], AF.Square), reads=[zkey], writes=[sq])
        P.op("pe", lambda e: e.matmul(ps1[:, 0:n], ones[:], z[:, k, 0:n], start=(k == 0), stop=(k == 15)), reads=[ones, zkey], writes=[ps1])
        P.op("pe", lambda e: e.matmul(ps2[:, 0:n], ones[:], sq[:, 0:n], start=(k == 0), stop=(k == 15)), reads=[ones, sq], writes=[ps2])
    mean = stp(); msq = stp(); rstd = stp()
    P.op("act", lambda e: e.activation(mean[:, 0:n], ps1[:, 0:n], AF.Copy, scale=inv_d), reads=[ps1], writes=[mean])
    P.op("dve", lambda e: e.tensor_tensor(msq[:, 0:n], mean[:, 0:n], mean[:, 0:n], ALU.mult), reads=[mean], writes=[msq])
    P.op("dve", lambda e: e.scalar_tensor_tensor(rstd[:, 0:n], ps2[:, 0:n], inv_d, msq[:, 0:n], ALU.mult, ALU.subtract), reads=[ps2, msq], writes=[rstd])
    P.op("act", lambda e: e.activation(rstd[:, 0:n], rstd[:, 0:n], AF.Sqrt, bias=epsc_col), reads=[rstd], writes=[rstd])
    P.op("dve", lambda e: e.reciprocal(rstd[:, 0:n], rstd[:, 0:n]), reads=[rstd], writes=[rstd])
    return mean, rstd


PKC = {}
_o = 0
for _n, _w in (("modsh", 96), ("modoff", 96), ("lng", 16), ("lnb", 16), ("rb", 1)):
    PKC[_n] = (_o, _w)
    _o += _w
NPKC = _o


def pack_C(inp, l, which, modsh):
    pk = np.zeros((128, NPKC), np.float32)
    pk[:, 0:96] = modsh
    pk[:, 96:192] = fm(inp["mod_offset"][l])
    pk[:, 192:208] = fm(inp["ln_post_g"][l, which])
    pk[:, 208:224] = fm(inp["ln_post_b"][l, which])
    pk[0:32, 224] = inp["moe_router_b"][l]
    return pk


def build_C():
    P = Prog()
    yc_d = P.din("ycatT", [D, TOK], BF16)
    x_d = P.din("x", [TOK, D])
    wo_d = P.din("w_out", [D, D])
    pk_d = P.din("pk", [128, NPKC])
    rw_d = P.din("router_w", [D, 32])
    ident_d = P.din("ident", [128, 128])
    o_x1 = P.dout("x1T", [D, TOK])
    o_h2 = P.dout("h2T", [D, TOK], BF16)
    o_g = P.dout("gateT", [32, TOK])

    pk = P.sb([128, NPKC], name="pk")
    ident = P.sb([128, 128], name="ident")
    ones = P.sb([128, 128], name="ones")
    epsc = P.sb([128, 1], name="epsc")
    modp = P.sb([128, 96], name="modp")
    g1p = P.sb([128, 16], name="g1p")
    sc2p = P.sb([128, 16], name="sc2p")
    rw = P.sb([128, 16, 32], name="rw")
    z = P.sb([128, 16, 512], name="z")
    ycT = P.sb([128, 16, 512], BF16, name="ycT")
    h2b = P.sb([128, 16, 512], BF16, name="h2b")
    xt = P.pool("xt", 2, [128, D])
    wb = P.pool("wb", 4, [128, 16, 128], BF16)
    wk = P.pool("wk", 8, [128, 512])
    stp = P.pool("stp", 6, [128, 512])
    pp = P.pool("pp", 6, [128, 512], psum=True)
    plg = P.ps([32, 512], name="plg")
    ptk = P.ps([128, 512], name="ptk")
    lgs = P.sb([32, 512], name="lgs")
    lt = P.sb([128, 4, 32], name="lt")
    gt = P.sb([128, 4, 32], name="gt")
    ex = P.sb([128, 4, 32], name="ex")
    m8 = P.sb([128, 4, 8], name="m8")
    sm = P.sb([128, 4, 4], name="sm")
    gT = P.sb([32, 512], name="gT")

    def col(n, j=0, w=1):
        o, _ = PKC[n]
        return pk[:, o + j:o + j + w]

    P.dma("sp", pk[:], pk_d.ap(), writes=[pk])
    P.dma("sp", ident[:], ident_d.ap(), writes=[ident])
    P.dma("sp", rw[:], rw_d.ap().rearrange("(k p) n -> p k n", p=128), writes=[rw])
    P.op("pool", lambda e: e.memset(ones[:], 1.0), writes=[ones])
    P.op("pool", lambda e: e.memset(epsc[:], LN_EPS), writes=[epsc])
    P.op("dve", lambda e: e.tensor_tensor(modp[:], col("modsh", 0, 96), col("modoff", 0, 96), ALU.add), reads=[pk], writes=[modp])
    P.op("dve", lambda e: e.tensor_scalar_add(g1p[:], modp[:, 32:48], 1.0), reads=[modp], writes=[g1p])
    P.op("dve", lambda e: e.tensor_scalar_add(sc2p[:], modp[:, 64:80], 1.0), reads=[modp], writes=[sc2p])
    dqc = [0]

    def dq():
        dqc[0] += 1
        return ("sp", "act")[dqc[0] % 2]

    for n in range(4):
        m0 = n * 512
        P.dma(dq(), ycT[:], yc_d.ap()[:, m0:m0 + 512].rearrange("(k p) t -> p k t", p=128), writes=[ycT])
        for s in range(4):
            xx = xt()
            P.dma(dq(), xx[:], x_d.ap()[m0 + s * 128:m0 + (s + 1) * 128, :], writes=[xx])
            for g in range(4):
                pt = pp()
                for q in range(4):
                    k = g * 4 + q
                    P.op("pe", lambda e: e.transpose(pt[:, q * 128:(q + 1) * 128], xx[:, k * 128:(k + 1) * 128], ident[:]), reads=[xx, ident], writes=[pt])
                P.op("act", lambda e: e.activation(z[:, g * 4:(g + 1) * 4, s * 128:(s + 1) * 128], pt[:].rearrange("p (q t) -> p q t", q=4), AF.Copy, scale=DN_ALPHA),
                     reads=[pt], writes=["z"])
        for dc in range(16):
            w = wb()
            P.dma("pool", w[:], wo_d.ap()[:, dc * 128:(dc + 1) * 128].rearrange("(k p) n -> p k n", p=128), writes=[w])
            ps = pp()
            for k in range(16):
                P.op("pe", lambda e: e.matmul(ps[:], w[:, k, :], ycT[:, k, :], start=(k == 0), stop=(k == 15)), reads=[w, ycT], writes=[ps])
            P.op("dve", lambda e: e.scalar_tensor_tensor(z[:, dc, :], ps[:], g1p[:, dc:dc + 1], z[:, dc, :], ALU.mult, ALU.add), reads=[ps, g1p, "z"], writes=["z"])
        mean, rstd = ln_fm(P, z, "z", 512, ones, epsc[:, 0:1], wk, pp, 1.0 / D, stp)
        for k in range(16):
            t = wk()
            P.op("dve", lambda e: e.tensor_tensor(t[:], z[:, k, :], mean[:], ALU.subtract), reads=["z", mean], writes=[t])
            P.op("dve", lambda e: e.tensor_tensor(t[:], t[:], rstd[:], ALU.mult), reads=[t, rstd], writes=[t])
            P.op("dve", lambda e: e.tensor_scalar(z[:, k, :], t[:], col("lng", k), col("lnb", k), ALU.mult, ALU.add), reads=[t, pk, "z"], writes=["z"])
        P.dma(dq(), o_x1.ap()[:, m0:m0 + 512].rearrange("(k p) t -> p k t", p=128), z[:], reads=["z"], writes=["o_x1"])
        mean, rstd = ln_fm(P, z, "z", 512, ones, epsc[:, 0:1], wk, pp, 1.0 / D, stp)
        for k in range(16):
            t = wk()
            P.op("dve", lambda e: e.tensor_tensor(t[:], z[:, k, :], mean[:], ALU.subtract), reads=["z", mean], writes=[t])
            P.op("dve", lambda e: e.tensor_tensor(t[:], t[:], rstd[:], ALU.mult), reads=[t, rstd], writes=[t])
            P.op("dve", lambda e: e.tensor_scalar(t[:], t[:], sc2p[:, k:k + 1], modp[:, 48 + k:49 + k], ALU.mult, ALU.add), reads=[t, sc2p, modp], writes=[t])
            P.op("pe", lambda e: e.matmul(plg[:], rw[:, k, :], t[:], start=(k == 0), stop=(k == 15)), reads=[rw, t], writes=[plg])
            P.op("act", lambda e: e.activation(h2b[:, k, :], t[:], AF.Copy), reads=[t], writes=[h2b])
        P.dma(dq(), o_h2.ap()[:, m0:m0 + 512].rearrange("(k p) t -> p k t", p=128), h2b[:], reads=[h2b], writes=["o_h2"])
        P.op("act", lambda e: e.activation(lgs[:], plg[:], AF.Identity, bias=pk[0:32, PKC["rb"][0]:PKC["rb"][0] + 1]), reads=[plg, pk], writes=[lgs])
        for s in range(4):
            P.op("pe", lambda e: e.transpose(ptk[:, s * 32:(s + 1) * 32], lgs[:, s * 128:(s + 1) * 128], ident[0:32, 0:32]), reads=[lgs, ident], writes=[ptk])
        P.op("dve", lambda e: e.tensor_copy(lt[:], ptk[:, 0:128].rearrange("p (s e) -> p s e", s=4)), reads=[ptk], writes=[lt])
        for s in range(4):
            P.op("dve", lambda e: e.max(m8[:, s, :], lt[:, s, :]), reads=[lt], writes=[m8])
        P.op("dve", lambda e: e.tensor_scalar_mul(sm[:, :, 0:1], m8[:, :, 0:1], -1.0), reads=[m8], writes=[sm])
        for s in range(4):
            P.op("act", lambda e: e.activation(ex[:, s, :], lt[:, s, :], AF.Exp, bias=sm[:, s, 0:1]), reads=[lt, sm], writes=[ex])
            P.op("dve", lambda e: e.tensor_scalar(gt[:, s, :], lt[:, s, :], m8[:, s, 3:4], None, ALU.is_ge), reads=[lt, m8], writes=[gt])
            P.op("dve", lambda e: e.tensor_tensor(gt[:, s, :], gt[:, s, :], ex[:, s, :], ALU.mult), reads=[gt, ex], writes=[gt])
            P.op("dve", lambda e: e.tensor_reduce(sm[:, s, 1:2], gt[:, s, :], AX.X, ALU.add), reads=[gt], writes=[sm])
            P.op("dve", lambda e: e.reciprocal(sm[:, s, 2:3], sm[:, s, 1:2]), reads=[sm], writes=[sm])
            P.op("dve", lambda e: e.tensor_scalar_mul(gt[:, s, :], gt[:, s, :], sm[:, s, 2:3]), reads=[gt, sm], writes=[gt])
        for s in range(4):
            P.op("pe", lambda e: e.transpose(ptk[0:32, 128 + s * 128:128 + (s + 1) * 128] if False else plg[:, s * 128:(s + 1) * 128], gt[:, s, :], ident[:]),
                 reads=[gt, ident, lgs], writes=[plg])
        P.op("act", lambda e: e.activation(gT[:], plg[:], AF.Copy), reads=[plg], writes=[gT])
        P.dma(dq(), o_g.ap()[:, m0:m0 + 512], gT[:], reads=[gT], writes=["o_g"])
    return P.finish(["o_x1", "o_h2", "o_g"]), P


NTOK = NB * SEQ
TP = 1024


def build_D():
    P = Prog()
    h2_d = P.din("h2T_all", [D, NTOK], BF16)
    g_d = P.din("gate4", [4, NTOK])
    w1_d = P.din("w1", [4, D, 2 * D])
    w2_d = P.din("w2", [4, D, D])
    b1_d = P.din("b1T", [128, 4 * 32])
    b2_d = P.din("b2", [4, D])
    o_p = P.dout("partT", [D, NTOK])

    b1T = P.sb([128, 4, 32], name="b1T")
    b2s = P.sb([4, D], name="b2s")
    h2 = P.sb([128, 16, TP], BF16, name="h2")
    acc = P.sb([128, 16, TP], name="acc")
    act = P.sb([128, 16, TP], BF16, name="act")
    g4 = P.sb([4, TP], name="g4")
    wbc = P.pool("wbc", 2, [128, TP])
    w1b = P.pool("w1b", 2, [128, 16, 256], BF16)
    w2b = P.pool("w2b", 3, [128, 16, 128], BF16)
    wk = P.pool("wk", 8, [128, 512])
    pp = P.pool("pp", 8, [128, 512], psum=True)
    P.dma("sp", b1T[:], b1_d.ap().rearrange("p (e j) -> p e j", e=4), writes=[b1T])
    P.dma("sp", b2s[:], b2_d.ap(), writes=[b2s])
    for p_ in range(NTOK // TP):
        t0 = p_ * TP
        P.dma("sp", h2[:], h2_d.ap()[:, t0:t0 + TP].rearrange("(k p) t -> p k t", p=128), writes=[h2])
        P.dma("act", g4[:], g_d.ap()[:, t0:t0 + TP], writes=[g4])
        for dc in range(16):
            for tt in range(2):
                ps = pp()
                P.op("pe", lambda e: e.matmul(ps[:], b2s[:, dc * 128:(dc + 1) * 128], g4[:, tt * 512:(tt + 1) * 512], start=True, stop=True), reads=[b2s, g4], writes=[ps])
                P.op("act", lambda e: e.activation(acc[:, dc, tt * 512:(tt + 1) * 512], ps[:], AF.Copy), reads=[ps], writes=[("acc", dc, tt)])
        for ex_ in range(4):
            wb_ = wbc()
            P.dma("act", wb_[:], g_d.ap()[ex_:ex_ + 1, t0:t0 + TP].partition_broadcast(128), writes=[wb_])
            for j in range(16):
                w = w1b()
                P.dma("pool", w[:, :, 0:128], w1_d.ap()[ex_, :, j * 128:(j + 1) * 128].rearrange("(k p) n -> p k n", p=128), writes=[w])
                P.dma("pool", w[:, :, 128:256], w1_d.ap()[ex_, :, D + j * 128:D + (j + 1) * 128].rearrange("(k p) n -> p k n", p=128), reads=[w], writes=[w])
                for tt in range(2):
                    ts = slice(tt * 512, (tt + 1) * 512)
                    psg = pp(); psl = pp()
                    for k in range(16):
                        P.op("pe", lambda e: e.matmul(psg[:], w[:, k, 0:128], h2[:, k, ts], start=(k == 0), stop=(k == 15)), reads=[w, h2], writes=[psg])
                    for k in range(16):
                        P.op("pe", lambda e: e.matmul(psl[:], w[:, k, 128:256], h2[:, k, ts], start=(k == 0), stop=(k == 15)), reads=[w, h2], writes=[psl])
                    gl = wk(); sg = wk(); ln = wk()
                    P.op("dve", lambda e: e.tensor_scalar(gl[:], psg[:], b1T[:, ex_, j:j + 1], 7.0, ALU.add, ALU.min), reads=[psg, b1T], writes=[gl])
                    P.op("act", lambda e: e.activation(sg[:], gl[:], AF.Sigmoid, scale=1.702), reads=[gl], writes=[sg])
                    P.op("dve", lambda e: e.tensor_scalar(ln[:], psl[:], b1T[:, ex_, 16 + j:17 + j], 7.0, ALU.add, ALU.min), reads=[psl, b1T], writes=[ln])
                    P.op("dve", lambda e: e.tensor_scalar(ln[:], ln[:], -7.0, 1.0, ALU.max, ALU.add), reads=[ln], writes=[ln])
                    P.op("dve", lambda e: e.tensor_tensor(gl[:], gl[:], sg[:], ALU.mult), reads=[gl, sg], writes=[gl])
                    P.op("dve", lambda e: e.tensor_tensor(gl[:], gl[:], ln[:], ALU.mult), reads=[gl, ln], writes=[gl])
                    P.op("dve", lambda e: e.tensor_tensor(act[:, j, ts], gl[:], wb_[:, ts], ALU.mult), reads=[gl, wb_], writes=[("act", j, tt)])
            for dc in range(16):
                w = w2b()
                P.dma("pool", w[:], w2_d.ap()[ex_, :, dc * 128:(dc + 1) * 128].rearrange("(k p) n -> p k n", p=128), writes=[w])
                for tt in range(2):
                    ts = slice(tt * 512, (tt + 1) * 512)
                    ps = pp()
                    for k in range(16):
                        P.op("pe", lambda e: e.matmul(ps[:], w[:, k, :], act[:, k, ts], start=(k == 0), stop=(k == 15)), reads=[w, ("act", k, tt)], writes=[ps])
                    P.op("dve", lambda e: e.tensor_tensor(acc[:, dc, ts], ps[:], acc[:, dc, ts], ALU.add), reads=[ps, ("acc", dc, tt)], writes=[("acc", dc, tt)])
        P.dma("sp", o_p.ap()[:, t0:t0 + TP].rearrange("(k p) t -> p k t", p=128), acc[:],
              reads=[("acc", dc, tt) for dc in range(16) for tt in range(2)], writes=["o_p"])
    return P.finish(["o_p"]), P


def build_E():
    P = Prog()
    part_d = P.din("parts", [8, D, TOK])
    x1_d = P.din("x1T", [D, TOK])
    pk_d = P.din("pk", [128, NPKC])
    ident_d = P.din("ident", [128, 128])
    o_x = P.dout("xout", [TOK, D])
    pk = P.sb([128, NPKC], name="pk")
    ident = P.sb([128, 128], name="ident")
    ones = P.sb([128, 128], name="ones")
    epsc = P.sb([128, 1], name="epsc")
    modp = P.sb([128, 96], name="modp")
    g2p = P.sb([128, 16], name="g2p")
    u = P.sb([128, 16, 512], name="u")
    x1 = P.sb([128, 16, 512], name="x1")
    pb_ = P.pool("pb", 2, [128, 16, 512])
    ot = P.pool("ot", 2, [128, D])
    wk = P.pool("wk", 8, [128, 512])
    stp = P.pool("stp", 6, [128, 512])
    pp = P.pool("pp", 7, [128, 512], psum=True)

    def col(n, j=0, w=1):
        o, _ = PKC[n]
        return pk[:, o + j:o + j + w]
    P.dma("sp", pk[:], pk_d.ap(), writes=[pk])
    P.dma("sp", ident[:], ident_d.ap(), writes=[ident])
    P.op("pool", lambda e: e.memset(ones[:], 1.0), writes=[ones])
    P.op("pool", lambda e: e.memset(epsc[:], LN_EPS), writes=[epsc])
    P.op("dve", lambda e: e.tensor_tensor(modp[:], col("modsh", 0, 96), col("modoff", 0, 96), ALU.add), reads=[pk], writes=[modp])
    P.op("dve", lambda e: e.tensor_scalar_add(g2p[:], modp[:, 80:96], 1.0), reads=[modp], writes=[g2p])
    dqc = [0]

    def dq():
        dqc[0] += 1
        return ("sp", "act")[dqc[0] % 2]
    for n in range(4):
        m0 = n * 512
        P.dma(dq(), x1[:], x1_d.ap()[:, m0:m0 + 512].rearrange("(k p) t -> p k t", p=128), writes=[x1])
        P.dma(dq(), u[:], part_d.ap()[0, :, m0:m0 + 512].rearrange("(k p) t -> p k t", p=128), writes=[u])
        for c in range(1, 8):
            pb = pb_()
            P.dma(dq(), pb[:], part_d.ap()[c, :, m0:m0 + 512].rearrange("(k p) t -> p k t", p=128), writes=[pb])
            P.op("dve" if c % 2 else "pool", lambda e: e.tensor_tensor(u[:], u[:], pb[:], ALU.add), reads=[u, pb], writes=[u])
        for k in range(16):
            P.op("dve", lambda e: e.tensor_scalar_mul(u[:, k, :], u[:, k, :], g2p[:, k:k + 1]), reads=[u, g2p], writes=[u])
        P.op("dve", lambda e: e.scalar_tensor_tensor(u[:], x1[:], DN_ALPHA, u[:], ALU.mult, ALU.add), reads=[u, x1], writes=[u])
        mean, rstd = ln_fm(P, u, u, 512, ones, epsc[:, 0:1], wk, pp, 1.0 / D, stp)
        for k in range(16):
            t = wk()
            P.op("dve", lambda e: e.tensor_tensor(t[:], u[:, k, :], mean[:], ALU.subtract), reads=[u, mean], writes=[t])
            P.op("dve", lambda e: e.tensor_tensor(t[:], t[:], rstd[:], ALU.mult), reads=[t, rstd], writes=[t])
            P.op("dve", lambda e: e.tensor_scalar(x1[:, k, :], t[:], col("lng", k), col("lnb", k), ALU.mult, ALU.add), reads=[t, pk, x1], writes=[x1])
        for s in range(4):
            o = ot()
            for g in range(4):
                pt = pp()
                for q in range(4):
                    k = g * 4 + q
                    P.op("pe", lambda e: e.transpose(pt[:, q * 128:(q + 1) * 128], x1[:, k, s * 128:(s + 1) * 128], ident[:]), reads=[x1, ident], writes=[pt])
                if g % 2 == 0:
                    P.op("act", lambda e: e.activation(o[:, g * 512:(g + 1) * 512], pt[:], AF.Copy), reads=[pt], writes=[o])
                else:
                    P.op("dve", lambda e: e.tensor_copy(o[:, g * 512:(g + 1) * 512], pt[:]), reads=[pt], writes=[o])
            P.dma(dq(), o_x.ap()[m0 + s * 128:m0 + (s + 1) * 128, :], o[:], reads=[o], writes=["o_x"])
    return P.finish(["o_x"]), P


def inputs_A(inp, l, c, x, vfirst):
    b, hf = c // 2, c % 2
    xh = np.zeros((TT, D), np.float32)
    if hf == 0:
        xh[HALO:] = x[b, 0:TOK]
    else:
        xh[:] = x[b, TOK - HALO:2 * TOK]
    m = {"xh": xh, "pk": pack_A(inp, l, b, hf),
         "pos": np.ascontiguousarray(inp["positions"][b:b + 1, hf * TOK:(hf + 1) * TOK]).astype(np.int32),
         "ident": np.eye(128, dtype=np.float32), "w_in": inp["w_in"][l],
         "w_uq": inp["mla_w_uq"][l], "w_ukv": inp["mla_w_ukv"][l], "rw_w1": inp["rw_w1"][l], "rw_w2": inp["rw_w2"][l],
         "rw_a1": inp["rw_a1"][l], "rw_a2": inp["rw_a2"][l], "rw_g1": inp["rw_g1"][l], "rw_g2": inp["rw_g2"][l]}
    if l > 0:
        m["rw_v1"] = inp["rw_v1"][l - 1]
        m["rw_v2"] = inp["rw_v2"][l - 1]
        m["vfirstT"] = np.ascontiguousarray(vfirst[b, hf * TOK:(hf + 1) * TOK].T)
    return m


_PROGS = {}


def _prog(name, fn):
    if name not in _PROGS:
        _PROGS[name] = fn()[0]
    return _PROGS[name]


def _run(nc, ims):
    return run_bass_kernel_spmd(nc, ims, core_ids=list(range(8))).results


def run_M(inp):
    ims = []
    cT4 = np.zeros((128, 64), np.float32)
    for b in range(NB):
        cT4[:, b::4] = fm(inp["c"][b])
    for c in range(8):
        ims.append({"cT4": cT4, "mod_w_s": np.ascontiguousarray(inp["mod_w"][:, c * 1536:(c + 1) * 1536]),
                    "mod_b_s": fm(inp["mod_b"][c * 1536:(c + 1) * 1536])})
    r = _run(_prog("M", build_M), ims)
    modsh = np.zeros((NB, 128, 96), np.float32)
    for c in range(8):
        mt = r[c]["modT"].reshape(128, 12, 4)
        for b in range(NB):
            modsh[b][:, c * 12:(c + 1) * 12] = mt[:, :, b]
    return modsh


def inputs_C(inp, l, c, x, A, Bres):
    b, hf = c // 2, c % 2
    ts = slice(hf * TOK, (hf + 1) * TOK)
    pair = lambda n: np.concatenate([Bres[2 * b][n], Bres[2 * b + 1][n]], 0)[:, ts]
    ycat = np.concatenate([A[c]["yconvT"], pair("y_mlaT"), pair("y_rwT"), pair("y_caT")], 0)
    return {"ycatT": np.ascontiguousarray(ycat), "x": np.ascontiguousarray(x[b, ts]), "w_out": inp["w_out"][l],
            "pk": pack_C(inp, l, 0, inp["modsh"][b]), "router_w": inp["moe_router_w"][l], "ident": np.eye(128, dtype=np.float32)}


def inputs_D(inp, l, c, Cres):
    h2 = np.concatenate([Cres[i]["h2T"] for i in range(8)], 1)
    g = np.concatenate([Cres[i]["gateT"] for i in range(8)], 1)
    b1 = inp["moe_b1"][l][4 * c:4 * c + 4]
    b1T = np.concatenate([fm(b1[e]) for e in range(4)], 1)
    return {"h2T_all": h2, "gate4": np.ascontiguousarray(g[4 * c:4 * c + 4]),
            "w1": inp["moe_w1"][l][4 * c:4 * c + 4], "w2": inp["moe_w2"][l][4 * c:4 * c + 4],
            "b1T": b1T, "b2": np.ascontiguousarray(inp["moe_b2"][l][4 * c:4 * c + 4])}


def inputs_E(inp, l, c, Cres, Dres):
    b = c // 2
    parts = np.stack([Dres[i]["partT"][:, c * TOK:(c + 1) * TOK] for i in range(8)], 0)
    return {"parts": parts, "x1T": Cres[c]["x1T"], "pk": pack_C(inp, l, 1, inp["modsh"][b]), "ident": np.eye(128, dtype=np.float32)}


def kernel(**inputs):
    inp = {k: np.asarray(v) for k, v in inputs.items()}
    inp["modsh"] = run_M(inp)
    x = inp["x"].astype(np.float32)
    vfirst = None
    for l in range(DEPTH):
        A = _run(_prog("A%d" % l, lambda: build_A(l)), [inputs_A(inp, l, c, x, vfirst) for c in range(8)])
        if l == 0:
            vfirst = np.zeros((NB, SEQ, 512), np.float32)
            for c in range(8):
                vfirst[c // 2, (c % 2) * TOK:(c % 2 + 1) * TOK] = A[c]["rw_vfirstT"].T
        Bres = _run(_prog("B", lambda: build_B(l)), [inputs_B(inp, l, c, A) for c in range(8)])
        Cres = _run(_prog("C", build_C), [inputs_C(inp, l, c, x, A, Bres) for c in range(8)])
        del A, Bres
        Dres = _run(_prog("D", build_D), [inputs_D(inp, l, c, Cres) for c in range(8)])
        Eres = _run(_prog("E", build_E), [inputs_E(inp, l, c, Cres, Dres) for c in range(8)])
        del Cres, Dres
        x = np.zeros((NB, SEQ, D), np.float32)
        for c in range(8):
            x[c // 2, (c % 2) * TOK:(c % 2 + 1) * TOK] = Eres[c]["xout"]
    return x
```

```python
import numpy as np
from contextlib import ExitStack
import concourse.bass as bass
import concourse.mybir as mybir
from concourse.bass_utils import run_bass_kernel_spmd

F32 = mybir.dt.float32
BF16 = mybir.dt.bfloat16
I32 = mybir.dt.int32
AF = mybir.ActivationFunctionType
ALU = mybir.AluOpType
AX = mybir.AxisListType

ENGS = ("pe", "act", "dve", "pool", "sp")
ND = 6

D = 2048
SEQ = 4096
NB = 4
TOK = 2048
HALO = 128
TT = TOK + HALO
DIN = 4800
LN_EPS = 1e-5
DEPTH = 2
DN_ALPHA = (2 * DEPTH) ** 0.25


class _Rec:
    def __init__(self):
        self.calls = []

    def __getattr__(self, name):
        def f(*a, **kw):
            self.calls.append((name, a, kw))
        return f


class Prog:
    def __init__(self):
        self.nc = bass.Bass("TRN2", target_bir_lowering=False)
        self.es = ExitStack()
        self.ops = {e: [] for e in ENGS}
        self.cnt = {e: 0 for e in ENGS}
        self.dcnt = {e: 0 for e in ENGS}
        self.waited = {e: {} for e in ENGS}
        self.lastw = {}
        self.readers = {}
        self.sems = {}
        self.n_t = 0
        self.rr = {}
        self.same_engine_sync = True

    def sem(self, key):
        if key not in self.sems:
            nm = "s_" + "_".join(str(x) for x in (key if isinstance(key, tuple) else (key,)))
            self.sems[key] = self.es.enter_context(self.nc.semaphore(nm))
        return self.sems[key]

    def sb(self, shape, dt=F32, name=None):
        self.n_t += 1
        return self.es.enter_context(self.nc.sbuf_tensor("sb_" + (name or "t%d" % self.n_t), list(shape), dt))

    def ps(self, shape, dt=F32, name=None):
        self.n_t += 1
        return self.es.enter_context(self.nc.psum_tensor("ps_" + (name or "p%d" % self.n_t), list(shape), dt))

    def din(self, name, shape, dt=F32):
        return self.nc.dram_tensor(name, list(shape), dt, kind="ExternalInput")

    def dout(self, name, shape, dt=F32):
        return self.nc.dram_tensor(name, list(shape), dt, kind="ExternalOutput")

    def pool(self, name, n, shape, dt=F32, psum=False):
        bufs = [(self.ps if psum else self.sb)(shape, dt, name="%s%d" % (name, i)) for i in range(n)]
        self.rr[name] = 0

        def nxt():
            b = bufs[self.rr[name] % n]
            self.rr[name] += 1
            return b
        return nxt

    @staticmethod
    def _k(k):
        if isinstance(k, (str, int)):
            return k
        if isinstance(k, tuple):
            return tuple(Prog._k(x) for x in k)
        return "T:" + k.name

    def _tok_wait(self, tok):
        if tok[0] == "c":
            return (("c", tok[1]), tok[2] + 1)
        q, d = tok[1], tok[2]
        return (("d", q, d % ND), 16 * (d // ND + 1))

    def _deps(self, eng, reads, writes):
        toks = set()
        for k in list(reads) + list(writes):
            if k in self.lastw:
                toks.add(self.lastw[k])
        for k in writes:
            for t in self.readers.get(k, ()):
                toks.add(t)
        waits = []
        for t in toks:
            if t[0] == "c" and t[1] == eng and (eng == "pe" or not self.same_engine_sync):
                continue
            sk, v = self._tok_wait(t)
            if self.waited[eng].get(sk, 0) >= v:
                continue
            self.waited[eng][sk] = v
            waits.append((sk, v))
        return waits

    def _commit(self, tok, reads, writes):
        for k in writes:
            self.lastw[k] = tok
            self.readers[k] = []
        for k in reads:
            self.readers.setdefault(k, []).append(tok)

    def op(self, eng, fn, reads=(), writes=()):
        rec = _Rec()
        fn(rec)
        (mname, margs, mkw), = rec.calls
        fn = lambda e, mname=mname, margs=margs, mkw=mkw: getattr(e, mname)(*margs, **mkw)
        reads = [self._k(k) for k in reads]
        writes = [self._k(k) for k in writes]
        waits = self._deps(eng, reads, writes)
        idx = self.cnt[eng]
        self.cnt[eng] += 1
        self.ops[eng].append((waits, fn, (("c", eng), 1)))
        self._commit(("c", eng, idx), reads, writes)

    def dma(self, q, out, in_, reads=(), writes=(), **kw):
        reads = [self._k(k) for k in reads]
        writes = [self._k(k) for k in writes]
        d = self.dcnt[q]
        waits = self._deps(q, reads, writes)
        if d >= ND:
            sk, v = self._tok_wait(("d", q, d - ND))
            if self.waited[q].get(sk, 0) < v:
                self.waited[q][sk] = v
                waits.append((sk, v))
        self.dcnt[q] += 1
        self.ops[q].append((waits, lambda e: e.dma_start(out=out, in_=in_, **kw), (("d", q, d % ND), 16)))
        self._commit(("d", q, d), reads, writes)

    def barrier(self):
        allw = []
        for e2 in ENGS:
            if self.cnt[e2] > 0:
                allw.append((("c", e2), self.cnt[e2]))
            for d in range(max(0, self.dcnt[e2] - ND), self.dcnt[e2]):
                allw.append(self._tok_wait(("d", e2, d)))
        for e in ENGS:
            waits = []
            for sk, v in allw:
                if self.waited[e].get(sk, 0) < v:
                    self.waited[e][sk] = v
                    waits.append((sk, v))
            if waits:
                self.ops[e].append((waits, None, None))

    def scope(self):
        prog = self

        class _S:
            def __enter__(s2):
                s2.outer = prog.es
                prog.es = ExitStack()
                return s2

            def __exit__(s2, *a):
                prog.barrier()
                prog.es.close()
                prog.es = s2.outer
                return False
        return _S()

    def finish(self, final_keys):
        final_keys = [self._k(k) for k in final_keys]
        waits = self._deps("sp", final_keys, ())
        self.ops["sp"].append((waits, None, None))
        nc = self.nc
        for e in ENGS:
            for waits, fn, inc in self.ops[e]:
                for sk, v in waits:
                    self.sem(sk)
                if inc is not None:
                    self.sem(inc[0])
        with nc.Block() as block:
            def emit(eng_name):
                def body(e):
                    for waits, fn, inc in self.ops[eng_name]:
                        for sk, v in waits:
                            e.wait_ge(self.sem(sk), v)
                        if fn is not None:
                            fn(e).then_inc(self.sem(inc[0]), inc[1])
                return body
            block.tensor(emit("pe"))
            block.scalar(emit("act"))
            block.vector(emit("dve"))
            block.gpsimd(emit("pool"))
            block.sync(emit("sp"))
        self.es.close()
        return nc


def fm(v):
    v = np.asarray(v, np.float32).reshape(-1, 128)
    return np.ascontiguousarray(v.T)


PKA = {}
_o = 0
for _n, _w in (("modb", 96), ("modoff", 96), ("cT", 16), ("dw", 124), ("convb", 4), ("clng", 4),
               ("clnb", 4), ("qn", 3), ("kvn", 2), ("murkv", 12), ("mux", 48), ("w0", 4), ("a0", 4),
               ("kk", 4), ("ka", 4), ("rk", 4), ("v0", 4), ("vmu", 16), ("invf", 1), ("flag", 1)):
    PKA[_n] = (_o, _w)
    _o += _w
NPKA = _o


def pack_A(inp, l, b, hf):
    pk = np.zeros((128, NPKA), np.float32)

    def put(n, a):
        o, w = PKA[n]
        a = np.asarray(a, np.float32).reshape(128, w)
        pk[:, o:o + w] = a
    put("modb", inp["modsh"][b])
    put("modoff", fm(inp["mod_offset"][l]))
    put("dw", np.transpose(inp["conv_dw"][l].T.reshape(4, 128, 31), (1, 0, 2)))
    put("convb", fm(inp["conv_b"][l]))
    put("clng", fm(inp["conv_ln_g"][l]))
    put("clnb", fm(inp["conv_ln_b"][l]))
    put("qn", fm(inp["mla_q_norm"][l]))
    put("kvn", fm(inp["mla_kv_norm"][l]))
    put("murkv", np.transpose(inp["rw_mu_rkv"][l].reshape(3, 4, 128), (2, 0, 1)))
    put("mux", np.transpose(inp["rw_mu_x"][l].reshape(3, 16, 128), (2, 0, 1)))
    put("w0", fm(inp["rw_w0"][l]))
    put("a0", fm(inp["rw_a0"][l]))
    put("kk", fm(inp["rw_k_k"][l]))
    put("ka", fm(inp["rw_k_a"][l]))
    put("rk", fm(inp["rw_r_k"][l].reshape(-1)))
    if l > 0:
        put("v0", fm(inp["rw_v0"][l - 1]))
        put("vmu", fm(inp["rw_vres_mu"][l - 1]))
    inv = (10000.0 ** (-np.arange(0, 64, 2, dtype=np.float32) / 64)).astype(np.float32)
    iv = np.zeros((128, 1), np.float32)
    iv[0:32, 0] = inv
    iv[32:64, 0] = inv
    put("invf", iv)
    put("flag", np.full((128, 1), 0.0 if hf == 0 else 1.0, np.float32))
    return pk


TWO_PI = float(2 * np.pi)
CW1 = 6.28125
CW2 = float(np.float32(2 * np.pi - 6.28125).view(np.int32) & ~0xFFF) if False else 0.0019350052
_c2 = np.float32(2 * np.pi - CW1)
_c2i = np.frombuffer(np.float32(_c2).tobytes(), np.uint32)[0] & np.uint32(0xFFFFF000)
CW2 = float(np.frombuffer(np.uint32(_c2i).tobytes(), np.float32)[0])
CW3 = float(2 * np.pi - CW1 - CW2)
MAGIC = 12582912.0


def build_A(l, dbg=False):
    P = Prog()
    xh = P.din("xh", [TT, D])
    pk_d = P.din("pk", [128, NPKA])
    pos_d = P.din("pos", [1, TOK], I32)
    ident_d = P.din("ident", [128, 128])
    win_d = P.din("w_in", [D, DIN])
    wuq_d = P.din("w_uq", [384, 768])
    wukv_d = P.din("w_ukv", [256, 1024])
    w1_d = P.din("rw_w1", [D, 96]); w2_d = P.din("rw_w2", [96, 512])
    a1_d = P.din("rw_a1", [D, 96]); a2_d = P.din("rw_a2", [96, 512])
    g1_d = P.din("rw_g1", [D, 256]); g2_d = P.din("rw_g2", [256, 512])
    if l > 0:
        v1_d = P.din("rw_v1", [D, 64]); v2_d = P.din("rw_v2", [64, 512])
        vf_d = P.din("vfirstT", [512, TOK])
    o_modp = P.dout("modp", [128, 96])
    o_yconv = P.dout("yconvT", [512, TOK], BF16)
    o_mq = P.dout("mla_qT", [4, 192, TOK], BF16)
    o_mk = P.dout("mla_kT", [4, 128, TOK], BF16)
    o_mkpe = P.dout("mla_kpeT", [64, TOK], BF16)
    o_mv = P.dout("mla_v", [TOK, 512], BF16)
    o_cq = P.dout("ca_qT", [512, TOK], BF16)
    o_ck = P.dout("ca_kT", [512, TOK], BF16)
    o_cv = P.dout("ca_v", [TOK, 512], BF16)
    o_rtm = {n: P.dout("rw_%s_tm" % n, [TOK, 512]) for n in ("r", "w", "k", "kk", "kka")}
    rfm_names = ("v", "bonus", "g") + (("vfirst",) if l == 0 else ())
    o_rfm = {n: P.dout("rw_%sT" % n, [512, TOK]) for n in rfm_names}
    finals = ["o_modp", "o_yconv", "o_mq", "o_mk", "o_mkpe", "o_mv", "o_cq", "o_ck", "o_cv"] + \
        ["o_rtm_" + n for n in o_rtm] + ["o_rfm_" + n for n in o_rfm]

    pk = P.sb([128, NPKA], name="pk")
    ident = P.sb([128, 128], name="ident")
    ones = P.sb([128, 128], name="ones")
    bones = P.sb([128, 128], name="bones")
    epsc = P.sb([128, 4], name="epsc")
    hT = P.sb([128, 16, TT], BF16, name="hT")
    modp = P.sb([128, 96], name="modp")
    sc1p = P.sb([128, 16], name="sc1p")
    cosT = P.sb([64, TOK], name="cosT")
    sinT = P.sb([64, TOK], name="sinT")
    wkb = P.pool("wkb", 6, [128, 512], BF16)
    wb = P.pool("wb", 4, [128, 16, 128], BF16)
    pp = P.pool("pp", 7, [128, 512], psum=True)

    def col(n, j=0, w=1):
        o, _ = PKA[n]
        return pk[:, o + j:o + j + w]

    P.dma("sp", pk[:], pk_d.ap(), writes=[pk])
    P.dma("sp", ident[:], ident_d.ap(), writes=[ident])
    P.op("pool", lambda e: e.memset(epsc[:, 0:1], LN_EPS), writes=[epsc])
    P.op("pool", lambda e: e.memset(epsc[:, 1:2], 1e-6), reads=[epsc], writes=[epsc])
    P.op("pool", lambda e: e.memset(epsc[:, 2:3], 64e-5), reads=[epsc], writes=[epsc])
    P.op("pool", lambda e: e.memset(ones[:], 1.0), writes=[ones])
    P.op("pool", lambda e: e.memset(bones[:], 0.0), writes=[bones])
    P.op("pool", lambda e: e.memset(bones[0:64, 0:64], 1.0), reads=[bones], writes=[bones])
    P.op("pool", lambda e: e.memset(bones[64:128, 64:128], 1.0), reads=[bones], writes=[bones])

    NTI = TT // 128
    hkeys = [("hT", i) for i in range(NTI)]
    with P.scope():
        big = P.sb([128, 8192], name="big")
        junk = P.sb([128, D], BF16, name="junk")
        st = P.pool("st", 8, [128, 4])
        P.op("dve", lambda e: e.tensor_tensor(modp[:], col("modb", 0, 96), col("modoff", 0, 96), ALU.add),
             reads=[pk], writes=[modp])
        P.op("dve", lambda e: e.tensor_scalar_add(sc1p[:], modp[:, 16:32], 1.0), reads=[modp], writes=[sc1p])
        P.dma("sp", o_modp.ap(), modp[:], reads=[modp], writes=["o_modp"])
        P.barrier()
        posi = big[0:64, 4096:6144].bitcast(I32)
        ang = big[0:64, 0:2048]
        rn = big[0:64, 2048:4096]
        P.dma("sp", posi, pos_d.ap().partition_broadcast(64), writes=["posi"])
        P.op("dve", lambda e: e.tensor_copy(ang, posi), reads=["posi"], writes=["ang"])
        P.op("dve", lambda e: e.tensor_scalar_mul(ang, ang, col("invf")[0:64, :]), reads=["ang", pk], writes=["ang"])
        P.op("dve", lambda e: e.tensor_scalar(rn, ang, 1.0 / TWO_PI, MAGIC, ALU.mult, ALU.add), reads=["ang"], writes=["rn"])
        P.op("dve", lambda e: e.tensor_scalar_add(rn, rn, -MAGIC), reads=["rn"], writes=["rn"])
        for cw in (CW1, CW2, CW3):
            P.op("dve", lambda e, cw=cw: e.scalar_tensor_tensor(ang, rn, -cw, ang, ALU.mult, ALU.add),
                 reads=["rn", "ang"], writes=["ang"])
        P.op("dve", lambda e: e.tensor_scalar(sinT[:], ang, float(np.pi), -float(np.pi), ALU.min, ALU.max),
             reads=["ang"], writes=[sinT])
        P.op("dve", lambda e: e.tensor_scalar_add(cosT[:], sinT[:], float(np.pi / 2)), reads=[sinT], writes=[cosT])
        P.op("dve", lambda e: e.tensor_scalar(rn, cosT[:], float(np.pi), -TWO_PI, ALU.is_gt, ALU.mult), reads=[cosT], writes=["rn"])
        P.op("dve", lambda e: e.tensor_tensor(cosT[:], cosT[:], rn, ALU.add), reads=[cosT, "rn"], writes=[cosT])
        P.op("dve", lambda e: e.tensor_scalar(cosT[:], cosT[:], float(np.pi), -float(np.pi), ALU.min, ALU.max), reads=[cosT], writes=[cosT])
        P.op("act", lambda e: e.activation(sinT[:], sinT[:], AF.Sin), reads=[sinT], writes=[sinT])
        P.op("act", lambda e: e.activation(cosT[:], cosT[:], AF.Sin), reads=[cosT], writes=[cosT])
        P.barrier()
        for i in range(NTI):
            xv = big[:, (i % 2) * 2048:(i % 2 + 1) * 2048]
            xk = ("xt", i % 2)
            P.dma("sp" if i % 2 == 0 else "act", xv, xh.ap()[i * 128:(i + 1) * 128, :], writes=[xk])
            s = st()
            P.op("act", lambda e, xv=xv, s=s: e.activation(junk[:], xv, AF.Identity, accum_out=s[:, 0:1]),
                 reads=[xk], writes=[junk, (s, 0)])
            P.op("act", lambda e, xv=xv, s=s: e.activation(junk[:], xv, AF.Square, accum_out=s[:, 1:2]),
                 reads=[xk], writes=[junk, (s, 1)])
            P.op("dve", lambda e, s=s: e.tensor_scalar_mul(s[:, 0:2], s[:, 0:2], 1.0 / D), reads=[(s, 0), (s, 1)], writes=[(s, 0), (s, 1)])
            P.op("dve", lambda e, s=s: e.tensor_tensor(s[:, 2:3], s[:, 0:1], s[:, 0:1], ALU.mult), reads=[(s, 0)], writes=[(s, 2)])
            P.op("dve", lambda e, s=s: e.tensor_tensor(s[:, 1:2], s[:, 1:2], s[:, 2:3], ALU.subtract), reads=[(s, 1), (s, 2)], writes=[(s, 1)])
            P.op("act", lambda e, s=s: e.activation(s[:, 1:2], s[:, 1:2], AF.Sqrt, bias=epsc[:, 0:1]), reads=[(s, 1), epsc], writes=[(s, 1)])
            P.op("dve", lambda e, s=s: e.reciprocal(s[:, 1:2], s[:, 1:2]), reads=[(s, 1)], writes=[(s, 1)])
            P.op("dve", lambda e, xv=xv, s=s: e.tensor_scalar(xv, xv, s[:, 0:1], s[:, 1:2], ALU.subtract, ALU.mult),
                 reads=[xk, (s, 0), (s, 1)], writes=[xk])
            for g in range(4):
                pt = pp()
                for q in range(4):
                    k = g * 4 + q
                    P.op("pe", lambda e, pt=pt, q=q, k=k, xv=xv: e.transpose(pt[:, q * 128:(q + 1) * 128], xv[:, k * 128:(k + 1) * 128], ident[:]),
                         reads=[xk, ident], writes=[pt])
                for q in range(4):
                    k = g * 4 + q
                    P.op("act", lambda e, pt=pt, q=q, k=k, i=i: e.activation(
                        hT[:, k, i * 128:(i + 1) * 128], pt[:, q * 128:(q + 1) * 128], AF.Identity,
                        bias=modp[:, k:k + 1], scale=sc1p[:, k:k + 1]),
                        reads=[pt, modp, sc1p], writes=[("hT", i)])
        P.op("dve", lambda e: e.tensor_scalar_mul(hT[:, :, 0:128], hT[:, :, 0:128], col("flag")), reads=[("hT", 0), pk], writes=[("hT", 0)])

    cnt = {"q": 0}

    def dq():
        cnt["q"] += 1
        return ("sp", "act")[cnt["q"] % 2]

    def load_wblock(src_ap, ncols=128, rows=16):
        w = wb()
        P.dma("pool", w[:, 0:rows, 0:ncols], src_ap.rearrange("(k p) n -> p k n", p=128), writes=[w])
        return w

    def proj(w, ncols, t0, n, shift=0):
        ps = pp()
        for k in range(16):
            P.op("pe", lambda e, ps=ps, w=w, k=k: e.matmul(ps[0:ncols, 0:n], w[:, k, 0:ncols], hT[:, k, t0 - shift:t0 - shift + n],
                                                       start=(k == 0), stop=(k == 15)),
                 reads=[w] + hkeys, writes=[ps])
        return ps

    def evac(dst, src, i, reads, writes):
        if i % 2 == 0:
            P.op("act", lambda e: e.activation(dst, src, AF.Copy), reads=reads, writes=writes)
        else:
            P.op("dve", lambda e: e.tensor_copy(dst, src), reads=reads, writes=writes)

    TILES = [(HALO + n * 512, 512) for n in range(4)]

    with P.scope():
        for name, c0, o_d in (("cq", 3264, o_cq), ("ck", 3776, o_ck)):
            for j in range(4):
                w = load_wblock(win_d.ap()[:, c0 + j * 128:c0 + (j + 1) * 128])
                for n, (t0, nn) in enumerate(TILES):
                    ps = proj(w, 128, t0, nn)
                    ob = wkb()
                    evac(ob[:], ps[:], n, [ps], [ob])
                    P.dma(dq(), o_d.ap()[j * 128:(j + 1) * 128, t0 - HALO:t0 - HALO + nn], ob[:], reads=[ob], writes=["o_" + name])
        wv = P.sb([128, 16, 512], BF16, name="wv")
        P.dma("pool", wv[:], win_d.ap()[:, 4288:4800].rearrange("(k p) n -> p k n", p=128), writes=[wv])
        for i in range(16):
            ps = pp()
            for k in range(16):
                P.op("pe", lambda e, ps=ps, k=k, i=i: e.matmul(ps[:], hT[:, k, HALO + i * 128:HALO + (i + 1) * 128], wv[:, k, :],
                                                            start=(k == 0), stop=(k == 15)),
                     reads=[wv] + hkeys, writes=[ps])
            ob = wkb()
            evac(ob[:], ps[:], i, [ps], [ob])
            P.dma(dq(), o_cv.ap()[i * 128:(i + 1) * 128, :], ob[:], reads=[ob], writes=["o_cv"])

    with P.scope():
        cqs = P.sb([128, 5, TOK], name="cqs")
        wuq = P.sb([128, 3, 768], BF16, name="wuq")
        wuqr = P.sb([128, 3, 4, 64], BF16, name="wuqr")
        wukv = P.sb([128, 2, 1024], BF16, name="wukv")
        wukvv = P.sb([128, 2, 512], BF16, name="wukvv")
        wk = P.pool("wkm", 10, [128, 512])
        cqn_p = P.pool("cqn", 2, [128, 3, 512], BF16)
        ckvn_p = P.pool("ckvn", 2, [128, 2, 512], BF16)
        P.dma("pool", wuq[:], wuq_d.ap().rearrange("(k p) n -> p k n", p=128), writes=[wuq])
        P.dma("pool", wukv[:], wukv_d.ap().rearrange("(k p) n -> p k n", p=128), writes=[wukv])
        for k in range(3):
            for h in range(4):
                P.op("dve", lambda e, k=k, h=h: e.tensor_scalar_mul(wuqr[:, k, h, 0:32], wuq[:, k, h * 192 + 160:h * 192 + 192], -1.0),
                     reads=[wuq], writes=[wuqr])
                P.op("dve", lambda e, k=k, h=h: e.tensor_copy(wuqr[:, k, h, 32:64], wuq[:, k, h * 192 + 128:h * 192 + 160]),
                     reads=[wuq], writes=[wuqr])
        for k in range(2):
            for h in range(4):
                P.op("dve", lambda e, k=k, h=h: e.tensor_copy(wukvv[:, k, h * 128:(h + 1) * 128], wukv[:, k, h * 256 + 128:h * 256 + 256]),
                     reads=[wukv], writes=[wukvv])
        for j in range(5):
            w = load_wblock(win_d.ap()[:, 1024 + j * 128:1024 + (j + 1) * 128])
            for n, (t0, nn) in enumerate(TILES):
                ps = proj(w, 128, t0, nn)
                evac(cqs[:, j, t0 - HALO:t0 - HALO + nn], ps[:], n, [ps], [("cqs", j, n)])
        wkp = load_wblock(win_d.ap()[:, 1664:1728], ncols=64)
        wkr = wb()
        P.op("dve", lambda e: e.tensor_scalar_mul(wkr[:, :, 0:32], wkp[:, :, 32:64], -1.0), reads=[wkp], writes=[wkr])
        P.op("dve", lambda e: e.tensor_copy(wkr[:, :, 32:64], wkp[:, :, 0:32]), reads=[wkp, wkr], writes=[wkr])

        def rope_out(ps1, ps2, m0, dst_ap, okey):
            t1 = wk(); t2 = wk(); ob = wkb()
            P.op("dve", lambda e: e.tensor_tensor(t1[0:64, :], ps1[0:64, :], cosT[:, m0:m0 + 512], ALU.mult), reads=[ps1, cosT], writes=[t1])
            P.op("dve", lambda e: e.tensor_tensor(t2[0:64, :], ps2[0:64, :], sinT[:, m0:m0 + 512], ALU.mult), reads=[ps2, sinT], writes=[t2])
            P.op("dve", lambda e: e.tensor_tensor(ob[0:64, :], t1[0:64, :], t2[0:64, :], ALU.add), reads=[t1, t2], writes=[ob])
            P.dma(dq(), dst_ap, ob[0:64, :], reads=[ob], writes=[okey])

        for n, (t0, nn) in enumerate(TILES):
            m0 = t0 - HALO
            ps1 = proj(wkp, 64, t0, nn)
            ps2 = proj(wkr, 64, t0, nn)
            rope_out(ps1, ps2, m0, o_mkpe.ap()[:, m0:m0 + 512], "o_mkpe")
        for n, (t0, nn) in enumerate(TILES):
            m0 = t0 - HALO
            normed = []
            for (j0, nj, pkn, inv_d, pool_) in ((0, 3, "qn", 1.0 / 384, cqn_p), (3, 2, "kvn", 1.0 / 256, ckvn_p)):
                pss = pp()
                for j in range(nj):
                    sq = wk()
                    P.op("act", lambda e, sq=sq, j=j: e.activation(sq[:], cqs[:, j0 + j, m0:m0 + 512], AF.Square),
                         reads=[("cqs", j0 + j, n)], writes=[sq])
                    P.op("pe", lambda e, sq=sq, j=j, pss=pss: e.matmul(pss[:], ones[:], sq[:], start=(j == 0), stop=(j == nj - 1)),
                         reads=[sq, ones], writes=[pss])
                rq = wk()
                P.op("act", lambda e, rq=rq, pss=pss: e.activation(rq[:], pss[:], AF.Sqrt, bias=epsc[:, 1:2], scale=inv_d),
                     reads=[pss, epsc], writes=[rq])
                P.op("dve", lambda e, rq=rq: e.reciprocal(rq[:], rq[:]), reads=[rq], writes=[rq])
                nb_ = pool_()
                for j in range(nj):
                    P.op("dve", lambda e, j=j, nb_=nb_, rq=rq: e.scalar_tensor_tensor(
                        nb_[:, j, :], cqs[:, j0 + j, m0:m0 + 512], col(pkn, j), rq[:], ALU.mult, ALU.mult),
                        reads=[("cqs", j0 + j, n), pk, rq], writes=[nb_])
                normed.append(nb_)
            cqn, ckvn = normed
            for h in range(4):
                ps = pp()
                for k in range(3):
                    P.op("pe", lambda e, ps=ps, k=k, h=h: e.matmul(ps[:], wuq[:, k, h * 192:h * 192 + 128], cqn[:, k, :], start=(k == 0), stop=(k == 2)),
                         reads=[wuq, cqn], writes=[ps])
                ob = wkb()
                evac(ob[:], ps[:], h, [ps], [ob])
                P.dma(dq(), o_mq.ap()[h, 0:128, m0:m0 + 512], ob[:], reads=[ob], writes=["o_mq"])
                ps1 = pp(); ps2 = pp()
                for k in range(3):
                    P.op("pe", lambda e, ps1=ps1, k=k, h=h: e.matmul(ps1[0:64, :], wuq[:, k, h * 192 + 128:h * 192 + 192], cqn[:, k, :], start=(k == 0), stop=(k == 2)),
                         reads=[wuq, cqn], writes=[ps1])
                for k in range(3):
                    P.op("pe", lambda e, ps2=ps2, k=k, h=h: e.matmul(ps2[0:64, :], wuqr[:, k, h, :], cqn[:, k, :], start=(k == 0), stop=(k == 2)),
                         reads=[wuqr, cqn], writes=[ps2])
                rope_out(ps1, ps2, m0, o_mq.ap()[h, 128:192, m0:m0 + 512], "o_mq")
                ps = pp()
                for k in range(2):
                    P.op("pe", lambda e, ps=ps, k=k, h=h: e.matmul(ps[:], wukv[:, k, h * 256:h * 256 + 128], ckvn[:, k, :], start=(k == 0), stop=(k == 1)),
                         reads=[wukv, ckvn], writes=[ps])
                ob = wkb()
                evac(ob[:], ps[:], h + 1, [ps], [ob])
                P.dma(dq(), o_mk.ap()[h, :, m0:m0 + 512], ob[:], reads=[ob], writes=["o_mk"])
            for s in range(4):
                ps = pp()
                for k in range(2):
                    P.op("pe", lambda e, ps=ps, k=k, s=s: e.matmul(ps[:], ckvn[:, k, s * 128:(s + 1) * 128], wukvv[:, k, :], start=(k == 0), stop=(k == 1)),
                         reads=[wukvv, ckvn], writes=[ps])
                ob = wkb()
                evac(ob[:], ps[:], s, [ps], [ob])
                P.dma(dq(), o_mv.ap()[m0 + s * 128:m0 + (s + 1) * 128, :], ob[:], reads=[ob], writes=["o_mv"])

    with P.scope():
        u = P.sb([128, TT], name="u")
        uc = P.sb([128, 4, TOK], name="uc")
        wk = P.pool("wkc", 10, [128, 512])
        CT = [(0, 512), (512, 512), (1024, 512), (1536, 512), (2048, 128)]
        for j in range(4):
            wva = load_wblock(win_d.ap()[:, j * 128:(j + 1) * 128])
            wga = load_wblock(win_d.ap()[:, 512 + j * 128:512 + (j + 1) * 128])
            for n, (t0, nn) in enumerate(CT):
                psv = proj(wva, 128, t0, nn)
                psg = proj(wga, 128, t0, nn)
                sg = wk()
                P.op("act", lambda e, sg=sg, psg=psg, nn=nn: e.activation(sg[:, 0:nn], psg[:, 0:nn], AF.Sigmoid), reads=[psg], writes=[sg])
                P.op("dve", lambda e, sg=sg, psv=psv, t0=t0, nn=nn: e.tensor_tensor(u[:, t0:t0 + nn], psv[:, 0:nn], sg[:, 0:nn], ALU.mult),
                     reads=[psv, sg], writes=[("u", n)])
            ukeys = [("u", n) for n in range(5)]
            o_dw = PKA["dw"][0]
            P.op("dve", lambda e, j=j: e.tensor_scalar(uc[:, j, :], u[:, 98:98 + TOK], pk[:, o_dw + j * 31:o_dw + j * 31 + 1], col("convb", j), ALU.mult, ALU.add),
                 reads=ukeys + [pk], writes=[("uc", j)])
            for i in range(1, 31):
                P.op("dve", lambda e, j=j, i=i: e.scalar_tensor_tensor(uc[:, j, :], u[:, 98 + i:98 + i + TOK], pk[:, o_dw + j * 31 + i:o_dw + j * 31 + i + 1], uc[:, j, :], ALU.mult, ALU.add),
                     reads=ukeys + [pk, ("uc", j)], writes=[("uc", j)])
        uckeys = [("uc", j) for j in range(4)]
        for n in range(4):
            m0 = n * 512
            pssum = pp(); pssq = pp()
            for j in range(4):
                sq = wk()
                P.op("act", lambda e, sq=sq, j=j: e.activation(sq[:], uc[:, j, m0:m0 + 512], AF.Square), reads=[("uc", j)], writes=[sq])
                P.op("pe", lambda e, j=j, pssum=pssum: e.matmul(pssum[:], ones[:], uc[:, j, m0:m0 + 512], start=(j == 0), stop=(j == 3)),
                     reads=[("uc", j), ones], writes=[pssum])
                P.op("pe", lambda e, j=j, pssq=pssq, sq=sq: e.matmul(pssq[:], ones[:], sq[:], start=(j == 0), stop=(j == 3)),
                     reads=[sq, ones], writes=[pssq])
            mean = wk(); msq = wk(); rstd = wk()
            P.op("act", lambda e, mean=mean, pssum=pssum: e.activation(mean[:], pssum[:], AF.Copy, scale=1.0 / 512), reads=[pssum], writes=[mean])
            P.op("dve", lambda e, mean=mean, msq=msq: e.tensor_tensor(msq[:], mean[:], mean[:], ALU.mult), reads=[mean], writes=[msq])
            P.op("dve", lambda e, msq=msq, pssq=pssq, rstd=rstd: e.scalar_tensor_tensor(rstd[:], pssq[:], 1.0 / 512, msq[:], ALU.mult, ALU.subtract),
                 reads=[pssq, msq], writes=[rstd])
            P.op("act", lambda e, rstd=rstd: e.activation(rstd[:], rstd[:], AF.Sqrt, bias=epsc[:, 0:1]), reads=[rstd, epsc], writes=[rstd])
            P.op("dve", lambda e, rstd=rstd: e.reciprocal(rstd[:], rstd[:]), reads=[rstd], writes=[rstd])
            for j in range(4):
                t1 = wk(); ob = wkb()
                P.op("dve", lambda e, t1=t1, j=j, mean=mean: e.tensor_tensor(t1[:], uc[:, j, m0:m0 + 512], mean[:], ALU.subtract), reads=[("uc", j), mean], writes=[t1])
                P.op("dve", lambda e, t1=t1, rstd=rstd: e.tensor_tensor(t1[:], t1[:], rstd[:], ALU.mult), reads=[t1, rstd], writes=[t1])
                P.op("act", lambda e, t1=t1, ob=ob, j=j: e.activation(ob[:], t1[:], AF.Silu, bias=col("clnb", j), scale=col("clng", j)), reads=[t1, pk], writes=[ob])
                P.dma(dq(), o_yconv.ap()[j * 128:(j + 1) * 128, m0:m0 + 512], ob[:], reads=[ob], writes=["o_yconv"])

    with P.scope():
        loras = [("w", w1_d, 96, 0, AF.Tanh), ("a", a1_d, 96, 1, AF.Identity), ("g", g1_d, 256, 2, AF.Sigmoid)]
        if l > 0:
            loras.append(("v", v1_d, 64, None, AF.Identity))
        lo = {}
        for nm, _, r, _, _ in loras:
            lo[nm] = P.sb([128, (r + 127) // 128, TOK], BF16, name="lo_" + nm)
        with P.scope():
            stg = P.sb([128, 16, 256], name="stg")
            stb = P.sb([128, 16, 256], name="stb")
            for nm, wd, r, murow, fn in loras:
                wa = P.sb([128, 16, r], BF16, name="wa_" + nm)
                wbb = P.sb([128, 16, r], BF16, name="wb_" + nm)
                P.dma("sp", stg[:, :, 0:r], wd.ap().rearrange("(k p) n -> p k n", p=128), writes=[stg])
                for k in range(16):
                    mcol = col("mux", murow * 16 + k) if murow is not None else col("vmu", k)
                    P.op("dve", lambda e, k=k, mcol=mcol, r=r: e.tensor_scalar_mul(stb[:, k, 0:r], stg[:, k, 0:r], mcol), reads=[stg, pk], writes=[stb])
                P.op("dve", lambda e, r=r, wbb=wbb: e.tensor_copy(wbb[:], stb[:, :, 0:r]), reads=[stb], writes=[wbb])
                P.op("dve", lambda e, r=r, wa=wa: e.tensor_tensor(wa[:], stg[:, :, 0:r], stb[:, :, 0:r], ALU.subtract), reads=[stg, stb], writes=[wa])
                for n, (t0, nn) in enumerate(TILES):
                    m0 = t0 - HALO
                    for c0 in range(0, r, 128):
                        cw = min(128, r - c0)
                        ps = pp()
                        for k in range(16):
                            P.op("pe", lambda e, ps=ps, k=k, c0=c0, cw=cw, wa=wa: e.matmul(ps[0:cw, :], wa[:, k, c0:c0 + cw], hT[:, k, t0:t0 + 512], start=(k == 0), stop=False),
                                 reads=[wa] + hkeys, writes=[ps])
                        for k in range(16):
                            P.op("pe", lambda e, ps=ps, k=k, c0=c0, cw=cw, wbb=wbb: e.matmul(ps[0:cw, :], wbb[:, k, c0:c0 + cw], hT[:, k, t0 - 1:t0 - 1 + 512], start=False, stop=(k == 15)),
                                 reads=[wbb] + hkeys, writes=[ps])
                        P.op("act", lambda e, ps=ps, c0=c0, cw=cw, nm=nm, fn=fn: e.activation(lo[nm][0:cw, c0 // 128, m0:m0 + 512], ps[0:cw, :], fn),
                             reads=[ps], writes=[("lo", nm, n)])
        w2s = P.sb([96, 512], BF16, name="w2s"); a2s = P.sb([96, 512], BF16, name="a2s")
        g2s = P.sb([128, 2, 512], BF16, name="g2s")
        P.dma("pool", w2s[:], w2_d.ap(), writes=[w2s])
        P.dma("pool", a2s[:], a2_d.ap(), writes=[a2s])
        P.dma("pool", g2s[:], g2_d.ap().rearrange("(k p) n -> p k n", p=128), writes=[g2s])
        if l > 0:
            v2s = P.sb([64, 512], BF16, name="v2s")
            P.dma("pool", v2s[:], v2_d.ap(), writes=[v2s])
        wk = P.pool("wkr", 30, [128, 512])

        def fm_out(name, src, j, m0):
            P.dma(dq(), o_rfm[name].ap()[j * 128:(j + 1) * 128, m0:m0 + 512], src[:], reads=[src], writes=["o_rfm_" + name])

        tcount = [0]

        def tm_out(name, src, j, m0):
            pt = pp()
            for s in range(4):
                P.op("pe", lambda e, pt=pt, s=s: e.transpose(pt[:, s * 128:(s + 1) * 128], src[:, s * 128:(s + 1) * 128], ident[:]),
                     reads=[src, ident], writes=[pt])
            ob = wk()
            tcount[0] += 1
            evac(ob[:], pt[:], tcount[0], [pt], [ob])
            P.dma(dq(), o_rtm[name].ap()[m0:m0 + 512, j * 128:(j + 1) * 128].rearrange("(s p) c -> p s c", p=128),
                  ob[:].rearrange("p (s c) -> p s c", s=4), reads=[ob], writes=["o_rtm_" + name])

        for j in range(4):
            wr_ = load_wblock(win_d.ap()[:, 1728 + j * 128:1728 + (j + 1) * 128])
            wk_ = load_wblock(win_d.ap()[:, 2240 + j * 128:2240 + (j + 1) * 128])
            wv_ = load_wblock(win_d.ap()[:, 2752 + j * 128:2752 + (j + 1) * 128])
            for n, (t0, nn) in enumerate(TILES):
                m0 = t0 - HALO
                mixed = []
                for idx, w_ in enumerate((wr_, wk_, wv_)):
                    ps_a = proj(w_, 128, t0, 512)
                    ps_s = proj(w_, 128, t0, 512, shift=1)
                    base = wk(); dd = wk(); mx = wk()
                    P.op("act", lambda e, base=base, ps_a=ps_a: e.activation(base[:], ps_a[:], AF.Copy), reads=[ps_a], writes=[base])
                    P.op("dve", lambda e, dd=dd, ps_s=ps_s, base=base: e.tensor_tensor(dd[:], ps_s[:], base[:], ALU.subtract), reads=[ps_s, base], writes=[dd])
                    P.op("dve", lambda e, dd=dd, mx=mx, base=base, idx=idx: e.scalar_tensor_tensor(mx[:], dd[:], col("murkv", idx * 4 + j), base[:], ALU.mult, ALU.add),
                         reads=[dd, base, pk], writes=[mx])
                    mixed.append(mx)
                rp, kp, vp = mixed
                ps = pp()
                P.op("pe", lambda e, ps=ps: e.matmul(ps[:], w2s[:, j * 128:(j + 1) * 128], lo["w"][0:96, 0, m0:m0 + 512], start=True, stop=True),
                     reads=[w2s, ("lo", "w", n)], writes=[ps])
                dec = wk()
                P.op("act", lambda e, ps=ps, dec=dec: e.activation(dec[:], ps[:], AF.Sigmoid, bias=col("w0", j)), reads=[ps, pk], writes=[dec])
                P.op("act", lambda e, dec=dec: e.activation(dec[:], dec[:], AF.Exp, scale=-float(np.exp(-0.5))), reads=[dec], writes=[dec])
                ps = pp()
                P.op("pe", lambda e, ps=ps: e.matmul(ps[:], a2s[:, j * 128:(j + 1) * 128], lo["a"][0:96, 0, m0:m0 + 512], start=True, stop=True),
                     reads=[a2s, ("lo", "a", n)], writes=[ps])
                at = wk()
                P.op("act", lambda e, ps=ps, at=at: e.activation(at[:], ps[:], AF.Sigmoid, bias=col("a0", j)), reads=[ps, pk], writes=[at])
                ps = pp()
                for k in range(2):
                    P.op("pe", lambda e, ps=ps, k=k: e.matmul(ps[:], g2s[:, k, j * 128:(j + 1) * 128], lo["g"][:, k, m0:m0 + 512], start=(k == 0), stop=(k == 1)),
                         reads=[g2s, ("lo", "g", n)], writes=[ps])
                gt = wk()
                P.op("dve", lambda e, ps=ps, gt=gt: e.tensor_copy(gt[:], ps[:]), reads=[ps], writes=[gt])
                fm_out("g", gt, j, m0)
                if l > 0:
                    ps = pp()
                    P.op("pe", lambda e, ps=ps: e.matmul(ps[:], v2s[:, j * 128:(j + 1) * 128], lo["v"][0:64, 0, m0:m0 + 512], start=True, stop=True),
                         reads=[v2s, ("lo", "v", n)], writes=[ps])
                    vg = wk(); vf = wk(); vpp = wk()
                    P.op("act", lambda e, ps=ps, vg=vg: e.activation(vg[:], ps[:], AF.Sigmoid, bias=col("v0", j)), reads=[ps, pk], writes=[vg])
                    P.dma(dq(), vf[:], vf_d.ap()[j * 128:(j + 1) * 128, m0:m0 + 512], writes=[vf])
                    P.op("dve", lambda e, vf=vf, vp=vp: e.tensor_tensor(vf[:], vf[:], vp[:], ALU.subtract), reads=[vf, vp], writes=[vf])
                    P.op("dve", lambda e, vf=vf, vg=vg: e.tensor_tensor(vf[:], vf[:], vg[:], ALU.mult), reads=[vf, vg], writes=[vf])
                    P.op("dve", lambda e, vf=vf, vp=vp, vpp=vpp: e.tensor_tensor(vpp[:], vf[:], vp[:], ALU.add), reads=[vf, vp], writes=[vpp])
                else:
                    vpp = vp
                    fm_out("vfirst", vp, j, m0)
                fm_out("v", vpp, j, m0)
                kkr = wk(); sq = wk(); nrm = wk(); kkn = wk()
                P.op("dve", lambda e, kkr=kkr, kp=kp: e.tensor_scalar_mul(kkr[:], kp[:], col("kk", j)), reads=[kp, pk], writes=[kkr])
                P.op("act", lambda e, kkr=kkr, sq=sq: e.activation(sq[:], kkr[:], AF.Square), reads=[kkr], writes=[sq])
                ps = pp()
                P.op("pe", lambda e, ps=ps, sq=sq: e.matmul(ps[:], bones[:], sq[:], start=True, stop=True), reads=[bones, sq], writes=[ps])
                P.op("act", lambda e, ps=ps, nrm=nrm: e.activation(nrm[:], ps[:], AF.Sqrt), reads=[ps], writes=[nrm])
                P.op("dve", lambda e, nrm=nrm: e.tensor_scalar_max(nrm[:], nrm[:], 1e-12), reads=[nrm], writes=[nrm])
                P.op("dve", lambda e, nrm=nrm: e.reciprocal(nrm[:], nrm[:]), reads=[nrm], writes=[nrm])
                P.op("dve", lambda e, nrm=nrm, kkr=kkr, kkn=kkn: e.tensor_tensor(kkn[:], kkr[:], nrm[:], ALU.mult), reads=[kkr, nrm], writes=[kkn])
                tt = wk(); k2 = wk(); kka = wk()
                P.op("dve", lambda e, tt=tt, at=at: e.tensor_scalar(tt[:], at[:], -1.0, col("ka", j), ALU.add, ALU.mult), reads=[at, pk], writes=[tt])
                P.op("dve", lambda e, tt=tt, k2=k2, kp=kp: e.scalar_tensor_tensor(k2[:], tt[:], 1.0, kp[:], ALU.add, ALU.mult), reads=[tt, kp], writes=[k2])
                P.op("dve", lambda e, kka=kka, kkn=kkn, at=at: e.tensor_tensor(kka[:], kkn[:], at[:], ALU.mult), reads=[kkn, at], writes=[kka])
                rk = wk(); bon = wk()
                P.op("dve", lambda e, rk=rk, rp=rp, k2=k2: e.scalar_tensor_tensor(rk[:], rp[:], col("rk", j), k2[:], ALU.mult, ALU.mult), reads=[rp, k2, pk], writes=[rk])
                ps = pp()
                P.op("pe", lambda e, ps=ps, rk=rk: e.matmul(ps[:], bones[:], rk[:], start=True, stop=True), reads=[bones, rk], writes=[ps])
                P.op("dve", lambda e, ps=ps, bon=bon, vpp=vpp: e.tensor_tensor(bon[:], ps[:], vpp[:], ALU.mult), reads=[ps, vpp], writes=[bon])
                fm_out("bonus", bon, j, m0)
                for name, src in (("r", rp), ("w", dec), ("k", k2), ("kk", kkn), ("kka", kka)):
                    tm_out(name, src, j, m0)

    nc = P.finish(finals)
    return nc, P


TC = 8
NEG = -30000.0


def band_bias(rel_table_h):
    q = np.arange(128)
    a, c = q // 64, q % 64
    kk = np.arange(640)
    u, kq = kk // 64, kk % 64
    w = 64 * (u[None, :] - a[:, None]) + kq[None, :]
    valid = np.where(a[:, None] == 0, u[None, :] <= 8, u[None, :] >= 1)
    rel = np.clip((512 + c[:, None]) - w, -256, 256) + 256
    out = rel_table_h[np.clip(rel, 0, 512)].astype(np.float32)
    return np.where(valid, out, np.float32(NEG)).astype(np.float32)


def build_B(l):
    P = Prog()
    mq_d = P.din("mla_qT", [2, 192, SEQ], BF16)
    mk_d = P.din("mla_kT", [2, 128, SEQ], BF16)
    mkpe_d = P.din("mla_kpeT", [64, SEQ], BF16)
    mv_d = P.din("mla_v", [SEQ, 256], BF16)
    cq_d = P.din("ca_qT", [2, 128, SEQ], BF16)
    ck_d = P.din("ca_kT", [2, 128, SEQ], BF16)
    cv_d = P.din("ca_v", [SEQ, 256], BF16)
    bias_d = P.din("ca_bias", [2, 128, 640])
    ident_d = P.din("ident", [128, 128])
    rwbc_d = P.din("rw_bc", [2, SEQ, 640])
    rv_d = P.din("rw_vT", [256, SEQ])
    rb_d = P.din("rw_bonusT", [256, SEQ])
    rg_d = P.din("rw_gT", [256, SEQ])
    gn_d = P.din("rw_gn", [128, 4])
    o_mla = P.dout("y_mlaT", [256, SEQ], BF16)
    o_ca = P.dout("y_caT", [256, SEQ], BF16)
    o_rw = P.dout("y_rwT", [256, SEQ], BF16)

    ident = P.sb([128, 128], name="ident")
    identb = P.sb([128, 128], BF16, name="identb")
    bones = P.sb([128, 128], name="bones")
    epsc = P.sb([128, 2], name="epsc")
    gn = P.sb([128, 4], name="gn")
    bias = P.sb([128, 2, 640], name="bias")
    qn = P.sb([128, SEQ], BF16, name="qn")
    qp = P.sb([64, SEQ], BF16, name="qp")
    kn = P.sb([128, SEQ], BF16, name="kn")
    kp = P.sb([64, SEQ], BF16, name="kp")
    vts = [P.sb([128, 32, 128], BF16, name="vt%d" % i) for i in range(2)]
    scb = [P.sb([128, SEQ], name="sc%d" % i) for i in range(2)]
    pb = [P.sb([128, SEQ], BF16, name="pb%d" % i) for i in range(2)]
    PT = P.pool("PT", 3, [128, 5, 128], BF16)
    outb = P.sb([128, SEQ], BF16, name="outb")
    stt_ = P.pool("stt", 6, [128, 4])
    dg = P.pool("dg", 3, [128, 128], BF16)
    psc = P.pool("psc", 3, [128, 512], psum=True)
    ppt = P.pool("ppt", 2, [128, 512], psum=True)
    pot = P.ps([128, 512], name="pot")
    Ss = [P.sb([128, 128], name="S%d" % i) for i in range(2)]
    tmps = [P.sb([128, 128], name="tmp%d" % i) for i in range(4)]
    nsk = P.pool("nsk", 8, [128, 2])
    bc = [P.sb([128, TC, 2, 5, 64], name="bc%d" % i) for i in range(2)]
    vsb = [P.sb([128, 2, 512], name="vsb%d" % i) for i in range(2)]
    ysb = P.sb([128, 2, SEQ], name="ysb")

    P.dma("act", ident[:], ident_d.ap(), writes=[ident])
    P.dma("act", gn[:], gn_d.ap(), writes=[gn])
    P.dma("act", bias[:], bias_d.ap().rearrange("h q k -> q h k"), writes=[bias])
    P.dma("act", kp[:], mkpe_d.ap(), writes=[kp])
    P.op("act", lambda e: e.activation(identb[:], ident[:], AF.Copy), reads=[ident], writes=[identb])
    P.op("pool", lambda e: e.memset(epsc[:, 0:1], 64e-5), writes=[epsc])
    P.op("pool", lambda e: e.memset(bones[:], 0.0), writes=[bones])
    P.op("pool", lambda e: e.memset(bones[0:64, 0:64], 1.0), reads=[bones], writes=[bones])
    P.op("pool", lambda e: e.memset(bones[64:128, 64:128], 1.0), reads=[bones], writes=[bones])
    P.op("dve", lambda e: e.memset(Ss[0][:], 0.0), writes=[(Ss[0], 0), (Ss[0], 1)])

    dqc = [0]

    def dq():
        dqc[0] += 1
        return ("act", "pool")[dqc[0] % 2]

    units = []
    otc = [0]

    def make_unit(kind, hh, i):
        ub = len(units) % 2
        sc = scb[ub]; pbuf = pb[ub]
        vt = vts[(len(units) // 32) % 2]
        st = {}
        if kind == "mla":
            kt0 = 0
            scale = 192.0 ** -0.5
        else:
            kt0 = max(0, i - 4)
            scale = 128.0 ** -0.5
        nkt = i - kt0 + 1
        nk = nkt * 128

        def stageA():
            if i == 0:
                if kind == "mla":
                    P.dma(dq(), qn[:], mq_d.ap()[hh, 0:128, :], writes=[qn])
                    P.dma(dq(), qp[:], mq_d.ap()[hh, 128:192, :], writes=[qp])
                    P.dma(dq(), kn[:], mk_d.ap()[hh], writes=[kn])
                    P.dma(dq(), vt[:], mv_d.ap()[:, hh * 128:(hh + 1) * 128].rearrange("(n p) d -> p n d", p=128), writes=[vt])
                else:
                    P.dma(dq(), qn[:], cq_d.ap()[hh], writes=[qn])
                    P.dma(dq(), kn[:], ck_d.ap()[hh], writes=[kn])
                    P.dma(dq(), vt[:], cv_d.ap()[:, hh * 128:(hh + 1) * 128].rearrange("(n p) d -> p n d", p=128), writes=[vt])
            qs = slice(i * 128, (i + 1) * 128)
            c0 = 0
            while c0 < nk:
                n_c = min(512, nk - c0)
                ps = psc()
                k0 = kt0 * 128 + c0
                P.op("pe", lambda e: e.matmul(ps[:, 0:n_c], qn[:, qs], kn[:, k0:k0 + n_c], start=True, stop=(kind != "mla")),
                     reads=[qn, kn], writes=[ps])
                if kind == "mla":
                    P.op("pe", lambda e: e.matmul(ps[:, 0:n_c], qp[:, qs], kp[:, k0:k0 + n_c], start=False, stop=True),
                         reads=[qp, kp], writes=[ps])
                    P.op("act", lambda e: e.activation(sc[:, c0:c0 + n_c], ps[:, 0:n_c], AF.Copy), reads=[ps], writes=[sc])
                else:
                    P.op("act", lambda e: e.activation(sc[:, c0:c0 + n_c], ps[:, 0:n_c], AF.Copy, scale=scale), reads=[ps], writes=[sc])
                c0 += n_c
            if kind == "mla":
                P.op("pool", lambda e: e.memset(sc[0:64, nk - 64:nk], NEG / scale), reads=[sc], writes=[sc])
            else:
                p0 = (kt0 - (i - 4)) * 128
                P.op("pool", lambda e: e.tensor_tensor(sc[:, 0:nk], sc[:, 0:nk], bias[:, hh, p0:p0 + nk], ALU.add), reads=[sc, bias], writes=[sc])

        def stageB():
            s4 = stt_()
            st["s4"] = s4
            P.op("dve", lambda e: e.tensor_reduce(s4[:, 0:1], sc[:, 0:nk], AX.X, ALU.max), reads=[sc], writes=[s4])
            esc = scale if kind == "mla" else 1.0
            P.op("dve", lambda e: e.tensor_scalar_mul(s4[:, 1:2], s4[:, 0:1], -esc), reads=[s4], writes=[s4])
            P.op("act", lambda e: e.activation(pbuf[:, 0:nk], sc[:, 0:nk], AF.Exp, bias=s4[:, 1:2], scale=esc, accum_out=s4[:, 2:3]),
                 reads=[sc, s4], writes=[pbuf, s4])

        def stageC():
            s4 = st["s4"]
            P.op("dve", lambda e: e.reciprocal(s4[:, 3:4], s4[:, 2:3]), reads=[s4], writes=[s4])
            d = dg()
            P.op("act", lambda e: e.activation(d[:], identb[:], AF.Copy, scale=s4[:, 3:4]), reads=[identb, s4], writes=[d])
            oc = (otc[0] % 4) * 128
            otc[0] += 1
            okey = ("pot", oc)
            for g0 in range(0, nkt, 4):
                gn_ = min(4, nkt - g0)
                pt = ppt()
                for t in range(gn_):
                    kt = g0 + t
                    P.op("pe", lambda e: e.matmul(pt[:, t * 128:(t + 1) * 128], pbuf[:, kt * 128:(kt + 1) * 128], d[:], start=True, stop=True),
                         reads=[pbuf, d], writes=[pt])
                ptb = PT()
                P.op("act", lambda e: e.activation(ptb[:, 0:gn_, :], pt[:, 0:gn_ * 128].rearrange("p (t k) -> p t k", t=gn_), AF.Copy),
                     reads=[pt], writes=[ptb])
                for t in range(gn_):
                    kt = g0 + t
                    P.op("pe", lambda e: e.matmul(pot[:, oc:oc + 128], vt[:, kt0 + kt, :], ptb[:, t, :], start=(kt == 0), stop=(kt == nkt - 1)),
                         reads=[vt, ptb], writes=[okey])
            P.op("act", lambda e: e.activation(outb[:, i * 128:(i + 1) * 128], pot[:, oc:oc + 128], AF.Copy), reads=[okey], writes=[outb])
            if i == 31:
                od = o_mla if kind == "mla" else o_ca
                P.dma(dq(), od.ap()[hh * 128:(hh + 1) * 128, :], outb[:], reads=[outb], writes=["o_" + kind])
        units.append((stageA, stageB, stageC))

    for kind in ("mla", "ca"):
        for hh in range(2):
            for i in range(32):
                make_unit(kind, hh, i)


    def scan_chunk(c):
        t0 = c * TC
        b = bc[c % 2]
        bkey = ("bc", c % 2)
        for half in range(2):
            P.dma("sp", b[half * 64:(half + 1) * 64].rearrange("p t g a k -> p t (g a k)"),
                  rwbc_d.ap()[half, t0:t0 + TC, :].partition_broadcast(64), writes=[bkey])
        if t0 % 512 == 0:
            vb = vsb[(t0 // 512) % 2]
            P.dma("sp", vb[:], rv_d.ap()[:, t0:t0 + 512].rearrange("(g p) t -> p g t", p=128), writes=[vb])
        vb = vsb[(t0 // 512) % 2]
        for tt in range(TC):
            t = t0 + tt
            ns = nsk()
            So = Ss[t % 2]; Sn = Ss[(t + 1) % 2]
            Sog = So[:].rearrange("p (g k) -> p g k", g=2)
            Sng = Sn[:].rearrange("p (g k) -> p g k", g=2)
            tm1 = tmps[(2 * t) % 4]; tm2 = tmps[(2 * t + 1) % 4]
            tm1g = tm1[:].rearrange("p (g k) -> p g k", g=2)
            tm2g = tm2[:].rearrange("p (g k) -> p g k", g=2)
            P.op("dve", lambda e: e.tensor_tensor(tm1g, Sog, b[:, tt, :, 0, :], ALU.mult), reads=[(So, 0), (So, 1), bkey], writes=[tm1])
            P.op("dve", lambda e: e.tensor_tensor(Sng, Sog, b[:, tt, :, 1, :], ALU.mult), reads=[(So, 0), (So, 1), bkey], writes=[(Sn, 0), (Sn, 1)])
            P.op("dve", lambda e: e.tensor_reduce(ns[:], tm1g, AX.X, ALU.add, negate=True), reads=[tm1], writes=[ns])
            for g in range(2):
                P.op("dve", lambda e: e.scalar_tensor_tensor(Sn[:, g * 64:(g + 1) * 64], b[:, tt, g, 3, :], vb[:, g, (t % 512):(t % 512) + 1],
                                                            Sn[:, g * 64:(g + 1) * 64], ALU.mult, ALU.add), reads=[(Sn, g), bkey, vb], writes=[(Sn, g)])
            for g in range(2):
                P.op("dve", lambda e: e.scalar_tensor_tensor(Sn[:, g * 64:(g + 1) * 64], b[:, tt, g, 2, :], ns[:, g:g + 1],
                                                            Sn[:, g * 64:(g + 1) * 64], ALU.mult, ALU.add), reads=[(Sn, g), bkey, ns], writes=[(Sn, g)])
            P.op("dve", lambda e: e.tensor_tensor(tm2g, Sng, b[:, tt, :, 4, :], ALU.mult), reads=[(Sn, 0), (Sn, 1), bkey], writes=[tm2])
            P.op("dve", lambda e: e.tensor_reduce(ysb[:, :, t], tm2g, AX.X, ALU.add), reads=[tm2], writes=[("ysb", t // 512)])

    NCH = SEQ // TC
    per = NCH // len(units)
    for c in range(NCH):
        u = c // per if c % per == 0 else None
        if u is not None and u < len(units):
            units[u][0]()
        scan_chunk(c)
        if u is not None and u < len(units):
            units[u][1]()
            if u >= 1:
                units[u - 1][2]()
    units[-1][2]()

    wkv = [scb[0][:, i * 512:(i + 1) * 512] for i in range(8)] + [scb[1][:, i * 512:(i + 1) * 512] for i in range(8)]
    wi = [0]

    def wk():
        wi[0] += 1
        j = wi[0] % 16
        return wkv[j], ("wkv", j)

    first = [True]
    for g in range(2):
        for n in range(8):
            m0 = n * 512
            extra = ([scb[0], scb[1]] + [("wkv", j) for j in range(16)]) if first[0] else []
            first[0] = False
            sq, sqk = wk()
            P.op("act", lambda e: e.activation(sq, ysb[:, g, m0:m0 + 512], AF.Square), reads=[("ysb", n)], writes=[sqk] + extra)
            ps1 = psc(); ps2 = psc()
            P.op("pe", lambda e: e.matmul(ps1[:], bones[:], ysb[:, g, m0:m0 + 512], start=True, stop=True), reads=[bones, ("ysb", n)], writes=[ps1])
            P.op("pe", lambda e: e.matmul(ps2[:], bones[:], sq, start=True, stop=True), reads=[bones, sqk], writes=[ps2])
            mean, mk_ = wk(); msq, msk = wk(); rstd, rsk = wk(); yn, ynk = wk(); bt, btk = wk(); gt, gtk = wk()
            P.op("act", lambda e: e.activation(mean, ps1[:], AF.Copy, scale=1.0 / 64), reads=[ps1], writes=[mk_])
            P.op("dve", lambda e: e.tensor_tensor(msq, mean, mean, ALU.mult), reads=[mk_], writes=[msk])
            P.op("dve", lambda e: e.scalar_tensor_tensor(rstd, ps2[:], 1.0 / 64, msq, ALU.mult, ALU.subtract), reads=[ps2, msk], writes=[rsk])
            P.op("act", lambda e: e.activation(rstd, rstd, AF.Sqrt, bias=epsc[:, 0:1]), reads=[rsk, epsc], writes=[rsk])
            P.op("dve", lambda e: e.reciprocal(rstd, rstd), reads=[rsk], writes=[rsk])
            P.op("dve", lambda e: e.tensor_tensor(yn, ysb[:, g, m0:m0 + 512], mean, ALU.subtract), reads=[("ysb", n), mk_], writes=[ynk])
            P.op("dve", lambda e: e.tensor_tensor(yn, yn, rstd, ALU.mult), reads=[ynk, rsk], writes=[ynk])
            P.op("dve", lambda e: e.tensor_scalar(yn, yn, gn[:, g:g + 1], gn[:, 2 + g:3 + g], ALU.mult, ALU.add), reads=[ynk, gn], writes=[ynk])
            P.dma(dq(), bt, rb_d.ap()[g * 128:(g + 1) * 128, m0:m0 + 512], writes=[btk])
            P.dma(dq(), gt, rg_d.ap()[g * 128:(g + 1) * 128, m0:m0 + 512], writes=[gtk])
            P.op("dve", lambda e: e.tensor_tensor(yn, yn, bt, ALU.add), reads=[ynk, btk], writes=[ynk])
            ob = PT()
            obv = ob[:].rearrange("p a b -> p (a b)")[:, 0:512]
            P.op("dve", lambda e: e.tensor_tensor(obv, yn, gt, ALU.mult), reads=[ynk, gtk], writes=[ob])
            P.dma(dq(), o_rw.ap()[g * 128:(g + 1) * 128, m0:m0 + 512], obv, reads=[ob], writes=["o_rw"])
    nc = P.finish(["o_mla", "o_ca", "o_rw"])
    return nc, P


def inputs_B(inp, l, c, A):
    b, g = c // 2, c % 2
    cat = lambda name, ax: np.concatenate([A[2 * b][name], A[2 * b + 1][name]], axis=ax)
    m = {"mla_qT": np.ascontiguousarray(cat("mla_qT", 2)[2 * g:2 * g + 2]),
         "mla_kT": np.ascontiguousarray(cat("mla_kT", 2)[2 * g:2 * g + 2]),
         "mla_kpeT": cat("mla_kpeT", 1),
         "mla_v": np.ascontiguousarray(cat("mla_v", 0)[:, 256 * g:256 * (g + 1)]),
         "ca_qT": np.ascontiguousarray(cat("ca_qT", 1)[256 * g:256 * (g + 1)].reshape(2, 128, SEQ)),
         "ca_kT": np.ascontiguousarray(cat("ca_kT", 1)[256 * g:256 * (g + 1)].reshape(2, 128, SEQ)),
         "ca_v": np.ascontiguousarray(cat("ca_v", 0)[:, 256 * g:256 * (g + 1)]),
         "ca_bias": np.stack([band_bias(inp["ca_rel_bias"][l][2 * g + hh]) for hh in range(2)]),
         "ident": np.eye(128, dtype=np.float32),
         "rw_vT": np.ascontiguousarray(cat("rw_vT", 1)[256 * g:256 * (g + 1)]),
         "rw_bonusT": np.ascontiguousarray(cat("rw_bonusT", 1)[256 * g:256 * (g + 1)]),
         "rw_gT": np.ascontiguousarray(cat("rw_gT", 1)[256 * g:256 * (g + 1)]),
         "rw_gn": np.concatenate([fm(inp["rw_gn_g"][l][256 * g:256 * (g + 1)]), fm(inp["rw_gn_b"][l][256 * g:256 * (g + 1)])], 1)}
    arrs = [cat("rw_%s_tm" % n, 0)[:, 256 * g:256 * (g + 1)].reshape(SEQ, 2, 2, 64) for n in ("kk", "w", "kka", "k", "r")]
    st = np.stack(arrs, 0)
    m["rw_bc"] = np.ascontiguousarray(np.transpose(st, (3, 1, 2, 0, 4))).reshape(2, SEQ, 640)
    return m


def build_M():
    P = Prog()
    ct_d = P.din("cT4", [128, 64])
    mw_d = P.din("mod_w_s", [D, 1536])
    mb_d = P.din("mod_b_s", [128, 12])
    o = P.dout("modT", [128, 48])
    sct = P.sb([128, 64], name="sct")
    mb = P.sb([128, 12], name="mb")
    res = P.sb([128, 12, 4], name="res")
    blk = [P.sb([128, 16, 128], name="blk%d" % i) for i in range(3)]
    ps = P.ps([128, 48], name="pm")
    P.dma("sp", sct[:], ct_d.ap(), writes=[sct])
    P.dma("sp", mb[:], mb_d.ap(), writes=[mb])
    sct0 = sct
    sct = P.sb([128, 64], name="sct2")
    P.op("act", lambda e: e.activation(sct[:], sct0[:], AF.Silu), reads=[sct0], writes=[sct])
    for j in range(12):
        b = blk[j % 3]
        P.dma(("sp", "act")[j % 2], b[:], mw_d.ap()[:, j * 128:(j + 1) * 128].rearrange("(k p) n -> p k n", p=128), writes=[b])
        for k in range(16):
            P.op("pe", lambda e: e.matmul(ps[:, j * 4:(j + 1) * 4], b[:, k, :], sct[:, k * 4:(k + 1) * 4], start=(k == 0), stop=(k == 15)),
                 reads=[b, sct], writes=[("pm", j)])
    for j in range(12):
        P.op("dve", lambda e: e.tensor_scalar_add(res[:, j, :], ps[:, j * 4:(j + 1) * 4], mb[:, j:j + 1]), reads=[("pm", jj) for jj in range(12)] + [mb], writes=[res])
    P.dma("sp", o.ap(), res[:].rearrange("p j b -> p (j b)"), reads=[res], writes=["o"])
    return P.finish(["o"]), P


def ln_fm(P, z, zkey, n, ones, epsc_col, wk, pp, inv_d, stp):
    ps1 = pp(); ps2 = pp()
    for k in range(16):
        sq = wk()
        P.op("act", lambda e: e.activation(sq[:, 0:n], z[:, k, 0:n], AF.Square), reads=[zkey], writes=[sq])
        P.op("pe", lambda e: e.matmul(ps1[:, 0:n], ones[:], z[:, k, 0:n], start=(k == 0), stop=(k == 15)), reads=[ones, zkey], writes=[ps1])
        P.op("pe", lambda e: e.matmul(ps2[:, 0:n], ones[:], sq[:, 0:n], start=(k == 0), stop=(k == 15)), reads=[ones, sq], writes=[ps2])
    mean = stp(); msq = stp(); rstd = stp()
    P.op("act", lambda e: e.activation(mean[:, 0:n], ps1[:, 0:n], AF.Copy, scale=inv_d), reads=[ps1], writes=[mean])
    P.op("dve", lambda e: e.tensor_tensor(msq[:, 0:n], mean[:, 0:n], mean[:, 0:n], ALU.mult), reads=[mean], writes=[msq])
    P.op("dve", lambda e: e.scalar_tensor_tensor(rstd[:, 0:n], ps2[:, 0:n], inv_d, msq[:, 0:n], ALU.mult, ALU.subtract), reads=[ps2, msq], writes=[rstd])
    P.op("act", lambda e: e.activation(rstd[:, 0:n], rstd[:, 0:n], AF.Sqrt, bias=epsc_col), reads=[rstd], writes=[rstd])
    P.op("dve", lambda e: e.reciprocal(rstd[:, 0:n], rstd[:, 0:n]), reads=[rstd], writes=[rstd])
    return mean, rstd


PKC = {}
_o = 0
for _n, _w in (("modsh", 96), ("modoff", 96), ("lng", 16), ("lnb", 16), ("rb", 1)):
    PKC[_n] = (_o, _w)
    _o += _w
NPKC = _o


def pack_C(inp, l, which, modsh):
    pk = np.zeros((128, NPKC), np.float32)
    pk[:, 0:96] = modsh
    pk[:, 96:192] = fm(inp["mod_offset"][l])
    pk[:, 192:208] = fm(inp["ln_post_g"][l, which])
    pk[:, 208:224] = fm(inp["ln_post_b"][l, which])
    pk[0:32, 224] = inp["moe_router_b"][l]
    return pk


def build_C():
    P = Prog()
    yc_d = P.din("ycatT", [D, TOK], BF16)
    x_d = P.din("x", [TOK, D])
    wo_d = P.din("w_out", [D, D])
    pk_d = P.din("pk", [128, NPKC])
    rw_d = P.din("router_w", [D, 32])
    ident_d = P.din("ident", [128, 128])
    o_x1 = P.dout("x1T", [D, TOK])
    o_h2 = P.dout("h2T", [D, TOK], BF16)
    o_g = P.dout("gateT", [32, TOK])

    pk = P.sb([128, NPKC], name="pk")
    ident = P.sb([128, 128], name="ident")
    ones = P.sb([128, 128], name="ones")
    epsc = P.sb([128, 1], name="epsc")
    modp = P.sb([128, 96], name="modp")
    g1p = P.sb([128, 16], name="g1p")
    sc2p = P.sb([128, 16], name="sc2p")
    rw = P.sb([128, 16, 32], name="rw")
    z = P.sb([128, 16, 512], name="z")
    ycT = P.sb([128, 16, 512], BF16, name="ycT")
    h2b = P.sb([128, 16, 512], BF16, name="h2b")
    xt = P.pool("xt", 2, [128, D])
    wb = P.pool("wb", 4, [128, 16, 128], BF16)
    wk = P.pool("wk", 8, [128, 512])
    stp = P.pool("stp", 6, [128, 512])
    pp = P.pool("pp", 6, [128, 512], psum=True)
    plg = P.ps([32, 512], name="plg")
    ptk = P.ps([128, 512], name="ptk")
    lgs = P.sb([32, 512], name="lgs")
    lt = P.sb([128, 4, 32], name="lt")
    gt = P.sb([128, 4, 32], name="gt")
    ex = P.sb([128, 4, 32], name="ex")
    m8 = P.sb([128, 4, 8], name="m8")
    sm = P.sb([128, 4, 4], name="sm")
    gT = P.sb([32, 512], name="gT")

    def col(n, j=0, w=1):
        o, _ = PKC[n]
        return pk[:, o + j:o + j + w]

    P.dma("sp", pk[:], pk_d.ap(), writes=[pk])
    P.dma("sp", ident[:], ident_d.ap(), writes=[ident])
    P.dma("sp", rw[:], rw_d.ap().rearrange("(k p) n -> p k n", p=128), writes=[rw])
    P.op("pool", lambda e: e.memset(ones[:], 1.0), writes=[ones])
    P.op("pool", lambda e: e.memset(epsc[:], LN_EPS), writes=[epsc])
    P.op("dve", lambda e: e.tensor_tensor(modp[:], col("modsh", 0, 96), col("modoff", 0, 96), ALU.add), reads=[pk], writes=[modp])
    P.op("dve", lambda e: e.tensor_scalar_add(g1p[:], modp[:, 32:48], 1.0), reads=[modp], writes=[g1p])
    P.op("dve", lambda e: e.tensor_scalar_add(sc2p[:], modp[:, 64:80], 1.0), reads=[modp], writes=[sc2p])
    dqc = [0]

    def dq():
        dqc[0] += 1
        return ("sp", "act")[dqc[0] % 2]

    for n in range(4):
        m0 = n * 512
        P.dma(dq(), ycT[:], yc_d.ap()[:, m0:m0 + 512].rearrange("(k p) t -> p k t", p=128), writes=[ycT])
        for s in range(4):
            xx = xt()
            P.dma(dq(), xx[:], x_d.ap()[m0 + s * 128:m0 + (s + 1) * 128, :], writes=[xx])
            for g in range(4):
                pt = pp()
                for q in range(4):
                    k = g * 4 + q
                    P.op("pe", lambda e: e.transpose(pt[:, q * 128:(q + 1) * 128], xx[:, k * 128:(k + 1) * 128], ident[:]), reads=[xx, ident], writes=[pt])
                P.op("act", lambda e: e.activation(z[:, g * 4:(g + 1) * 4, s * 128:(s + 1) * 128], pt[:].rearrange("p (q t) -> p q t", q=4), AF.Copy, scale=DN_ALPHA),
                     reads=[pt], writes=["z"])
        for dc in range(16):
            w = wb()
            P.dma("pool", w[:], wo_d.ap()[:, dc * 128:(dc + 1) * 128].rearrange("(k p) n -> p k n", p=128), writes=[w])
            ps = pp()
            for k in range(16):
                P.op("pe", lambda e: e.matmul(ps[:], w[:, k, :], ycT[:, k, :], start=(k == 0), stop=(k == 15)), reads=[w, ycT], writes=[ps])
            P.op("dve", lambda e: e.scalar_tensor_tensor(z[:, dc, :], ps[:], g1p[:, dc:dc + 1], z[:, dc, :], ALU.mult, ALU.add), reads=[ps, g1p, "z"], writes=["z"])
        mean, rstd = ln_fm(P, z, "z", 512, ones, epsc[:, 0:1], wk, pp, 1.0 / D, stp)
        for k in range(16):
            t = wk()
            P.op("dve", lambda e: e.tensor_tensor(t[:], z[:, k, :], mean[:], ALU.subtract), reads=["z", mean], writes=[t])
            P.op("dve", lambda e: e.tensor_tensor(t[:], t[:], rstd[:], ALU.mult), reads=[t, rstd], writes=[t])
            P.op("dve", lambda e: e.tensor_scalar(z[:, k, :], t[:], col("lng", k), col("lnb", k), ALU.mult, ALU.add), reads=[t, pk, "z"], writes=["z"])
        P.dma(dq(), o_x1.ap()[:, m0:m0 + 512].rearrange("(k p) t -> p k t", p=128), z[:], reads=["z"], writes=["o_x1"])
        mean, rstd = ln_fm(P, z, "z", 512, ones, epsc[:, 0:1], wk, pp, 1.0 / D, stp)
        for k in range(16):
            t = wk()
            P.op("dve", lambda e: e.tensor_tensor(t[:], z[:, k, :], mean[:], ALU.subtract), reads=["z", mean], writes=[t])
            P.op("dve", lambda e: e.tensor_tensor(t[:], t[:], rstd[:], ALU.mult), reads=[t, rstd], writes=[t])
            P.op("dve", lambda e: e.tensor_scalar(t[:], t[:], sc2p[:, k:k + 1], modp[:, 48 + k:49 + k], ALU.mult, ALU.add), reads=[t, sc2p, modp], writes=[t])
            P.op("pe", lambda e: e.matmul(plg[:], rw[:, k, :], t[:], start=(k == 0), stop=(k == 15)), reads=[rw, t], writes=[plg])
            P.op("act", lambda e: e.activation(h2b[:, k, :], t[:], AF.Copy), reads=[t], writes=[h2b])
        P.dma(dq(), o_h2.ap()[:, m0:m0 + 512].rearrange("(k p) t -> p k t", p=128), h2b[:], reads=[h2b], writes=["o_h2"])
        P.op("act", lambda e: e.activation(lgs[:], plg[:], AF.Identity, bias=pk[0:32, PKC["rb"][0]:PKC["rb"][0] + 1]), reads=[plg, pk], writes=[lgs])
        for s in range(4):
            P.op("pe", lambda e: e.transpose(ptk[:, s * 32:(s + 1) * 32], lgs[:, s * 128:(s + 1) * 128], ident[0:32, 0:32]), reads=[lgs, ident], writes=[ptk])
        P.op("dve", lambda e: e.tensor_copy(lt[:], ptk[:, 0:128].rearrange("p (s e) -> p s e", s=4)), reads=[ptk], writes=[lt])
        for s in range(4):
            P.op("dve", lambda e: e.max(m8[:, s, :], lt[:, s, :]), reads=[lt], writes=[m8])
        P.op("dve", lambda e: e.tensor_scalar_mul(sm[:, :, 0:1], m8[:, :, 0:1], -1.0), reads=[m8], writes=[sm])
        for s in range(4):
            P.op("act", lambda e: e.activation(ex[:, s, :], lt[:, s, :], AF.Exp, bias=sm[:, s, 0:1]), reads=[lt, sm], writes=[ex])
            P.op("dve", lambda e: e.tensor_scalar(gt[:, s, :], lt[:, s, :], m8[:, s, 3:4], None, ALU.is_ge), reads=[lt, m8], writes=[gt])
            P.op("dve", lambda e: e.tensor_tensor(gt[:, s, :], gt[:, s, :], ex[:, s, :], ALU.mult), reads=[gt, ex], writes=[gt])
            P.op("dve", lambda e: e.tensor_reduce(sm[:, s, 1:2], gt[:, s, :], AX.X, ALU.add), reads=[gt], writes=[sm])
            P.op("dve", lambda e: e.reciprocal(sm[:, s, 2:3], sm[:, s, 1:2]), reads=[sm], writes=[sm])
            P.op("dve", lambda e: e.tensor_scalar_mul(gt[:, s, :], gt[:, s, :], sm[:, s, 2:3]), reads=[gt, sm], writes=[gt])
        for s in range(4):
            P.op("pe", lambda e: e.transpose(ptk[0:32, 128 + s * 128:128 + (s + 1) * 128] if False else plg[:, s * 128:(s + 1) * 128], gt[:, s, :], ident[:]),
                 reads=[gt, ident, lgs], writes=[plg])
        P.op("act", lambda e: e.activation(gT[:], plg[:], AF.Copy), reads=[plg], writes=[gT])
        P.dma(dq(), o_g.ap()[:, m0:m0 + 512], gT[:], reads=[gT], writes=["o_g"])
    return P.finish(["o_x1", "o_h2", "o_g"]), P


NTOK = NB * SEQ
TP = 1024


def build_D():
    P = Prog()
    NS = 256
    h2_d = P.din("h2T_all", [D, NTOK], BF16)
    g_d = P.din("gate4", [4, NTOK])
    w1_d = P.din("w1", [4, D, 2 * D])
    w2_d = P.din("w2", [4, D, D])
    b1_d = P.din("b1T", [128, 4 * 32])
    b2_d = P.din("b2", [4, D])
    cst_d = P.din("cstD", [128, 1024])
    o_p = P.dout("partT", [D, NTOK])

    cst = P.sb([128, 1024], name="cst")
    identb = P.sb([128, 128], BF16, name="identb")
    b1T = P.sb([128, 4, 32], name="b1T")
    hw = P.sb([128, 16 * TP], BF16, name="hw")
    h2v = hw[:].rearrange("p (k t) -> p k t", k=16)
    w1v = [hw[:, i * 4096:(i + 1) * 4096].rearrange("p (k n) -> p k n", k=16) for i in range(2)]
    w2v = [hw[:, 8192 + i * 4096:8192 + (i + 1) * 4096].rearrange("p (k n) -> p k n", k=16) for i in range(2)]
    h2tm = P.sb([128, 8, D], BF16, name="h2tm")
    acc = P.sb([128, 16, TP], name="acc")
    act = P.sb([128, 16, NS], BF16, name="act")
    h2c = P.sb([128, 16, NS], BF16, name="h2c")
    sel = P.sb([128, 8, NS], BF16, name="sel")
    selT = [P.sb([128, TP], name="selT%d" % i) for i in range(2)]
    ysl = P.sb([128, 2, D], name="ysl")
    b2bc = P.sb([128, D], name="b2bc")
    wbc = P.sb([128, TP], name="wbc")
    g4 = P.sb([4, TP], name="g4")
    pos4 = P.sb([4, TP], name="pos4")
    one4 = P.sb([4, TP], name="one4")
    postm = P.sb([128, 8, 4], name="postm")
    wk = P.pool("wk", 6, [128, NS])
    pp = P.pool("pp", 5, [128, 512], psum=True)
    ptb = P.pool("ptb", 2, [128, 1024], BF16, psum=True)
    ppos = P.ps([128, 32], name="ppos")
    iota_row = cst[:, 0:256]
    selk = lambda e_: cst[0:4, 512 + e_ * 128:512 + (e_ + 1) * 128]
    P.dma("sp", cst[:], cst_d.ap(), writes=[cst])
    P.dma("sp", b1T[:], b1_d.ap().rearrange("p (e j) -> p e j", e=4), writes=[b1T])
    P.op("act", lambda e: e.activation(identb[:], cst[:, 384:512], AF.Copy), reads=[cst], writes=[identb])
    P.op("dve", lambda e: e.memset(one4[:], 1.0), writes=[one4])
    HW = [("hw", 0), ("hw", 1), ("hw", 2), ("hw", 3)]
    w1s = P.nc.dram_tensor("w1s", [4, 16, 128, 4096], BF16, kind="Internal")
    w2s = P.nc.dram_tensor("w2s", [4, 8, 128, 4096], BF16, kind="Internal")
    cq = [0]
    for ex_ in range(4):
        for j in range(16):
            i = cq[0] % 4
            cq[0] += 1
            wfl = hw[:, i * 4096:(i + 1) * 4096]
            w = wfl.rearrange("p (k n) -> p k n", k=16)
            P.dma("pool", w[:, :, 0:128], w1_d.ap()[ex_, :, j * 128:(j + 1) * 128].rearrange("(k p) n -> p k n", p=128), writes=[("hw", i)])
            P.dma("pool", w[:, :, 128:256], w1_d.ap()[ex_, :, D + j * 128:D + (j + 1) * 128].rearrange("(k p) n -> p k n", p=128), reads=[("hw", i)], writes=[("hw", i)])
            P.dma(("sp", "act")[i % 2], w1s.ap()[ex_, j], wfl, reads=[("hw", i)], writes=[("w1s", ex_, j)])
        for nb in range(8):
            i = cq[0] % 4
            cq[0] += 1
            wfl = hw[:, i * 4096:(i + 1) * 4096]
            w = wfl.rearrange("p (k n) -> p k n", k=16)
            P.dma("pool", w, w2_d.ap()[ex_, :, nb * 256:(nb + 1) * 256].rearrange("(k p) n -> p k n", p=128), writes=[("hw", i)])
            P.dma(("sp", "act")[i % 2], w2s.ap()[ex_, nb], wfl, reads=[("hw", i)], writes=[("w2s", ex_, nb)])
    wrr = [0]
    for p_ in range(NTOK // TP):
        t0 = p_ * TP
        P.dma("sp", h2v, h2_d.ap()[:, t0:t0 + TP].rearrange("(k p) t -> p k t", p=128), writes=HW)
        P.dma("act", g4[:], g_d.ap()[:, t0:t0 + TP], writes=[g4])
        for blk in range(8):
            for kh in range(2):
                pt = ptb()
                for q in range(8):
                    k = kh * 8 + q
                    P.op("pe", lambda e: e.transpose(pt[:, q * 128:(q + 1) * 128], h2v[:, k, blk * 128:(blk + 1) * 128], identb[:]), reads=HW + [identb], writes=[pt])
                if (blk + kh) % 2 == 0:
                    P.op("act", lambda e: e.activation(h2tm[:, blk, kh * 1024:(kh + 1) * 1024], pt[:], AF.Copy), reads=[pt], writes=[("h2tm", blk)])
                else:
                    P.op("dve", lambda e: e.tensor_copy(h2tm[:, blk, kh * 1024:(kh + 1) * 1024], pt[:]), reads=[pt], writes=[("h2tm", blk)])
        h2k = [("h2tm", blk) for blk in range(8)]
        m4 = g4
        P.op("dve", lambda e: e.tensor_scalar(g4[:], g4[:], 0.0, None, ALU.is_gt), reads=[g4], writes=[g4])
        P.op("dve", lambda e: e.tensor_tensor_scan(pos4[:], one4[:], m4[:], 0.0, ALU.mult, ALU.add), reads=[m4, one4], writes=[pos4])
        P.op("dve", lambda e: e.tensor_tensor(pos4[:], pos4[:], m4[:], ALU.mult), reads=[pos4, m4], writes=[pos4])
        P.op("dve", lambda e: e.tensor_scalar_add(pos4[:], pos4[:], -1.0), reads=[pos4], writes=[pos4])
        for blk in range(8):
            P.op("pe", lambda e: e.transpose(ppos[:, blk * 4:(blk + 1) * 4], pos4[:, blk * 128:(blk + 1) * 128], cst[0:4, 384:388]), reads=[pos4, cst], writes=[ppos])
        P.op("dve", lambda e: e.tensor_copy(postm[:], ppos[:].rearrange("p (b e) -> p b e", b=8)), reads=[ppos], writes=[postm])
        for dc in range(16):
            P.op("pool", lambda e: e.memset(acc[:, dc, :], 0.0), writes=[("acc", dc, 0), ("acc", dc, 1)])
        for ex_ in range(4):
            P.dma("act", wbc[:], g_d.ap()[ex_:ex_ + 1, t0:t0 + TP].partition_broadcast(128), writes=[wbc])
            P.dma("act", b2bc[:], b2_d.ap()[ex_:ex_ + 1, :].partition_broadcast(128), writes=[b2bc])
            for blk in range(8):
                P.op("dve", lambda e: e.tensor_scalar(sel[:, blk, :], iota_row, postm[:, blk, ex_:ex_ + 1], None, ALU.is_equal), reads=[cst, postm], writes=[sel])
            for tt in range(2):
                ps = pp()
                P.op("pe", lambda e: e.matmul(ps[:], selk(ex_), pos4[:, tt * 512:(tt + 1) * 512], start=True, stop=True), reads=[cst, pos4], writes=[ps])
                for st in range(2):
                    P.op("dve", lambda e: e.tensor_scalar(selT[st][:, tt * 512:(tt + 1) * 512], ps[:], cst[:, 256 + st:257 + st], None, ALU.is_equal), reads=[ps, cst], writes=[selT[st]])
            for st in range(2):
                P.op("dve", lambda e: e.tensor_tensor(selT[st][:], selT[st][:], wbc[:], ALU.mult), reads=[selT[st], wbc], writes=[selT[st]])
            for kp in range(8):
                ps = pp()
                for q in range(2):
                    k = kp * 2 + q
                    for blk in range(8):
                        P.op("pe", lambda e: e.matmul(ps[:, q * NS:(q + 1) * NS], h2tm[:, blk, k * 128:(k + 1) * 128], sel[:, blk, :], start=(blk == 0), stop=(blk == 7)),
                             reads=h2k + [sel], writes=[ps])
                if kp % 2 == 0:
                    P.op("act", lambda e: e.activation(h2c[:, kp * 2:kp * 2 + 2, :], ps[:].rearrange("p (q s) -> p q s", q=2), AF.Copy), reads=[ps], writes=[h2c])
                else:
                    P.op("dve", lambda e: e.tensor_copy(h2c[:, kp * 2:kp * 2 + 2, :], ps[:].rearrange("p (q s) -> p q s", q=2)), reads=[ps], writes=[h2c])
            for j in range(16):
                i = wrr[0] % 4
                wrr[0] += 1
                w = hw[:, i * 4096:(i + 1) * 4096].rearrange("p (k n) -> p k n", k=16)
                wkey = ("hw", i)
                P.dma(("sp", "pool")[wrr[0] % 2], hw[:, i * 4096:(i + 1) * 4096], w1s.ap()[ex_, j], reads=[("w1s", ex_, j)], writes=[wkey])
                ps = pp()
                for k in range(16):
                    P.op("pe", lambda e: e.matmul(ps[:, 0:NS], w[:, k, 0:128], h2c[:, k, :], start=(k == 0), stop=(k == 15)), reads=[wkey, h2c], writes=[ps])
                for k in range(16):
                    P.op("pe", lambda e: e.matmul(ps[:, NS:2 * NS], w[:, k, 128:256], h2c[:, k, :], start=(k == 0), stop=(k == 15)), reads=[wkey, h2c], writes=[ps])
                gl = wk(); sg = wk(); ln = wk()
                P.op("dve", lambda e: e.tensor_scalar(gl[:], ps[:, 0:NS], b1T[:, ex_, j:j + 1], 7.0, ALU.add, ALU.min), reads=[ps, b1T], writes=[gl])
                P.op("act", lambda e: e.activation(sg[:], gl[:], AF.Sigmoid, scale=1.702), reads=[gl], writes=[sg])
                P.op("dve", lambda e: e.tensor_scalar(ln[:], ps[:, NS:2 * NS], b1T[:, ex_, 16 + j:17 + j], 7.0, ALU.add, ALU.min), reads=[ps, b1T], writes=[ln])
                P.op("dve", lambda e: e.tensor_scalar(ln[:], ln[:], -7.0, 1.0, ALU.max, ALU.add), reads=[ln], writes=[ln])
                P.op("dve", lambda e: e.tensor_tensor(gl[:], gl[:], sg[:], ALU.mult), reads=[gl, sg], writes=[gl])
                P.op("dve", lambda e: e.tensor_tensor(act[:, j, :], gl[:], ln[:], ALU.mult), reads=[gl, ln], writes=[("act", j)])
            actk = [("act", j) for j in range(16)]
            for nb in range(8):
                i = wrr[0] % 4
                wrr[0] += 1
                w = hw[:, i * 4096:(i + 1) * 4096].rearrange("p (k n) -> p k n", k=16)
                wkey = ("hw", i)
                P.dma(("sp", "pool")[wrr[0] % 2], hw[:, i * 4096:(i + 1) * 4096], w2s.ap()[ex_, nb], reads=[("w2s", ex_, nb)], writes=[wkey])
                ps = pp()
                for st in range(2):
                    for k in range(16):
                        P.op("pe", lambda e: e.matmul(ps[:, st * 256:(st + 1) * 256], act[:, k, st * 128:(st + 1) * 128], w[:, k, :], start=(k == 0), stop=(k == 15)),
                             reads=[wkey] + actk, writes=[ps])
                P.op("dve", lambda e: e.tensor_tensor(ysl[:, 0, nb * 256:(nb + 1) * 256], ps[:, 0:256], b2bc[:, nb * 256:(nb + 1) * 256], ALU.add),
                     reads=[ps, b2bc], writes=[("ysl", nb)])
                P.op("dve", lambda e: e.tensor_tensor(ysl[:, 1, nb * 256:(nb + 1) * 256], ps[:, 256:512], b2bc[:, nb * 256:(nb + 1) * 256], ALU.add),
                     reads=[ps, b2bc, ("ysl", nb)], writes=[("ysl", nb)])
            yk = [("ysl", nb) for nb in range(8)]
            for dc in range(16):
                for tt in range(2):
                    ts = slice(tt * 512, (tt + 1) * 512)
                    ps = pp()
                    for st in range(2):
                        P.op("pe", lambda e: e.matmul(ps[:], ysl[:, st, dc * 128:(dc + 1) * 128], selT[st][:, ts], start=(st == 0), stop=(st == 1)),
                             reads=yk + [selT[0], selT[1]], writes=[ps])
                    P.op("dve", lambda e: e.tensor_tensor(acc[:, dc, ts], ps[:], acc[:, dc, ts], ALU.add), reads=[ps, ("acc", dc, tt)], writes=[("acc", dc, tt)])
        P.dma("sp", o_p.ap()[:, t0:t0 + TP].rearrange("(k p) t -> p k t", p=128), acc[:],
              reads=[("acc", dc, tt) for dc in range(16) for tt in range(2)], writes=["o_p"])
    return P.finish(["o_p"]), P


def consts_D():
    c = np.zeros((128, 1024), np.float32)
    c[:, 0:256] = np.arange(256, dtype=np.float32)[None, :]
    c[:, 256] = np.arange(128, dtype=np.float32)
    c[:, 257] = np.arange(128, dtype=np.float32) + 128
    c[:, 384:512] = np.eye(128, dtype=np.float32)
    for e_ in range(4):
        c[e_, 512 + e_ * 128:512 + (e_ + 1) * 128] = 1.0
    return c


def build_E():
    P = Prog()
    part_d = P.din("parts", [8, D, TOK])
    x1_d = P.din("x1T", [D, TOK])
    pk_d = P.din("pk", [128, NPKC])
    ident_d = P.din("ident", [128, 128])
    o_x = P.dout("xout", [TOK, D])
    pk = P.sb([128, NPKC], name="pk")
    ident = P.sb([128, 128], name="ident")
    ones = P.sb([128, 128], name="ones")
    epsc = P.sb([128, 1], name="epsc")
    modp = P.sb([128, 96], name="modp")
    g2p = P.sb([128, 16], name="g2p")
    u = P.sb([128, 16, 512], name="u")
    x1 = P.sb([128, 16, 512], name="x1")
    pb_ = P.pool("pb", 2, [128, 16, 512])
    ot = P.pool("ot", 2, [128, D])
    wk = P.pool("wk", 8, [128, 512])
    stp = P.pool("stp", 6, [128, 512])
    pp = P.pool("pp", 7, [128, 512], psum=True)

    def col(n, j=0, w=1):
        o, _ = PKC[n]
        return pk[:, o + j:o + j + w]
    P.dma("sp", pk[:], pk_d.ap(), writes=[pk])
    P.dma("sp", ident[:], ident_d.ap(), writes=[ident])
    P.op("pool", lambda e: e.memset(ones[:], 1.0), writes=[ones])
    P.op("pool", lambda e: e.memset(epsc[:], LN_EPS), writes=[epsc])
    P.op("dve", lambda e: e.tensor_tensor(modp[:], col("modsh", 0, 96), col("modoff", 0, 96), ALU.add), reads=[pk], writes=[modp])
    P.op("dve", lambda e: e.tensor_scalar_add(g2p[:], modp[:, 80:96], 1.0), reads=[modp], writes=[g2p])
    dqc = [0]

    def dq():
        dqc[0] += 1
        return ("sp", "act")[dqc[0] % 2]
    for n in range(4):
        m0 = n * 512
        P.dma(dq(), x1[:], x1_d.ap()[:, m0:m0 + 512].rearrange("(k p) t -> p k t", p=128), writes=[x1])
        P.dma(dq(), u[:], part_d.ap()[0, :, m0:m0 + 512].rearrange("(k p) t -> p k t", p=128), writes=[u])
        for c in range(1, 8):
            pb = pb_()
            P.dma(dq(), pb[:], part_d.ap()[c, :, m0:m0 + 512].rearrange("(k p) t -> p k t", p=128), writes=[pb])
            P.op("dve" if c % 2 else "pool", lambda e: e.tensor_tensor(u[:], u[:], pb[:], ALU.add), reads=[u, pb], writes=[u])
        for k in range(16):
            P.op("dve", lambda e: e.tensor_scalar_mul(u[:, k, :], u[:, k, :], g2p[:, k:k + 1]), reads=[u, g2p], writes=[u])
        P.op("dve", lambda e: e.scalar_tensor_tensor(u[:], x1[:], DN_ALPHA, u[:], ALU.mult, ALU.add), reads=[u, x1], writes=[u])
        mean, rstd = ln_fm(P, u, u, 512, ones, epsc[:, 0:1], wk, pp, 1.0 / D, stp)
        for k in range(16):
            t = wk()
            P.op("dve", lambda e: e.tensor_tensor(t[:], u[:, k, :], mean[:], ALU.subtract), reads=[u, mean], writes=[t])
            P.op("dve", lambda e: e.tensor_tensor(t[:], t[:], rstd[:], ALU.mult), reads=[t, rstd], writes=[t])
            P.op("dve", lambda e: e.tensor_scalar(x1[:, k, :], t[:], col("lng", k), col("lnb", k), ALU.mult, ALU.add), reads=[t, pk, x1], writes=[x1])
        for s in range(4):
            o = ot()
            for g in range(4):
                pt = pp()
                for q in range(4):
                    k = g * 4 + q
                    P.op("pe", lambda e: e.transpose(pt[:, q * 128:(q + 1) * 128], x1[:, k, s * 128:(s + 1) * 128], ident[:]), reads=[x1, ident], writes=[pt])
                if g % 2 == 0:
                    P.op("act", lambda e: e.activation(o[:, g * 512:(g + 1) * 512], pt[:], AF.Copy), reads=[pt], writes=[o])
                else:
                    P.op("dve", lambda e: e.tensor_copy(o[:, g * 512:(g + 1) * 512], pt[:]), reads=[pt], writes=[o])
            P.dma(dq(), o_x.ap()[m0 + s * 128:m0 + (s + 1) * 128, :], o[:], reads=[o], writes=["o_x"])
    return P.finish(["o_x"]), P


def inputs_A(inp, l, c, x, vfirst):
    b, hf = c // 2, c % 2
    xh = np.zeros((TT, D), np.float32)
    if hf == 0:
        xh[HALO:] = x[b, 0:TOK]
    else:
        xh[:] = x[b, TOK - HALO:2 * TOK]
    m = {"xh": xh, "pk": pack_A(inp, l, b, hf),
         "pos": np.ascontiguousarray(inp["positions"][b:b + 1, hf * TOK:(hf + 1) * TOK]).astype(np.int32),
         "ident": np.eye(128, dtype=np.float32), "w_in": inp["w_in"][l],
         "w_uq": inp["mla_w_uq"][l], "w_ukv": inp["mla_w_ukv"][l], "rw_w1": inp["rw_w1"][l], "rw_w2": inp["rw_w2"][l],
         "rw_a1": inp["rw_a1"][l], "rw_a2": inp["rw_a2"][l], "rw_g1": inp["rw_g1"][l], "rw_g2": inp["rw_g2"][l]}
    if l > 0:
        m["rw_v1"] = inp["rw_v1"][l - 1]
        m["rw_v2"] = inp["rw_v2"][l - 1]
        m["vfirstT"] = np.ascontiguousarray(vfirst[b, hf * TOK:(hf + 1) * TOK].T)
    return m


_PROGS = {}


def _prog(name, fn):
    if name not in _PROGS:
        _PROGS[name] = fn()[0]
    return _PROGS[name]


def _run(nc, ims):
    return run_bass_kernel_spmd(nc, ims, core_ids=list(range(8))).results


def run_M(inp):
    ims = []
    cT4 = np.zeros((128, 64), np.float32)
    for b in range(NB):
        cT4[:, b::4] = fm(inp["c"][b])
    for c in range(8):
        ims.append({"cT4": cT4, "mod_w_s": np.ascontiguousarray(inp["mod_w"][:, c * 1536:(c + 1) * 1536]),
                    "mod_b_s": fm(inp["mod_b"][c * 1536:(c + 1) * 1536])})
    r = _run(_prog("M", build_M), ims)
    modsh = np.zeros((NB, 128, 96), np.float32)
    for c in range(8):
        mt = r[c]["modT"].reshape(128, 12, 4)
        for b in range(NB):
            modsh[b][:, c * 12:(c + 1) * 12] = mt[:, :, b]
    return modsh


def inputs_C(inp, l, c, x, A, Bres):
    b, hf = c // 2, c % 2
    ts = slice(hf * TOK, (hf + 1) * TOK)
    pair = lambda n: np.concatenate([Bres[2 * b][n], Bres[2 * b + 1][n]], 0)[:, ts]
    ycat = np.concatenate([A[c]["yconvT"], pair("y_mlaT"), pair("y_rwT"), pair("y_caT")], 0)
    return {"ycatT": np.ascontiguousarray(ycat), "x": np.ascontiguousarray(x[b, ts]), "w_out": inp["w_out"][l],
            "pk": pack_C(inp, l, 0, inp["modsh"][b]), "router_w": inp["moe_router_w"][l], "ident": np.eye(128, dtype=np.float32)}


def inputs_D(inp, l, c, Cres):
    h2 = np.concatenate([Cres[i]["h2T"] for i in range(8)], 1)
    g = np.concatenate([Cres[i]["gateT"] for i in range(8)], 1)
    b1 = inp["moe_b1"][l][4 * c:4 * c + 4]
    b1T = np.concatenate([fm(b1[e]) for e in range(4)], 1)
    return {"h2T_all": h2, "gate4": np.ascontiguousarray(g[4 * c:4 * c + 4]),
            "w1": inp["moe_w1"][l][4 * c:4 * c + 4], "w2": inp["moe_w2"][l][4 * c:4 * c + 4],
            "b1T": b1T, "b2": np.ascontiguousarray(inp["moe_b2"][l][4 * c:4 * c + 4]), "cstD": consts_D()}


def inputs_E(inp, l, c, Cres, Dres):
    b = c // 2
    parts = np.stack([Dres[i]["partT"][:, c * TOK:(c + 1) * TOK] for i in range(8)], 0)
    return {"parts": parts, "x1T": Cres[c]["x1T"], "pk": pack_C(inp, l, 1, inp["modsh"][b]), "ident": np.eye(128, dtype=np.float32)}


def kernel(**inputs):
    inp = {k: np.asarray(v) for k, v in inputs.items()}
    inp["modsh"] = run_M(inp)
    x = inp["x"].astype(np.float32)
    vfirst = None
    for l in range(DEPTH):
        A = _run(_prog("A%d" % l, lambda: build_A(l)), [inputs_A(inp, l, c, x, vfirst) for c in range(8)])
        if l == 0:
            vfirst = np.zeros((NB, SEQ, 512), np.float32)
            for c in range(8):
                vfirst[c // 2, (c % 2) * TOK:(c % 2 + 1) * TOK] = A[c]["rw_vfirstT"].T
        Bres = _run(_prog("B", lambda: build_B(l)), [inputs_B(inp, l, c, A) for c in range(8)])
        Cres = _run(_prog("C", build_C), [inputs_C(inp, l, c, x, A, Bres) for c in range(8)])
        del A, Bres
        Dres = _run(_prog("D", build_D), [inputs_D(inp, l, c, Cres) for c in range(8)])
        Eres = _run(_prog("E", build_E), [inputs_E(inp, l, c, Cres, Dres) for c in range(8)])
        del Cres, Dres
        x = np.zeros((NB, SEQ, D), np.float32)
        for c in range(8):
            x[c // 2, (c % 2) * TOK:(c % 2 + 1) * TOK] = Eres[c]["xout"]
    return x
```

```python
import numpy as np
from contextlib import ExitStack
import concourse.bass as bass
import concourse.mybir as mybir
from concourse.bass_utils import run_bass_kernel_spmd

F32 = mybir.dt.float32
BF16 = mybir.dt.bfloat16
I32 = mybir.dt.int32
AF = mybir.ActivationFunctionType
ALU = mybir.AluOpType
AX = mybir.AxisListType

ENGS = ("pe", "act", "dve", "pool", "sp")
ND = 6

D = 2048
SEQ = 4096
NB = 4
TOK = 2048
HALO = 128
TT = TOK + HALO
DIN = 4800
LN_EPS = 1e-5
DEPTH = 2
DN_ALPHA = (2 * DEPTH) ** 0.25


class _Rec:
    def __init__(self):
        self.calls = []

    def __getattr__(self, name):
        def f(*a, **kw):
            self.calls.append((name, a, kw))
        return f


class Prog:
    def __init__(self):
        self.nc = bass.Bass("TRN2", target_bir_lowering=False)
        self.es = ExitStack()
        self.ops = {e: [] for e in ENGS}
        self.cnt = {e: 0 for e in ENGS}
        self.dcnt = {e: 0 for e in ENGS}
        self.waited = {e: {} for e in ENGS}
        self.lastw = {}
        self.readers = {}
        self.sems = {}
        self.n_t = 0
        self.rr = {}
        self.same_engine_sync = True

    def sem(self, key):
        if key not in self.sems:
            nm = "s_" + "_".join(str(x) for x in (key if isinstance(key, tuple) else (key,)))
            self.sems[key] = self.es.enter_context(self.nc.semaphore(nm))
        return self.sems[key]

    def sb(self, shape, dt=F32, name=None):
        self.n_t += 1
        return self.es.enter_context(self.nc.sbuf_tensor("sb_" + (name or "t%d" % self.n_t), list(shape), dt))

    def ps(self, shape, dt=F32, name=None):
        self.n_t += 1
        return self.es.enter_context(self.nc.psum_tensor("ps_" + (name or "p%d" % self.n_t), list(shape), dt))

    def din(self, name, shape, dt=F32):
        return self.nc.dram_tensor(name, list(shape), dt, kind="ExternalInput")

    def dout(self, name, shape, dt=F32):
        return self.nc.dram_tensor(name, list(shape), dt, kind="ExternalOutput")

    def pool(self, name, n, shape, dt=F32, psum=False):
        bufs = [(self.ps if psum else self.sb)(shape, dt, name="%s%d" % (name, i)) for i in range(n)]
        self.rr[name] = 0

        def nxt():
            b = bufs[self.rr[name] % n]
            self.rr[name] += 1
            return b
        return nxt

    @staticmethod
    def _k(k):
        if isinstance(k, (str, int)):
            return k
        if isinstance(k, tuple):
            return tuple(Prog._k(x) for x in k)
        return "T:" + k.name

    def _tok_wait(self, tok):
        if tok[0] == "c":
            return (("c", tok[1]), tok[2] + 1)
        q, d = tok[1], tok[2]
        return (("d", q, d % ND), 16 * (d // ND + 1))

    def _deps(self, eng, reads, writes):
        toks = set()
        for k in list(reads) + list(writes):
            if k in self.lastw:
                toks.add(self.lastw[k])
        for k in writes:
            for t in self.readers.get(k, ()):
                toks.add(t)
        waits = []
        for t in toks:
            if t[0] == "c" and t[1] == eng and (eng == "pe" or not self.same_engine_sync):
                continue
            sk, v = self._tok_wait(t)
            if self.waited[eng].get(sk, 0) >= v:
                continue
            self.waited[eng][sk] = v
            waits.append((sk, v))
        return waits

    def _commit(self, tok, reads, writes):
        for k in writes:
            self.lastw[k] = tok
            self.readers[k] = []
        for k in reads:
            self.readers.setdefault(k, []).append(tok)

    def op(self, eng, fn, reads=(), writes=()):
        rec = _Rec()
        fn(rec)
        (mname, margs, mkw), = rec.calls
        fn = lambda e, mname=mname, margs=margs, mkw=mkw: getattr(e, mname)(*margs, **mkw)
        reads = [self._k(k) for k in reads]
        writes = [self._k(k) for k in writes]
        waits = self._deps(eng, reads, writes)
        idx = self.cnt[eng]
        self.cnt[eng] += 1
        self.ops[eng].append((waits, fn, (("c", eng), 1)))
        self._commit(("c", eng, idx), reads, writes)

    def dma(self, q, out, in_, reads=(), writes=(), **kw):
        reads = [self._k(k) for k in reads]
        writes = [self._k(k) for k in writes]
        d = self.dcnt[q]
        waits = self._deps(q, reads, writes)
        if d >= ND:
            sk, v = self._tok_wait(("d", q, d - ND))
            if self.waited[q].get(sk, 0) < v:
                self.waited[q][sk] = v
                waits.append((sk, v))
        self.dcnt[q] += 1
        self.ops[q].append((waits, lambda e: e.dma_start(out=out, in_=in_, **kw), (("d", q, d % ND), 16)))
        self._commit(("d", q, d), reads, writes)

    def barrier(self):
        allw = []
        for e2 in ENGS:
            if self.cnt[e2] > 0:
                allw.append((("c", e2), self.cnt[e2]))
            for d in range(max(0, self.dcnt[e2] - ND), self.dcnt[e2]):
                allw.append(self._tok_wait(("d", e2, d)))
        for e in ENGS:
            waits = []
            for sk, v in allw:
                if self.waited[e].get(sk, 0) < v:
                    self.waited[e][sk] = v
                    waits.append((sk, v))
            if waits:
                self.ops[e].append((waits, None, None))

    def scope(self):
        prog = self

        class _S:
            def __enter__(s2):
                s2.outer = prog.es
                prog.es = ExitStack()
                return s2

            def __exit__(s2, *a):
                prog.barrier()
                prog.es.close()
                prog.es = s2.outer
                return False
        return _S()

    def finish(self, final_keys):
        final_keys = [self._k(k) for k in final_keys]
        waits = self._deps("sp", final_keys, ())
        self.ops["sp"].append((waits, None, None))
        nc = self.nc
        for e in ENGS:
            for waits, fn, inc in self.ops[e]:
                for sk, v in waits:
                    self.sem(sk)
                if inc is not None:
                    self.sem(inc[0])
        with nc.Block() as block:
            def emit(eng_name):
                def body(e):
                    for waits, fn, inc in self.ops[eng_name]:
                        for sk, v in waits:
                            e.wait_ge(self.sem(sk), v)
                        if fn is not None:
                            fn(e).then_inc(self.sem(inc[0]), inc[1])
                return body
            block.tensor(emit("pe"))
            block.scalar(emit("act"))
            block.vector(emit("dve"))
            block.gpsimd(emit("pool"))
            block.sync(emit("sp"))
        self.es.close()
        return nc


def fm(v):
    v = np.asarray(v, np.float32).reshape(-1, 128)
    return np.ascontiguousarray(v.T)


PKA = {}
_o = 0
for _n, _w in (("modb", 96), ("modoff", 96), ("cT", 16), ("dw", 124), ("convb", 4), ("clng", 4),
               ("clnb", 4), ("qn", 3), ("kvn", 2), ("murkv", 12), ("mux", 48), ("w0", 4), ("a0", 4),
               ("kk", 4), ("ka", 4), ("rk", 4), ("v0", 4), ("vmu", 16), ("invf", 1), ("flag", 1)):
    PKA[_n] = (_o, _w)
    _o += _w
NPKA = _o


def pack_A(inp, l, b, hf):
    pk = np.zeros((128, NPKA), np.float32)

    def put(n, a):
        o, w = PKA[n]
        a = np.asarray(a, np.float32).reshape(128, w)
        pk[:, o:o + w] = a
    put("modb", inp["modsh"][b])
    put("modoff", fm(inp["mod_offset"][l]))
    put("dw", np.transpose(inp["conv_dw"][l].T.reshape(4, 128, 31), (1, 0, 2)))
    put("convb", fm(inp["conv_b"][l]))
    put("clng", fm(inp["conv_ln_g"][l]))
    put("clnb", fm(inp["conv_ln_b"][l]))
    put("qn", fm(inp["mla_q_norm"][l]))
    put("kvn", fm(inp["mla_kv_norm"][l]))
    put("murkv", np.transpose(inp["rw_mu_rkv"][l].reshape(3, 4, 128), (2, 0, 1)))
    put("mux", np.transpose(inp["rw_mu_x"][l].reshape(3, 16, 128), (2, 0, 1)))
    put("w0", fm(inp["rw_w0"][l]))
    put("a0", fm(inp["rw_a0"][l]))
    put("kk", fm(inp["rw_k_k"][l]))
    put("ka", fm(inp["rw_k_a"][l]))
    put("rk", fm(inp["rw_r_k"][l].reshape(-1)))
    if l > 0:
        put("v0", fm(inp["rw_v0"][l - 1]))
        put("vmu", fm(inp["rw_vres_mu"][l - 1]))
    inv = (10000.0 ** (-np.arange(0, 64, 2, dtype=np.float32) / 64)).astype(np.float32)
    iv = np.zeros((128, 1), np.float32)
    iv[0:32, 0] = inv
    iv[32:64, 0] = inv
    put("invf", iv)
    put("flag", np.full((128, 1), 0.0 if hf == 0 else 1.0, np.float32))
    return pk


TWO_PI = float(2 * np.pi)
CW1 = 6.28125
CW2 = float(np.float32(2 * np.pi - 6.28125).view(np.int32) & ~0xFFF) if False else 0.0019350052
_c2 = np.float32(2 * np.pi - CW1)
_c2i = np.frombuffer(np.float32(_c2).tobytes(), np.uint32)[0] & np.uint32(0xFFFFF000)
CW2 = float(np.frombuffer(np.uint32(_c2i).tobytes(), np.float32)[0])
CW3 = float(2 * np.pi - CW1 - CW2)
MAGIC = 12582912.0


def build_A(l, dbg=False):
    P = Prog()
    xh = P.din("xh", [TT, D])
    pk_d = P.din("pk", [128, NPKA])
    pos_d = P.din("pos", [1, TOK], I32)
    ident_d = P.din("ident", [128, 128])
    win_d = P.din("w_in", [D, DIN])
    wuq_d = P.din("w_uq", [384, 768])
    wukv_d = P.din("w_ukv", [256, 1024])
    w1_d = P.din("rw_w1", [D, 96]); w2_d = P.din("rw_w2", [96, 512])
    a1_d = P.din("rw_a1", [D, 96]); a2_d = P.din("rw_a2", [96, 512])
    g1_d = P.din("rw_g1", [D, 256]); g2_d = P.din("rw_g2", [256, 512])
    if l > 0:
        v1_d = P.din("rw_v1", [D, 64]); v2_d = P.din("rw_v2", [64, 512])
        vf_d = P.din("vfirstT", [512, TOK])
    o_modp = P.dout("modp", [128, 96])
    o_yconv = P.dout("yconvT", [512, TOK], BF16)
    o_mq = P.dout("mla_qT", [4, 192, TOK], BF16)
    o_mk = P.dout("mla_kT", [4, 128, TOK], BF16)
    o_mkpe = P.dout("mla_kpeT", [64, TOK], BF16)
    o_mv = P.dout("mla_v", [TOK, 512], BF16)
    o_cq = P.dout("ca_qT", [512, TOK], BF16)
    o_ck = P.dout("ca_kT", [512, TOK], BF16)
    o_cv = P.dout("ca_v", [TOK, 512], BF16)
    o_rtm = {n: P.dout("rw_%s_tm" % n, [TOK, 512]) for n in ("r", "w", "k", "kk", "kka")}
    rfm_names = ("v", "bonus", "g") + (("vfirst",) if l == 0 else ())
    o_rfm = {n: P.dout("rw_%sT" % n, [512, TOK]) for n in rfm_names}
    finals = ["o_modp", "o_yconv", "o_mq", "o_mk", "o_mkpe", "o_mv", "o_cq", "o_ck", "o_cv"] + \
        ["o_rtm_" + n for n in o_rtm] + ["o_rfm_" + n for n in o_rfm]

    pk = P.sb([128, NPKA], name="pk")
    ident = P.sb([128, 128], name="ident")
    ones = P.sb([128, 128], name="ones")
    bones = P.sb([128, 128], name="bones")
    epsc = P.sb([128, 4], name="epsc")
    hT = P.sb([128, 16, TT], BF16, name="hT")
    modp = P.sb([128, 96], name="modp")
    sc1p = P.sb([128, 16], name="sc1p")
    cosT = P.sb([64, TOK], name="cosT")
    sinT = P.sb([64, TOK], name="sinT")
    wkb = P.pool("wkb", 6, [128, 512], BF16)
    wb = P.pool("wb", 4, [128, 16, 128], BF16)
    pp = P.pool("pp", 7, [128, 512], psum=True)

    def col(n, j=0, w=1):
        o, _ = PKA[n]
        return pk[:, o + j:o + j + w]

    P.dma("sp", pk[:], pk_d.ap(), writes=[pk])
    P.dma("sp", ident[:], ident_d.ap(), writes=[ident])
    P.op("pool", lambda e: e.memset(epsc[:, 0:1], LN_EPS), writes=[epsc])
    P.op("pool", lambda e: e.memset(epsc[:, 1:2], 1e-6), reads=[epsc], writes=[epsc])
    P.op("pool", lambda e: e.memset(epsc[:, 2:3], 64e-5), reads=[epsc], writes=[epsc])
    P.op("pool", lambda e: e.memset(ones[:], 1.0), writes=[ones])
    P.op("pool", lambda e: e.memset(bones[:], 0.0), writes=[bones])
    P.op("pool", lambda e: e.memset(bones[0:64, 0:64], 1.0), reads=[bones], writes=[bones])
    P.op("pool", lambda e: e.memset(bones[64:128, 64:128], 1.0), reads=[bones], writes=[bones])

    NTI = TT // 128
    hkeys = [("hT", i) for i in range(NTI)]
    with P.scope():
        big = P.sb([128, 8192], name="big")
        junk = P.sb([128, D], BF16, name="junk")
        st = P.pool("st", 8, [128, 4])
        P.op("dve", lambda e: e.tensor_tensor(modp[:], col("modb", 0, 96), col("modoff", 0, 96), ALU.add),
             reads=[pk], writes=[modp])
        P.op("dve", lambda e: e.tensor_scalar_add(sc1p[:], modp[:, 16:32], 1.0), reads=[modp], writes=[sc1p])
        P.dma("sp", o_modp.ap(), modp[:], reads=[modp], writes=["o_modp"])
        P.barrier()
        posi = big[0:64, 4096:6144].bitcast(I32)
        ang = big[0:64, 0:2048]
        rn = big[0:64, 2048:4096]
        P.dma("sp", posi, pos_d.ap().partition_broadcast(64), writes=["posi"])
        P.op("dve", lambda e: e.tensor_copy(ang, posi), reads=["posi"], writes=["ang"])
        P.op("dve", lambda e: e.tensor_scalar_mul(ang, ang, col("invf")[0:64, :]), reads=["ang", pk], writes=["ang"])
        P.op("dve", lambda e: e.tensor_scalar(rn, ang, 1.0 / TWO_PI, MAGIC, ALU.mult, ALU.add), reads=["ang"], writes=["rn"])
        P.op("dve", lambda e: e.tensor_scalar_add(rn, rn, -MAGIC), reads=["rn"], writes=["rn"])
        for cw in (CW1, CW2, CW3):
            P.op("dve", lambda e, cw=cw: e.scalar_tensor_tensor(ang, rn, -cw, ang, ALU.mult, ALU.add),
                 reads=["rn", "ang"], writes=["ang"])
        P.op("dve", lambda e: e.tensor_scalar(sinT[:], ang, float(np.pi), -float(np.pi), ALU.min, ALU.max),
             reads=["ang"], writes=[sinT])
        P.op("dve", lambda e: e.tensor_scalar_add(cosT[:], sinT[:], float(np.pi / 2)), reads=[sinT], writes=[cosT])
        P.op("dve", lambda e: e.tensor_scalar(rn, cosT[:], float(np.pi), -TWO_PI, ALU.is_gt, ALU.mult), reads=[cosT], writes=["rn"])
        P.op("dve", lambda e: e.tensor_tensor(cosT[:], cosT[:], rn, ALU.add), reads=[cosT, "rn"], writes=[cosT])
        P.op("dve", lambda e: e.tensor_scalar(cosT[:], cosT[:], float(np.pi), -float(np.pi), ALU.min, ALU.max), reads=[cosT], writes=[cosT])
        P.op("act", lambda e: e.activation(sinT[:], sinT[:], AF.Sin), reads=[sinT], writes=[sinT])
        P.op("act", lambda e: e.activation(cosT[:], cosT[:], AF.Sin), reads=[cosT], writes=[cosT])
        P.barrier()
        for i in range(NTI):
            xv = big[:, (i % 2) * 2048:(i % 2 + 1) * 2048]
            xk = ("xt", i % 2)
            P.dma("sp" if i % 2 == 0 else "act", xv, xh.ap()[i * 128:(i + 1) * 128, :], writes=[xk])
            s = st()
            P.op("act", lambda e, xv=xv, s=s: e.activation(junk[:], xv, AF.Identity, accum_out=s[:, 0:1]),
                 reads=[xk], writes=[junk, (s, 0)])
            P.op("act", lambda e, xv=xv, s=s: e.activation(junk[:], xv, AF.Square, accum_out=s[:, 1:2]),
                 reads=[xk], writes=[junk, (s, 1)])
            P.op("dve", lambda e, s=s: e.tensor_scalar_mul(s[:, 0:2], s[:, 0:2], 1.0 / D), reads=[(s, 0), (s, 1)], writes=[(s, 0), (s, 1)])
            P.op("dve", lambda e, s=s: e.tensor_tensor(s[:, 2:3], s[:, 0:1], s[:, 0:1], ALU.mult), reads=[(s, 0)], writes=[(s, 2)])
            P.op("dve", lambda e, s=s: e.tensor_tensor(s[:, 1:2], s[:, 1:2], s[:, 2:3], ALU.subtract), reads=[(s, 1), (s, 2)], writes=[(s, 1)])
            P.op("act", lambda e, s=s: e.activation(s[:, 1:2], s[:, 1:2], AF.Sqrt, bias=epsc[:, 0:1]), reads=[(s, 1), epsc], writes=[(s, 1)])
            P.op("dve", lambda e, s=s: e.reciprocal(s[:, 1:2], s[:, 1:2]), reads=[(s, 1)], writes=[(s, 1)])
            P.op("dve", lambda e, xv=xv, s=s: e.tensor_scalar(xv, xv, s[:, 0:1], s[:, 1:2], ALU.subtract, ALU.mult),
                 reads=[xk, (s, 0), (s, 1)], writes=[xk])
            for g in range(4):
                pt = pp()
                for q in range(4):
                    k = g * 4 + q
                    P.op("pe", lambda e, pt=pt, q=q, k=k, xv=xv: e.transpose(pt[:, q * 128:(q + 1) * 128], xv[:, k * 128:(k + 1) * 128], ident[:]),
                         reads=[xk, ident], writes=[pt])
                for q in range(4):
                    k = g * 4 + q
                    P.op("act", lambda e, pt=pt, q=q, k=k, i=i: e.activation(
                        hT[:, k, i * 128:(i + 1) * 128], pt[:, q * 128:(q + 1) * 128], AF.Identity,
                        bias=modp[:, k:k + 1], scale=sc1p[:, k:k + 1]),
                        reads=[pt, modp, sc1p], writes=[("hT", i)])
        P.op("dve", lambda e: e.tensor_scalar_mul(hT[:, :, 0:128], hT[:, :, 0:128], col("flag")), reads=[("hT", 0), pk], writes=[("hT", 0)])

    cnt = {"q": 0}

    def dq():
        cnt["q"] += 1
        return ("sp", "act")[cnt["q"] % 2]

    def load_wblock(src_ap, ncols=128, rows=16):
        w = wb()
        P.dma("pool", w[:, 0:rows, 0:ncols], src_ap.rearrange("(k p) n -> p k n", p=128), writes=[w])
        return w

    def proj(w, ncols, t0, n, shift=0):
        ps = pp()
        for k in range(16):
            P.op("pe", lambda e, ps=ps, w=w, k=k: e.matmul(ps[0:ncols, 0:n], w[:, k, 0:ncols], hT[:, k, t0 - shift:t0 - shift + n],
                                                       start=(k == 0), stop=(k == 15)),
                 reads=[w] + hkeys, writes=[ps])
        return ps

    def evac(dst, src, i, reads, writes):
        if i % 2 == 0:
            P.op("act", lambda e: e.activation(dst, src, AF.Copy), reads=reads, writes=writes)
        else:
            P.op("dve", lambda e: e.tensor_copy(dst, src), reads=reads, writes=writes)

    TILES = [(HALO + n * 512, 512) for n in range(4)]

    with P.scope():
        for name, c0, o_d in (("cq", 3264, o_cq), ("ck", 3776, o_ck)):
            for j in range(4):
                w = load_wblock(win_d.ap()[:, c0 + j * 128:c0 + (j + 1) * 128])
                for n, (t0, nn) in enumerate(TILES):
                    ps = proj(w, 128, t0, nn)
                    ob = wkb()
                    evac(ob[:], ps[:], n, [ps], [ob])
                    P.dma(dq(), o_d.ap()[j * 128:(j + 1) * 128, t0 - HALO:t0 - HALO + nn], ob[:], reads=[ob], writes=["o_" + name])
        wv = P.sb([128, 16, 512], BF16, name="wv")
        P.dma("pool", wv[:], win_d.ap()[:, 4288:4800].rearrange("(k p) n -> p k n", p=128), writes=[wv])
        for i in range(16):
            ps = pp()
            for k in range(16):
                P.op("pe", lambda e, ps=ps, k=k, i=i: e.matmul(ps[:], hT[:, k, HALO + i * 128:HALO + (i + 1) * 128], wv[:, k, :],
                                                            start=(k == 0), stop=(k == 15)),
                     reads=[wv] + hkeys, writes=[ps])
            ob = wkb()
            evac(ob[:], ps[:], i, [ps], [ob])
            P.dma(dq(), o_cv.ap()[i * 128:(i + 1) * 128, :], ob[:], reads=[ob], writes=["o_cv"])

    with P.scope():
        cqs = P.sb([128, 5, TOK], name="cqs")
        wuq = P.sb([128, 3, 768], BF16, name="wuq")
        wuqr = P.sb([128, 3, 4, 64], BF16, name="wuqr")
        wukv = P.sb([128, 2, 1024], BF16, name="wukv")
        wukvv = P.sb([128, 2, 512], BF16, name="wukvv")
        wk = P.pool("wkm", 10, [128, 512])
        cqn_p = P.pool("cqn", 2, [128, 3, 512], BF16)
        ckvn_p = P.pool("ckvn", 2, [128, 2, 512], BF16)
        P.dma("pool", wuq[:], wuq_d.ap().rearrange("(k p) n -> p k n", p=128), writes=[wuq])
        P.dma("pool", wukv[:], wukv_d.ap().rearrange("(k p) n -> p k n", p=128), writes=[wukv])
        for k in range(3):
            for h in range(4):
                P.op("dve", lambda e, k=k, h=h: e.tensor_scalar_mul(wuqr[:, k, h, 0:32], wuq[:, k, h * 192 + 160:h * 192 + 192], -1.0),
                     reads=[wuq], writes=[wuqr])
                P.op("dve", lambda e, k=k, h=h: e.tensor_copy(wuqr[:, k, h, 32:64], wuq[:, k, h * 192 + 128:h * 192 + 160]),
                     reads=[wuq], writes=[wuqr])
        for k in range(2):
            for h in range(4):
                P.op("dve", lambda e, k=k, h=h: e.tensor_copy(wukvv[:, k, h * 128:(h + 1) * 128], wukv[:, k, h * 256 + 128:h * 256 + 256]),
                     reads=[wukv], writes=[wukvv])
        for j in range(5):
            w = load_wblock(win_d.ap()[:, 1024 + j * 128:1024 + (j + 1) * 128])
            for n, (t0, nn) in enumerate(TILES):
                ps = proj(w, 128, t0, nn)
                evac(cqs[:, j, t0 - HALO:t0 - HALO + nn], ps[:], n, [ps], [("cqs", j, n)])
        wkp = load_wblock(win_d.ap()[:, 1664:1728], ncols=64)
        wkr = wb()
        P.op("dve", lambda e: e.tensor_scalar_mul(wkr[:, :, 0:32], wkp[:, :, 32:64], -1.0), reads=[wkp], writes=[wkr])
        P.op("dve", lambda e: e.tensor_copy(wkr[:, :, 32:64], wkp[:, :, 0:32]), reads=[wkp, wkr], writes=[wkr])

        def rope_out(ps1, ps2, m0, dst_ap, okey):
            t1 = wk(); t2 = wk(); ob = wkb()
            P.op("dve", lambda e: e.tensor_tensor(t1[0:64, :], ps1[0:64, :], cosT[:, m0:m0 + 512], ALU.mult), reads=[ps1, cosT], writes=[t1])
            P.op("dve", lambda e: e.tensor_tensor(t2[0:64, :], ps2[0:64, :], sinT[:, m0:m0 + 512], ALU.mult), reads=[ps2, sinT], writes=[t2])
            P.op("dve", lambda e: e.tensor_tensor(ob[0:64, :], t1[0:64, :], t2[0:64, :], ALU.add), reads=[t1, t2], writes=[ob])
            P.dma(dq(), dst_ap, ob[0:64, :], reads=[ob], writes=[okey])

        for n, (t0, nn) in enumerate(TILES):
            m0 = t0 - HALO
            ps1 = proj(wkp, 64, t0, nn)
            ps2 = proj(wkr, 64, t0, nn)
            rope_out(ps1, ps2, m0, o_mkpe.ap()[:, m0:m0 + 512], "o_mkpe")
        for n, (t0, nn) in enumerate(TILES):
            m0 = t0 - HALO
            normed = []
            for (j0, nj, pkn, inv_d, pool_) in ((0, 3, "qn", 1.0 / 384, cqn_p), (3, 2, "kvn", 1.0 / 256, ckvn_p)):
                pss = pp()
                for j in range(nj):
                    sq = wk()
                    P.op("act", lambda e, sq=sq, j=j: e.activation(sq[:], cqs[:, j0 + j, m0:m0 + 512], AF.Square),
                         reads=[("cqs", j0 + j, n)], writes=[sq])
                    P.op("pe", lambda e, sq=sq, j=j, pss=pss: e.matmul(pss[:], ones[:], sq[:], start=(j == 0), stop=(j == nj - 1)),
                         reads=[sq, ones], writes=[pss])
                rq = wk()
                P.op("act", lambda e, rq=rq, pss=pss: e.activation(rq[:], pss[:], AF.Sqrt, bias=epsc[:, 1:2], scale=inv_d),
                     reads=[pss, epsc], writes=[rq])
                P.op("dve", lambda e, rq=rq: e.reciprocal(rq[:], rq[:]), reads=[rq], writes=[rq])
                nb_ = pool_()
                for j in range(nj):
                    P.op("dve", lambda e, j=j, nb_=nb_, rq=rq: e.scalar_tensor_tensor(
                        nb_[:, j, :], cqs[:, j0 + j, m0:m0 + 512], col(pkn, j), rq[:], ALU.mult, ALU.mult),
                        reads=[("cqs", j0 + j, n), pk, rq], writes=[nb_])
                normed.append(nb_)
            cqn, ckvn = normed
            for h in range(4):
                ps = pp()
                for k in range(3):
                    P.op("pe", lambda e, ps=ps, k=k, h=h: e.matmul(ps[:], wuq[:, k, h * 192:h * 192 + 128], cqn[:, k, :], start=(k == 0), stop=(k == 2)),
                         reads=[wuq, cqn], writes=[ps])
                ob = wkb()
                evac(ob[:], ps[:], h, [ps], [ob])
                P.dma(dq(), o_mq.ap()[h, 0:128, m0:m0 + 512], ob[:], reads=[ob], writes=["o_mq"])
                ps1 = pp(); ps2 = pp()
                for k in range(3):
                    P.op("pe", lambda e, ps1=ps1, k=k, h=h: e.matmul(ps1[0:64, :], wuq[:, k, h * 192 + 128:h * 192 + 192], cqn[:, k, :], start=(k == 0), stop=(k == 2)),
                         reads=[wuq, cqn], writes=[ps1])
                for k in range(3):
                    P.op("pe", lambda e, ps2=ps2, k=k, h=h: e.matmul(ps2[0:64, :], wuqr[:, k, h, :], cqn[:, k, :], start=(k == 0), stop=(k == 2)),
                         reads=[wuqr, cqn], writes=[ps2])
                rope_out(ps1, ps2, m0, o_mq.ap()[h, 128:192, m0:m0 + 512], "o_mq")
                ps = pp()
                for k in range(2):
                    P.op("pe", lambda e, ps=ps, k=k, h=h: e.matmul(ps[:], wukv[:, k, h * 256:h * 256 + 128], ckvn[:, k, :], start=(k == 0), stop=(k == 1)),
                         reads=[wukv, ckvn], writes=[ps])
                ob = wkb()
                evac(ob[:], ps[:], h + 1, [ps], [ob])
                P.dma(dq(), o_mk.ap()[h, :, m0:m0 + 512], ob[:], reads=[ob], writes=["o_mk"])
            for s in range(4):
                ps = pp()
                for k in range(2):
                    P.op("pe", lambda e, ps=ps, k=k, s=s: e.matmul(ps[:], ckvn[:, k, s * 128:(s + 1) * 128], wukvv[:, k, :], start=(k == 0), stop=(k == 1)),
                         reads=[wukvv, ckvn], writes=[ps])
                ob = wkb()
                evac(ob[:], ps[:], s, [ps], [ob])
                P.dma(dq(), o_mv.ap()[m0 + s * 128:m0 + (s + 1) * 128, :], ob[:], reads=[ob], writes=["o_mv"])

    with P.scope():
        u = P.sb([128, TT], name="u")
        uc = P.sb([128, 4, TOK], name="uc")
        wk = P.pool("wkc", 10, [128, 512])
        CT = [(0, 512), (512, 512), (1024, 512), (1536, 512), (2048, 128)]
        for j in range(4):
            wva = load_wblock(win_d.ap()[:, j * 128:(j + 1) * 128])
            wga = load_wblock(win_d.ap()[:, 512 + j * 128:512 + (j + 1) * 128])
            for n, (t0, nn) in enumerate(CT):
                psv = proj(wva, 128, t0, nn)
                psg = proj(wga, 128, t0, nn)
                sg = wk()
                P.op("act", lambda e, sg=sg, psg=psg, nn=nn: e.activation(sg[:, 0:nn], psg[:, 0:nn], AF.Sigmoid), reads=[psg], writes=[sg])
                P.op("dve", lambda e, sg=sg, psv=psv, t0=t0, nn=nn: e.tensor_tensor(u[:, t0:t0 + nn], psv[:, 0:nn], sg[:, 0:nn], ALU.mult),
                     reads=[psv, sg], writes=[("u", n)])
            ukeys = [("u", n) for n in range(5)]
            o_dw = PKA["dw"][0]
            P.op("dve", lambda e, j=j: e.tensor_scalar(uc[:, j, :], u[:, 98:98 + TOK], pk[:, o_dw + j * 31:o_dw + j * 31 + 1], col("convb", j), ALU.mult, ALU.add),
                 reads=ukeys + [pk], writes=[("uc", j)])
            for i in range(1, 31):
                P.op("dve", lambda e, j=j, i=i: e.scalar_tensor_tensor(uc[:, j, :], u[:, 98 + i:98 + i + TOK], pk[:, o_dw + j * 31 + i:o_dw + j * 31 + i + 1], uc[:, j, :], ALU.mult, ALU.add),
                     reads=ukeys + [pk, ("uc", j)], writes=[("uc", j)])
        uckeys = [("uc", j) for j in range(4)]
        for n in range(4):
            m0 = n * 512
            pssum = pp(); pssq = pp()
            for j in range(4):
                sq = wk()
                P.op("act", lambda e, sq=sq, j=j: e.activation(sq[:], uc[:, j, m0:m0 + 512], AF.Square), reads=[("uc", j)], writes=[sq])
                P.op("pe", lambda e, j=j, pssum=pssum: e.matmul(pssum[:], ones[:], uc[:, j, m0:m0 + 512], start=(j == 0), stop=(j == 3)),
                     reads=[("uc", j), ones], writes=[pssum])
                P.op("pe", lambda e, j=j, pssq=pssq, sq=sq: e.matmul(pssq[:], ones[:], sq[:], start=(j == 0), stop=(j == 3)),
                     reads=[sq, ones], writes=[pssq])
            mean = wk(); msq = wk(); rstd = wk()
            P.op("act", lambda e, mean=mean, pssum=pssum: e.activation(mean[:], pssum[:], AF.Copy, scale=1.0 / 512), reads=[pssum], writes=[mean])
            P.op("dve", lambda e, mean=mean, msq=msq: e.tensor_tensor(msq[:], mean[:], mean[:], ALU.mult), reads=[mean], writes=[msq])
            P.op("dve", lambda e, msq=msq, pssq=pssq, rstd=rstd: e.scalar_tensor_tensor(rstd[:], pssq[:], 1.0 / 512, msq[:], ALU.mult, ALU.subtract),
                 reads=[pssq, msq], writes=[rstd])
            P.op("act", lambda e, rstd=rstd: e.activation(rstd[:], rstd[:], AF.Sqrt, bias=epsc[:, 0:1]), reads=[rstd, epsc], writes=[rstd])
            P.op("dve", lambda e, rstd=rstd: e.reciprocal(rstd[:], rstd[:]), reads=[rstd], writes=[rstd])
            for j in range(4):
                t1 = wk(); ob = wkb()
                P.op("dve", lambda e, t1=t1, j=j, mean=mean: e.tensor_tensor(t1[:], uc[:, j, m0:m0 + 512], mean[:], ALU.subtract), reads=[("uc", j), mean], writes=[t1])
                P.op("dve", lambda e, t1=t1, rstd=rstd: e.tensor_tensor(t1[:], t1[:], rstd[:], ALU.mult), reads=[t1, rstd], writes=[t1])
                P.op("act", lambda e, t1=t1, ob=ob, j=j: e.activation(ob[:], t1[:], AF.Silu, bias=col("clnb", j), scale=col("clng", j)), reads=[t1, pk], writes=[ob])
                P.dma(dq(), o_yconv.ap()[j * 128:(j + 1) * 128, m0:m0 + 512], ob[:], reads=[ob], writes=["o_yconv"])

    with P.scope():
        loras = [("w", w1_d, 96, 0, AF.Tanh), ("a", a1_d, 96, 1, AF.Identity), ("g", g1_d, 256, 2, AF.Sigmoid)]
        if l > 0:
            loras.append(("v", v1_d, 64, None, AF.Identity))
        lo = {}
        for nm, _, r, _, _ in loras:
            lo[nm] = P.sb([128, (r + 127) // 128, TOK], BF16, name="lo_" + nm)
        with P.scope():
            stg = P.sb([128, 16, 256], name="stg")
            stb = P.sb([128, 16, 256], name="stb")
            for nm, wd, r, murow, fn in loras:
                wa = P.sb([128, 16, r], BF16, name="wa_" + nm)
                wbb = P.sb([128, 16, r], BF16, name="wb_" + nm)
                P.dma("sp", stg[:, :, 0:r], wd.ap().rearrange("(k p) n -> p k n", p=128), writes=[stg])
                for k in range(16):
                    mcol = col("mux", murow * 16 + k) if murow is not None else col("vmu", k)
                    P.op("dve", lambda e, k=k, mcol=mcol, r=r: e.tensor_scalar_mul(stb[:, k, 0:r], stg[:, k, 0:r], mcol), reads=[stg, pk], writes=[stb])
                P.op("dve", lambda e, r=r, wbb=wbb: e.tensor_copy(wbb[:], stb[:, :, 0:r]), reads=[stb], writes=[wbb])
                P.op("dve", lambda e, r=r, wa=wa: e.tensor_tensor(wa[:], stg[:, :, 0:r], stb[:, :, 0:r], ALU.subtract), reads=[stg, stb], writes=[wa])
                for n, (t0, nn) in enumerate(TILES):
                    m0 = t0 - HALO
                    for c0 in range(0, r, 128):
                        cw = min(128, r - c0)
                        ps = pp()
                        for k in range(16):
                            P.op("pe", lambda e, ps=ps, k=k, c0=c0, cw=cw, wa=wa: e.matmul(ps[0:cw, :], wa[:, k, c0:c0 + cw], hT[:, k, t0:t0 + 512], start=(k == 0), stop=False),
                                 reads=[wa] + hkeys, writes=[ps])
                        for k in range(16):
                            P.op("pe", lambda e, ps=ps, k=k, c0=c0, cw=cw, wbb=wbb: e.matmul(ps[0:cw, :], wbb[:, k, c0:c0 + cw], hT[:, k, t0 - 1:t0 - 1 + 512], start=False, stop=(k == 15)),
                                 reads=[wbb] + hkeys, writes=[ps])
                        P.op("act", lambda e, ps=ps, c0=c0, cw=cw, nm=nm, fn=fn: e.activation(lo[nm][0:cw, c0 // 128, m0:m0 + 512], ps[0:cw, :], fn),
                             reads=[ps], writes=[("lo", nm, n)])
        w2s = P.sb([96, 512], BF16, name="w2s"); a2s = P.sb([96, 512], BF16, name="a2s")
        g2s = P.sb([128, 2, 512], BF16, name="g2s")
        P.dma("pool", w2s[:], w2_d.ap(), writes=[w2s])
        P.dma("pool", a2s[:], a2_d.ap(), writes=[a2s])
        P.dma("pool", g2s[:], g2_d.ap().rearrange("(k p) n -> p k n", p=128), writes=[g2s])
        if l > 0:
            v2s = P.sb([64, 512], BF16, name="v2s")
            P.dma("pool", v2s[:], v2_d.ap(), writes=[v2s])
        wk = P.pool("wkr", 30, [128, 512])

        def fm_out(name, src, j, m0):
            P.dma(dq(), o_rfm[name].ap()[j * 128:(j + 1) * 128, m0:m0 + 512], src[:], reads=[src], writes=["o_rfm_" + name])

        tcount = [0]

        def tm_out(name, src, j, m0):
            pt = pp()
            for s in range(4):
                P.op("pe", lambda e, pt=pt, s=s: e.transpose(pt[:, s * 128:(s + 1) * 128], src[:, s * 128:(s + 1) * 128], ident[:]),
                     reads=[src, ident], writes=[pt])
            ob = wk()
            tcount[0] += 1
            evac(ob[:], pt[:], tcount[0], [pt], [ob])
            P.dma(dq(), o_rtm[name].ap()[m0:m0 + 512, j * 128:(j + 1) * 128].rearrange("(s p) c -> p s c", p=128),
                  ob[:].rearrange("p (s c) -> p s c", s=4), reads=[ob], writes=["o_rtm_" + name])

        for j in range(4):
            wr_ = load_wblock(win_d.ap()[:, 1728 + j * 128:1728 + (j + 1) * 128])
            wk_ = load_wblock(win_d.ap()[:, 2240 + j * 128:2240 + (j + 1) * 128])
            wv_ = load_wblock(win_d.ap()[:, 2752 + j * 128:2752 + (j + 1) * 128])
            for n, (t0, nn) in enumerate(TILES):
                m0 = t0 - HALO
                mixed = []
                for idx, w_ in enumerate((wr_, wk_, wv_)):
                    ps_a = proj(w_, 128, t0, 512)
                    ps_s = proj(w_, 128, t0, 512, shift=1)
                    base = wk(); dd = wk(); mx = wk()
                    P.op("act", lambda e, base=base, ps_a=ps_a: e.activation(base[:], ps_a[:], AF.Copy), reads=[ps_a], writes=[base])
                    P.op("dve", lambda e, dd=dd, ps_s=ps_s, base=base: e.tensor_tensor(dd[:], ps_s[:], base[:], ALU.subtract), reads=[ps_s, base], writes=[dd])
                    P.op("dve", lambda e, dd=dd, mx=mx, base=base, idx=idx: e.scalar_tensor_tensor(mx[:], dd[:], col("murkv", idx * 4 + j), base[:], ALU.mult, ALU.add),
                         reads=[dd, base, pk], writes=[mx])
                    mixed.append(mx)
                rp, kp, vp = mixed
                ps = pp()
                P.op("pe", lambda e, ps=ps: e.matmul(ps[:], w2s[:, j * 128:(j + 1) * 128], lo["w"][0:96, 0, m0:m0 + 512], start=True, stop=True),
                     reads=[w2s, ("lo", "w", n)], writes=[ps])
                dec = wk()
                P.op("act", lambda e, ps=ps, dec=dec: e.activation(dec[:], ps[:], AF.Sigmoid, bias=col("w0", j)), reads=[ps, pk], writes=[dec])
                P.op("act", lambda e, dec=dec: e.activation(dec[:], dec[:], AF.Exp, scale=-float(np.exp(-0.5))), reads=[dec], writes=[dec])
                ps = pp()
                P.op("pe", lambda e, ps=ps: e.matmul(ps[:], a2s[:, j * 128:(j + 1) * 128], lo["a"][0:96, 0, m0:m0 + 512], start=True, stop=True),
                     reads=[a2s, ("lo", "a", n)], writes=[ps])
                at = wk()
                P.op("act", lambda e, ps=ps, at=at: e.activation(at[:], ps[:], AF.Sigmoid, bias=col("a0", j)), reads=[ps, pk], writes=[at])
                ps = pp()
                for k in range(2):
                    P.op("pe", lambda e, ps=ps, k=k: e.matmul(ps[:], g2s[:, k, j * 128:(j + 1) * 128], lo["g"][:, k, m0:m0 + 512], start=(k == 0), stop=(k == 1)),
                         reads=[g2s, ("lo", "g", n)], writes=[ps])
                gt = wk()
                P.op("dve", lambda e, ps=ps, gt=gt: e.tensor_copy(gt[:], ps[:]), reads=[ps], writes=[gt])
                fm_out("g", gt, j, m0)
                if l > 0:
                    ps = pp()
                    P.op("pe", lambda e, ps=ps: e.matmul(ps[:], v2s[:, j * 128:(j + 1) * 128], lo["v"][0:64, 0, m0:m0 + 512], start=True, stop=True),
                         reads=[v2s, ("lo", "v", n)], writes=[ps])
                    vg = wk(); vf = wk(); vpp = wk()
                    P.op("act", lambda e, ps=ps, vg=vg: e.activation(vg[:], ps[:], AF.Sigmoid, bias=col("v0", j)), reads=[ps, pk], writes=[vg])
                    P.dma(dq(), vf[:], vf_d.ap()[j * 128:(j + 1) * 128, m0:m0 + 512], writes=[vf])
                    P.op("dve", lambda e, vf=vf, vp=vp: e.tensor_tensor(vf[:], vf[:], vp[:], ALU.subtract), reads=[vf, vp], writes=[vf])
                    P.op("dve", lambda e, vf=vf, vg=vg: e.tensor_tensor(vf[:], vf[:], vg[:], ALU.mult), reads=[vf, vg], writes=[vf])
                    P.op("dve", lambda e, vf=vf, vp=vp, vpp=vpp: e.tensor_tensor(vpp[:], vf[:], vp[:], ALU.add), reads=[vf, vp], writes=[vpp])
                else:
                    vpp = vp
                    fm_out("vfirst", vp, j, m0)
                fm_out("v", vpp, j, m0)
                kkr = wk(); sq = wk(); nrm = wk(); kkn = wk()
                P.op("dve", lambda e, kkr=kkr, kp=kp: e.tensor_scalar_mul(kkr[:], kp[:], col("kk", j)), reads=[kp, pk], writes=[kkr])
                P.op("act", lambda e, kkr=kkr, sq=sq: e.activation(sq[:], kkr[:], AF.Square), reads=[kkr], writes=[sq])
                ps = pp()
                P.op("pe", lambda e, ps=ps, sq=sq: e.matmul(ps[:], bones[:], sq[:], start=True, stop=True), reads=[bones, sq], writes=[ps])
                P.op("act", lambda e, ps=ps, nrm=nrm: e.activation(nrm[:], ps[:], AF.Sqrt), reads=[ps], writes=[nrm])
                P.op("dve", lambda e, nrm=nrm: e.tensor_scalar_max(nrm[:], nrm[:], 1e-12), reads=[nrm], writes=[nrm])
                P.op("dve", lambda e, nrm=nrm: e.reciprocal(nrm[:], nrm[:]), reads=[nrm], writes=[nrm])
                P.op("dve", lambda e, nrm=nrm, kkr=kkr, kkn=kkn: e.tensor_tensor(kkn[:], kkr[:], nrm[:], ALU.mult), reads=[kkr, nrm], writes=[kkn])
                tt = wk(); k2 = wk(); kka = wk()
                P.op("dve", lambda e, tt=tt, at=at: e.tensor_scalar(tt[:], at[:], -1.0, col("ka", j), ALU.add, ALU.mult), reads=[at, pk], writes=[tt])
                P.op("dve", lambda e, tt=tt, k2=k2, kp=kp: e.scalar_tensor_tensor(k2[:], tt[:], 1.0, kp[:], ALU.add, ALU.mult), reads=[tt, kp], writes=[k2])
                P.op("dve", lambda e, kka=kka, kkn=kkn, at=at: e.tensor_tensor(kka[:], kkn[:], at[:], ALU.mult), reads=[kkn, at], writes=[kka])
                rk = wk(); bon = wk()
                P.op("dve", lambda e, rk=rk, rp=rp, k2=k2: e.scalar_tensor_tensor(rk[:], rp[:], col("rk", j), k2[:], ALU.mult, ALU.mult), reads=[rp, k2, pk], writes=[rk])
                ps = pp()
                P.op("pe", lambda e, ps=ps, rk=rk: e.matmul(ps[:], bones[:], rk[:], start=True, stop=True), reads=[bones, rk], writes=[ps])
                P.op("dve", lambda e, ps=ps, bon=bon, vpp=vpp: e.tensor_tensor(bon[:], ps[:], vpp[:], ALU.mult), reads=[ps, vpp], writes=[bon])
                fm_out("bonus", bon, j, m0)
                for name, src in (("r", rp), ("w", dec), ("k", k2), ("kk", kkn), ("kka", kka)):
                    tm_out(name, src, j, m0)

    nc = P.finish(finals)
    return nc, P


TC = 8
NEG = -30000.0


def band_bias(rel_table_h):
    q = np.arange(128)
    a, c = q // 64, q % 64
    kk = np.arange(640)
    u, kq = kk // 64, kk % 64
    w = 64 * (u[None, :] - a[:, None]) + kq[None, :]
    valid = np.where(a[:, None] == 0, u[None, :] <= 8, u[None, :] >= 1)
    rel = np.clip((512 + c[:, None]) - w, -256, 256) + 256
    out = rel_table_h[np.clip(rel, 0, 512)].astype(np.float32)
    return np.where(valid, out, np.float32(NEG)).astype(np.float32)


def build_B(l):
    P = Prog()
    mq_d = P.din("mla_qT", [2, 192, SEQ], BF16)
    mk_d = P.din("mla_kT", [2, 128, SEQ], BF16)
    mkpe_d = P.din("mla_kpeT", [64, SEQ], BF16)
    mv_d = P.din("mla_v", [SEQ, 256], BF16)
    cq_d = P.din("ca_qT", [2, 128, SEQ], BF16)
    ck_d = P.din("ca_kT", [2, 128, SEQ], BF16)
    cv_d = P.din("ca_v", [SEQ, 256], BF16)
    bias_d = P.din("ca_bias", [2, 128, 640])
    ident_d = P.din("ident", [128, 128])
    rwbc_d = P.din("rw_bc", [2, SEQ, 640])
    rv_d = P.din("rw_vT", [256, SEQ])
    rb_d = P.din("rw_bonusT", [256, SEQ])
    rg_d = P.din("rw_gT", [256, SEQ])
    gn_d = P.din("rw_gn", [128, 4])
    o_mla = P.dout("y_mlaT", [256, SEQ], BF16)
    o_ca = P.dout("y_caT", [256, SEQ], BF16)
    o_rw = P.dout("y_rwT", [256, SEQ], BF16)

    ident = P.sb([128, 128], name="ident")
    identb = P.sb([128, 128], BF16, name="identb")
    bones = P.sb([128, 128], name="bones")
    epsc = P.sb([128, 2], name="epsc")
    gn = P.sb([128, 4], name="gn")
    bias = P.sb([128, 2, 640], name="bias")
    qn = P.sb([128, SEQ], BF16, name="qn")
    qp = P.sb([64, SEQ], BF16, name="qp")
    kn = P.sb([128, SEQ], BF16, name="kn")
    kp = P.sb([64, SEQ], BF16, name="kp")
    vts = [P.sb([128, 32, 128], BF16, name="vt%d" % i) for i in range(2)]
    scb = [P.sb([128, SEQ], name="sc%d" % i) for i in range(2)]
    pb = [P.sb([128, SEQ], BF16, name="pb%d" % i) for i in range(2)]
    PT = P.pool("PT", 3, [128, 5, 128], BF16)
    outb = P.sb([128, SEQ], BF16, name="outb")
    stt_ = P.pool("stt", 6, [128, 4])
    dg = P.pool("dg", 3, [128, 128], BF16)
    psc = P.pool("psc", 3, [128, 512], psum=True)
    ppt = P.pool("ppt", 2, [128, 512], psum=True)
    pot = P.ps([128, 512], name="pot")
    Ss = [P.sb([128, 128], name="S%d" % i) for i in range(2)]
    tmps = [P.sb([128, 128], name="tmp%d" % i) for i in range(4)]
    nsk = P.pool("nsk", 8, [128, 2])
    bc = [P.sb([128, TC, 2, 5, 64], name="bc%d" % i) for i in range(2)]
    vsb = [P.sb([128, 2, 512], name="vsb%d" % i) for i in range(2)]
    ysb = P.sb([128, 2, SEQ], name="ysb")

    P.dma("act", ident[:], ident_d.ap(), writes=[ident])
    P.dma("act", gn[:], gn_d.ap(), writes=[gn])
    P.dma("act", bias[:], bias_d.ap().rearrange("h q k -> q h k"), writes=[bias])
    P.dma("act", kp[:], mkpe_d.ap(), writes=[kp])
    P.op("act", lambda e: e.activation(identb[:], ident[:], AF.Copy), reads=[ident], writes=[identb])
    P.op("pool", lambda e: e.memset(epsc[:, 0:1], 64e-5), writes=[epsc])
    P.op("pool", lambda e: e.memset(bones[:], 0.0), writes=[bones])
    P.op("pool", lambda e: e.memset(bones[0:64, 0:64], 1.0), reads=[bones], writes=[bones])
    P.op("pool", lambda e: e.memset(bones[64:128, 64:128], 1.0), reads=[bones], writes=[bones])
    P.op("dve", lambda e: e.memset(Ss[0][:], 0.0), writes=[(Ss[0], 0), (Ss[0], 1)])

    dqc = [0]

    def dq():
        dqc[0] += 1
        return ("act", "pool")[dqc[0] % 2]

    units = []
    otc = [0]

    def make_unit(kind, hh, i):
        ub = len(units) % 2
        sc = scb[ub]; pbuf = pb[ub]
        vt = vts[(len(units) // 32) % 2]
        st = {}
        if kind == "mla":
            kt0 = 0
            scale = 192.0 ** -0.5
        else:
            kt0 = max(0, i - 4)
            scale = 128.0 ** -0.5
        nkt = i - kt0 + 1
        nk = nkt * 128

        def stageA():
            if i == 0:
                if kind == "mla":
                    P.dma(dq(), qn[:], mq_d.ap()[hh, 0:128, :], writes=[qn])
                    P.dma(dq(), qp[:], mq_d.ap()[hh, 128:192, :], writes=[qp])
                    P.dma(dq(), kn[:], mk_d.ap()[hh], writes=[kn])
                    P.dma(dq(), vt[:], mv_d.ap()[:, hh * 128:(hh + 1) * 128].rearrange("(n p) d -> p n d", p=128), writes=[vt])
                else:
                    P.dma(dq(), qn[:], cq_d.ap()[hh], writes=[qn])
                    P.dma(dq(), kn[:], ck_d.ap()[hh], writes=[kn])
                    P.dma(dq(), vt[:], cv_d.ap()[:, hh * 128:(hh + 1) * 128].rearrange("(n p) d -> p n d", p=128), writes=[vt])
            qs = slice(i * 128, (i + 1) * 128)
            c0 = 0
            while c0 < nk:
                n_c = min(512, nk - c0)
                ps = psc()
                k0 = kt0 * 128 + c0
                P.op("pe", lambda e: e.matmul(ps[:, 0:n_c], qn[:, qs], kn[:, k0:k0 + n_c], start=True, stop=(kind != "mla")),
                     reads=[qn, kn], writes=[ps])
                if kind == "mla":
                    P.op("pe", lambda e: e.matmul(ps[:, 0:n_c], qp[:, qs], kp[:, k0:k0 + n_c], start=False, stop=True),
                         reads=[qp, kp], writes=[ps])
                    P.op("act", lambda e: e.activation(sc[:, c0:c0 + n_c], ps[:, 0:n_c], AF.Copy), reads=[ps], writes=[sc])
                else:
                    P.op("act", lambda e: e.activation(sc[:, c0:c0 + n_c], ps[:, 0:n_c], AF.Copy, scale=scale), reads=[ps], writes=[sc])
                c0 += n_c
            if kind == "mla":
                P.op("pool", lambda e: e.memset(sc[0:64, nk - 64:nk], NEG / scale), reads=[sc], writes=[sc])
            else:
                p0 = (kt0 - (i - 4)) * 128
                P.op("pool", lambda e: e.tensor_tensor(sc[:, 0:nk], sc[:, 0:nk], bias[:, hh, p0:p0 + nk], ALU.add), reads=[sc, bias], writes=[sc])

        def stageB():
            s4 = stt_()
            st["s4"] = s4
            P.op("dve", lambda e: e.tensor_reduce(s4[:, 0:1], sc[:, 0:nk], AX.X, ALU.max), reads=[sc], writes=[s4])
            esc = scale if kind == "mla" else 1.0
            P.op("dve", lambda e: e.tensor_scalar_mul(s4[:, 1:2], s4[:, 0:1], -esc), reads=[s4], writes=[s4])
            P.op("act", lambda e: e.activation(pbuf[:, 0:nk], sc[:, 0:nk], AF.Exp, bias=s4[:, 1:2], scale=esc, accum_out=s4[:, 2:3]),
                 reads=[sc, s4], writes=[pbuf, s4])

        def stageC():
            s4 = st["s4"]
            P.op("dve", lambda e: e.reciprocal(s4[:, 3:4], s4[:, 2:3]), reads=[s4], writes=[s4])
            d = dg()
            P.op("act", lambda e: e.activation(d[:], identb[:], AF.Copy, scale=s4[:, 3:4]), reads=[identb, s4], writes=[d])
            oc = (otc[0] % 4) * 128
            otc[0] += 1
            okey = ("pot", oc)
            for g0 in range(0, nkt, 4):
                gn_ = min(4, nkt - g0)
                pt = ppt()
                for t in range(gn_):
                    kt = g0 + t
                    P.op("pe", lambda e: e.matmul(pt[:, t * 128:(t + 1) * 128], pbuf[:, kt * 128:(kt + 1) * 128], d[:], start=True, stop=True),
                         reads=[pbuf, d], writes=[pt])
                ptb = PT()
                P.op("act", lambda e: e.activation(ptb[:, 0:gn_, :], pt[:, 0:gn_ * 128].rearrange("p (t k) -> p t k", t=gn_), AF.Copy),
                     reads=[pt], writes=[ptb])
                for t in range(gn_):
                    kt = g0 + t
                    P.op("pe", lambda e: e.matmul(pot[:, oc:oc + 128], vt[:, kt0 + kt, :], ptb[:, t, :], start=(kt == 0), stop=(kt == nkt - 1)),
                         reads=[vt, ptb], writes=[okey])
            P.op("act", lambda e: e.activation(outb[:, i * 128:(i + 1) * 128], pot[:, oc:oc + 128], AF.Copy), reads=[okey], writes=[outb])
            if i == 31:
                od = o_mla if kind == "mla" else o_ca
                P.dma(dq(), od.ap()[hh * 128:(hh + 1) * 128, :], outb[:], reads=[outb], writes=["o_" + kind])
        units.append((stageA, stageB, stageC))

    for kind in ("mla", "ca"):
        for hh in range(2):
            for i in range(32):
                make_unit(kind, hh, i)


    def scan_chunk(c):
        t0 = c * TC
        b = bc[c % 2]
        bkey = ("bc", c % 2)
        for half in range(2):
            P.dma("sp", b[half * 64:(half + 1) * 64].rearrange("p t g a k -> p t (g a k)"),
                  rwbc_d.ap()[half, t0:t0 + TC, :].partition_broadcast(64), writes=[bkey])
        if t0 % 512 == 0:
            vb = vsb[(t0 // 512) % 2]
            P.dma("sp", vb[:], rv_d.ap()[:, t0:t0 + 512].rearrange("(g p) t -> p g t", p=128), writes=[vb])
        vb = vsb[(t0 // 512) % 2]
        for tt in range(TC):
            t = t0 + tt
            ns = nsk()
            So = Ss[t % 2]; Sn = Ss[(t + 1) % 2]
            Sog = So[:].rearrange("p (g k) -> p g k", g=2)
            Sng = Sn[:].rearrange("p (g k) -> p g k", g=2)
            tm1 = tmps[(2 * t) % 4]; tm2 = tmps[(2 * t + 1) % 4]
            tm1g = tm1[:].rearrange("p (g k) -> p g k", g=2)
            tm2g = tm2[:].rearrange("p (g k) -> p g k", g=2)
            P.op("dve", lambda e: e.tensor_tensor(tm1g, Sog, b[:, tt, :, 0, :], ALU.mult), reads=[(So, 0), (So, 1), bkey], writes=[tm1])
            P.op("dve", lambda e: e.tensor_tensor(Sng, Sog, b[:, tt, :, 1, :], ALU.mult), reads=[(So, 0), (So, 1), bkey], writes=[(Sn, 0), (Sn, 1)])
            P.op("dve", lambda e: e.tensor_reduce(ns[:], tm1g, AX.X, ALU.add, negate=True), reads=[tm1], writes=[ns])
            for g in range(2):
                P.op("dve", lambda e: e.scalar_tensor_tensor(Sn[:, g * 64:(g + 1) * 64], b[:, tt, g, 3, :], vb[:, g, (t % 512):(t % 512) + 1],
                                                            Sn[:, g * 64:(g + 1) * 64], ALU.mult, ALU.add), reads=[(Sn, g), bkey, vb], writes=[(Sn, g)])
            for g in range(2):
                P.op("dve", lambda e: e.scalar_tensor_tensor(Sn[:, g * 64:(g + 1) * 64], b[:, tt, g, 2, :], ns[:, g:g + 1],
                                                            Sn[:, g * 64:(g + 1) * 64], ALU.mult, ALU.add), reads=[(Sn, g), bkey, ns], writes=[(Sn, g)])
            P.op("dve", lambda e: e.tensor_tensor(tm2g, Sng, b[:, tt, :, 4, :], ALU.mult), reads=[(Sn, 0), (Sn, 1), bkey], writes=[tm2])
            P.op("dve", lambda e: e.tensor_reduce(ysb[:, :, t], tm2g, AX.X, ALU.add), reads=[tm2], writes=[("ysb", t // 512)])

    NCH = SEQ // TC
    per = NCH // len(units)
    for c in range(NCH):
        u = c // per if c % per == 0 else None
        if u is not None and u < len(units):
            units[u][0]()
        scan_chunk(c)
        if u is not None and u < len(units):
            units[u][1]()
            if u >= 1:
                units[u - 1][2]()
    units[-1][2]()

    wkv = [scb[0][:, i * 512:(i + 1) * 512] for i in range(8)] + [scb[1][:, i * 512:(i + 1) * 512] for i in range(8)]
    wi = [0]

    def wk():
        wi[0] += 1
        j = wi[0] % 16
        return wkv[j], ("wkv", j)

    first = [True]
    for g in range(2):
        for n in range(8):
            m0 = n * 512
            extra = ([scb[0], scb[1]] + [("wkv", j) for j in range(16)]) if first[0] else []
            first[0] = False
            sq, sqk = wk()
            P.op("act", lambda e: e.activation(sq, ysb[:, g, m0:m0 + 512], AF.Square), reads=[("ysb", n)], writes=[sqk] + extra)
            ps1 = psc(); ps2 = psc()
            P.op("pe", lambda e: e.matmul(ps1[:], bones[:], ysb[:, g, m0:m0 + 512], start=True, stop=True), reads=[bones, ("ysb", n)], writes=[ps1])
            P.op("pe", lambda e: e.matmul(ps2[:], bones[:], sq, start=True, stop=True), reads=[bones, sqk], writes=[ps2])
            mean, mk_ = wk(); msq, msk = wk(); rstd, rsk = wk(); yn, ynk = wk(); bt, btk = wk(); gt, gtk = wk()
            P.op("act", lambda e: e.activation(mean, ps1[:], AF.Copy, scale=1.0 / 64), reads=[ps1], writes=[mk_])
            P.op("dve", lambda e: e.tensor_tensor(msq, mean, mean, ALU.mult), reads=[mk_], writes=[msk])
            P.op("dve", lambda e: e.scalar_tensor_tensor(rstd, ps2[:], 1.0 / 64, msq, ALU.mult, ALU.subtract), reads=[ps2, msk], writes=[rsk])
            P.op("act", lambda e: e.activation(rstd, rstd, AF.Sqrt, bias=epsc[:, 0:1]), reads=[rsk, epsc], writes=[rsk])
            P.op("dve", lambda e: e.reciprocal(rstd, rstd), reads=[rsk], writes=[rsk])
            P.op("dve", lambda e: e.tensor_tensor(yn, ysb[:, g, m0:m0 + 512], mean, ALU.subtract), reads=[("ysb", n), mk_], writes=[ynk])
            P.op("dve", lambda e: e.tensor_tensor(yn, yn, rstd, ALU.mult), reads=[ynk, rsk], writes=[ynk])
            P.op("dve", lambda e: e.tensor_scalar(yn, yn, gn[:, g:g + 1], gn[:, 2 + g:3 + g], ALU.mult, ALU.add), reads=[ynk, gn], writes=[ynk])
            P.dma(dq(), bt, rb_d.ap()[g * 128:(g + 1) * 128, m0:m0 + 512], writes=[btk])
            P.dma(dq(), gt, rg_d.ap()[g * 128:(g + 1) * 128, m0:m0 + 512], writes=[gtk])
            P.op("dve", lambda e: e.tensor_tensor(yn, yn, bt, ALU.add), reads=[ynk, btk], writes=[ynk])
            ob = PT()
            obv = ob[:].rearrange("p a b -> p (a b)")[:, 0:512]
            P.op("dve", lambda e: e.tensor_tensor(obv, yn, gt, ALU.mult), reads=[ynk, gtk], writes=[ob])
            P.dma(dq(), o_rw.ap()[g * 128:(g + 1) * 128, m0:m0 + 512], obv, reads=[ob], writes=["o_rw"])
    nc = P.finish(["o_mla", "o_ca", "o_rw"])
    return nc, P


def inputs_B(inp, l, c, A):
    b, g = c // 2, c % 2
    cat = lambda name, ax: np.concatenate([A[2 * b][name], A[2 * b + 1][name]], axis=ax)
    m = {"mla_qT": np.ascontiguousarray(cat("mla_qT", 2)[2 * g:2 * g + 2]),
         "mla_kT": np.ascontiguousarray(cat("mla_kT", 2)[2 * g:2 * g + 2]),
         "mla_kpeT": cat("mla_kpeT", 1),
         "mla_v": np.ascontiguousarray(cat("mla_v", 0)[:, 256 * g:256 * (g + 1)]),
         "ca_qT": np.ascontiguousarray(cat("ca_qT", 1)[256 * g:256 * (g + 1)].reshape(2, 128, SEQ)),
         "ca_kT": np.ascontiguousarray(cat("ca_kT", 1)[256 * g:256 * (g + 1)].reshape(2, 128, SEQ)),
         "ca_v": np.ascontiguousarray(cat("ca_v", 0)[:, 256 * g:256 * (g + 1)]),
         "ca_bias": np.stack([band_bias(inp["ca_rel_bias"][l][2 * g + hh]) for hh in range(2)]),
         "ident": np.eye(128, dtype=np.float32),
         "rw_vT": np.ascontiguousarray(cat("rw_vT", 1)[256 * g:256 * (g + 1)]),
         "rw_bonusT": np.ascontiguousarray(cat("rw_bonusT", 1)[256 * g:256 * (g + 1)]),
         "rw_gT": np.ascontiguousarray(cat("rw_gT", 1)[256 * g:256 * (g + 1)]),
         "rw_gn": np.concatenate([fm(inp["rw_gn_g"][l][256 * g:256 * (g + 1)]), fm(inp["rw_gn_b"][l][256 * g:256 * (g + 1)])], 1)}
    arrs = [cat("rw_%s_tm" % n, 0)[:, 256 * g:256 * (g + 1)].reshape(SEQ, 2, 2, 64) for n in ("kk", "w", "kka", "k", "r")]
    st = np.stack(arrs, 0)
    m["rw_bc"] = np.ascontiguousarray(np.transpose(st, (3, 1, 2, 0, 4))).reshape(2, SEQ, 640)
    return m


def build_M():
    P = Prog()
    ct_d = P.din("cT4", [128, 64])
    mw_d = P.din("mod_w_s", [D, 1536])
    mb_d = P.din("mod_b_s", [128, 12])
    o = P.dout("modT", [128, 48])
    sct = P.sb([128, 64], name="sct")
    mb = P.sb([128, 12], name="mb")
    res = P.sb([128, 12, 4], name="res")
    blk = [P.sb([128, 16, 128], name="blk%d" % i) for i in range(3)]
    ps = P.ps([128, 48], name="pm")
    P.dma("sp", sct[:], ct_d.ap(), writes=[sct])
    P.dma("sp", mb[:], mb_d.ap(), writes=[mb])
    sct0 = sct
    sct = P.sb([128, 64], name="sct2")
    P.op("act", lambda e: e.activation(sct[:], sct0[:], AF.Silu), reads=[sct0], writes=[sct])
    for j in range(12):
        b = blk[j % 3]
        P.dma(("sp", "act")[j % 2], b[:], mw_d.ap()[:, j * 128:(j + 1) * 128].rearrange("(k p) n -> p k n", p=128), writes=[b])
        for k in range(16):
            P.op("pe", lambda e: e.matmul(ps[:, j * 4:(j + 1) * 4], b[:, k, :], sct[:, k * 4:(k + 1) * 4], start=(k == 0), stop=(k == 15)),
                 reads=[b, sct], writes=[("pm", j)])
    for j in range(12):
        P.op("dve", lambda e: e.tensor_scalar_add(res[:, j, :], ps[:, j * 4:(j + 1) * 4], mb[:, j:j + 1]), reads=[("pm", jj) for jj in range(12)] + [mb], writes=[res])
    P.dma("sp", o.ap(), res[:].rearrange("p j b -> p (j b)"), reads=[res], writes=["o"])
    return P.finish(["o"]), P


def ln_fm(P, z, zkey, n, ones, epsc_col, wk, pp, inv_d, stp):
    ps1 = pp(); ps2 = pp()
    for k in range(16):
        sq = wk()
        P.op("act", lambda e: e.activation(sq[:, 0:n], z[:, k, 0:n], AF.Square), reads=[zkey], writes=[sq])
        P.op("pe", lambda e: e.matmul(ps1[:, 0:n], ones[:], z[:, k, 0:n], start=(k == 0), stop=(k == 15)), reads=[ones, zkey], writes=[ps1])
        P.op("pe", lambda e: e.matmul(ps2[:, 0:n], ones[:], sq[:, 0:n], start=(k == 0), stop=(k == 15)), reads=[ones, sq], writes=[ps2])
    mean = stp(); msq = stp(); rstd = stp()
    P.op("act", lambda e: e.activation(mean[:, 0:n], ps1[:, 0:n], AF.Copy, scale=inv_d), reads=[ps1], writes=[mean])
    P.op("dve", lambda e: e.tensor_tensor(msq[:, 0:n], mean[:, 0:n], mean[:, 0:n], ALU.mult), reads=[mean], writes=[msq])
    P.op("dve", lambda e: e.scalar_tensor_tensor(rstd[:, 0:n], ps2[:, 0:n], inv_d, msq[:, 0:n], ALU.mult, ALU.subtract), reads=[ps2, msq], writes=[rstd])
    P.op("act", lambda e: e.activation(rstd[:, 0:n], rstd[:, 0:n], AF.Sqrt, bias=epsc_col), reads=[rstd], writes=[rstd])
    P.op("dve", lambda e: e.reciprocal(rstd[:, 0:n], rstd[:, 0:n]), reads=[rstd], writes=[rstd])
    return mean, rstd


PKC = {}
_o = 0
for _n, _w in (("modsh", 96), ("modoff", 96), ("lng", 16), ("lnb", 16), ("rb", 1)):
    PKC[_n] = (_o, _w)
    _o += _w
NPKC = _o


def pack_C(inp, l, which, modsh):
    pk = np.zeros((128, NPKC), np.float32)
    pk[:, 0:96] = modsh
    pk[:, 96:192] = fm(inp["mod_offset"][l])
    pk[:, 192:208] = fm(inp["ln_post_g"][l, which])
    pk[:, 208:224] = fm(inp["ln_post_b"][l, which])
    pk[0:32, 224] = inp["moe_router_b"][l]
    return pk


def build_C():
    P = Prog()
    yc_d = P.din("ycatT", [D, TOK], BF16)
    x_d = P.din("x", [TOK, D])
    wo_d = P.din("w_out", [D, D])
    pk_d = P.din("pk", [128, NPKC])
    rw_d = P.din("router_w", [D, 32])
    ident_d = P.din("ident", [128, 128])
    o_x1 = P.dout("x1T", [D, TOK])
    o_h2 = P.dout("h2T", [D, TOK], BF16)
    o_g = P.dout("gateT", [32, TOK])

    pk = P.sb([128, NPKC], name="pk")
    ident = P.sb([128, 128], name="ident")
    ones = P.sb([128, 128], name="ones")
    epsc = P.sb([128, 1], name="epsc")
    modp = P.sb([128, 96], name="modp")
    g1p = P.sb([128, 16], name="g1p")
    sc2p = P.sb([128, 16], name="sc2p")
    rw = P.sb([128, 16, 32], name="rw")
    z = P.sb([128, 16, 512], name="z")
    ycT = P.sb([128, 16, 512], BF16, name="ycT")
    h2b = P.sb([128, 16, 512], BF16, name="h2b")
    xt = P.pool("xt", 2, [128, D])
    wb = P.pool("wb", 4, [128, 16, 128], BF16)
    wk = P.pool("wk", 8, [128, 512])
    stp = P.pool("stp", 6, [128, 512])
    pp = P.pool("pp", 6, [128, 512], psum=True)
    plg = P.ps([32, 512], name="plg")
    ptk = P.ps([128, 512], name="ptk")
    lgs = P.sb([32, 512], name="lgs")
    lt = P.sb([128, 4, 32], name="lt")
    gt = P.sb([128, 4, 32], name="gt")
    ex = P.sb([128, 4, 32], name="ex")
    m8 = P.sb([128, 4, 8], name="m8")
    sm = P.sb([128, 4, 4], name="sm")
    gT = P.sb([32, 512], name="gT")

    def col(n, j=0, w=1):
        o, _ = PKC[n]
        return pk[:, o + j:o + j + w]

    P.dma("sp", pk[:], pk_d.ap(), writes=[pk])
    P.dma("sp", ident[:], ident_d.ap(), writes=[ident])
    P.dma("sp", rw[:], rw_d.ap().rearrange("(k p) n -> p k n", p=128), writes=[rw])
    P.op("pool", lambda e: e.memset(ones[:], 1.0), writes=[ones])
    P.op("pool", lambda e: e.memset(epsc[:], LN_EPS), writes=[epsc])
    P.op("dve", lambda e: e.tensor_tensor(modp[:], col("modsh", 0, 96), col("modoff", 0, 96), ALU.add), reads=[pk], writes=[modp])
    P.op("dve", lambda e: e.tensor_scalar_add(g1p[:], modp[:, 32:48], 1.0), reads=[modp], writes=[g1p])
    P.op("dve", lambda e: e.tensor_scalar_add(sc2p[:], modp[:, 64:80], 1.0), reads=[modp], writes=[sc2p])
    dqc = [0]

    def dq():
        dqc[0] += 1
        return ("sp", "act")[dqc[0] % 2]

    for n in range(4):
        m0 = n * 512
        P.dma(dq(), ycT[:], yc_d.ap()[:, m0:m0 + 512].rearrange("(k p) t -> p k t", p=128), writes=[ycT])
        for s in range(4):
            xx = xt()
            P.dma(dq(), xx[:], x_d.ap()[m0 + s * 128:m0 + (s + 1) * 128, :], writes=[xx])
            for g in range(4):
                pt = pp()
                for q in range(4):
                    k = g * 4 + q
                    P.op("pe", lambda e: e.transpose(pt[:, q * 128:(q + 1) * 128], xx[:, k * 128:(k + 1) * 128], ident[:]), reads=[xx, ident], writes=[pt])
                P.op("act", lambda e: e.activation(z[:, g * 4:(g + 1) * 4, s * 128:(s + 1) * 128], pt[:].rearrange("p (q t) -> p q t", q=4), AF.Copy, scale=DN_ALPHA),
                     reads=[pt], writes=["z"])
        for dc in range(16):
            w = wb()
            P.dma("pool", w[:], wo_d.ap()[:, dc * 128:(dc + 1) * 128].rearrange("(k p) n -> p k n", p=128), writes=[w])
            ps = pp()
            for k in range(16):
                P.op("pe", lambda e: e.matmul(ps[:], w[:, k, :], ycT[:, k, :], start=(k == 0), stop=(k == 15)), reads=[w, ycT], writes=[ps])
            P.op("dve", lambda e: e.scalar_tensor_tensor(z[:, dc, :], ps[:], g1p[:, dc:dc + 1], z[:, dc, :], ALU.mult, ALU.add), reads=[ps, g1p, "z"], writes=["z"])
        mean, rstd = ln_fm(P, z, "z", 512, ones, epsc[:, 0:1], wk, pp, 1.0 / D, stp)
        for k in range(16):
            t = wk()
            P.op("dve", lambda e: e.tensor_tensor(t[:], z[:, k, :], mean[:], ALU.subtract), reads=["z", mean], writes=[t])
            P.op("dve", lambda e: e.tensor_tensor(t[:], t[:], rstd[:], ALU.mult), reads=[t, rstd], writes=[t])
            P.op("dve", lambda e: e.tensor_scalar(z[:, k, :], t[:], col("lng", k), col("lnb", k), ALU.mult, ALU.add), reads=[t, pk, "z"], writes=["z"])
        P.dma(dq(), o_x1.ap()[:, m0:m0 + 512].rearrange("(k p) t -> p k t", p=128), z[:], reads=["z"], writes=["o_x1"])
        mean, rstd = ln_fm(P, z, "z", 512, ones, epsc[:, 0:1], wk, pp, 1.0 / D, stp)
        for k in range(16):
            t = wk()
            P.op("dve", lambda e: e.tensor_tensor(t[:], z[:, k, :], mean[:], ALU.subtract), reads=["z", mean], writes=[t])
            P.op("dve", lambda e: e.tensor_tensor(t[:], t[:], rstd[:], ALU.mult), reads=[t, rstd], writes=[t])
            P.op("dve", lambda e: e.tensor_scalar(t[:], t[:], sc2p[:, k:k + 1], modp[:, 48 + k:49 + k], ALU.mult, ALU.add), reads=[t, sc2p, modp], writes=[t])
            P.op("pe", lambda e: e.matmul(plg[:], rw[:, k, :], t[:], start=(k == 0), stop=(k == 15)), reads=[rw, t], writes=[plg])
            P.op("act", lambda e: e.activation(h2b[:, k, :], t[:], AF.Copy), reads=[t], writes=[h2b])
        P.dma(dq(), o_h2.ap()[:, m0:m0 + 512].rearrange("(k p) t -> p k t", p=128), h2b[:], reads=[h2b], writes=["o_h2"])
        P.op("act", lambda e: e.activation(lgs[:], plg[:], AF.Identity, bias=pk[0:32, PKC["rb"][0]:PKC["rb"][0] + 1]), reads=[plg, pk], writes=[lgs])
        for s in range(4):
            P.op("pe", lambda e: e.transpose(ptk[:, s * 32:(s + 1) * 32], lgs[:, s * 128:(s + 1) * 128], ident[0:32, 0:32]), reads=[lgs, ident], writes=[ptk])
        P.op("dve", lambda e: e.tensor_copy(lt[:], ptk[:, 0:128].rearrange("p (s e) -> p s e", s=4)), reads=[ptk], writes=[lt])
        for s in range(4):
            P.op("dve", lambda e: e.max(m8[:, s, :], lt[:, s, :]), reads=[lt], writes=[m8])
        P.op("dve", lambda e: e.tensor_scalar_mul(sm[:, :, 0:1], m8[:, :, 0:1], -1.0), reads=[m8], writes=[sm])
        for s in range(4):
            P.op("act", lambda e: e.activation(ex[:, s, :], lt[:, s, :], AF.Exp, bias=sm[:, s, 0:1]), reads=[lt, sm], writes=[ex])
            P.op("dve", lambda e: e.tensor_scalar(gt[:, s, :], lt[:, s, :], m8[:, s, 3:4], None, ALU.is_ge), reads=[lt, m8], writes=[gt])
            P.op("dve", lambda e: e.tensor_tensor(gt[:, s, :], gt[:, s, :], ex[:, s, :], ALU.mult), reads=[gt, ex], writes=[gt])
            P.op("dve", lambda e: e.tensor_reduce(sm[:, s, 1:2], gt[:, s, :], AX.X, ALU.add), reads=[gt], writes=[sm])
            P.op("dve", lambda e: e.reciprocal(sm[:, s, 2:3], sm[:, s, 1:2]), reads=[sm], writes=[sm])
            P.op("dve", lambda e: e.tensor_scalar_mul(gt[:, s, :], gt[:, s, :], sm[:, s, 2:3]), reads=[gt, sm], writes=[gt])
        for s in range(4):
            P.op("pe", lambda e: e.transpose(ptk[0:32, 128 + s * 128:128 + (s + 1) * 128] if False else plg[:, s * 128:(s + 1) * 128], gt[:, s, :], ident[:]),
                 reads=[gt, ident, lgs], writes=[plg])
        P.op("act", lambda e: e.activation(gT[:], plg[:], AF.Copy), reads=[plg], writes=[gT])
        P.dma(dq(), o_g.ap()[:, m0:m0 + 512], gT[:], reads=[gT], writes=["o_g"])
    return P.finish(["o_x1", "o_h2", "o_g"]), P


NTOK = NB * SEQ
TP = 1024


def build_D():
    P = Prog()
    NS = 256
    h2_d = P.din("h2T_all", [D, NTOK], BF16)
    g_d = P.din("gate4", [4, NTOK])
    w1_d = P.din("w1", [4, D, 2 * D])
    w2_d = P.din("w2", [4, D, D])
    b1_d = P.din("b1T", [128, 4 * 32])
    b2_d = P.din("b2", [4, D])
    cst_d = P.din("cstD", [128, 1024])
    o_p = P.dout("partT", [D, NTOK])

    cst = P.sb([128, 1024], name="cst")
    identb = P.sb([128, 128], BF16, name="identb")
    b1T = P.sb([128, 4, 32], name="b1T")
    hw = P.sb([128, 16 * TP], BF16, name="hw")
    h2v = hw[:].rearrange("p (k t) -> p k t", k=16)
    w1v = [hw[:, i * 4096:(i + 1) * 4096].rearrange("p (k n) -> p k n", k=16) for i in range(2)]
    w2v = [hw[:, 8192 + i * 4096:8192 + (i + 1) * 4096].rearrange("p (k n) -> p k n", k=16) for i in range(2)]
    h2tm = P.sb([128, 8, D], BF16, name="h2tm")
    acc = P.sb([128, 16, TP], name="acc")
    act = P.sb([128, 16, NS], BF16, name="act")
    h2c = P.sb([128, 16, NS], BF16, name="h2c")
    sel = P.sb([128, 8, NS], BF16, name="sel")
    selT = [P.sb([128, TP], name="selT%d" % i) for i in range(2)]
    ysl = P.sb([128, 2, D], name="ysl")
    b2bc = P.sb([128, D], name="b2bc")
    wbc = P.sb([128, TP], name="wbc")
    g4 = P.sb([4, TP], name="g4")
    pos4 = P.sb([4, TP], name="pos4")
    one4 = P.sb([4, TP], name="one4")
    postm = P.sb([128, 8, 4], name="postm")
    wk = P.pool("wk", 6, [128, NS])
    pp = P.pool("pp", 5, [128, 512], psum=True)
    ptb = P.pool("ptb", 2, [128, 1024], BF16, psum=True)
    ppos = P.ps([128, 32], name="ppos")
    iota_row = cst[:, 0:256]
    selk = lambda e_: cst[0:4, 512 + e_ * 128:512 + (e_ + 1) * 128]
    P.dma("sp", cst[:], cst_d.ap(), writes=[cst])
    P.dma("sp", b1T[:], b1_d.ap().rearrange("p (e j) -> p e j", e=4), writes=[b1T])
    P.op("act", lambda e: e.activation(identb[:], cst[:, 384:512], AF.Copy), reads=[cst], writes=[identb])
    P.op("dve", lambda e: e.memset(one4[:], 1.0), writes=[one4])
    HW = [("hw", 0), ("hw", 1), ("hw", 2), ("hw", 3)]
    w1s = P.nc.dram_tensor("w1s", [4, 16, 128, 4096], BF16, kind="Internal")
    w2s = P.nc.dram_tensor("w2s", [4, 8, 128, 4096], BF16, kind="Internal")
    cq = [0]
    for ex_ in range(4):
        for j in range(16):
            i = cq[0] % 4
            cq[0] += 1
            wfl = hw[:, i * 4096:(i + 1) * 4096]
            w = wfl.rearrange("p (k n) -> p k n", k=16)
            P.dma("pool", w[:, :, 0:128], w1_d.ap()[ex_, :, j * 128:(j + 1) * 128].rearrange("(k p) n -> p k n", p=128), writes=[("hw", i)])
            P.dma("pool", w[:, :, 128:256], w1_d.ap()[ex_, :, D + j * 128:D + (j + 1) * 128].rearrange("(k p) n -> p k n", p=128), reads=[("hw", i)], writes=[("hw", i)])
            P.dma(("sp", "act")[i % 2], w1s.ap()[ex_, j], wfl, reads=[("hw", i)], writes=[("w1s", ex_, j)])
        for nb in range(8):
            i = cq[0] % 4
            cq[0] += 1
            wfl = hw[:, i * 4096:(i + 1) * 4096]
            w = wfl.rearrange("p (k n) -> p k n", k=16)
            P.dma("pool", w, w2_d.ap()[ex_, :, nb * 256:(nb + 1) * 256].rearrange("(k p) n -> p k n", p=128), writes=[("hw", i)])
            P.dma(("sp", "act")[i % 2], w2s.ap()[ex_, nb], wfl, reads=[("hw", i)], writes=[("w2s", ex_, nb)])
    wrr = [0]
    for p_ in range(NTOK // TP):
        t0 = p_ * TP
        P.dma("sp", h2v, h2_d.ap()[:, t0:t0 + TP].rearrange("(k p) t -> p k t", p=128), writes=HW)
        P.dma("act", g4[:], g_d.ap()[:, t0:t0 + TP], writes=[g4])
        for blk in range(8):
            for kh in range(2):
                pt = ptb()
                for q in range(8):
                    k = kh * 8 + q
                    P.op("pe", lambda e: e.transpose(pt[:, q * 128:(q + 1) * 128], h2v[:, k, blk * 128:(blk + 1) * 128], identb[:]), reads=HW + [identb], writes=[pt])
                if (blk + kh) % 2 == 0:
                    P.op("act", lambda e: e.activation(h2tm[:, blk, kh * 1024:(kh + 1) * 1024], pt[:], AF.Copy), reads=[pt], writes=[("h2tm", blk)])
                else:
                    P.op("dve", lambda e: e.tensor_copy(h2tm[:, blk, kh * 1024:(kh + 1) * 1024], pt[:]), reads=[pt], writes=[("h2tm", blk)])
        h2k = [("h2tm", blk) for blk in range(8)]
        m4 = g4
        P.op("dve", lambda e: e.tensor_scalar(g4[:], g4[:], 0.0, None, ALU.is_gt), reads=[g4], writes=[g4])
        P.op("dve", lambda e: e.tensor_tensor_scan(pos4[:], one4[:], m4[:], 0.0, ALU.mult, ALU.add), reads=[m4, one4], writes=[pos4])
        P.op("dve", lambda e: e.tensor_tensor(pos4[:], pos4[:], m4[:], ALU.mult), reads=[pos4, m4], writes=[pos4])
        P.op("dve", lambda e: e.tensor_scalar_add(pos4[:], pos4[:], -1.0), reads=[pos4], writes=[pos4])
        for blk in range(8):
            P.op("pe", lambda e: e.transpose(ppos[:, blk * 4:(blk + 1) * 4], pos4[:, blk * 128:(blk + 1) * 128], cst[0:4, 384:388]), reads=[pos4, cst], writes=[ppos])
        P.op("dve", lambda e: e.tensor_copy(postm[:], ppos[:].rearrange("p (b e) -> p b e", b=8)), reads=[ppos], writes=[postm])
        for dc in range(16):
            P.op("pool", lambda e: e.memset(acc[:, dc, :], 0.0), writes=[("acc", dc, 0), ("acc", dc, 1)])
        for ex_ in range(4):
            P.dma("act", wbc[:], g_d.ap()[ex_:ex_ + 1, t0:t0 + TP].partition_broadcast(128), writes=[wbc])
            P.dma("act", b2bc[:], b2_d.ap()[ex_:ex_ + 1, :].partition_broadcast(128), writes=[b2bc])
            for blk in range(8):
                P.op("dve", lambda e: e.tensor_scalar(sel[:, blk, :], iota_row, postm[:, blk, ex_:ex_ + 1], None, ALU.is_equal), reads=[cst, postm], writes=[sel])
            for tt in range(2):
                ps = pp()
                P.op("pe", lambda e: e.matmul(ps[:], selk(ex_), pos4[:, tt * 512:(tt + 1) * 512], start=True, stop=True), reads=[cst, pos4], writes=[ps])
                for st in range(2):
                    P.op("dve", lambda e: e.tensor_scalar(selT[st][:, tt * 512:(tt + 1) * 512], ps[:], cst[:, 256 + st:257 + st], None, ALU.is_equal), reads=[ps, cst], writes=[selT[st]])
            for st in range(2):
                P.op("dve", lambda e: e.tensor_tensor(selT[st][:], selT[st][:], wbc[:], ALU.mult), reads=[selT[st], wbc], writes=[selT[st]])
            for kp in range(8):
                ps = pp()
                for q in range(2):
                    k = kp * 2 + q
                    for blk in range(8):
                        P.op("pe", lambda e: e.matmul(ps[:, q * NS:(q + 1) * NS], h2tm[:, blk, k * 128:(k + 1) * 128], sel[:, blk, :], start=(blk == 0), stop=(blk == 7)),
                             reads=h2k + [sel], writes=[ps])
                if kp % 2 == 0:
                    P.op("act", lambda e: e.activation(h2c[:, kp * 2:kp * 2 + 2, :], ps[:].rearrange("p (q s) -> p q s", q=2), AF.Copy), reads=[ps], writes=[h2c])
                else:
                    P.op("dve", lambda e: e.tensor_copy(h2c[:, kp * 2:kp * 2 + 2, :], ps[:].rearrange("p (q s) -> p q s", q=2)), reads=[ps], writes=[h2c])
            for j in range(16):
                i = wrr[0] % 4
                wrr[0] += 1
                w = hw[:, i * 4096:(i + 1) * 4096].rearrange("p (k n) -> p k n", k=16)
                wkey = ("hw", i)
                P.dma(("sp", "pool")[wrr[0] % 2], hw[:, i * 4096:(i + 1) * 4096], w1s.ap()[ex_, j], reads=[("w1s", ex_, j)], writes=[wkey])
                ps = pp()
                for k in range(16):
                    P.op("pe", lambda e: e.matmul(ps[:, 0:NS], w[:, k, 0:128], h2c[:, k, :], start=(k == 0), stop=(k == 15)), reads=[wkey, h2c], writes=[ps])
                for k in range(16):
                    P.op("pe", lambda e: e.matmul(ps[:, NS:2 * NS], w[:, k, 128:256], h2c[:, k, :], start=(k == 0), stop=(k == 15)), reads=[wkey, h2c], writes=[ps])
                gl = wk(); sg = wk(); ln = wk()
                P.op("dve", lambda e: e.tensor_scalar(gl[:], ps[:, 0:NS], b1T[:, ex_, j:j + 1], 7.0, ALU.add, ALU.min), reads=[ps, b1T], writes=[gl])
                P.op("act", lambda e: e.activation(sg[:], gl[:], AF.Sigmoid, scale=1.702), reads=[gl], writes=[sg])
                P.op("dve", lambda e: e.tensor_scalar(ln[:], ps[:, NS:2 * NS], b1T[:, ex_, 16 + j:17 + j], 7.0, ALU.add, ALU.min), reads=[ps, b1T], writes=[ln])
                P.op("dve", lambda e: e.tensor_scalar(ln[:], ln[:], -7.0, 1.0, ALU.max, ALU.add), reads=[ln], writes=[ln])
                P.op("dve", lambda e: e.tensor_tensor(gl[:], gl[:], sg[:], ALU.mult), reads=[gl, sg], writes=[gl])
                P.op("dve", lambda e: e.tensor_tensor(act[:, j, :], gl[:], ln[:], ALU.mult), reads=[gl, ln], writes=[("act", j)])
            actk = [("act", j) for j in range(16)]
            for nb in range(8):
                i = wrr[0] % 4
                wrr[0] += 1
                w = hw[:, i * 4096:(i + 1) * 4096].rearrange("p (k n) -> p k n", k=16)
                wkey = ("hw", i)
                P.dma(("sp", "pool")[wrr[0] % 2], hw[:, i * 4096:(i + 1) * 4096], w2s.ap()[ex_, nb], reads=[("w2s", ex_, nb)], writes=[wkey])
                ps = pp()
                for st in range(2):
                    for k in range(16):
                        P.op("pe", lambda e: e.matmul(ps[:, st * 256:(st + 1) * 256], act[:, k, st * 128:(st + 1) * 128], w[:, k, :], start=(k == 0), stop=(k == 15)),
                             reads=[wkey] + actk, writes=[ps])
                P.op("dve", lambda e: e.tensor_tensor(ysl[:, 0, nb * 256:(nb + 1) * 256], ps[:, 0:256], b2bc[:, nb * 256:(nb + 1) * 256], ALU.add),
                     reads=[ps, b2bc], writes=[("ysl", nb)])
                P.op("dve", lambda e: e.tensor_tensor(ysl[:, 1, nb * 256:(nb + 1) * 256], ps[:, 256:512], b2bc[:, nb * 256:(nb + 1) * 256], ALU.add),
                     reads=[ps, b2bc, ("ysl", nb)], writes=[("ysl", nb)])
            yk = [("ysl", nb) for nb in range(8)]
            for dc in range(16):
                for tt in range(2):
                    ts = slice(tt * 512, (tt + 1) * 512)
                    ps = pp()
                    for st in range(2):
                        P.op("pe", lambda e: e.matmul(ps[:], ysl[:, st, dc * 128:(dc + 1) * 128], selT[st][:, ts], start=(st == 0), stop=(st == 1)),
                             reads=yk + [selT[0], selT[1]], writes=[ps])
                    P.op("dve", lambda e: e.tensor_tensor(acc[:, dc, ts], ps[:], acc[:, dc, ts], ALU.add), reads=[ps, ("acc", dc, tt)], writes=[("acc", dc, tt)])
        P.dma("sp", o_p.ap()[:, t0:t0 + TP].rearrange("(k p) t -> p k t", p=128), acc[:],
              reads=[("acc", dc, tt) for dc in range(16) for tt in range(2)], writes=["o_p"])
    return P.finish(["o_p"]), P


def consts_D():
    c = np.zeros((128, 1024), np.float32)
    c[:, 0:256] = np.arange(256, dtype=np.float32)[None, :]
    c[:, 256] = np.arange(128, dtype=np.float32)
    c[:, 257] = np.arange(128, dtype=np.float32) + 128
    c[:, 384:512] = np.eye(128, dtype=np.float32)
    for e_ in range(4):
        c[e_, 512 + e_ * 128:512 + (e_ + 1) * 128] = 1.0
    return c


def build_E():
    P = Prog()
    part_d = P.din("parts", [8, D, TOK])
    x1_d = P.din("x1T", [D, TOK])
    pk_d = P.din("pk", [128, NPKC])
    ident_d = P.din("ident", [128, 128])
    o_x = P.dout("xout", [TOK, D])
    pk = P.sb([128, NPKC], name="pk")
    ident = P.sb([128, 128], name="ident")
    ones = P.sb([128, 128], name="ones")
    epsc = P.sb([128, 1], name="epsc")
    modp = P.sb([128, 96], name="modp")
    g2p = P.sb([128, 16], name="g2p")
    u = P.sb([128, 16, 512], name="u")
    x1 = P.sb([128, 16, 512], name="x1")
    pb_ = P.pool("pb", 2, [128, 16, 512])
    ot = P.pool("ot", 2, [128, D])
    wk = P.pool("wk", 8, [128, 512])
    stp = P.pool("stp", 6, [128, 512])
    pp = P.pool("pp", 7, [128, 512], psum=True)

    def col(n, j=0, w=1):
        o, _ = PKC[n]
        return pk[:, o + j:o + j + w]
    P.dma("sp", pk[:], pk_d.ap(), writes=[pk])
    P.dma("sp", ident[:], ident_d.ap(), writes=[ident])
    P.op("pool", lambda e: e.memset(ones[:], 1.0), writes=[ones])
    P.op("pool", lambda e: e.memset(epsc[:], LN_EPS), writes=[epsc])
    P.op("dve", lambda e: e.tensor_tensor(modp[:], col("modsh", 0, 96), col("modoff", 0, 96), ALU.add), reads=[pk], writes=[modp])
    P.op("dve", lambda e: e.tensor_scalar_add(g2p[:], modp[:, 80:96], 1.0), reads=[modp], writes=[g2p])
    dqc = [0]

    def dq():
        dqc[0] += 1
        return ("sp", "act")[dqc[0] % 2]
    for n in range(4):
        m0 = n * 512
        P.dma(dq(), x1[:], x1_d.ap()[:, m0:m0 + 512].rearrange("(k p) t -> p k t", p=128), writes=[x1])
        P.dma(dq(), u[:], part_d.ap()[0, :, m0:m0 + 512].rearrange("(k p) t -> p k t", p=128), writes=[u])
        for c in range(1, 8):
            pb = pb_()
            P.dma(dq(), pb[:], part_d.ap()[c, :, m0:m0 + 512].rearrange("(k p) t -> p k t", p=128), writes=[pb])
            P.op("dve" if c % 2 else "pool", lambda e: e.tensor_tensor(u[:], u[:], pb[:], ALU.add), reads=[u, pb], writes=[u])
        for k in range(16):
            P.op("dve", lambda e: e.tensor_scalar_mul(u[:, k, :], u[:, k, :], g2p[:, k:k + 1]), reads=[u, g2p], writes=[u])
        P.op("dve", lambda e: e.scalar_tensor_tensor(u[:], x1[:], DN_ALPHA, u[:], ALU.mult, ALU.add), reads=[u, x1], writes=[u])
        mean, rstd = ln_fm(P, u, u, 512, ones, epsc[:, 0:1], wk, pp, 1.0 / D, stp)
        for k in range(16):
            t = wk()
            P.op("dve", lambda e: e.tensor_tensor(t[:], u[:, k, :], mean[:], ALU.subtract), reads=[u, mean], writes=[t])
            P.op("dve", lambda e: e.tensor_tensor(t[:], t[:], rstd[:], ALU.mult), reads=[t, rstd], writes=[t])
            P.op("dve", lambda e: e.tensor_scalar(x1[:, k, :], t[:], col("lng", k), col("lnb", k), ALU.mult, ALU.add), reads=[t, pk, x1], writes=[x1])
        for s in range(4):
            o = ot()
            for g in range(4):
                pt = pp()
                for q in range(4):
                    k = g * 4 + q
                    P.op("pe", lambda e: e.transpose(pt[:, q * 128:(q + 1) * 128], x1[:, k, s * 128:(s + 1) * 128], ident[:]), reads=[x1, ident], writes=[pt])
                if g % 2 == 0:
                    P.op("act", lambda e: e.activation(o[:, g * 512:(g + 1) * 512], pt[:], AF.Copy), reads=[pt], writes=[o])
                else:
                    P.op("dve", lambda e: e.tensor_copy(o[:, g * 512:(g + 1) * 512], pt[:]), reads=[pt], writes=[o])
            P.dma(dq(), o_x.ap()[m0 + s * 128:m0 + (s + 1) * 128, :], o[:], reads=[o], writes=["o_x"])
    return P.finish(["o_x"]), P


def inputs_A(inp, l, c, x, vfirst):
    b, hf = c // 2, c % 2
    xh = np.zeros((TT, D), np.float32)
    if hf == 0:
        xh[HALO:] = x[b, 0:TOK]
    else:
        xh[:] = x[b, TOK - HALO:2 * TOK]
    m = {"xh": xh, "pk": pack_A(inp, l, b, hf),
         "pos": np.ascontiguousarray(inp["positions"][b:b + 1, hf * TOK:(hf + 1) * TOK]).astype(np.int32),
         "ident": np.eye(128, dtype=np.float32), "w_in": inp["w_in"][l],
         "w_uq": inp["mla_w_uq"][l], "w_ukv": inp["mla_w_ukv"][l], "rw_w1": inp["rw_w1"][l], "rw_w2": inp["rw_w2"][l],
         "rw_a1": inp["rw_a1"][l], "rw_a2": inp["rw_a2"][l], "rw_g1": inp["rw_g1"][l], "rw_g2": inp["rw_g2"][l]}
    if l > 0:
        m["rw_v1"] = inp["rw_v1"][l - 1]
        m["rw_v2"] = inp["rw_v2"][l - 1]
        m["vfirstT"] = np.ascontiguousarray(vfirst[b, hf * TOK:(hf + 1) * TOK].T)
    return m


_PROGS = {}


def _prog(name, fn):
    if name not in _PROGS:
        _PROGS[name] = fn()[0]
    return _PROGS[name]


def _run(nc, ims):
    return run_bass_kernel_spmd(nc, ims, core_ids=list(range(8))).results


def run_M(inp):
    ims = []
    cT4 = np.zeros((128, 64), np.float32)
    for b in range(NB):
        cT4[:, b::4] = fm(inp["c"][b])
    for c in range(8):
        ims.append({"cT4": cT4, "mod_w_s": np.ascontiguousarray(inp["mod_w"][:, c * 1536:(c + 1) * 1536]),
                    "mod_b_s": fm(inp["mod_b"][c * 1536:(c + 1) * 1536])})
    r = _run(_prog("M", build_M), ims)
    modsh = np.zeros((NB, 128, 96), np.float32)
    for c in range(8):
        mt = r[c]["modT"].reshape(128, 12, 4)
        for b in range(NB):
            modsh[b][:, c * 12:(c + 1) * 12] = mt[:, :, b]
    return modsh


def inputs_C(inp, l, c, x, A, Bres):
    b, hf = c // 2, c % 2
    ts = slice(hf * TOK, (hf + 1) * TOK)
    pair = lambda n: np.concatenate([Bres[2 * b][n], Bres[2 * b + 1][n]], 0)[:, ts]
    ycat = np.concatenate([A[c]["yconvT"], pair("y_mlaT"), pair("y_rwT"), pair("y_caT")], 0)
    return {"ycatT": np.ascontiguousarray(ycat), "x": np.ascontiguousarray(x[b, ts]), "w_out": inp["w_out"][l],
            "pk": pack_C(inp, l, 0, inp["modsh"][b]), "router_w": inp["moe_router_w"][l], "ident": np.eye(128, dtype=np.float32)}


PERM_D = np.arange(NB * SEQ).reshape(1024, 16).T.reshape(-1)
INV_D = np.argsort(PERM_D)


def inputs_D(inp, l, c, Cres):
    h2 = np.ascontiguousarray(np.concatenate([Cres[i]["h2T"] for i in range(8)], 1)[:, PERM_D])
    g = np.ascontiguousarray(np.concatenate([Cres[i]["gateT"] for i in range(8)], 1)[:, PERM_D])
    b1 = inp["moe_b1"][l][4 * c:4 * c + 4]
    b1T = np.concatenate([fm(b1[e]) for e in range(4)], 1)
    return {"h2T_all": h2, "gate4": np.ascontiguousarray(g[4 * c:4 * c + 4]),
            "w1": inp["moe_w1"][l][4 * c:4 * c + 4], "w2": inp["moe_w2"][l][4 * c:4 * c + 4],
            "b1T": b1T, "b2": np.ascontiguousarray(inp["moe_b2"][l][4 * c:4 * c + 4]), "cstD": consts_D()}


def inputs_E(inp, l, c, Cres, Dres):
    b = c // 2
    parts = np.stack([Dres[i]["partT"][:, INV_D[c * TOK:(c + 1) * TOK]] for i in range(8)], 0)
    return {"parts": parts, "x1T": Cres[c]["x1T"], "pk": pack_C(inp, l, 1, inp["modsh"][b]), "ident": np.eye(128, dtype=np.float32)}


def kernel(**inputs):
    inp = {k: np.asarray(v) for k, v in inputs.items()}
    inp["modsh"] = run_M(inp)
    x = inp["x"].astype(np.float32)
    vfirst = None
    for l in range(DEPTH):
        A = _run(_prog("A%d" % l, lambda: build_A(l)), [inputs_A(inp, l, c, x, vfirst) for c in range(8)])
        if l == 0:
            vfirst = np.zeros((NB, SEQ, 512), np.float32)
            for c in range(8):
                vfirst[c // 2, (c % 2) * TOK:(c % 2 + 1) * TOK] = A[c]["rw_vfirstT"].T
        Bres = _run(_prog("B", lambda: build_B(l)), [inputs_B(inp, l, c, A) for c in range(8)])
        Cres = _run(_prog("C", build_C), [inputs_C(inp, l, c, x, A, Bres) for c in range(8)])
        del A, Bres
        Dres = _run(_prog("D", build_D), [inputs_D(inp, l, c, Cres) for c in range(8)])
        Eres = _run(_prog("E", build_E), [inputs_E(inp, l, c, Cres, Dres) for c in range(8)])
        del Cres, Dres
        x = np.zeros((NB, SEQ, D), np.float32)
        for c in range(8):
            x[c // 2, (c % 2) * TOK:(c % 2 + 1) * TOK] = Eres[c]["xout"]
    return x
```
